# Optimizing a Trainium2 kernel written in Bass

```python
import math
import jax
import jax.numpy as jnp
from jax import lax
import numpy as np

D_MODEL = 2048
BATCH = 2
SEQ = 4096
DEPTH = 2

SSM_GROUP = 16
SSM_STATE = 64
SSM_WIDTH = 1024
SSM_GROUPS = SSM_WIDTH // SSM_GROUP
HEAD_DIM = 128
DILATION_PATTERN = ((128, 1), (512, 4), (2048, 16))
HEADS_PER_GROUP = 4
N_ATT_GROUPS = len(DILATION_PATTERN)
N_HEADS = N_ATT_GROUPS * HEADS_PER_GROUP
ATT_WIDTH = N_HEADS * HEAD_DIM
ATT_OUT_WIDTH = HEADS_PER_GROUP * HEAD_DIM
Q_BLOCK = 128
NEG_INF = -1e30
N_BUCKETS = 32
MAX_DISTANCE = 2048
SPLIT_POINTS = (SSM_WIDTH,
                SSM_WIDTH + ATT_WIDTH,
                SSM_WIDTH + 2 * ATT_WIDTH,
                SSM_WIDTH + 3 * ATT_WIDTH,
                SSM_WIDTH + 3 * ATT_WIDTH + D_MODEL)
IN_WIDTH = SSM_WIDTH + 3 * ATT_WIDTH + 2 * D_MODEL
D_FF = 5632
N_EXPERTS = 8
TOP_K = 2
D_FF_EXPERT = 7168
EXPERT_BLOCK = 128
N_DENSE = (DEPTH + 1) // 2
N_MOE = DEPTH // 2
N_MOD = 6
EPS = 1e-6

kernel_name = "hybrid_s5_dilated_attn_moe_block"


def rms_norm(x, g):
    x32 = x.astype(jnp.float32)
    y = x32 * lax.rsqrt(jnp.mean(x32 * x32, axis=-1, keepdims=True) + EPS)
    return y.astype(x.dtype) * g


def modulate(h, shift, scale):
    return h * (1 + scale[:, None, :]) + shift[:, None, :]


def swiglu(h, w_gate, w_up, w_down):
    return (jax.nn.silu(h @ w_gate) * (h @ w_up)) @ w_down


def s5_mixer(u, a_re, a_im, log_dt, b_re, b_im, c_re, c_im, d_skip, w_glu, b_glu):
    bsz, seqlen, _ = u.shape
    f32 = jnp.float32
    lam = lax.complex(a_re.astype(f32), a_im.astype(f32))
    dt = jnp.exp(log_dt.astype(f32))[:, None]
    a_bar = jnp.exp(lam * dt)
    b_bar = ((a_bar - 1.0) / lam)[:, :, None] * lax.complex(b_re.astype(f32), b_im.astype(f32))
    c_mat = lax.complex(c_re.astype(f32), c_im.astype(f32))
    u32 = u.astype(f32)
    u_g = u32.reshape(bsz, seqlen, SSM_GROUPS, SSM_GROUP).astype(jnp.complex64)
    bu = jnp.einsum('blgh,gph->blgp', u_g, b_bar)
    a_seq = jnp.broadcast_to(a_bar, bu.shape)

    def combine(left, right):
        a_l, s_l = left
        a_r, s_r = right
        return a_r * a_l, a_r * s_l + s_r

    _, states = lax.associative_scan(combine, (a_seq, bu), axis=1)
    y = jnp.einsum('blgp,ghp->blgh', states, c_mat).real.reshape(bsz, seqlen, SSM_WIDTH)
    y = y + d_skip.astype(f32) * u32
    y = jax.nn.gelu(y).astype(u.dtype)
    return y * jax.nn.sigmoid(y @ w_glu + b_glu)


def t5_causal_bucket(dist):
    max_exact = N_BUCKETS // 2
    d32 = jnp.maximum(dist, 1).astype(jnp.float32)
    large = max_exact + (jnp.log(d32 / max_exact) / math.log(MAX_DISTANCE / max_exact)
                         * (N_BUCKETS - max_exact)).astype(jnp.int32)
    return jnp.where(dist < max_exact, dist, jnp.minimum(large, N_BUCKETS - 1))


def dilated_attention(q, k, v, rel_bias):
    bsz, seqlen = q.shape[0], q.shape[1]
    n_blocks = seqlen // Q_BLOCK
    scale = HEAD_DIM ** -0.5
    f32 = jnp.float32
    offsets, biases, qs, ks, vs = [], [], [], [], []
    for g, (window, dilation) in enumerate(DILATION_PATTERN):
        hs = slice(g * HEADS_PER_GROUP, (g + 1) * HEADS_PER_GROUP)
        offs = jnp.arange(window // dilation + 1, dtype=jnp.int32) * dilation
        offsets.append(offs)
        biases.append(rel_bias[t5_causal_bucket(offs)][:, hs].T.astype(f32))
        qs.append(q[:, :, hs])
        ks.append(k[:, :, hs])
        vs.append(v[:, :, hs])

    def block(blk):
        start = blk * Q_BLOCK
        t = start + jnp.arange(Q_BLOCK, dtype=jnp.int32)
        outs, lses = [], []
        for g in range(N_ATT_GROUPS):
            kpos = t[:, None] - offsets[g][None, :]
            valid = kpos >= 0
            kidx = jnp.maximum(kpos, 0)
            qb = lax.dynamic_slice_in_dim(qs[g], start, Q_BLOCK, axis=1)
            kb = ks[g][:, kidx]
            vb = vs[g][:, kidx].astype(f32)
            logits = jnp.einsum('bqhd,bqkhd->bhqk', qb, kb).astype(f32) * scale
            logits = logits + biases[g][None, :, None, :]
            logits = jnp.where(valid[None, None], logits, NEG_INF)
            m = jnp.max(logits, axis=-1, keepdims=True)
            p = jnp.exp(logits - m)
            s = jnp.sum(p, axis=-1)
            o = jnp.einsum('bhqk,bqkhd->bqhd', p, vb) / jnp.transpose(s, (0, 2, 1))[..., None]
            outs.append(o)
            lses.append(m[..., 0] + jnp.log(s))
        w = jax.nn.softmax(jnp.stack(lses), axis=0)
        o = jnp.einsum('gbhq,gbqhd->bqhd', w, jnp.stack(outs))
        return o.astype(q.dtype)

    out = lax.map(block, jnp.arange(n_blocks))
    return jnp.transpose(out, (1, 0, 2, 3, 4)).reshape(bsz, seqlen, ATT_OUT_WIDTH)


def moe_swiglu(h, w_router, w_gate, w_up, w_down):
    n_tok = h.shape[0]
    n_assign = n_tok * TOP_K
    logits = (h @ w_router).astype(jnp.float32)
    top_logit, top_expert = lax.top_k(logits, TOP_K)
    top_gate = jax.nn.softmax(top_logit, axis=-1)
    flat_expert = top_expert.reshape(-1)
    flat_token = jnp.repeat(jnp.arange(n_tok, dtype=jnp.int32), TOP_K)
    order = jnp.argsort(flat_expert)
    sorted_expert = flat_expert[order]
    sorted_token = flat_token[order]
    sorted_gate = top_gate.reshape(-1)[order]
    counts = jnp.bincount(flat_expert, length=N_EXPERTS)
    padded = (counts + EXPERT_BLOCK - 1) // EXPERT_BLOCK * EXPERT_BLOCK
    padded_end = jnp.cumsum(padded)
    start = jnp.cumsum(counts) - counts
    dest = (padded_end - padded)[sorted_expert] + jnp.arange(n_assign, dtype=jnp.int32) - start[sorted_expert]
    n_rows = n_assign + N_EXPERTS * EXPERT_BLOCK
    n_blocks = n_rows // EXPERT_BLOCK
    row_token = jnp.zeros((n_rows,), jnp.int32).at[dest].set(sorted_token)
    block_expert = jnp.minimum(
        jnp.searchsorted(padded_end, jnp.arange(n_blocks, dtype=jnp.int32) * EXPERT_BLOCK, side='right'),
        N_EXPERTS - 1)
    x_rows = h[row_token].reshape(n_blocks, EXPERT_BLOCK, h.shape[-1])

    def expert_block(args):
        xb, e = args
        return swiglu(xb, w_gate[e], w_up[e], w_down[e])

    y_rows = lax.map(expert_block, (x_rows, block_expert)).reshape(n_rows, h.shape[-1])
    contrib = y_rows[dest] * sorted_gate[:, None].astype(h.dtype)
    return jax.ops.segment_sum(contrib, sorted_token, num_segments=n_tok)


def setup_inputs(seed: int = 0) -> dict:
    key = jax.random.key(seed)
    ks = iter(jax.random.split(key, 32))
    f32 = jnp.float32

    def normal(shape, std):
        return jax.random.normal(next(ks), shape, f32) * std

    x = normal((BATCH, SEQ, D_MODEL), 1.0)
    c = normal((BATCH, D_MODEL), 1.0)
    w_mod = normal((DEPTH, D_MODEL, N_MOD * D_MODEL), 0.5 * D_MODEL ** -0.5)
    b_mod = normal((DEPTH, N_MOD * D_MODEL), 0.02)
    norm_mix_g = 1.0 + normal((DEPTH, D_MODEL), 0.05)
    norm_ffn_g = 1.0 + normal((DEPTH, D_MODEL), 0.05)
    w_in = normal((DEPTH, D_MODEL, IN_WIDTH), D_MODEL ** -0.5)
    ssm_a_re = -0.5 + normal((DEPTH, SSM_GROUPS, SSM_STATE), 0.01)
    ssm_a_im = math.pi * jnp.arange(SSM_STATE, dtype=f32) + normal((DEPTH, SSM_GROUPS, SSM_STATE), 0.01)
    ssm_log_dt = jax.random.uniform(next(ks), (DEPTH, SSM_GROUPS), f32, math.log(1e-3), math.log(1e-1))
    ssm_b_re = normal((DEPTH, SSM_GROUPS, SSM_STATE, SSM_GROUP), (2 * SSM_GROUP) ** -0.5)
    ssm_b_im = normal((DEPTH, SSM_GROUPS, SSM_STATE, SSM_GROUP), (2 * SSM_GROUP) ** -0.5)
    ssm_c_re = normal((DEPTH, SSM_GROUPS, SSM_GROUP, SSM_STATE), SSM_STATE ** -0.5)
    ssm_c_im = normal((DEPTH, SSM_GROUPS, SSM_GROUP, SSM_STATE), SSM_STATE ** -0.5)
    ssm_d = normal((DEPTH, SSM_WIDTH), 1.0)
    w_glu = normal((DEPTH, SSM_WIDTH, SSM_WIDTH), SSM_WIDTH ** -0.5)
    b_glu = normal((DEPTH, SSM_WIDTH), 0.02)
    rel_bias = normal((N_BUCKETS, N_HEADS), 0.5)
    w_branch_ssm = normal((DEPTH, SSM_WIDTH, D_MODEL), SSM_WIDTH ** -0.5)
    w_branch_att = normal((DEPTH, ATT_OUT_WIDTH, D_MODEL), ATT_OUT_WIDTH ** -0.5)
    w_out = normal((DEPTH, D_MODEL, D_MODEL), D_MODEL ** -0.5)
    ffn_w_gate = normal((N_DENSE, D_MODEL, D_FF), D_MODEL ** -0.5)
    ffn_w_up = normal((N_DENSE, D_MODEL, D_FF), D_MODEL ** -0.5)
    ffn_w_down = normal((N_DENSE, D_FF, D_MODEL), D_FF ** -0.5)
    moe_router = normal((N_MOE, D_MODEL, N_EXPERTS), D_MODEL ** -0.5)
    moe_w_gate = normal((N_MOE, N_EXPERTS, D_MODEL, D_FF_EXPERT), D_MODEL ** -0.5)
    moe_w_up = normal((N_MOE, N_EXPERTS, D_MODEL, D_FF_EXPERT), D_MODEL ** -0.5)
    moe_w_down = normal((N_MOE, N_EXPERTS, D_FF_EXPERT, D_MODEL), D_FF_EXPERT ** -0.5)
    final_norm_g = 1.0 + normal((D_MODEL,), 0.05)
    return {"x": x, "c": c, "w_mod": w_mod, "b_mod": b_mod,
            "norm_mix_g": norm_mix_g, "norm_ffn_g": norm_ffn_g, "w_in": w_in,
            "ssm_a_re": ssm_a_re, "ssm_a_im": ssm_a_im, "ssm_log_dt": ssm_log_dt,
            "ssm_b_re": ssm_b_re, "ssm_b_im": ssm_b_im, "ssm_c_re": ssm_c_re, "ssm_c_im": ssm_c_im,
            "ssm_d": ssm_d, "w_glu": w_glu, "b_glu": b_glu, "rel_bias": rel_bias,
            "w_branch_ssm": w_branch_ssm, "w_branch_att": w_branch_att, "w_out": w_out,
            "ffn_w_gate": ffn_w_gate, "ffn_w_up": ffn_w_up, "ffn_w_down": ffn_w_down,
            "moe_router": moe_router, "moe_w_gate": moe_w_gate, "moe_w_up": moe_w_up,
            "moe_w_down": moe_w_down, "final_norm_g": final_norm_g}


def reference(x, c, w_mod, b_mod, norm_mix_g, norm_ffn_g, w_in,
              ssm_a_re, ssm_a_im, ssm_log_dt, ssm_b_re, ssm_b_im, ssm_c_re, ssm_c_im,
              ssm_d, w_glu, b_glu, rel_bias, w_branch_ssm, w_branch_att, w_out,
              ffn_w_gate, ffn_w_up, ffn_w_down, moe_router, moe_w_gate, moe_w_up,
              moe_w_down, final_norm_g):
    bsz, seqlen, _ = x.shape
    cond = jax.nn.silu(c)
    for i in range(DEPTH):
        mod = cond @ w_mod[i] + b_mod[i]
        shift_m, scale_m, gate_m, shift_f, scale_f, gate_f = jnp.split(mod, N_MOD, axis=-1)
        h = modulate(rms_norm(x, norm_mix_g[i]), shift_m, scale_m)
        proj = h @ w_in[i]
        u, q, k, v, g_ssm, g_att = jnp.split(proj, SPLIT_POINTS, axis=-1)
        y_ssm = s5_mixer(u, ssm_a_re[i], ssm_a_im[i], ssm_log_dt[i], ssm_b_re[i], ssm_b_im[i],
                         ssm_c_re[i], ssm_c_im[i], ssm_d[i], w_glu[i], b_glu[i])
        y_att = dilated_attention(q.reshape(bsz, seqlen, N_HEADS, HEAD_DIM),
                                  k.reshape(bsz, seqlen, N_HEADS, HEAD_DIM),
                                  v.reshape(bsz, seqlen, N_HEADS, HEAD_DIM), rel_bias)
        merged = (jax.nn.sigmoid(g_ssm) * (y_ssm @ w_branch_ssm[i])
                  + jax.nn.sigmoid(g_att) * (y_att @ w_branch_att[i]))
        x = x + gate_m[:, None, :] * (merged @ w_out[i])
        h = modulate(rms_norm(x, norm_ffn_g[i]), shift_f, scale_f).reshape(bsz * seqlen, D_MODEL)
        j = i // 2
        if i % 2 == 0:
            f = swiglu(h, ffn_w_gate[j], ffn_w_up[j], ffn_w_down[j])
        else:
            f = moe_swiglu(h, moe_router[j], moe_w_gate[j], moe_w_up[j], moe_w_down[j])
        x = x + gate_f[:, None, :] * f.reshape(bsz, seqlen, D_MODEL)
    return rms_norm(x, final_norm_g)
```

```python
import contextlib
import numpy as np
import ml_dtypes
import concourse.bass as bass
import concourse.mybir as mybir
from concourse.bass_utils import run_bass_kernel_spmd

F32 = mybir.dt.float32
BF16 = mybir.dt.bfloat16
I32 = mybir.dt.int32
ALU = mybir.AluOpType
AF = mybir.ActivationFunctionType
NPBF = ml_dtypes.bfloat16

ENGS = ("pe", "dve", "act", "pool", "sp")

D = 2048
DC = 16
NCORES = 8
T = 1024
SEQ = 4096
EPS = 1e-6
IN_W = 9728
DFF = 5632
DFFE = 7168
NEXP = 8


class Prog:
    def __init__(self, nc):
        self.nc = nc
        self.ops = {e: [] for e in ENGS}
        self.last_w = {}
        self.readers = {}
        self.ndma = {}

    def _deps(self, reads, writes):
        deps = []
        for k in reads:
            t = self.last_w.get(k)
            if t is not None:
                deps.append(t)
        for k in writes:
            t = self.last_w.get(k)
            if t is not None:
                deps.append(t)
            deps.extend(self.readers.get(k, ()))
        return deps

    def _commit(self, tok, reads, writes):
        for k in reads:
            self.readers.setdefault(k, []).append(tok)
        for k in writes:
            self.last_w[k] = tok
            self.readers[k] = []

    def op(self, eng, fn, reads=(), writes=(), signal=True):
        deps = self._deps(reads, writes)
        tok = ("c", eng, len(self.ops[eng]))
        self.ops[eng].append(dict(kind="c", fn=fn, deps=deps, signal=signal or eng != "pe"))
        self._commit(tok, reads, writes)
        return tok

    def dma(self, q, fn, reads=(), writes=(), inc=16, sem=None):
        if sem is None:
            sem = writes[0]
        deps = self._deps(reads, writes)
        self.ndma[sem] = self.ndma.get(sem, 0) + inc
        tok = ("d", sem, self.ndma[sem])
        self.ops[q].append(dict(kind="d", fn=fn, deps=deps, inc=inc, sem=sem))
        self._commit(tok, reads, writes)
        return tok

    def wait_all_on(self, eng, toks):
        self.ops[eng].append(dict(kind="w", deps=list(toks)))

    def barrier(self):
        toks = []
        for e in ENGS:
            for i in range(len(self.ops[e]) - 1, -1, -1):
                o = self.ops[e][i]
                if o["kind"] == "c" and o["signal"]:
                    toks.append(("c", e, i))
                    break
        for k, v in self.ndma.items():
            toks.append(("d", k, v))
        for e in ENGS:
            self.ops[e].append(dict(kind="w", deps=list(toks)))

    def emit(self, st):
        nc = self.nc
        tick_at = {}
        for e in ENGS:
            n = 0
            ticks = []
            for o in self.ops[e]:
                if o["kind"] == "c" and o["signal"]:
                    n += 1
                ticks.append(n)
            res = [None] * len(ticks)
            nxt = None
            for i in range(len(ticks) - 1, -1, -1):
                o = self.ops[e][i]
                if o["kind"] == "c" and o["signal"]:
                    nxt = ticks[i]
                res[i] = nxt
            tick_at[e] = res
        csem = {e: st.enter_context(nc.semaphore("c_" + e)) for e in ENGS if e != "sp"}
        dkeys = []
        dset = set()
        for e in ENGS:
            for o in self.ops[e]:
                if o["kind"] == "d" and o["sem"] not in dset:
                    dset.add(o["sem"])
                    dkeys.append(o["sem"])
        dsem = {k: st.enter_context(nc.semaphore("d%d" % i)) for i, k in enumerate(dkeys)}
        block = st.enter_context(nc.Block())

        def make(e):
            def body(eng):
                seen = {}
                for o in self.ops[e]:
                    for t in o["deps"]:
                        if t[0] == "c":
                            if t[1] == e and e == "pe":
                                continue
                            v = tick_at[t[1]][t[2]]
                            assert v is not None, ("unsignaled dep", t)
                            key = ("c", t[1])
                            sem = csem[t[1]]
                        else:
                            v = t[2]
                            key = ("d", t[1])
                            sem = dsem[t[1]]
                        if seen.get(key, 0) >= v:
                            continue
                        seen[key] = v
                        eng.wait_ge(sem, v)
                    if o["kind"] == "c":
                        ins = o["fn"](eng)
                        if o["signal"]:
                            ins.then_inc(csem[e], 1)
                    elif o["kind"] == "d":
                        ins = o["fn"](eng)
                        ins.then_inc(dsem[o["sem"]], o["inc"])
            return body

        for e in ENGS:
            if not self.ops[e]:
                continue
            dec = {"pe": block.tensor, "dve": block.vector, "act": block.scalar,
                   "pool": block.gpsimd, "sp": block.sync}[e]
            dec(make(e))


class Ctx:
    def __init__(self):
        self.nc = bass.Bass("TRN2", target_bir_lowering=False)
        self.st = contextlib.ExitStack()
        self.P = Prog(self.nc)
        self.ps = []
        self.ps_i = 0
        self.out_toks = []
        self.cv_i = 0
        self._n = 0
        self.phase_st = None
        self.arena = None
        self.arena_off = 0

    def din(self, name, shape, dt):
        return self.nc.dram_tensor(name, list(shape), dt, kind="ExternalInput").ap()

    def dout(self, name, shape, dt):
        return self.nc.dram_tensor(name, list(shape), dt, kind="ExternalOutput").ap()

    ARENA_BYTES = 160 * 1024

    def sb(self, name, shape, dt):
        if self.phase_st is None:
            return self.st.enter_context(self.nc.sbuf_tensor(name, list(shape), dt))
        esz = mybir.dt.size(dt)
        n = int(np.prod(shape[1:]))
        nbytes = (n * esz + 63) // 64 * 64
        off = self.arena_off
        assert off + nbytes <= self.ARENA_BYTES, ("arena overflow", name, off, nbytes)
        self.arena_off += nbytes
        v = self.arena[0:shape[0], off // 2:(off + n * esz) // 2]
        if dt != BF16:
            v = v.bitcast(dt)
        if len(shape) > 2:
            names = " ".join("d%d" % i for i in range(1, len(shape)))
            v = v.rearrange("p (%s) -> p %s" % (names, names), **{"d%d" % i: shape[i] for i in range(1, len(shape))})
        return v

    def phase_begin(self):
        if self.arena is None:
            self.arena = self.st.enter_context(self.nc.sbuf_tensor("arena", [128, self.ARENA_BYTES // 2], BF16))
        self.phase_st = True
        self.arena_off = 0

    def phase_end(self):
        self.P.barrier()
        self.phase_st = None

    def alloc_psum(self, n=8):
        for i in range(n):
            self.ps.append(self.st.enter_context(self.nc.psum_tensor("ps%d" % i, [128, 512], F32)))

    def psum(self):
        i = self.ps_i % len(self.ps)
        self.ps_i += 1
        return self.ps[i], ("ps", i)

    def load(self, dst_ap, src_ap, key, q="sp"):
        return self.P.dma(q, lambda e: e.dma_start(out=dst_ap, in_=src_ap), writes=[key])

    def store(self, dst_ap, src_ap, rkeys, wkey, q="sp", sem=None):
        t = self.P.dma(q, lambda e: e.dma_start(out=dst_ap, in_=src_ap), reads=rkeys, writes=[wkey],
                       sem=sem if sem is not None else ("st", wkey))
        self.out_toks.append(t)
        return t

    def conv_eng(self):
        e = ("pool", "act", "dve")[self.cv_i % 3]
        self.cv_i += 1
        return e

    def copy(self, eng, out, in_, reads, writes):
        if eng == "act":
            return self.P.op("act", lambda e: e.activation(out=out, in_=in_, func=AF.Identity), reads=reads, writes=writes)
        return self.P.op(eng, lambda e: e.tensor_copy(out=out, in_=in_), reads=reads, writes=writes)

    def finish(self):
        last = {}
        for t in self.out_toks:
            last[t[1]] = t
        self.P.wait_all_on("sp", list(last.values()))
        self.P.emit(self.st)
        self.st.close()
        return self.nc


def mm_group(cx, ps_ap, ps_key, pairs, reads):
    n = len(pairs)
    for i, (l, r) in enumerate(pairs):
        cx.P.op("pe", lambda e, l=l, r=r, i=i: e.matmul(ps_ap, lhsT=l, rhs=r, start=(i == 0), stop=(i == n - 1)),
                reads=reads, writes=[ps_key], signal=(i == n - 1))


class WLoader:
    def __init__(self, cx, name, KC, ncol, nslots=2, nstage=4, kq=4):
        self.cx, self.KC, self.ncol, self.kq = cx, KC, ncol, kq
        self.name = name
        self.wb = [cx.sb("%s_wb%d" % (name, i), [128, KC, ncol], BF16) for i in range(nslots)]
        self.stg = [cx.sb("%s_st%d" % (name, i), [128, kq, ncol], F32) for i in range(nstage)]
        self.si = 0
        self.wi = 0

    def load(self, w_rows_ap, ncol=None):
        cx = self.cx
        ncol = ncol or self.ncol
        slot = self.wi % len(self.wb)
        self.wi += 1
        wb = self.wb[slot]
        key = (self.name, "wb", slot)
        for k0 in range(0, self.KC, self.kq):
            kn = min(self.kq, self.KC - k0)
            s = self.si % len(self.stg)
            self.si += 1
            stg = self.stg[s]
            skey = (self.name, "st", s)
            src = w_rows_ap[k0 * 128:(k0 + kn) * 128, :].rearrange("(c p) n -> p c n", p=128)
            cx.P.dma("sp", lambda e, stg=stg, src=src, kn=kn: e.dma_start(out=stg[:, 0:kn, 0:ncol], in_=src),
                     writes=[skey])
            cx.copy(cx.conv_eng(), wb[:, k0:k0 + kn, 0:ncol], stg[:, 0:kn, 0:ncol], [skey], [(key, k0)])
        return wb, [(key, k0) for k0 in range(0, self.KC, self.kq)]


def build_mod():
    cx = Ctx()
    P = cx.P
    NCOL = 3072
    cT = cx.din("cT", [128, DC, 2], F32)
    W = cx.din("W", [D, NCOL], F32)
    b = cx.din("b", [2, NCOL], F32)
    y = cx.dout("y", [2, NCOL], F32)
    cx.alloc_psum(2)
    c_sb = cx.sb("c_sb", [128, DC, 2], F32)
    cond = cx.sb("cond", [128, DC, 2], F32)
    b_sb = cx.sb("b_sb", [2, NCOL], F32)
    o_sb = cx.sb("o_sb", [2, NCOL], F32)
    wst = [cx.sb("wst%d" % i, [128, DC, 512], F32) for i in range(2)]
    cx.load(c_sb[:], cT, "c_sb")
    cx.load(b_sb[:], b, "b_sb")
    P.op("act", lambda e: e.activation(out=cond[:], in_=c_sb[:], func=AF.Silu), reads=["c_sb"], writes=["cond"])
    for ct in range(NCOL // 512):
        s = ct % 2
        for k0 in range(0, DC, 4):
            src = W[k0 * 128:(k0 + 4) * 128, ct * 512:(ct + 1) * 512].rearrange("(c p) n -> p c n", p=128)
            P.dma("sp", lambda e, s=s, k0=k0, src=src: e.dma_start(out=wst[s][:, k0:k0 + 4, :], in_=src),
                  writes=[("wst", s)])
        ps, pk = cx.psum()
        mm_group(cx, ps[0:2, :], pk, [(cond[:, kc, :], wst[s][:, kc, :]) for kc in range(DC)], ["cond", ("wst", s)])
        P.op("dve", lambda e, ps=ps, ct=ct: e.tensor_tensor(out=o_sb[:, ct * 512:(ct + 1) * 512], in0=ps[0:2, :],
                                                        in1=b_sb[:, ct * 512:(ct + 1) * 512], op=ALU.add),
             reads=[pk, "b_sb"], writes=["o_sb"])
    cx.store(y, o_sb[:], ["o_sb"], "y")
    return cx.finish()


def emit_rmsnorm_mod(cx, xT, xkey, gm, shift, hT, hkey, ones16, tag):
    P = cx.P
    sq = [cx.sb("%s_sq%d" % (tag, i), [128, 512], BF16) for i in range(2)]
    rstd = cx.sb("%s_rstd" % tag, [128, T], F32)
    tmp = [cx.sb("%s_tmp%d" % (tag, i), [128, 512], F32) for i in range(2)]
    for half in range(T // 512):
        hs = slice(half * 512, (half + 1) * 512)
        ps, pk = cx.psum()
        for kc in range(DC):
            s = kc % 2
            P.op("act", lambda e, s=s, kc=kc, hs=hs: e.activation(out=sq[s][:], in_=xT[:, kc, hs], func=AF.Square),
                 reads=[xkey], writes=[(tag, "sq", s)])
            P.op("pe", lambda e, s=s, kc=kc, ps=ps: e.matmul(ps[:], lhsT=ones16[:], rhs=sq[s][:], start=(kc == 0),
                                                          stop=(kc == DC - 1)),
                 reads=[(tag, "sq", s), "ones16"], writes=[pk], signal=True)
        rk = (tag, "rstd", half)
        P.op("dve", lambda e, ps=ps, hs=hs: e.tensor_scalar(out=rstd[:, hs], in0=ps[:], scalar1=1.0 / D, scalar2=EPS,
                                                    op0=ALU.mult, op1=ALU.add), reads=[pk], writes=[rk])
        P.op("act", lambda e, hs=hs: e.activation(out=rstd[:, hs], in_=rstd[:, hs], func=AF.Sqrt), reads=[rk], writes=[rk])
        P.op("dve", lambda e, hs=hs: e.reciprocal(out=rstd[:, hs], in_=rstd[:, hs]), reads=[rk], writes=[rk])
        for kc in range(DC):
            s = kc % 2
            P.op("dve", lambda e, s=s, kc=kc, hs=hs: e.tensor_tensor(out=tmp[s][:], in0=xT[:, kc, hs], in1=rstd[:, hs], op=ALU.mult),
                 reads=[xkey, rk], writes=[(tag, "tmp", s)])
            P.op("act", lambda e, s=s, kc=kc, hs=hs: e.activation(out=hT[:, kc, hs], in_=tmp[s][:], func=AF.Identity,
                                                        scale=gm[:, kc:kc + 1], bias=shift[:, kc:kc + 1]),
                 reads=[(tag, "tmp", s), "modv"], writes=[hkey])
    return rstd


def emit_modvecs(cx, modv, gnorm, gm):
    P = cx.P
    P.op("dve", lambda e: e.tensor_scalar(out=gm[:], in0=modv[:, 1, :], scalar1=1.0, scalar2=None, op0=ALU.add),
         reads=["modv_in"], writes=["gm_tmp"])
    P.op("dve", lambda e: e.tensor_tensor(out=gm[:], in0=gm[:], in1=gnorm[:], op=ALU.mult),
         reads=["gm_tmp", "gnorm"], writes=["modv"])


def emit_linear(cx, wl, W, KC, N, inT, in_keys, evac, ngrp=256):
    for g0 in range(0, N, ngrp):
        gn = min(ngrp, N - g0)
        wb, wkeys = wl.load(W[:, g0:g0 + gn], gn)
        for j in range(gn // 128):
            nt = g0 // 128 + j
            for half in range(T // 512):
                ps, pk = cx.psum()
                mm_group(cx, ps[:], pk,
                         [(wb[:, kc, j * 128:(j + 1) * 128], inT[:, kc, half * 512:(half + 1) * 512]) for kc in range(KC)],
                         list(wkeys) + list(in_keys))
                evac(nt, half, ps, pk)


def build_inproj():
    cx = Ctx()
    P = cx.P
    xTd = cx.din("xT", [128, DC, T], F32)
    modd = cx.din("modv", [128, 6, DC], F32)
    gnd = cx.din("gnorm", [128, DC], F32)
    W = cx.din("W", [D, IN_W], F32)
    out = cx.dout("projT", [IN_W, T], BF16)
    cx.alloc_psum(8)
    xT = cx.sb("xT_sb", [128, DC, T], F32)
    hT = cx.sb("hT_sb", [128, DC, T], BF16)
    modv = cx.sb("modv_sb", [128, 6, DC], F32)
    gnorm = cx.sb("gnorm_sb", [128, DC], F32)
    gm = cx.sb("gm_sb", [128, DC], F32)
    ones16 = cx.sb("ones16", [128, 128], BF16)
    osb = [cx.sb("osb%d" % i, [128, T], BF16) for i in range(3)]
    P.op("pool", lambda e: e.memset(ones16[:], 1.0), writes=["ones16"])
    for k0 in range(0, DC, 4):
        P.dma("sp", lambda e, k0=k0: e.dma_start(out=xT[:, k0:k0 + 4, :], in_=xTd[:, k0:k0 + 4, :]), writes=["xT"], sem="xT")
    cx.load(modv[:], modd, "modv_in")
    cx.load(gnorm[:], gnd, "gnorm")
    emit_modvecs(cx, modv, gnorm, gm)
    emit_rmsnorm_mod(cx, xT, "xT", gm, modv[:, 0, :], hT, "hT", ones16, "n1")
    wl = WLoader(cx, "win", DC, 256)
    cnt = [0]

    def evac(nt, half, ps, pk):
        s = nt % 3
        eng = "act" if (cnt[0] % 2 == 0) else "dve"
        cnt[0] += 1
        cx.copy(eng, osb[s][:, half * 512:(half + 1) * 512], ps[:], [pk], [("osb", s, half)])
        if half == T // 512 - 1:
            cx.store(out[nt * 128:(nt + 1) * 128, :], osb[s][:], [("osb", s, h) for h in range(T // 512)], ("out", nt),
                     sem=("st_osb", s))

    emit_linear(cx, wl, W, DC, IN_W, hT, ["hT"], evac)
    return cx.finish()


def fm(a2d):
    F, n = a2d.shape
    return np.ascontiguousarray(a2d.reshape(F // 128, 128, n).transpose(1, 0, 2))


def vec_fm(v):
    return np.ascontiguousarray(v.reshape(-1, 128).T)


_cache = {}


def get_nc(name, builder):
    if name not in _cache:
        _cache[name] = builder()
    return _cache[name]


def run(nc, in_maps):
    res = run_bass_kernel_spmd(nc, in_maps, core_ids=list(range(NCORES)))
    return res.results


def run_mod(c, w_mod, b_mod):
    nc = get_nc("mod", build_mod)
    cT = np.ascontiguousarray(c.T.reshape(DC, 128, 2).transpose(1, 0, 2))
    ins = []
    for i in range(NCORES):
        l, q = i // 4, i % 4
        cols = slice(q * 3072, (q + 1) * 3072)
        ins.append({"cT": cT, "W": np.ascontiguousarray(w_mod[l][:, cols]),
                    "b": np.ascontiguousarray(np.broadcast_to(b_mod[l][cols], (2, 3072)))})
    res = run(nc, ins)
    mod = np.zeros((2, 2, 6 * D), np.float32)
    for i in range(NCORES):
        l, q = i // 4, i % 4
        mod[l][:, q * 3072:(q + 1) * 3072] = res[i]["y"]
    return mod


def modv_layout(mod_lb):
    return np.ascontiguousarray(mod_lb.reshape(6, DC, 128).transpose(2, 0, 1))


def x_to_cores(x):
    outs = []
    for i in range(NCORES):
        b, q = i // 4, i % 4
        outs.append(fm(np.ascontiguousarray(x[b, q * T:(q + 1) * T, :].T)))
    return outs


def run_inproj(xTs, mod_l, gnorm, w_in_l):
    nc = get_nc("inproj", build_inproj)
    ins = []
    for i in range(NCORES):
        b = i // 4
        ins.append({"xT": xTs[i], "modv": modv_layout(mod_l[b]), "gnorm": vec_fm(gnorm), "W": w_in_l})
    res = run(nc, ins)
    projT = np.zeros((2, IN_W, SEQ), NPBF)
    for i in range(NCORES):
        b, q = i // 4, i % 4
        projT[b][:, q * T:(q + 1) * T] = res[i]["projT"]
    return projT


DILS = (1, 4, 16)


def build_attn():
    cx = Ctx()
    P = cx.P
    QTd = cx.din("QT", [128, 3, SEQ], BF16)
    KTd = cx.din("KT", [128, 3, SEQ], BF16)
    Vd = cx.din("V", [128, 3, 32, 128], BF16)
    Bmd = cx.din("Bm", [128, 3, 256], F32)
    out = cx.dout("oT", [128, SEQ], BF16)
    cx.alloc_psum(8)
    QT = cx.sb("QT_sb", [128, 3, SEQ], BF16)
    KT = cx.sb("KT_sb", [128, 3, SEQ], BF16)
    V = cx.sb("V_sb", [128, 3, 32, 128], BF16)
    Bm = cx.sb("Bm_sb", [128, 3, 256], F32)
    ones16 = cx.sb("ones16", [128, 128], BF16)
    num = cx.sb("num", [128, SEQ], F32)
    den = cx.sb("den", [128, SEQ], F32)
    o16 = cx.sb("o16", [128, SEQ], BF16)
    lg = [cx.sb("lg%d" % i, [128, 256], F32) for i in range(3)]
    pT = [cx.sb("pT%d" % i, [128, 256], BF16) for i in range(3)]
    P.op("pool", lambda e: e.memset(ones16[:], 1.0), writes=["ones16"])
    Vf = V[:].rearrange("p g t d -> p g (t d)")
    Vdf = Vd.rearrange("p g t d -> p g (t d)")
    for g in range(3):
        cx.load(QT[:, g, :], QTd[:, g, :], ("QT", g))
        cx.load(KT[:, g, :], KTd[:, g, :], ("KT", g))
        cx.load(Vf[:, g, :], Vdf[:, g, :], ("V", g))
    cx.load(Bm[:], Bmd, "Bm")
    scale = 128 ** -0.5
    blk = 0
    import os
    DBG = os.environ.get("ATT_DBG", "")
    if DBG == "loads":
        P.op("pool", lambda e: e.memset(o16[:], 0.0), reads=[("QT", 0), ("QT", 1), ("QT", 2), ("KT", 0), ("KT", 1), ("KT", 2), ("V", 0), ("V", 1), ("V", 2), "Bm"], writes=["o16"])
        cx.store(out, o16[:], ["o16"], "oT")
        return cx.finish()
    for g in range(3 if DBG != "g0" else 1):
        d = DILS[g]
        run = SEQ // d
        numv = num[:].rearrange("p (m d) -> p m d", d=d)
        denv = den[:].rearrange("p (m d) -> p m d", d=d)
        for r in range(d):
            for i in range(run // 128):
                p0 = r * run + 128 * i
                nc_ = 256 if i > 0 else 128
                s = blk % 3
                blk += 1
                ps, pk = cx.psum()
                P.op("pe", lambda e, ps=ps, g=g, p0=p0: e.matmul(ps[:, 0:128], lhsT=KT[:, g, p0:p0 + 128],
                                                               rhs=QT[:, g, p0:p0 + 128], start=True, stop=True),
                     reads=[("KT", g), ("QT", g)], writes=[pk], signal=(i == 0))
                if i > 0:
                    P.op("pe", lambda e, ps=ps, g=g, p0=p0: e.matmul(ps[:, 128:256], lhsT=KT[:, g, p0 - 128:p0],
                                                                   rhs=QT[:, g, p0:p0 + 128], start=True, stop=True),
                         reads=[("KT", g), ("QT", g)], writes=[pk])
                LV = int(os.environ.get("ATT_LV", "9"))
                if LV == 1:
                    P.op("dve", lambda e, ps=ps, s=s, n=nc_: e.tensor_copy(out=lg[s][:, 0:n], in_=ps[:, 0:n]), reads=[pk], writes=[("lg", s)])
                    continue
                P.op("dve", lambda e, ps=ps, g=g, s=s, n=nc_: e.scalar_tensor_tensor(
                    out=lg[s][:, 0:n], in0=ps[:, 0:n], scalar=scale, in1=Bm[:, g, 0:n], op0=ALU.mult, op1=ALU.add),
                    reads=[pk, "Bm"], writes=[("lg", s)])
                if LV == 2:
                    continue
                P.op("act", lambda e, s=s, n=nc_: e.activation(out=pT[s][:, 0:n], in_=lg[s][:, 0:n], func=AF.Exp),
                     reads=[("lg", s)], writes=[("pT", s)])
                if LV == 3:
                    continue
                ps2, pk2 = cx.psum()
                td = p0 // 128
                P.op("pe", lambda e, ps2=ps2, g=g, td=td, s=s, i=i: e.matmul(
                    ps2[:, 0:128], lhsT=V[:, g, td, :], rhs=pT[s][:, 0:128], start=True, stop=(i == 0)),
                    reads=[("V", g), ("pT", s)], writes=[pk2], signal=False)
                if i > 0:
                    P.op("pe", lambda e, ps2=ps2, g=g, td=td, s=s: e.matmul(
                        ps2[:, 0:128], lhsT=V[:, g, td - 1, :], rhs=pT[s][:, 128:256], start=False, stop=True),
                        reads=[("V", g), ("pT", s)], writes=[pk2], signal=False)
                P.op("pe", lambda e, ps2=ps2, s=s, i=i: e.matmul(
                    ps2[:, 128:256], lhsT=ones16[:], rhs=pT[s][:, 0:128], start=True, stop=(i == 0)),
                    reads=["ones16", ("pT", s)], writes=[pk2], signal=(i == 0))
                if i > 0:
                    P.op("pe", lambda e, ps2=ps2, s=s: e.matmul(
                        ps2[:, 128:256], lhsT=ones16[:], rhs=pT[s][:, 128:256], start=False, stop=True),
                        reads=["ones16", ("pT", s)], writes=[pk2])
                if LV == 4:
                    P.op("dve", lambda e, ps2=ps2, s=s: e.tensor_copy(out=lg[s][:, 0:256], in_=ps2[:, 0:256]), reads=[pk2], writes=[("lg", s)])
                    continue
                nv = numv[:, 128 * i:128 * (i + 1), r]
                dv = denv[:, 128 * i:128 * (i + 1), r]
                if g == 0:
                    P.op("dve", lambda e, ps2=ps2, nv=nv: e.tensor_copy(out=nv, in_=ps2[:, 0:128]), reads=[pk2], writes=["num"])
                    P.op("dve", lambda e, ps2=ps2, dv=dv: e.tensor_copy(out=dv, in_=ps2[:, 128:256]), reads=[pk2], writes=["den"])
                else:
                    P.op("dve", lambda e, ps2=ps2, nv=nv: e.tensor_tensor(out=nv, in0=ps2[:, 0:128], in1=nv, op=ALU.add),
                         reads=[pk2, "num"], writes=["num"])
                    P.op("dve", lambda e, ps2=ps2, dv=dv: e.tensor_tensor(out=dv, in0=ps2[:, 128:256], in1=dv, op=ALU.add),
                         reads=[pk2, "den"], writes=["den"])
    if int(os.environ.get("ATT_LV", "9")) < 9:
        P.op("dve", lambda e: e.memset(o16[:], 0.0), reads=[("lg", 0), ("lg", 1), ("lg", 2), ("pT", 0), ("pT", 1), ("pT", 2), "num", "den"], writes=["o16"])
        cx.store(out, o16[:], ["o16"], "oT")
        return cx.finish()
    P.op("dve", lambda e: e.reciprocal(out=den[:], in_=den[:]), reads=["den"], writes=["den"])
    P.op("dve", lambda e: e.tensor_tensor(out=o16[:], in0=num[:], in1=den[:], op=ALU.mult), reads=["num", "den"], writes=["o16"])
    cx.store(out, o16[:], ["o16"], "oT")
    return cx.finish()


def t5_bucket_np(dist):
    dist = np.asarray(dist, np.int32)
    max_exact = 16
    d32 = np.maximum(dist, 1).astype(np.float32)
    large = max_exact + (np.log(d32 / np.float32(max_exact)) / np.float32(np.log(2048 / 16)) * np.float32(16)).astype(np.int32)
    return np.where(dist < max_exact, dist, np.minimum(large, 31))


def attn_bias_mats(rel_bias, slot):
    Bm = np.full((128, 3, 256), -1e30, np.float32)
    ik = np.arange(128)[:, None]
    jq = np.arange(128)[None, :]
    for g, d in enumerate(DILS):
        head = 4 * g + slot
        rel = jq - ik
        bd = rel_bias[t5_bucket_np(np.maximum(rel, 0) * d), head]
        Bm[:, g, 0:128] = np.where(rel >= 0, bd, np.float32(-1e30))
        rel2 = jq - ik + 128
        bo = rel_bias[t5_bucket_np(np.minimum(rel2, 128) * d), head]
        Bm[:, g, 128:256] = np.where(rel2 <= 128, bo, np.float32(-1e30))
    return Bm


def run_attn(projT, rel_bias):
    nc = get_nc("attn", build_attn)
    ins = []
    for i in range(NCORES):
        b, slot = i // 4, i % 4
        QT = np.zeros((128, 3, SEQ), NPBF)
        KT = np.zeros((128, 3, SEQ), NPBF)
        V = np.zeros((128, 3, 32, 128), NPBF)
        for g, d in enumerate(DILS):
            head = 4 * g + slot
            perm = np.arange(SEQ).reshape(SEQ // d, d).T.reshape(-1)
            QT[:, g, :] = projT[b, 1024 + head * 128:1024 + (head + 1) * 128, :][:, perm]
            KT[:, g, :] = projT[b, 2560 + head * 128:2560 + (head + 1) * 128, :][:, perm]
            vt = projT[b, 4096 + head * 128:4096 + (head + 1) * 128, :][:, perm]
            V[:, g, :, :] = vt.T.reshape(32, 128, 128).transpose(1, 0, 2)
        ins.append({"QT": QT, "KT": KT, "V": V, "Bm": attn_bias_mats(rel_bias, slot)})
    res = run(nc, ins)
    yT = np.zeros((2, 512, SEQ), NPBF)
    for i in range(NCORES):
        b, slot = i // 4, i % 4
        yT[b, slot * 128:(slot + 1) * 128, :] = res[i]["oT"]
    return yT


NPW = 33
TWO_PI = 6.283185307179586
C1 = 6.28125
C2 = TWO_PI - C1


def ssm_nvec():
    n = [7 - k for k in range(8)] + [t - 7 for t in range(8)] + [t + 1 for t in range(8)] + [8 * 2 ** j for j in range(9)]
    return np.asarray(n, np.float32)


def build_ssm():
    cx = Ctx()
    P = cx.P
    G = 8
    NCH = 512
    Ud = cx.din("U", [128, G, 2, NCH], BF16)
    prm = cx.din("prm", [128, 3, G], F32)
    Bd = cx.din("Bri", [128, 2, G, 16], F32)
    Cd = cx.din("Cri", [128, 2, G, 16], F32)
    Dd = cx.din("Dcol", [128, G], F32)
    nvd = cx.din("nvec", [128, NPW], F32)
    mkd = cx.din("maskT", [128, 128], F32)
    idd = cx.din("ident", [128, 128], F32)
    out = cx.dout("ypre", [128, G, 2, NCH], F32)
    cx.alloc_psum(8)
    U = cx.sb("U_sb", [128, G, 2, NCH], BF16)
    prm_s = cx.sb("prm_s", [128, 3, G], F32)
    Bri = cx.sb("Bri_s", [128, 2, G, 16], F32)
    Cri = cx.sb("Cri_s", [128, 2, G, 16], F32)
    Dcol = cx.sb("Dcol_s", [128, G], F32)
    nvec = cx.sb("nvec_s", [128, NPW], F32)
    maskT = cx.sb("maskT_s", [128, 128], F32)
    ident = cx.sb("ident_s", [128, 128], F32)
    for g in range(G):
        cx.load(U[:, g, :, :].rearrange("p b c -> p (b c)"), Ud[:, g, :, :].rearrange("p b c -> p (b c)"), ("U", g))
    cx.load(prm_s[:], prm, "prm")
    cx.load(Bri[:].rearrange("p a g h -> p (a g h)"), Bd.rearrange("p a g h -> p (a g h)"), "Bri")
    cx.load(Cri[:].rearrange("p a g h -> p (a g h)"), Cd.rearrange("p a g h -> p (a g h)"), "Cri")
    cx.load(Dcol[:], Dd, "Dcol")
    cx.load(nvec[:], nvd, "nvec")
    cx.load(maskT[:], mkd, "maskT")
    cx.load(ident[:], idd, "ident")

    sm = lambda name, shape: cx.sb(name, shape, F32)
    dt_ = sm("dt_", [128, G]); lr = sm("lr", [128, G]); li = sm("li", [128, G])
    ang = sm("ang", [128, G, NPW]); mag = sm("mag", [128, G, NPW]); kf = sm("kf", [128, G, NPW])
    ki = cx.sb("ki", [128, G, NPW], I32)
    msk = sm("msk", [128, G, NPW]); rs = sm("rs", [128, G, NPW]); rc = sm("rc", [128, G, NPW])
    Pr = sm("Pr", [128, G, NPW]); Pi = sm("Pi", [128, G, NPW]); nPi = sm("nPi", [128, G, NPW])
    K = "ssmprep"

    def dve(fn, reads=(), writes=()):
        P.op("dve", fn, reads=[K] + list(reads), writes=[K] + list(writes))

    def act(fn, reads=(), writes=()):
        P.op("act", fn, reads=[K] + list(reads), writes=[K] + list(writes))

    act(lambda e: e.activation(out=dt_[:], in_=prm_s[:, 2, :], func=AF.Exp), reads=["prm"])
    dve(lambda e: e.tensor_tensor(out=lr[:], in0=prm_s[:, 0, :], in1=dt_[:], op=ALU.mult))
    dve(lambda e: e.tensor_tensor(out=li[:], in0=prm_s[:, 1, :], in1=dt_[:], op=ALU.mult))
    for g in range(G):
        dve(lambda e, g=g: e.tensor_scalar(out=ang[:, g, :], in0=nvec[:], scalar1=li[:, g:g + 1], scalar2=None, op0=ALU.mult),
            reads=["nvec"])
        act(lambda e, g=g: e.activation(out=mag[:, g, :], in_=nvec[:], func=AF.Exp, scale=lr[:, g:g + 1]), reads=["nvec"])

    def reduce_sin(src_fn, dst):
        dve(lambda e: e.tensor_scalar(out=kf[:], in0=src_fn(), scalar1=1.0 / TWO_PI, scalar2=None, op0=ALU.mult))
        dve(lambda e: e.tensor_copy(out=ki[:], in_=kf[:]))
        dve(lambda e: e.tensor_copy(out=kf[:], in_=ki[:]))
        dve(lambda e: e.scalar_tensor_tensor(out=dst[:], in0=kf[:], scalar=-C1, in1=src_fn(), op0=ALU.mult, op1=ALU.add))
        dve(lambda e: e.scalar_tensor_tensor(out=dst[:], in0=kf[:], scalar=-C2, in1=dst[:], op0=ALU.mult, op1=ALU.add))
        dve(lambda e: e.tensor_scalar(out=msk[:], in0=dst[:], scalar1=float(np.pi), scalar2=None, op0=ALU.is_gt))
        dve(lambda e: e.scalar_tensor_tensor(out=dst[:], in0=msk[:], scalar=-TWO_PI, in1=dst[:], op0=ALU.mult, op1=ALU.add))
        dve(lambda e: e.tensor_scalar(out=msk[:], in0=dst[:], scalar1=-float(np.pi), scalar2=None, op0=ALU.is_lt))
        dve(lambda e: e.scalar_tensor_tensor(out=dst[:], in0=msk[:], scalar=TWO_PI, in1=dst[:], op0=ALU.mult, op1=ALU.add))
        dve(lambda e: e.tensor_scalar(out=dst[:], in0=dst[:], scalar1=3.1415925, scalar2=-3.1415925, op0=ALU.min, op1=ALU.max))
        act(lambda e: e.activation(out=dst[:], in_=dst[:], func=AF.Sin))

    reduce_sin(lambda: ang[:], rs)
    dve(lambda e: e.tensor_scalar(out=ang[:], in0=ang[:], scalar1=float(np.pi / 2), scalar2=None, op0=ALU.add))
    reduce_sin(lambda: ang[:], rc)
    dve(lambda e: e.tensor_tensor(out=Pr[:], in0=mag[:], in1=rc[:], op=ALU.mult))
    dve(lambda e: e.tensor_tensor(out=Pi[:], in0=mag[:], in1=rs[:], op=ALU.mult))
    dve(lambda e: e.tensor_scalar(out=nPi[:], in0=Pi[:], scalar1=-1.0, scalar2=None, op0=ALU.mult))

    xr = sm("xr", [128, G]); abi = sm("abi", [128, G]); den_ = sm("den_", [128, G]); t1 = sm("t1", [128, G]); t2 = sm("t2", [128, G])
    cr = sm("cr", [128, G]); ci = sm("ci", [128, G]); nci = sm("nci", [128, G])
    are = prm_s[:, 0, :]
    aim = prm_s[:, 1, :]
    dve(lambda e: e.tensor_scalar(out=xr[:], in0=Pr[:, :, 16], scalar1=-1.0, scalar2=None, op0=ALU.add))
    dve(lambda e: e.tensor_copy(out=abi[:], in_=Pi[:, :, 16]))
    dve(lambda e: e.tensor_tensor(out=t1[:], in0=are, in1=are, op=ALU.mult))
    dve(lambda e: e.tensor_tensor(out=t2[:], in0=aim, in1=aim, op=ALU.mult))
    dve(lambda e: e.tensor_tensor(out=den_[:], in0=t1[:], in1=t2[:], op=ALU.add))
    dve(lambda e: e.reciprocal(out=den_[:], in_=den_[:]))
    dve(lambda e: e.tensor_tensor(out=t1[:], in0=xr[:], in1=are, op=ALU.mult))
    dve(lambda e: e.tensor_tensor(out=t2[:], in0=abi[:], in1=aim, op=ALU.mult))
    dve(lambda e: e.tensor_tensor(out=cr[:], in0=t1[:], in1=t2[:], op=ALU.add))
    dve(lambda e: e.tensor_tensor(out=cr[:], in0=cr[:], in1=den_[:], op=ALU.mult))
    dve(lambda e: e.tensor_tensor(out=t1[:], in0=abi[:], in1=are, op=ALU.mult))
    dve(lambda e: e.tensor_tensor(out=t2[:], in0=xr[:], in1=aim, op=ALU.mult))
    dve(lambda e: e.tensor_tensor(out=ci[:], in0=t1[:], in1=t2[:], op=ALU.subtract))
    dve(lambda e: e.tensor_tensor(out=ci[:], in0=ci[:], in1=den_[:], op=ALU.mult))
    dve(lambda e: e.tensor_scalar(out=nci[:], in0=ci[:], scalar1=-1.0, scalar2=None, op0=ALU.mult))

    Bbr = sm("Bbr", [128, G, 16]); Bbi = sm("Bbi", [128, G, 16])
    BX = sm("BX", [128, G, 16]); BY = sm("BY", [128, G, 16]); nBX = sm("nBX", [128, G, 16])
    CX = sm("CX", [128, G, 16]); CY = sm("CY", [128, G, 16])
    for g in range(G):
        dve(lambda e, g=g: e.tensor_scalar(out=Bbr[:, g, :], in0=Bri[:, 0, g, :], scalar1=cr[:, g:g + 1], scalar2=None, op0=ALU.mult), reads=["Bri"])
        dve(lambda e, g=g: e.scalar_tensor_tensor(out=Bbr[:, g, :], in0=Bri[:, 1, g, :], scalar=nci[:, g:g + 1], in1=Bbr[:, g, :],
                                               op0=ALU.mult, op1=ALU.add))
        dve(lambda e, g=g: e.tensor_scalar(out=Bbi[:, g, :], in0=Bri[:, 1, g, :], scalar1=cr[:, g:g + 1], scalar2=None, op0=ALU.mult))
        dve(lambda e, g=g: e.scalar_tensor_tensor(out=Bbi[:, g, :], in0=Bri[:, 0, g, :], scalar=ci[:, g:g + 1], in1=Bbi[:, g, :],
                                               op0=ALU.mult, op1=ALU.add))
    lo, hi = slice(0, 64), slice(64, 128)
    dve(lambda e: e.tensor_copy(out=BX[lo], in_=Bbr[lo]))
    dve(lambda e: e.tensor_copy(out=BX[hi], in_=Bbi[hi]))
    dve(lambda e: e.tensor_scalar(out=BY[lo], in0=Bbi[lo], scalar1=-1.0, scalar2=None, op0=ALU.mult))
    dve(lambda e: e.tensor_copy(out=BY[hi], in_=Bbr[hi]))
    dve(lambda e: e.tensor_scalar(out=nBX[:], in0=BX[:], scalar1=-1.0, scalar2=None, op0=ALU.mult))
    dve(lambda e: e.tensor_copy(out=CX[lo], in_=Cri[lo, 0, :, :]), reads=["Cri"])
    dve(lambda e: e.tensor_scalar(out=CX[hi], in0=Cri[hi, 1, :, :], scalar1=-1.0, scalar2=None, op0=ALU.mult))
    dve(lambda e: e.tensor_scalar(out=CY[lo], in0=Cri[lo, 1, :, :], scalar1=-1.0, scalar2=None, op0=ALU.mult))
    dve(lambda e: e.tensor_scalar(out=CY[hi], in0=Cri[hi, 0, :, :], scalar1=-1.0, scalar2=None, op0=ALU.mult))

    BcA = sm("BcA", [128, G, 8, 16]); BcB = sm("BcB", [128, G, 8, 16]); Cm = sm("Cm", [128, G, 8, 16]); Cc = sm("Cc", [128, G, 8, 16])
    for g in range(G):
        for j in range(8):
            dve(lambda e, g=g, j=j: e.tensor_scalar(out=BcA[:, g, j, :], in0=BX[:, g, :], scalar1=Pr[:, g, j:j + 1], scalar2=None, op0=ALU.mult))
            dve(lambda e, g=g, j=j: e.scalar_tensor_tensor(out=BcA[:, g, j, :], in0=BY[:, g, :], scalar=Pi[:, g, j:j + 1], in1=BcA[:, g, j, :],
                                                        op0=ALU.mult, op1=ALU.add))
            dve(lambda e, g=g, j=j: e.tensor_scalar(out=BcB[:, g, j, :], in0=BY[:, g, :], scalar1=Pr[:, g, j:j + 1], scalar2=None, op0=ALU.mult))
            dve(lambda e, g=g, j=j: e.scalar_tensor_tensor(out=BcB[:, g, j, :], in0=nBX[:, g, :], scalar=Pi[:, g, j:j + 1], in1=BcB[:, g, j, :],
                                                        op0=ALU.mult, op1=ALU.add))
            for dst, k0 in ((Cm, 8), (Cc, 16)):
                dve(lambda e, g=g, j=j, dst=dst, k0=k0: e.tensor_scalar(out=dst[:, g, j, :], in0=CX[:, g, :], scalar1=Pr[:, g, k0 + j:k0 + j + 1],
                                                                   scalar2=None, op0=ALU.mult))
                dve(lambda e, g=g, j=j, dst=dst, k0=k0: e.scalar_tensor_tensor(out=dst[:, g, j, :], in0=CY[:, g, :], scalar=Pi[:, g, k0 + j:k0 + j + 1],
                                                                          in1=dst[:, g, j, :], op0=ALU.mult, op1=ALU.add))

    MT16 = cx.sb("MT16", [128, G, 128], BF16)
    BaT16 = cx.sb("BaT16", [128, G, 128], BF16)
    BbT16 = cx.sb("BbT16", [128, G, 128], BF16)
    Cc16 = cx.sb("Cc16", [128, G, 128], BF16)
    dve(lambda e: e.tensor_copy(out=Cc16[:].rearrange("p g n -> p (g n)"), in_=Cc[:].rearrange("p g t h -> p (g t h)")), writes=["Cc16"])
    for g in range(G):
        bca = BcA[:, g, :, :].rearrange("p s h -> p (s h)")
        bcb = BcB[:, g, :, :].rearrange("p s h -> p (s h)")
        cm = Cm[:, g, :, :].rearrange("p t h -> p (t h)")
        ps, pk = cx.psum()
        P.op("pe", lambda e, ps=ps, bca=bca, cm=cm: e.matmul(ps[:, 0:128], lhsT=bca, rhs=cm, start=True, stop=True), reads=[K], writes=[pk])
        P.op("dve", lambda e, ps=ps, g=g: e.tensor_tensor(out=MT16[:, g, :], in0=ps[:, 0:128], in1=maskT[:], op=ALU.mult),
             reads=[pk, "maskT"], writes=[("MT16", g)])
        ps, pk = cx.psum()
        P.op("pe", lambda e, ps=ps, bca=bca: e.transpose(ps[:, 0:128], bca, ident[:]), reads=[K, "ident"], writes=[pk])
        P.op("dve", lambda e, ps=ps, g=g: e.tensor_copy(out=BaT16[:, g, :], in_=ps[:, 0:128]), reads=[pk], writes=[("BaT16", g)])
        ps, pk = cx.psum()
        P.op("pe", lambda e, ps=ps, bcb=bcb: e.transpose(ps[:, 0:128], bcb, ident[:]), reads=[K, "ident"], writes=[pk])
        P.op("dve", lambda e, ps=ps, g=g: e.tensor_copy(out=BbT16[:, g, :], in_=ps[:, 0:128]), reads=[pk], writes=[("BbT16", g)])

    SA = [cx.sb("SA%d" % i, [128, 2, NCH], F32) for i in range(2)]
    SB = [cx.sb("SB%d" % i, [128, 2, NCH], F32) for i in range(2)]
    S16 = [cx.sb("S16_%d" % i, [128, 2, NCH], BF16) for i in range(2)]
    ysb = [cx.sb("ysb%d" % i, [128, 2, NCH], F32) for i in range(2)]
    for i in range(2):
        P.op("pool", lambda e, i=i: e.memset(S16[i][:], 0.0), writes=[("S16", i)])
    for g in range(G):
        for (WT, dst, dk) in ((BaT16, SA[0], "SA0"), (BbT16, SB[0], "SB0")):
            for b in range(2):
                ps, pk = cx.psum()
                P.op("pe", lambda e, ps=ps, WT=WT, g=g, b=b: e.matmul(ps[:], lhsT=WT[:, g, :], rhs=U[:, g, b, :], start=True, stop=True),
                     reads=[("BaT16", g), ("BbT16", g), ("U", g)], writes=[pk])
                P.op("dve", lambda e, ps=ps, dst=dst, b=b: e.tensor_copy(out=dst[:, b, :], in_=ps[:]), reads=[pk], writes=[(dk, b)])
        cur = 0
        for j in range(9):
            d = 2 ** j
            nxt = 1 - cur
            oA, oB, nA, nB = SA[cur], SB[cur], SA[nxt], SB[nxt]
            kA = ["SA%d" % cur, ("SA%d" % cur, 0), ("SA%d" % cur, 1)]
            kB = ["SB%d" % cur, ("SB%d" % cur, 0), ("SB%d" % cur, 1)]
            wA = ["SA%d" % nxt, ("SA%d" % nxt, 0), ("SA%d" % nxt, 1)]
            wB = ["SB%d" % nxt, ("SB%d" % nxt, 0), ("SB%d" % nxt, 1)]
            pr = Pr[:, g, 24 + j:25 + j]
            pi = Pi[:, g, 24 + j:25 + j]
            npi = nPi[:, g, 24 + j:25 + j]
            P.op("dve", lambda e, oA=oA, nA=nA, d=d, pr=pr: e.scalar_tensor_tensor(
                out=nA[:, :, d:NCH], in0=oA[:, :, 0:NCH - d], scalar=pr, in1=oA[:, :, d:NCH], op0=ALU.mult, op1=ALU.add),
                reads=kA + [K], writes=wA)
            P.op("dve", lambda e, oB=oB, nA=nA, d=d, pi=pi: e.scalar_tensor_tensor(
                out=nA[:, :, d:NCH], in0=oB[:, :, 0:NCH - d], scalar=pi, in1=nA[:, :, d:NCH], op0=ALU.mult, op1=ALU.add),
                reads=kB + wA, writes=wA)
            P.op("act", lambda e, oA=oA, nA=nA, d=d: e.activation(out=nA[:, :, 0:d], in_=oA[:, :, 0:d], func=AF.Identity), reads=kA, writes=wA)
            P.op("dve", lambda e, oB=oB, nB=nB, d=d, pr=pr: e.scalar_tensor_tensor(
                out=nB[:, :, d:NCH], in0=oB[:, :, 0:NCH - d], scalar=pr, in1=oB[:, :, d:NCH], op0=ALU.mult, op1=ALU.add),
                reads=kB, writes=wB)
            P.op("dve", lambda e, oA=oA, nB=nB, d=d, npi=npi: e.scalar_tensor_tensor(
                out=nB[:, :, d:NCH], in0=oA[:, :, 0:NCH - d], scalar=npi, in1=nB[:, :, d:NCH], op0=ALU.mult, op1=ALU.add),
                reads=kA + wB, writes=wB)
            P.op("act", lambda e, oB=oB, nB=nB, d=d: e.activation(out=nB[:, :, 0:d], in_=oB[:, :, 0:d], func=AF.Identity), reads=kB, writes=wB)
            cur = nxt
        fin = SA[cur]
        s16 = S16[g % 2]
        P.op("act", lambda e, fin=fin, s16=s16: e.activation(out=s16[:, :, 1:NCH], in_=fin[:, :, 0:NCH - 1], func=AF.Identity),
             reads=["SA%d" % cur], writes=[("S16", g % 2)])
        for b in range(2):
            ps, pk = cx.psum()
            P.op("pe", lambda e, ps=ps, g=g, b=b: e.matmul(ps[:], lhsT=MT16[:, g, :], rhs=U[:, g, b, :], start=True, stop=False),
                 reads=[("MT16", g), ("U", g)], writes=[pk], signal=False)
            P.op("pe", lambda e, ps=ps, g=g, b=b, s16=s16: e.matmul(ps[:], lhsT=Cc16[:, g, :], rhs=s16[:, b, :], start=False, stop=True),
                 reads=["Cc16", ("S16", g % 2)], writes=[pk])
            y = ysb[g % 2]
            P.op("dve", lambda e, ps=ps, g=g, b=b, y=y: e.scalar_tensor_tensor(
                out=y[:, b, :], in0=U[:, g, b, :], scalar=Dcol[:, g:g + 1], in1=ps[:], op0=ALU.mult, op1=ALU.add),
                reads=[pk, "Dcol", ("U", g)], writes=[("ysb", g % 2, b)])
        cx.store(out[:, g, :, :].rearrange("p b c -> p (b c)"), ysb[g % 2][:].rearrange("p b c -> p (b c)"),
                 [("ysb", g % 2, 0), ("ysb", g % 2, 1)], ("out", g), sem=("st_ysb", g % 2))
    return cx.finish()


def ssm_inputs(projT, l, inp, core):
    G = 8
    gs = slice(core * G, (core + 1) * G)
    u = projT[:, core * 128:(core + 1) * 128, :]
    U = u.reshape(2, G, 16, 512, 8).transpose(4, 2, 1, 0, 3).reshape(128, G, 2, 512)
    dup = lambda a: np.concatenate([a, a], axis=0)
    are = inp["ssm_a_re"][l][gs].T
    aim = inp["ssm_a_im"][l][gs].T
    ldt = np.broadcast_to(inp["ssm_log_dt"][l][gs][None, :], (64, G))
    prm = dup(np.stack([are, aim, ldt], axis=1))
    Bri = dup(np.stack([inp["ssm_b_re"][l][gs].transpose(1, 0, 2), inp["ssm_b_im"][l][gs].transpose(1, 0, 2)], axis=1))
    Cri = dup(np.stack([inp["ssm_c_re"][l][gs].transpose(2, 0, 1), inp["ssm_c_im"][l][gs].transpose(2, 0, 1)], axis=1))
    dsk = inp["ssm_d"][l][core * 128:(core + 1) * 128].reshape(G, 16)
    Dcol = np.tile(dsk.T, (8, 1))
    sidx = np.arange(128) // 16
    maskT = (sidx[:, None] <= sidx[None, :]).astype(np.float32)
    return {"U": np.ascontiguousarray(U), "prm": np.ascontiguousarray(prm, dtype=np.float32),
            "Bri": np.ascontiguousarray(Bri, dtype=np.float32), "Cri": np.ascontiguousarray(Cri, dtype=np.float32),
            "Dcol": np.ascontiguousarray(Dcol, dtype=np.float32),
            "nvec": np.ascontiguousarray(np.broadcast_to(ssm_nvec()[None, :], (128, NPW))),
            "maskT": maskT, "ident": np.eye(128, dtype=np.float32)}


def run_ssm(projT, l, inp):
    nc = get_nc("ssm", build_ssm)
    ins = [ssm_inputs(projT, l, inp, i) for i in range(NCORES)]
    res = run(nc, ins)
    yT = np.zeros((2, 1024, SEQ), np.float32)
    for i in range(NCORES):
        y = res[i]["ypre"]
        yT[:, i * 128:(i + 1) * 128, :] = y.reshape(8, 16, 8, 2, 512).transpose(3, 2, 1, 4, 0).reshape(2, 128, SEQ)
    return yT


def build_post(moe):
    cx = Ctx()
    P = cx.P
    xTd = cx.din("xT", [128, DC, T], F32)
    ypd = cx.din("ypreT", [128, 8, T], F32)
    yad = cx.din("yattT", [128, 4, T], BF16)
    gsd = cx.din("gsT", [128, DC, T], BF16)
    gad = cx.din("gaT", [128, DC, T], BF16)
    modd = cx.din("modv", [128, 6, DC], F32)
    gnd = cx.din("gnorm", [128, DC], F32)
    bgd = cx.din("bglu", [128, 8], F32)
    Wglu = cx.din("w_glu", [1024, 1024], F32)
    Wbs = cx.din("w_bs", [1024, D], F32)
    Wba = cx.din("w_ba", [512, D], F32)
    Wout = cx.din("w_out", [D, D], F32)
    xo = cx.dout("xT_out", [128, DC, T], F32)
    ho = cx.dout("h2T", [128, DC, T], BF16)
    if moe:
        wrd = cx.din("w_router", [128, DC, NEXP], F32)
        rwo = cx.dout("rw", [T // 128, 128, NEXP], F32)
    cx.alloc_psum(8)
    xT = cx.sb("xT_sb", [128, DC, T], F32)
    bufA = cx.sb("bufA", [128, DC, T], BF16)
    y32 = bufA[:].bitcast(F32).rearrange("p (k t) -> p k t", k=8) if False else None
    y32t = cx.sb("y32", [128, 8, T // 2], F32) if False else None
    y16 = cx.sb("y16", [128, 8, T], BF16)
    yg16 = cx.sb("yg16", [128, 8, T], BF16)
    ya16 = cx.sb("ya16", [128, 4, T], BF16)
    modv = cx.sb("modv_sb", [128, 6, DC], F32)
    gnorm = cx.sb("gnorm_sb", [128, DC], F32)
    gm = cx.sb("gm_sb", [128, DC], F32)
    bglu = cx.sb("bglu_sb", [128, 8], F32)
    ones16 = cx.sb("ones16", [128, 128], BF16)
    P.op("pool", lambda e: e.memset(ones16[:], 1.0), writes=["ones16"])
    for k0 in range(0, DC, 4):
        P.dma("sp", lambda e, k0=k0: e.dma_start(out=xT[:, k0:k0 + 4, :], in_=xTd[:, k0:k0 + 4, :]), writes=["xT"], sem="xT")
    cx.load(ya16[:], yad, "ya16")
    cx.load(modv[:], modd, "modv_in")
    cx.load(gnorm[:], gnd, "gnorm")
    cx.load(bglu[:], bgd, "bglu")
    wl = WLoader(cx, "wl", DC, 128, nslots=4, nstage=4)

    y32 = bufA[:].rearrange("p k t -> p (k t)").bitcast(F32).rearrange("p (k t) -> p k t", k=8)
    yp = [cx.sb("yp%d" % i, [128, T], F32) for i in range(2)]
    ta = [cx.sb("ta%d" % i, [128, T], F32) for i in range(2)]
    for kc in range(8):
        s = kc % 2
        cx.load(yp[s][:], ypd[:, kc, :], ("yp", s))
        P.op("act", lambda e, s=s: e.activation(out=ta[s][:], in_=yp[s][:], func=AF.Square), reads=[("yp", s)], writes=[("ta", s)])
        P.op("dve", lambda e, s=s: e.tensor_scalar(out=ta[s][:], in0=ta[s][:], scalar1=0.044715, scalar2=1.0, op0=ALU.mult, op1=ALU.add),
             reads=[("ta", s)], writes=[("ta", s)])
        P.op("dve", lambda e, s=s: e.tensor_tensor(out=ta[s][:], in0=ta[s][:], in1=yp[s][:], op=ALU.mult), reads=[("ta", s), ("yp", s)], writes=[("ta", s)])
        P.op("act", lambda e, s=s: e.activation(out=ta[s][:], in_=ta[s][:], func=AF.Sigmoid, scale=1.5957691216057308),
             reads=[("ta", s)], writes=[("ta", s)])
        P.op("dve", lambda e, s=s, kc=kc: e.tensor_tensor(out=y32[:, kc, :], in0=ta[s][:], in1=yp[s][:], op=ALU.mult),
             reads=[("ta", s), ("yp", s)], writes=["bufA"])
        P.op("pool", lambda e, kc=kc: e.tensor_copy(out=y16[:, kc, :], in_=y32[:, kc, :]), reads=["bufA"], writes=["y16"])

    sg = [cx.sb("sg%d" % i, [128, 512], F32) for i in range(2)]
    cnt = [0]

    def evac_glu(nt, half, ps, pk):
        s = cnt[0] % 2
        cnt[0] += 1
        hs = slice(half * 512, (half + 1) * 512)
        P.op("act", lambda e: e.activation(out=sg[s][:], in_=ps[:], func=AF.Sigmoid, bias=bglu[:, nt:nt + 1]),
             reads=[pk, "bglu"], writes=[("sg", s)])
        P.op("dve", lambda e: e.tensor_tensor(out=yg16[:, nt, hs], in0=sg[s][:], in1=y32[:, nt, hs], op=ALU.mult),
             reads=[("sg", s), "bufA"], writes=["yg16"])

    wl.KC = 8
    emit_linear(cx, wl, Wglu, 8, 1024, y16, ["y16"], evac_glu, ngrp=128)

    gts = [cx.sb("gts%d" % i, [128, 512], BF16) for i in range(2)]
    gta = [cx.sb("gta%d" % i, [128, 512], BF16) for i in range(2)]
    m1 = [cx.sb("m1_%d" % i, [128, 512], F32) for i in range(2)]
    m2 = [cx.sb("m2_%d" % i, [128, 512], F32) for i in range(2)]
    it = 0
    for nt in range(DC):
        wl.KC = 8
        wbs, kbs = wl.load(Wbs[:, nt * 128:(nt + 1) * 128], 128)
        wl.KC = 4
        wba, kba = wl.load(Wba[:, nt * 128:(nt + 1) * 128], 128)
        for half in range(2):
            s = it % 2
            it += 1
            hs = slice(half * 512, (half + 1) * 512)
            cx.load(gts[s][:], gsd[:, nt, hs], ("gts", s))
            cx.load(gta[s][:], gad[:, nt, hs], ("gta", s))
            ps1, pk1 = cx.psum()
            mm_group(cx, ps1[:], pk1, [(wbs[:, kc, 0:128], yg16[:, kc, hs]) for kc in range(8)], list(kbs) + ["yg16"])
            ps2, pk2 = cx.psum()
            mm_group(cx, ps2[:], pk2, [(wba[:, kc, 0:128], ya16[:, kc, hs]) for kc in range(4)], list(kba) + ["ya16"])
            P.op("act", lambda e, s=s: e.activation(out=m1[s][:], in_=gts[s][:], func=AF.Sigmoid), reads=[("gts", s)], writes=[("m1", s)])
            P.op("act", lambda e, s=s: e.activation(out=m2[s][:], in_=gta[s][:], func=AF.Sigmoid), reads=[("gta", s)], writes=[("m2", s)])
            P.op("dve", lambda e, s=s, ps1=ps1: e.tensor_tensor(out=m1[s][:], in0=m1[s][:], in1=ps1[:], op=ALU.mult), reads=[("m1", s), pk1], writes=[("m1", s)])
            P.op("dve", lambda e, s=s, ps2=ps2: e.tensor_tensor(out=m2[s][:], in0=m2[s][:], in1=ps2[:], op=ALU.mult), reads=[("m2", s), pk2], writes=[("m2", s)])
            P.op("pool", lambda e, s=s, nt=nt, hs=hs: e.tensor_tensor(out=bufA[:, nt, hs], in0=m1[s][:], in1=m2[s][:], op=ALU.add),
                 reads=[("m1", s), ("m2", s)], writes=["bufA"])

    def evac_out(nt, half, ps, pk):
        hs = slice(half * 512, (half + 1) * 512)
        P.op("dve", lambda e: e.scalar_tensor_tensor(out=xT[:, nt, hs], in0=ps[:], scalar=modv[:, 2, nt:nt + 1], in1=xT[:, nt, hs],
                                                    op0=ALU.mult, op1=ALU.add), reads=[pk, "modv_in", "xT"], writes=["xT"])

    wl.KC = DC
    emit_linear(cx, wl, Wout, DC, D, bufA, ["bufA"], evac_out, ngrp=128)
    for k0 in range(0, DC, 4):
        cx.store(xo[:, k0:k0 + 4, :], xT[:, k0:k0 + 4, :], ["xT"], ("xo", k0), sem="st_x")

    P.op("dve", lambda e: e.tensor_scalar(out=gm[:], in0=modv[:, 4, :], scalar1=1.0, scalar2=None, op0=ALU.add), reads=["modv_in"], writes=["gm_tmp"])
    P.op("dve", lambda e: e.tensor_tensor(out=gm[:], in0=gm[:], in1=gnorm[:], op=ALU.mult), reads=["gm_tmp", "gnorm"], writes=["modv"])
    rstd = emit_rmsnorm_mod(cx, xT, "xT", gm, modv[:, 3, :], bufA, "bufA", ones16, "n2")
    for k0 in range(0, DC, 4):
        cx.store(ho[:, k0:k0 + 4, :], bufA[:, k0:k0 + 4, :], ["bufA"], ("ho", k0), sem="st_h")

    if moe:
        wr = cx.sb("wr_sb", [128, DC, NEXP], F32)
        cx.load(wr[:], wrd, "wr")
        h32 = [cx.sb("h32_%d" % i, [128, 128], F32) for i in range(3)]
        lgt = cx.sb("lgt", [128, NEXP], F32)
        mx8 = cx.sb("mx8", [128, 8], F32)
        nm1 = cx.sb("nm1", [128, 1], F32)
        sel = cx.sb("sel", [128, NEXP], F32)
        ex = cx.sb("ex", [128, NEXP], F32)
        ssum = cx.sb("ssum", [128, 1], F32)
        rwt = [cx.sb("rwt%d" % i, [128, NEXP], F32) for i in range(2)]
        R = "router"
        for tt in range(T // 128):
            ts_ = slice(tt * 128, (tt + 1) * 128)
            ps, pk = cx.psum()
            for kc in range(DC):
                s = kc % 3
                P.op("dve", lambda e, s=s, kc=kc, ts_=ts_: e.tensor_tensor(out=h32[s][:], in0=xT[:, kc, ts_], in1=rstd[:, ts_], op=ALU.mult),
                     reads=["xT", ("n2", "rstd", tt // 4)], writes=[("h32", s)])
                P.op("act", lambda e, s=s, kc=kc: e.activation(out=h32[s][:], in_=h32[s][:], func=AF.Identity, scale=gm[:, kc:kc + 1],
                                                            bias=modv[:, 3, kc:kc + 1]), reads=[("h32", s), "modv"], writes=[("h32", s)])
                P.op("pe", lambda e, s=s, kc=kc, ps=ps: e.matmul(ps[:, 0:NEXP], lhsT=h32[s][:], rhs=wr[:, kc, :], start=(kc == 0), stop=(kc == DC - 1)),
                     reads=[("h32", s), "wr"], writes=[pk], signal=True)
            P.op("dve", lambda e, ps=ps: e.tensor_copy(out=lgt[:], in_=ps[:, 0:NEXP]), reads=[pk, R], writes=[R])
            P.op("dve", lambda e: e.max(out=mx8[:], in_=lgt[:]), reads=[R], writes=[R])
            P.op("dve", lambda e: e.tensor_scalar(out=nm1[:], in0=mx8[:, 0:1], scalar1=-1.0, scalar2=None, op0=ALU.mult), reads=[R], writes=[R])
            P.op("dve", lambda e: e.tensor_scalar(out=sel[:], in0=lgt[:], scalar1=mx8[:, 1:2], scalar2=None, op0=ALU.is_ge), reads=[R], writes=[R])
            P.op("act", lambda e: e.activation(out=ex[:], in_=lgt[:], func=AF.Exp, bias=nm1[:, 0:1]), reads=[R], writes=[R])
            P.op("dve", lambda e: e.tensor_tensor(out=ex[:], in0=ex[:], in1=sel[:], op=ALU.mult), reads=[R], writes=[R])
            P.op("dve", lambda e: e.tensor_reduce(out=ssum[:], in_=ex[:], axis=mybir.AxisListType.X, op=ALU.add), reads=[R], writes=[R])
            P.op("dve", lambda e: e.reciprocal(out=ssum[:], in_=ssum[:]), reads=[R], writes=[R])
            o = rwt[tt % 2]
            P.op("dve", lambda e, o=o: e.tensor_scalar(out=o[:], in0=ex[:], scalar1=ssum[:, 0:1], scalar2=None, op0=ALU.mult),
                 reads=[R], writes=[R, ("rwt", tt % 2)])
            cx.store(rwo[tt], o[:], [("rwt", tt % 2)], ("rwo", tt), sem=("st_rw", tt % 2))
    return cx.finish()


def post_inputs(i, xTs, ypreT, yattT, projT, mod_l, l, inp, moe):
    b, q = i // 4, i % 4
    ts_ = slice(q * T, (q + 1) * T)
    m = {"xT": xTs[i], "ypreT": fm(ypreT[b][:, ts_]), "yattT": fm(yattT[b][:, ts_]),
         "gsT": fm(projT[b, 5632:7680, ts_]), "gaT": fm(projT[b, 7680:9728, ts_]),
         "modv": modv_layout(mod_l[b]), "gnorm": vec_fm(inp["norm_ffn_g"][l]), "bglu": vec_fm(inp["b_glu"][l]),
         "w_glu": np.ascontiguousarray(inp["w_glu"][l]), "w_bs": np.ascontiguousarray(inp["w_branch_ssm"][l]),
         "w_ba": np.ascontiguousarray(inp["w_branch_att"][l]), "w_out": np.ascontiguousarray(inp["w_out"][l])}
    if moe:
        m["w_router"] = np.ascontiguousarray(inp["moe_router"][l // 2].reshape(DC, 128, NEXP).transpose(1, 0, 2))
    return m


def run_post(xTs, ypreT, yattT, projT, mod_l, l, inp, moe):
    nc = get_nc("post%d" % int(moe), lambda: build_post(moe))
    ins = [post_inputs(i, xTs, ypreT, yattT, projT, mod_l, l, inp, moe) for i in range(NCORES)]
    res = run(nc, ins)
    xo = [res[i]["xT_out"] for i in range(NCORES)]
    h2 = [res[i]["h2T"] for i in range(NCORES)]
    rw = [res[i]["rw"].reshape(T, NEXP) for i in range(NCORES)] if moe else None
    return xo, h2, rw


FG = 4


def ffn_bufs(cx, tag, with_rw):
    g16 = [cx.sb("%s_g16_%d" % (tag, i), [128, FG, T], BF16) for i in range(2)]
    sgl = [cx.sb("%s_sg%d" % (tag, i), [128, 512], F32) for i in range(2)]
    tt = [cx.sb("%s_tt%d" % (tag, i), [128, 512], F32) for i in range(2)] if with_rw else None
    return g16, sgl, tt


def emit_ffn(cx, bufs, hT, hkeys, nft, get_gu, get_d, out_evac, tag, rwb=None, rwkey=None):
    P = cx.P
    g16, sgl, tt = bufs
    it = 0
    ngroups = (nft + FG - 1) // FG
    for fg in range(ngroups):
        gb = g16[fg % 2]
        nj = min(FG, nft - fg * FG)
        for j in range(nj):
            ft = fg * FG + j
            wg, wu, wkeys = get_gu(ft)
            for half in range(T // 512):
                hs = slice(half * 512, (half + 1) * 512)
                psg, pkg = cx.psum()
                mm_group(cx, psg[:], pkg, [(wg[:, kc, 0:128], hT[:, kc, hs]) for kc in range(DC)], list(wkeys) + list(hkeys))
                psu, pku = cx.psum()
                mm_group(cx, psu[:], pku, [(wu[:, kc, 0:128], hT[:, kc, hs]) for kc in range(DC)], list(wkeys) + list(hkeys))
                s = it % 2
                it += 1
                P.op("act", lambda e, s=s, psg=psg: e.activation(out=sgl[s][:], in_=psg[:], func=AF.Silu), reads=[pkg], writes=[(tag, "sg", s)])
                if rwb is None:
                    P.op("dve", lambda e, s=s, psu=psu, gb=gb, j=j, hs=hs: e.tensor_tensor(out=gb[:, j, hs], in0=sgl[s][:], in1=psu[:], op=ALU.mult),
                         reads=[(tag, "sg", s), pku], writes=[(tag, "g16", fg % 2, j)])
                else:
                    P.op("dve", lambda e, s=s, psu=psu: e.tensor_tensor(out=tt[s][:], in0=sgl[s][:], in1=psu[:], op=ALU.mult),
                         reads=[(tag, "sg", s), pku], writes=[(tag, "tt", s)])
                    P.op("pool", lambda e, s=s, gb=gb, j=j, hs=hs: e.tensor_tensor(out=gb[:, j, hs], in0=tt[s][:], in1=rwb[:, hs], op=ALU.mult),
                         reads=[(tag, "tt", s), rwkey], writes=[(tag, "g16", fg % 2, j)])
        wd, dkeys = get_d(fg)
        for dc in range(DC):
            for half in range(T // 512):
                hs = slice(half * 512, (half + 1) * 512)
                ps, pk = cx.psum()
                mm_group(cx, ps[:], pk, [(wd[:, j, dc * 128:(dc + 1) * 128], gb[:, j, hs]) for j in range(nj)],
                         list(dkeys) + [(tag, "g16", fg % 2, j) for j in range(nj)])
                out_evac(fg, dc, half, ps, pk)


class DLoader:
    def __init__(self, cx, name):
        self.cx = cx
        self.name = name
        self.wd = cx.sb(name + "_wd", [128, FG, D], BF16)
        self.stg = [cx.sb("%s_st%d" % (name, i), [128, D], F32) for i in range(2)]
        self.si = 0

    def load(self, Wd, fg, nj):
        cx = self.cx
        keys = []
        for j in range(nj):
            ft = fg * FG + j
            s = self.si % 2
            self.si += 1
            stg = self.stg[s]
            skey = (self.name, "st", s)
            cx.P.dma("sp", lambda e, stg=stg, ft=ft: e.dma_start(out=stg[:], in_=Wd[ft * 128:(ft + 1) * 128, :]), writes=[skey])
            key = (self.name, "wd", j)
            cx.copy(cx.conv_eng(), self.wd[:, j, :], stg[:], [skey], [key])
            keys.append(key)
        return self.wd, keys


def build_ffn():
    cx = Ctx()
    P = cx.P
    xTd = cx.din("xT", [128, DC, T], F32)
    hTd = cx.din("h2T", [128, DC, T], BF16)
    gfd = cx.din("gatef", [128, DC], F32)
    Wg = cx.din("w_gate", [D, DFF], F32)
    Wu = cx.din("w_up", [D, DFF], F32)
    Wd = cx.din("w_down", [DFF, D], F32)
    xo = cx.dout("xT_out", [128, DC, T], F32)
    cx.alloc_psum(8)
    xT = cx.sb("xT_sb", [128, DC, T], F32)
    hT = cx.sb("hT_sb", [128, DC, T], BF16)
    gf = cx.sb("gf_sb", [128, DC], F32)
    for k0 in range(0, DC, 4):
        P.dma("sp", lambda e, k0=k0: e.dma_start(out=hT[:, k0:k0 + 4, :], in_=hTd[:, k0:k0 + 4, :]), writes=["hT"], sem="hT")
    for k0 in range(0, DC, 4):
        P.dma("sp", lambda e, k0=k0: e.dma_start(out=xT[:, k0:k0 + 4, :], in_=xTd[:, k0:k0 + 4, :]), writes=["xT"], sem="xT")
    cx.load(gf[:], gfd, "gf")
    wl = WLoader(cx, "wgu", DC, 128, nslots=4, nstage=4)
    dl = DLoader(cx, "wdl")
    nft = DFF // 128

    def get_gu(ft):
        wg, k1 = wl.load(Wg[:, ft * 128:(ft + 1) * 128], 128)
        wu, k2 = wl.load(Wu[:, ft * 128:(ft + 1) * 128], 128)
        return wg, wu, list(k1) + list(k2)

    def get_d(fg):
        return dl.load(Wd, fg, min(FG, nft - fg * FG))

    def out_evac(fg, dc, half, ps, pk):
        hs = slice(half * 512, (half + 1) * 512)
        P.op("dve", lambda e: e.scalar_tensor_tensor(out=xT[:, dc, hs], in0=ps[:], scalar=gf[:, dc:dc + 1], in1=xT[:, dc, hs],
                                                    op0=ALU.mult, op1=ALU.add), reads=[pk, "gf", "xT"], writes=["xT"])

    emit_ffn(cx, ffn_bufs(cx, "ffn", False), hT, ["hT"], nft, get_gu, get_d, out_evac, "ffn")
    for k0 in range(0, DC, 4):
        cx.store(xo[:, k0:k0 + 4, :], xT[:, k0:k0 + 4, :], ["xT"], ("xo", k0), sem="st_x")
    return cx.finish()


NCHUNK = SEQ * 2 // T


def build_moe():
    cx = Ctx()
    P = cx.P
    nc = cx.nc
    hTd = cx.din("h2T", [NCHUNK, 128, DC, T], BF16)
    rwd = cx.din("rwb", [NCHUNK, 128, T], F32)
    Wg = cx.din("w_gate", [D, DFFE], F32)
    Wu = cx.din("w_up", [D, DFFE], F32)
    Wd = cx.din("w_down", [DFFE, D], F32)
    po = cx.dout("partial", [NCHUNK, 128, DC, T], BF16)
    Wg16 = nc.dram_tensor("Wg16", [D, DFFE], BF16, kind="Internal").ap()
    Wu16 = nc.dram_tensor("Wu16", [D, DFFE], BF16, kind="Internal").ap()
    Wd16 = nc.dram_tensor("Wd16", [DFFE, D], BF16, kind="Internal").ap()
    cx.alloc_psum(8)
    nft = DFFE // 128
    stg = [cx.sb("cv_st%d" % i, [128, 4, 512], F32) for i in range(3)]
    o16 = [cx.sb("cv_o%d" % i, [128, 4, 512], BF16) for i in range(3)]
    ci = 0
    for (Wsrc, Wdst, name, KC_, ncols) in ((Wg, Wg16, "Wg16", DC, DFFE), (Wu, Wu16, "Wu16", DC, DFFE), (Wd, Wd16, "Wd16", nft, D)):
        for kq in range(0, KC_, 4):
            for cb in range(ncols // 512):
                s = ci % 3
                ci += 1
                src = Wsrc[kq * 128:(kq + 4) * 128, cb * 512:(cb + 1) * 512].rearrange("(c p) n -> p c n", p=128)
                dst = Wdst[kq * 128:(kq + 4) * 128, cb * 512:(cb + 1) * 512].rearrange("(c p) n -> p c n", p=128)
                P.dma("sp", lambda e, s=s, src=src: e.dma_start(out=stg[s][:], in_=src), writes=[("cvst", s)])
                cx.copy(cx.conv_eng(), o16[s][:], stg[s][:], [("cvst", s)], [("cvo", s)])
                P.dma("pool", lambda e, s=s, dst=dst: e.dma_start(out=dst, in_=o16[s][:]), reads=[("cvo", s)], writes=[(name, kq // 4, cb)],
                      sem=("cvout", s))
    hT = cx.sb("hT_sb", [128, DC, T], BF16)
    rwb = cx.sb("rwb_sb", [128, T], F32)
    acc = cx.sb("acc", [128, DC, T], F32)
    wgt = [cx.sb("wg%d" % i, [128, DC, 128], BF16) for i in range(2)]
    wut = [cx.sb("wu%d" % i, [128, DC, 128], BF16) for i in range(2)]
    wdt = cx.sb("wdt", [128, FG, D], BF16)
    o16c = [cx.sb("oc%d" % i, [128, T], BF16) for i in range(2)]
    gi = [0]
    bufs = ffn_bufs(cx, "moe", True)
    for ch in range(NCHUNK):
        for k0 in range(0, DC, 4):
            P.dma("sp", lambda e, k0=k0, ch=ch: e.dma_start(out=hT[:, k0:k0 + 4, :], in_=hTd[ch, :, k0:k0 + 4, :]), writes=["hT"], sem="hT")
        cx.load(rwb[:], rwd[ch], "rwb")

        def get_gu(ft):
            s = gi[0] % 2
            gi[0] += 1
            dep = [("Wg16", kq, ft // 4) for kq in range(4)] + [("Wu16", kq, ft // 4) for kq in range(4)]
            srcg = Wg16[:, ft * 128:(ft + 1) * 128].rearrange("(c p) n -> p c n", p=128)
            srcu = Wu16[:, ft * 128:(ft + 1) * 128].rearrange("(c p) n -> p c n", p=128)
            P.dma("sp", lambda e, s=s, srcg=srcg: e.dma_start(out=wgt[s][:], in_=srcg), reads=dep, writes=[("wgt", s)])
            P.dma("sp", lambda e, s=s, srcu=srcu: e.dma_start(out=wut[s][:], in_=srcu), reads=dep, writes=[("wut", s)])
            return wgt[s], wut[s], [("wgt", s), ("wut", s)]

        def get_d(fg):
            keys = []
            for j in range(FG):
                ft = fg * FG + j
                dep = [("Wd16", ft // 4, cb) for cb in range(D // 512)]
                P.dma("sp", lambda e, j=j, ft=ft: e.dma_start(out=wdt[:, j, :], in_=Wd16[ft * 128:(ft + 1) * 128, :]), reads=dep, writes=[("wdt", j)])
                keys.append(("wdt", j))
            return wdt, keys

        def out_evac(fg, dc, half, ps, pk):
            hs = slice(half * 512, (half + 1) * 512)
            if fg == 0:
                P.op("dve", lambda e: e.tensor_copy(out=acc[:, dc, hs], in_=ps[:]), reads=[pk], writes=[("acc", dc, half)])
            else:
                P.op("dve", lambda e: e.tensor_tensor(out=acc[:, dc, hs], in0=ps[:], in1=acc[:, dc, hs], op=ALU.add),
                     reads=[pk, ("acc", dc, half)], writes=[("acc", dc, half)])

        emit_ffn(cx, bufs, hT, ["hT"], nft, get_gu, get_d, out_evac, "moe", rwb=rwb, rwkey="rwb")
        for dc in range(DC):
            s = dc % 2
            P.op("act", lambda e, s=s, dc=dc: e.activation(out=o16c[s][:], in_=acc[:, dc, :], func=AF.Identity),
                 reads=[("acc", dc, 0), ("acc", dc, 1)], writes=[("oc", s)])
            cx.store(po[ch, :, dc, :], o16c[s][:], [("oc", s)], ("po", ch, dc), sem=("st_oc", s))
    return cx.finish()


def build_final():
    cx = Ctx()
    P = cx.P
    xTd = cx.din("xT", [128, DC, T], F32)
    pd = cx.din("partials", [NEXP, 128, DC, T], BF16)
    gfd = cx.din("gatef", [128, DC], F32)
    gnd = cx.din("gfin", [128, DC], F32)
    out = cx.dout("outT", [128, DC, T], F32)
    cx.alloc_psum(4)
    xT = cx.sb("xT_sb", [128, DC, T], F32)
    gf = cx.sb("gf_sb", [128, DC], F32)
    gfin = cx.sb("gfin_sb", [128, DC], F32)
    zero = cx.sb("zero_sb", [128, DC], F32)
    ones16 = cx.sb("ones16", [128, 128], BF16)
    P.op("pool", lambda e: e.memset(ones16[:], 1.0), writes=["ones16"])
    P.op("pool", lambda e: e.memset(zero[:], 0.0), writes=["modv"])
    for k0 in range(0, DC, 4):
        P.dma("sp", lambda e, k0=k0: e.dma_start(out=xT[:, k0:k0 + 4, :], in_=xTd[:, k0:k0 + 4, :]), writes=["xT_in"], sem="xT")
    cx.load(gf[:], gfd, "gf")
    cx.load(gfin[:], gnd, "gfin")
    pb = [cx.sb("pb%d" % i, [128, NEXP, T], BF16) for i in range(2)]
    acc = [cx.sb("facc%d" % i, [128, T], F32) for i in range(2)]
    for kc in range(DC):
        s = kc % 2
        for e_ in range(NEXP):
            P.dma("sp", lambda e, s=s, e_=e_, kc=kc: e.dma_start(out=pb[s][:, e_, :], in_=pd[e_, :, kc, :]), writes=[("pb", s)], sem=("pb", s))
        P.op("dve", lambda e, s=s: e.tensor_tensor(out=acc[s][:], in0=pb[s][:, 0, :], in1=pb[s][:, 1, :], op=ALU.add), reads=[("pb", s)], writes=[("facc", s)])
        for e_ in range(2, NEXP):
            P.op("dve", lambda e, s=s, e_=e_: e.tensor_tensor(out=acc[s][:], in0=acc[s][:], in1=pb[s][:, e_, :], op=ALU.add),
                 reads=[("pb", s), ("facc", s)], writes=[("facc", s)])
        P.op("dve", lambda e, s=s, kc=kc: e.scalar_tensor_tensor(out=xT[:, kc, :], in0=acc[s][:], scalar=gf[:, kc:kc + 1], in1=xT[:, kc, :],
                                                              op0=ALU.mult, op1=ALU.add), reads=[("facc", s), "gf", "xT_in"], writes=["xT"])
    oT = cx.sb("oT_sb", [128, DC // 2, T], F32)
    P.op("dve", lambda e: e.tensor_copy(out=gfin[:], in_=gfin[:]), reads=["gfin"], writes=["modv"])
    rstd = emit_rmsnorm_stats(cx, xT, "xT", ones16, "nf")
    tmpf = [cx.sb("nf_t%d" % i, [128, T], F32) for i in range(2)]
    for kc in range(DC):
        s = kc % 2
        P.op("dve", lambda e, s=s, kc=kc: e.tensor_tensor(out=tmpf[s][:], in0=xT[:, kc, :], in1=rstd[:], op=ALU.mult),
             reads=["xT", "nf_rstd"], writes=[("nft", s)])
        P.op("act", lambda e, s=s, kc=kc: e.activation(out=tmpf[s][:], in_=tmpf[s][:], func=AF.Identity, scale=gfin[:, kc:kc + 1]),
             reads=[("nft", s), "modv"], writes=[("nft", s)])
        cx.store(out[:, kc, :], tmpf[s][:], [("nft", s)], ("out", kc), sem=("st_nft", s))
    return cx.finish()


def emit_rmsnorm_stats(cx, xT, xkey, ones16, tag):
    P = cx.P
    sq = [cx.sb("%s_sq%d" % (tag, i), [128, 512], BF16) for i in range(2)]
    rstd = cx.sb("%s_rstd" % tag, [128, T], F32)
    for half in range(T // 512):
        hs = slice(half * 512, (half + 1) * 512)
        ps, pk = cx.psum()
        for kc in range(DC):
            s = kc % 2
            P.op("act", lambda e, s=s, kc=kc, hs=hs: e.activation(out=sq[s][:], in_=xT[:, kc, hs], func=AF.Square),
                 reads=[xkey], writes=[(tag, "sq", s)])
            P.op("pe", lambda e, s=s, kc=kc, ps=ps: e.matmul(ps[:], lhsT=ones16[:], rhs=sq[s][:], start=(kc == 0), stop=(kc == DC - 1)),
                 reads=[(tag, "sq", s), "ones16"], writes=[pk], signal=True)
        rk = tag + "_rstd"
        P.op("dve", lambda e, ps=ps, hs=hs: e.tensor_scalar(out=rstd[:, hs], in0=ps[:], scalar1=1.0 / D, scalar2=EPS, op0=ALU.mult, op1=ALU.add),
             reads=[pk, rk], writes=[rk])
        P.op("act", lambda e, hs=hs: e.activation(out=rstd[:, hs], in_=rstd[:, hs], func=AF.Sqrt), reads=[rk], writes=[rk])
        P.op("dve", lambda e, hs=hs: e.reciprocal(out=rstd[:, hs], in_=rstd[:, hs]), reads=[rk], writes=[rk])
    return rstd


def run_ffn(xTs, h2s, mod_l, inp):
    nc = get_nc("ffn", build_ffn)
    wg = np.ascontiguousarray(inp["ffn_w_gate"][0]); wu = np.ascontiguousarray(inp["ffn_w_up"][0]); wd = np.ascontiguousarray(inp["ffn_w_down"][0])
    ins = []
    for i in range(NCORES):
        b = i // 4
        ins.append({"xT": xTs[i], "h2T": h2s[i], "gatef": np.ascontiguousarray(modv_layout(mod_l[b])[:, 5, :]),
                    "w_gate": wg, "w_up": wu, "w_down": wd})
    res = run(nc, ins)
    return [res[i]["xT_out"] for i in range(NCORES)]


def run_moe(h2s, rws, inp):
    nc = get_nc("moe", build_moe)
    h2all = np.ascontiguousarray(np.stack(h2s))
    rwall = np.stack(rws)
    ins = []
    for e in range(NCORES):
        rwb = np.ascontiguousarray(np.broadcast_to(rwall[:, None, :, e], (NCHUNK, 128, T)))
        ins.append({"h2T": h2all, "rwb": rwb, "w_gate": np.ascontiguousarray(inp["moe_w_gate"][0][e]),
                    "w_up": np.ascontiguousarray(inp["moe_w_up"][0][e]), "w_down": np.ascontiguousarray(inp["moe_w_down"][0][e])})
    res = run(nc, ins)
    return [res[e]["partial"] for e in range(NCORES)]


def run_final(xTs, partials, mod_l, gfin):
    nc = get_nc("final", build_final)
    ins = []
    for i in range(NCORES):
        b = i // 4
        ins.append({"xT": xTs[i], "partials": np.ascontiguousarray(np.stack([partials[e][i] for e in range(NEXP)])),
                    "gatef": np.ascontiguousarray(modv_layout(mod_l[b])[:, 5, :]), "gfin": vec_fm(gfin)})
    res = run(nc, ins)
    out = np.zeros((2, SEQ, D), np.float32)
    for i in range(NCORES):
        b, q = i // 4, i % 4
        out[b, q * T:(q + 1) * T, :] = res[i]["outT"].transpose(1, 0, 2).reshape(D, T).T
    return out


def kernel(**inputs):
    inp = {k: np.asarray(v) for k, v in inputs.items()}
    mod = run_mod(inp["c"], inp["w_mod"], inp["b_mod"])
    xTs = x_to_cores(inp["x"])
    out = None
    for l in range(2):
        projT = run_inproj(xTs, mod[l], inp["norm_mix_g"][l], np.ascontiguousarray(inp["w_in"][l]))
        ypreT = run_ssm(projT, l, inp)
        yattT = run_attn(projT, inp["rel_bias"])
        moe = (l % 2 == 1)
        xTs, h2s, rws = run_post(xTs, ypreT, yattT, projT, mod[l], l, inp, moe)
        if not moe:
            xTs = run_ffn(xTs, h2s, mod[l], inp)
        else:
            hcs, posms = run_route(h2s, rws)
            ys = run_moe3(hcs, inp)
            out = run_final3(xTs, ys, posms, rws, mod[l], inp["final_norm_g"])
    return out


CAP = 4096
NTOK = 8192
NTT = NTOK // 128


def build_moe2():
    cx = Ctx()
    P = cx.P
    nc = cx.nc
    h2d = cx.din("h2tm", [NTOK, D], BF16)
    rwd = cx.din("rwc", [128, NTT], F32)
    Ld = cx.din("Ltri", [128, 128], F32)
    idd = cx.din("ident", [128, 128], F32)
    Wg = cx.din("w_gate", [D, DFFE], F32)
    Wu = cx.din("w_up", [D, DFFE], F32)
    Wd = cx.din("w_down", [DFFE, D], F32)
    po = cx.dout("partial", [NTOK, D], BF16)
    Wg16 = nc.dram_tensor("Wg16", [D, DFFE], BF16, kind="Internal").ap()
    Wu16 = nc.dram_tensor("Wu16", [D, DFFE], BF16, kind="Internal").ap()
    Wd16 = nc.dram_tensor("Wd16", [DFFE, D], BF16, kind="Internal").ap()
    Xc = nc.dram_tensor("Xc", [CAP, D], BF16, kind="Internal").ap()
    Yc = nc.dram_tensor("Yc", [CAP, D], F32, kind="Internal").ap()
    for i in range(6):
        cx.ps.append(cx.st.enter_context(nc.psum_tensor("ps%d" % i, [128, 512], F32)))
    psb = [cx.st.enter_context(nc.psum_tensor("psb%d" % i, [128, 1024], BF16)) for i in range(2)]
    nft = DFFE // 128
    rwc = cx.sb("rwc_sb", [128, NTT], F32)
    posi = cx.sb("posi", [128, NTT], I32)
    identb = cx.sb("identb", [128, 128], BF16)
    cx.load(rwc[:], rwd, "rwc")

    cx.phase_begin()
    stg = [cx.sb("cv_st%d" % i, [128, 4, 512], F32) for i in range(3)]
    o16 = [cx.sb("cv_o%d" % i, [128, 4, 512], BF16) for i in range(3)]
    Ltri = cx.sb("Ltri_sb", [128, 128], F32)
    identf = cx.sb("identf", [128, 128], F32)
    ones32 = cx.sb("ones32", [128, 128], F32)
    onesr = cx.sb("onesr", [128, NTT], F32)
    m = cx.sb("m_sb", [128, NTT], F32)
    S = cx.sb("S_sb", [128, NTT], F32)
    incl = cx.sb("incl", [128, NTT], F32)
    posf = cx.sb("posf", [128, NTT], F32)
    z16 = cx.sb("z16", [128, D], BF16)
    hrow = [cx.sb("hrow%d" % i, [128, D], BF16) for i in range(4)]
    cx.load(Ltri[:], Ld, "Ltri")
    cx.load(identf[:], idd, "identf")
    P.op("dve", lambda e: e.tensor_copy(out=identb[:], in_=identf[:]), reads=["identf"], writes=["identb"])
    P.op("pool", lambda e: e.memset(ones32[:], 1.0), writes=["ones32"])
    P.op("pool", lambda e: e.memset(onesr[:], 1.0), writes=["onesr"])
    P.op("pool", lambda e: e.memset(z16[:], 0.0), writes=["z16"])
    C = "cmp"
    P.op("dve", lambda e: e.tensor_scalar(out=m[:], in0=rwc[:], scalar1=0.0, scalar2=None, op0=ALU.is_gt), reads=["rwc"], writes=[C])
    ps, pk = cx.psum()
    P.op("pe", lambda e: e.matmul(ps[:, 0:NTT], lhsT=Ltri[:], rhs=m[:], start=True, stop=True), reads=[C, "Ltri"], writes=[pk])
    P.op("pe", lambda e: e.matmul(ps[:, NTT:2 * NTT], lhsT=ones32[:], rhs=m[:], start=True, stop=True), reads=[C, "ones32"], writes=[pk])
    P.op("dve", lambda e: e.tensor_copy(out=S[:], in_=ps[:, NTT:2 * NTT]), reads=[pk, C], writes=[C])
    P.op("dve", lambda e: e.tensor_tensor_scan(out=incl[:], data0=onesr[:], data1=S[:], initial=0.0, op0=ALU.mult, op1=ALU.add),
         reads=[C, "onesr"], writes=[C])
    P.op("dve", lambda e: e.tensor_tensor(out=incl[:], in0=incl[:], in1=S[:], op=ALU.subtract), reads=[C], writes=[C])
    P.op("dve", lambda e: e.tensor_tensor(out=posf[:], in0=ps[:, 0:NTT], in1=incl[:], op=ALU.add), reads=[pk, C], writes=[C])
    P.op("dve", lambda e: e.tensor_scalar(out=m[:], in0=m[:], scalar1=-1.0e6, scalar2=1.0e6, op0=ALU.mult, op1=ALU.add), reads=[C], writes=[C])
    P.op("dve", lambda e: e.tensor_tensor(out=posf[:], in0=posf[:], in1=m[:], op=ALU.add), reads=[C], writes=[C])
    P.op("dve", lambda e: e.tensor_copy(out=posi[:], in_=posf[:]), reads=[C], writes=["posi"])
    for r in range(CAP // 128):
        P.dma("sp", lambda e, r=r: e.dma_start(out=Xc[r * 128:(r + 1) * 128, :], in_=z16[:]), reads=["z16"], writes=[("Xcz", r)], sem="xcz")
    xcz = [("Xcz", r) for r in range(CAP // 128)]
    for j in range(NTT):
        s = j % 4
        cx.load(hrow[s][:], h2d[j * 128:(j + 1) * 128, :], ("hrow", s))
        P.dma("pool", lambda e, j=j, s=s: e.indirect_dma_start(
            out=Xc[:, :], out_offset=bass.IndirectOffsetOnAxis(ap=posi[:, j:j + 1], axis=0), in_=hrow[s][:, :], in_offset=None,
            bounds_check=CAP - 1, oob_is_err=False), reads=[("hrow", s), "posi"] + (xcz if j < 4 else []), writes=[("Xcs", j)], sem=("sc", s))
    xcs = [("Xcs", j) for j in range(NTT)]
    ci = 0
    for (Wsrc, Wdst, name, KC_, ncols) in ((Wg, Wg16, "Wg16", DC, DFFE), (Wu, Wu16, "Wu16", DC, DFFE), (Wd, Wd16, "Wd16", nft, D)):
        for kq in range(0, KC_, 4):
            for cb in range(ncols // 512):
                s = ci % 3
                ci += 1
                src = Wsrc[kq * 128:(kq + 4) * 128, cb * 512:(cb + 1) * 512].rearrange("(c p) n -> p c n", p=128)
                dst = Wdst[kq * 128:(kq + 4) * 128, cb * 512:(cb + 1) * 512].rearrange("(c p) n -> p c n", p=128)
                P.dma("sp", lambda e, s=s, src=src: e.dma_start(out=stg[s][:], in_=src), writes=[("cvst", s)])
                cx.copy(cx.conv_eng(), o16[s][:], stg[s][:], [("cvst", s)], [("cvo", s)])
                P.dma("act", lambda e, s=s, dst=dst: e.dma_start(out=dst, in_=o16[s][:]), reads=[("cvo", s)], writes=[(name, kq // 4, cb)],
                      sem=("cvout", s))
    cx.phase_end()

    cx.phase_begin()
    hT = cx.sb("hT_sb", [128, DC, T], BF16)
    accR = cx.sb("accR", [128, T // 128, D], F32)
    wgt = [cx.sb("wg%d" % i, [128, DC, 128], BF16) for i in range(2)]
    wut = [cx.sb("wu%d" % i, [128, DC, 128], BF16) for i in range(2)]
    wdt = cx.sb("wdt", [128, FG, D], BF16)
    xrow = [cx.sb("xrow%d" % i, [128, D], BF16) for i in range(2)]
    g16 = [cx.sb("g16_%d" % i, [128, FG, T], BF16) for i in range(2)]
    sgl = [cx.sb("sg%d" % i, [128, 512], F32) for i in range(2)]
    gi = 0
    it = 0
    ti = 0
    for ch in range(CAP // T):
        for rt in range(T // 128):
            s = (ch * 8 + rt) % 2
            row0 = ch * T + rt * 128
            cx.P.dma("sp", lambda e, s=s, row0=row0: e.dma_start(out=xrow[s][:], in_=Xc[row0:row0 + 128, :]),
                     reads=(xcs + xcz) if (ch == 0 and rt < 2) else [], writes=[("xrow", s)])
            for kq in range(DC // 4):
                pb = psb[ti % 2]
                pbk = ("psb", ti % 2)
                ti += 1
                for q in range(4):
                    kc = kq * 4 + q
                    P.op("pe", lambda e, pb=pb, q=q, kc=kc, s=s: e.transpose(pb[:, q * 128:(q + 1) * 128], xrow[s][:, kc * 128:(kc + 1) * 128], identb[:]),
                         reads=[("xrow", s), "identb"], writes=[pbk], signal=(q == 3))
                eng = "act" if (ti % 2 == 0) else "dve"
                dst = hT[:, kq * 4:(kq + 1) * 4, rt * 128:(rt + 1) * 128]
                srcv = pb[:, 0:512].rearrange("p (k n) -> p k n", k=4)
                cx.copy(eng, dst, srcv, [pbk], ["hT"])
        for fg in range(nft // FG):
            gb = g16[fg % 2]
            for j in range(FG):
                ft = fg * FG + j
                s = gi % 2
                gi += 1
                dep = [("Wg16", kq, ft // 4) for kq in range(4)] + [("Wu16", kq, ft // 4) for kq in range(4)]
                srcg = Wg16[:, ft * 128:(ft + 1) * 128].rearrange("(c p) n -> p c n", p=128)
                srcu = Wu16[:, ft * 128:(ft + 1) * 128].rearrange("(c p) n -> p c n", p=128)
                P.dma("sp", lambda e, s=s, srcg=srcg: e.dma_start(out=wgt[s][:], in_=srcg), reads=dep if ch == 0 else [], writes=[("wgt", s)])
                P.dma("sp", lambda e, s=s, srcu=srcu: e.dma_start(out=wut[s][:], in_=srcu), reads=dep if ch == 0 else [], writes=[("wut", s)])
                for half in range(T // 512):
                    hs = slice(half * 512, (half + 1) * 512)
                    psg, pkg = cx.psum()
                    mm_group(cx, psg[:], pkg, [(wgt[s][:, kc, :], hT[:, kc, hs]) for kc in range(DC)], [("wgt", s), "hT"])
                    psu, pku = cx.psum()
                    mm_group(cx, psu[:], pku, [(wut[s][:, kc, :], hT[:, kc, hs]) for kc in range(DC)], [("wut", s), "hT"])
                    s2 = it % 2
                    it += 1
                    P.op("act", lambda e, s2=s2, psg=psg: e.activation(out=sgl[s2][:], in_=psg[:], func=AF.Silu), reads=[pkg], writes=[("sg", s2)])
                    P.op("dve", lambda e, s2=s2, psu=psu, gb=gb, j=j, hs=hs: e.tensor_tensor(out=gb[:, j, hs], in0=sgl[s2][:], in1=psu[:], op=ALU.mult),
                         reads=[("sg", s2), pku], writes=[("g16", fg % 2, j)])
            for j in range(FG):
                ft = fg * FG + j
                dep = [("Wd16", ft // 4, cb) for cb in range(D // 512)]
                P.dma("sp", lambda e, j=j, ft=ft: e.dma_start(out=wdt[:, j, :], in_=Wd16[ft * 128:(ft + 1) * 128, :]),
                      reads=dep if ch == 0 else [], writes=[("wdt", j)])
            di = 0
            for rt in range(T // 128):
                for db in range(D // 512):
                    ps, pk = cx.psum()
                    mm_group(cx, ps[:], pk, [(gb[:, j, rt * 128:(rt + 1) * 128], wdt[:, j, db * 512:(db + 1) * 512]) for j in range(FG)],
                             [("wdt", j) for j in range(FG)] + [("g16", fg % 2, j) for j in range(FG)])
                    dsl = accR[:, rt, db * 512:(db + 1) * 512]
                    eng = "dve" if (di % 4 != 3) else "pool"
                    di += 1
                    if fg == 0:
                        P.op("dve", lambda e, dsl=dsl, ps=ps: e.tensor_copy(out=dsl, in_=ps[:]), reads=[pk], writes=[("accR", rt, db)])
                    else:
                        P.op("dve", lambda e, dsl=dsl, ps=ps: e.tensor_tensor(out=dsl, in0=ps[:], in1=dsl, op=ALU.add),
                             reads=[pk, ("accR", rt, db)], writes=[("accR", rt, db)])
        for rt in range(T // 128):
            row0 = ch * T + rt * 128
            P.dma("sp", lambda e, rt=rt, row0=row0: e.dma_start(out=Yc[row0:row0 + 128, :], in_=accR[:, rt, :]),
                  reads=[("accR", rt, db) for db in range(D // 512)], writes=[("Yc", ch, rt)], sem=("st_acc", rt % 2))
    ycs = [("Yc", ch, rt) for ch in range(CAP // T) for rt in range(T // 128)]
    cx.phase_end()

    cx.phase_begin()
    zt = [cx.sb("zt%d" % i, [128, D], F32) for i in range(3)]
    ot = [cx.sb("ot%d" % i, [128, D], BF16) for i in range(3)]
    for i in range(3):
        P.op("pool", lambda e, i=i: e.memset(zt[i][:], 0.0), writes=[("zt", i)])
    for j in range(NTT):
        s = j % 3
        P.dma("pool", lambda e, j=j, s=s: e.indirect_dma_start(
            out=zt[s][:, :], out_offset=None, in_=Yc[:, :], in_offset=bass.IndirectOffsetOnAxis(ap=posi[:, j:j + 1], axis=0),
            bounds_check=CAP - 1, oob_is_err=False), reads=["posi", ("zt", s)], writes=[("zt", s)], sem=("ga", s))
        if j % 2 == 0:
            P.op("dve", lambda e, j=j, s=s: e.tensor_scalar(out=ot[s][:], in0=zt[s][:], scalar1=rwc[:, j:j + 1], scalar2=None, op0=ALU.mult),
                 reads=[("zt", s), "rwc"], writes=[("ot", s)])
        else:
            P.op("act", lambda e, j=j, s=s: e.activation(out=ot[s][:], in_=zt[s][:], func=AF.Identity, scale=rwc[:, j:j + 1]),
                 reads=[("zt", s), "rwc"], writes=[("ot", s)])
        cx.store(po[j * 128:(j + 1) * 128, :], ot[s][:], [("ot", s)], ("po", j), sem=("st_ot", s))
    cx.phase_end()
    return cx.finish()


def build_final2():
    cx = Ctx()
    P = cx.P
    NT_ = T // 128
    xd = cx.din("x", [T, D], F32)
    pd = cx.din("partials", [NEXP, T, D], BF16)
    gfd = cx.din("gatef", [128, D], F32)
    gnd = cx.din("gfin", [128, D], F32)
    out = cx.dout("out", [T, D], F32)
    gf = cx.sb("gf_sb", [128, D], F32)
    gfin = cx.sb("gfin_sb", [128, D], F32)
    cx.load(gf[:], gfd, "gf")
    cx.load(gfin[:], gnd, "gfin")
    xt = [cx.sb("xt%d" % i, [128, D], F32) for i in range(2)]
    pb = [cx.sb("pb%d" % i, [128, NEXP, D], BF16) for i in range(2)]
    acc = [cx.sb("acc%d" % i, [128, D], F32) for i in range(2)]
    sqj = cx.sb("sqj", [128, D], F32)
    ss = [cx.sb("ss%d" % i, [128, 1], F32) for i in range(2)]
    for tt in range(NT_):
        s = tt % 2
        rows = slice(tt * 128, (tt + 1) * 128)
        cx.load(xt[s][:], xd[rows, :], ("xt", s))
        for e_ in range(NEXP):
            P.dma("sp", lambda e, s=s, e_=e_, rows=rows: e.dma_start(out=pb[s][:, e_, :], in_=pd[e_, rows, :]), writes=[("pb", s, e_)], sem=("pb", s))
        pbk = [("pb", s, e_) for e_ in range(NEXP)]
        P.op("dve", lambda e, s=s: e.tensor_tensor(out=acc[s][:], in0=pb[s][:, 0, :], in1=pb[s][:, 1, :], op=ALU.add), reads=pbk, writes=[("acc", s)])
        for e_ in range(2, NEXP):
            eng = "dve" if e_ % 2 == 0 else "pool"
            P.op(eng, lambda e, s=s, e_=e_: e.tensor_tensor(out=acc[s][:], in0=acc[s][:], in1=pb[s][:, e_, :], op=ALU.add),
                 reads=pbk + [("acc", s)], writes=[("acc", s)])
        P.op("dve", lambda e, s=s: e.tensor_tensor(out=acc[s][:], in0=acc[s][:], in1=gf[:], op=ALU.mult), reads=[("acc", s), "gf"], writes=[("acc", s)])
        P.op("dve", lambda e, s=s: e.tensor_tensor(out=xt[s][:], in0=xt[s][:], in1=acc[s][:], op=ALU.add), reads=[("acc", s), ("xt", s)], writes=[("xt", s)])
        P.op("act", lambda e, s=s: e.activation(out=sqj[:], in_=xt[s][:], func=AF.Square, accum_out=ss[s][:]), reads=[("xt", s)], writes=["sqj", ("ss", s)])
        P.op("dve", lambda e, s=s: e.tensor_scalar(out=ss[s][:], in0=ss[s][:], scalar1=1.0 / D, scalar2=EPS, op0=ALU.mult, op1=ALU.add),
             reads=[("ss", s)], writes=[("ss", s)])
        P.op("act", lambda e, s=s: e.activation(out=ss[s][:], in_=ss[s][:], func=AF.Sqrt), reads=[("ss", s)], writes=[("ss", s)])
        P.op("dve", lambda e, s=s: e.reciprocal(out=ss[s][:], in_=ss[s][:]), reads=[("ss", s)], writes=[("ss", s)])
        P.op("dve", lambda e, s=s: e.scalar_tensor_tensor(out=acc[s][:], in0=xt[s][:], scalar=ss[s][:, 0:1], in1=gfin[:], op0=ALU.mult, op1=ALU.mult),
             reads=[("xt", s), ("ss", s), "gfin", ("acc", s)], writes=[("acc", s)])
        cx.store(out[rows, :], acc[s][:], [("acc", s)], ("out", tt), sem=("st_acc", s))
    return cx.finish()


def run_moe2(h2s, rws, inp):
    nc = get_nc("moe2", build_moe2)
    h2tm = np.ascontiguousarray(np.concatenate([h.transpose(1, 0, 2).reshape(D, T).T for h in h2s], axis=0))
    rwall = np.concatenate(rws, axis=0)
    kk = np.arange(128)
    Ltri = (kk[:, None] < kk[None, :]).astype(np.float32)
    ins = []
    for e in range(NCORES):
        ins.append({"h2tm": h2tm, "rwc": np.ascontiguousarray(rwall[:, e].reshape(NTT, 128).T), "Ltri": Ltri,
                    "ident": np.eye(128, dtype=np.float32),
                    "w_gate": np.ascontiguousarray(inp["moe_w_gate"][0][e]), "w_up": np.ascontiguousarray(inp["moe_w_up"][0][e]),
                    "w_down": np.ascontiguousarray(inp["moe_w_down"][0][e])})
    res = run(nc, ins)
    return [res[e]["partial"] for e in range(NCORES)]


def run_final2(xTs, partials, mod_l, gfin):
    nc = get_nc("final2", build_final2)
    ins = []
    for i in range(NCORES):
        b = i // 4
        gatef = mod_l[b].reshape(6, D)[5]
        ins.append({"x": np.ascontiguousarray(xTs[i].transpose(1, 0, 2).reshape(D, T).T),
                    "partials": np.ascontiguousarray(np.stack([partials[e][i * T:(i + 1) * T] for e in range(NEXP)])),
                    "gatef": np.ascontiguousarray(np.broadcast_to(gatef[None, :], (128, D))),
                    "gfin": np.ascontiguousarray(np.broadcast_to(gfin[None, :], (128, D)))})
    res = run(nc, ins)
    out = np.zeros((2, SEQ, D), np.float32)
    for i in range(NCORES):
        b, q = i // 4, i % 4
        out[b, q * T:(q + 1) * T, :] = res[i]["out"]
    return out


SEG = 512
NR = SEG // 128


def build_route():
    cx = Ctx()
    P = cx.P
    nc = cx.nc
    NT_ = T // 128
    hTd = cx.din("h2T", [128, DC, T], BF16)
    rwd = cx.din("rw", [128, NT_, NEXP], F32)
    Ld = cx.din("Ltri", [128, 128], F32)
    idd = cx.din("ident", [128, 128], F32)
    iod = cx.din("iota", [128, 128], F32)
    hco = cx.dout("hc", [NEXP, 128, DC, SEG], BF16)
    pso = cx.dout("posm", [128, NT_, NEXP], F32)
    for i in range(6):
        cx.ps.append(cx.st.enter_context(nc.psum_tensor("ps%d" % i, [128, 512], F32)))
    psb = [cx.st.enter_context(nc.psum_tensor("psb%d" % i, [128, 1024], BF16)) for i in range(2)]
    hT = cx.sb("hT_sb", [128, DC, T], BF16)
    htm = cx.sb("htm", [128, NT_, D], BF16)
    rw = cx.sb("rw_sb", [128, NT_, NEXP], F32)
    Ltri = cx.sb("Ltri_sb", [128, 128], F32)
    identf = cx.sb("identf", [128, 128], F32)
    identb = cx.sb("identb", [128, 128], BF16)
    iota = cx.sb("iota_sb", [128, 128], F32)
    ones32 = cx.sb("ones32", [128, 128], F32)
    for k0 in range(0, DC, 4):
        P.dma("sp", lambda e, k0=k0: e.dma_start(out=hT[:, k0:k0 + 4, :], in_=hTd[:, k0:k0 + 4, :]), writes=["hT"], sem="hT")
    cx.load(rw[:], rwd, "rw")
    cx.load(Ltri[:], Ld, "Ltri")
    cx.load(identf[:], idd, "identf")
    cx.load(iota[:], iod, "iota")
    P.op("dve", lambda e: e.tensor_copy(out=identb[:], in_=identf[:]), reads=["identf"], writes=["identb"])
    P.op("pool", lambda e: e.memset(ones32[:], 1.0), writes=["ones32"])
    ti = 0
    for tt in range(NT_):
        for kq in range(DC // 4):
            pb = psb[ti % 2]
            pbk = ("psb", ti % 2)
            ti += 1
            for q in range(4):
                kc = kq * 4 + q
                P.op("pe", lambda e, pb=pb, q=q, kc=kc, tt=tt: e.transpose(pb[:, q * 128:(q + 1) * 128], hT[:, kc, tt * 128:(tt + 1) * 128], identb[:]),
                     reads=["hT", "identb"], writes=[pbk], signal=(q == 3))
            cx.copy("act" if ti % 2 else "dve", htm[:, tt, kq * 512:(kq + 1) * 512], pb[:, 0:512], [pbk], [("htm", tt)])
    C = "cmp"
    NF = NT_ * NEXP
    m = cx.sb("m_sb", [128, NT_, NEXP], F32)
    S = cx.sb("S_sb", [128, NT_, NEXP], F32)
    offs = cx.sb("offs", [128, NT_, NEXP], F32)
    pos = cx.sb("pos", [128, NT_, NEXP], F32)
    posr = cx.sb("posr", [128, NR, NT_, NEXP], F32)
    mf = m[:].rearrange("p t e -> p (t e)")
    P.op("dve", lambda e: e.tensor_scalar(out=m[:], in0=rw[:], scalar1=0.0, scalar2=None, op0=ALU.is_gt), reads=["rw"], writes=[C])
    ps, pk = cx.psum()
    P.op("pe", lambda e: e.matmul(ps[:, 0:NF], lhsT=Ltri[:], rhs=mf, start=True, stop=True), reads=[C, "Ltri"], writes=[pk])
    P.op("pe", lambda e: e.matmul(ps[:, NF:2 * NF], lhsT=ones32[:], rhs=mf, start=True, stop=True), reads=[C, "ones32"], writes=[pk])
    P.op("dve", lambda e: e.tensor_copy(out=S[:].rearrange("p t e -> p (t e)"), in_=ps[:, NF:2 * NF]), reads=[pk, C], writes=[C])
    P.op("pool", lambda e: e.memset(offs[:, 0, :], 0.0), reads=[C], writes=[C])
    for tt in range(1, NT_):
        P.op("dve", lambda e, tt=tt: e.tensor_tensor(out=offs[:, tt, :], in0=offs[:, tt - 1, :], in1=S[:, tt - 1, :], op=ALU.add), reads=[C], writes=[C])
    P.op("dve", lambda e: e.tensor_tensor(out=pos[:].rearrange("p t e -> p (t e)"), in0=ps[:, 0:NF], in1=offs[:].rearrange("p t e -> p (t e)"), op=ALU.add),
         reads=[pk, C], writes=[C])
    P.op("dve", lambda e: e.tensor_scalar(out=pos[:], in0=pos[:], scalar1=1.0e6, scalar2=None, op0=ALU.add), reads=[C], writes=[C])
    P.op("dve", lambda e: e.tensor_tensor(out=pos[:], in0=pos[:], in1=m[:], op=ALU.mult), reads=[C], writes=[C])
    P.op("dve", lambda e: e.tensor_scalar(out=pos[:], in0=pos[:], scalar1=-1.0e6, scalar2=None, op0=ALU.add), reads=[C], writes=[C])
    cx.store(pso, pos[:], [C], "pso")
    for r in range(NR):
        P.op("dve", lambda e, r=r: e.tensor_scalar(out=posr[:, r, :, :], in0=pos[:], scalar1=-128.0 * r, scalar2=None, op0=ALU.add), reads=[C], writes=[C])
    Pm = [cx.sb("Pm%d" % i, [128, NT_, NR, 128], BF16) for i in range(2)]
    hcb = [cx.sb("hcb%d" % i, [128, DC, SEG], BF16) for i in range(2)]
    htk = [("htm", tt) for tt in range(NT_)]
    for ex in range(NEXP):
        s = ex % 2
        for tt in range(NT_):
            for r in range(min(tt, NR - 1) + 1):
                eng = "dve" if (tt + r) % 2 == 0 else "pool"
                P.op(eng, lambda e, s=s, tt=tt, r=r, ex=ex: e.tensor_scalar(out=Pm[s][:, tt, r, :], in0=iota[:], scalar1=posr[:, r, tt, ex:ex + 1],
                                                                        scalar2=None, op0=ALU.is_equal), reads=[C, "iota"], writes=[("Pm", s)])
        for r in range(NR):
            for kq in range(DC // 4):
                ps, pk = cx.psum()
                for q in range(4):
                    for tt in range(r, NT_):
                        kc = kq * 4 + q
                        P.op("pe", lambda e, ps=ps, q=q, kc=kc, tt=tt, r=r, s=s: e.matmul(
                            ps[:, q * 128:(q + 1) * 128], lhsT=htm[:, tt, kc * 128:(kc + 1) * 128], rhs=Pm[s][:, tt, r, :],
                            start=(tt == r), stop=(tt == NT_ - 1)), reads=htk + [("Pm", s)], writes=[pk], signal=(tt == NT_ - 1 and q == 3))
                cx.copy("act" if (kq % 2) else "dve", hcb[s][:, kq * 4:(kq + 1) * 4, r * 128:(r + 1) * 128],
                        ps[:].rearrange("p (k n) -> p k n", k=4), [pk], [("hcb", s)])
        cx.store(hco[ex].rearrange("p k n -> p (k n)"), hcb[s][:].rearrange("p k n -> p (k n)"), [("hcb", s)], ("hco", ex), sem=("st_hcb", s))
    return cx.finish()


NCH3 = CAP // T


def build_moe3():
    cx = Ctx()
    P = cx.P
    nc = cx.nc
    hcd = cx.din("hc", [NCH3, 128, DC, T], BF16)
    Wg = cx.din("w_gate", [D, DFFE], F32)
    Wu = cx.din("w_up", [D, DFFE], F32)
    Wd = cx.din("w_down", [DFFE, D], F32)
    yo = cx.dout("y", [CAP, D], BF16)
    Wg16 = nc.dram_tensor("Wg16", [D, DFFE], BF16, kind="Internal").ap()
    Wu16 = nc.dram_tensor("Wu16", [D, DFFE], BF16, kind="Internal").ap()
    Wd16 = nc.dram_tensor("Wd16", [DFFE, D], BF16, kind="Internal").ap()
    cx.alloc_psum(8)
    nft = DFFE // 128
    cx.phase_begin()
    stg = [cx.sb("cv_st%d" % i, [128, 4, 512], F32) for i in range(3)]
    o16 = [cx.sb("cv_o%d" % i, [128, 4, 512], BF16) for i in range(3)]
    ci = 0
    for (Wsrc, Wdst, name, KC_, ncols) in ((Wg, Wg16, "Wg16", DC, DFFE), (Wu, Wu16, "Wu16", DC, DFFE), (Wd, Wd16, "Wd16", nft, D)):
        for kq in range(0, KC_, 4):
            for cb in range(ncols // 512):
                s = ci % 3
                ci += 1
                src = Wsrc[kq * 128:(kq + 4) * 128, cb * 512:(cb + 1) * 512].rearrange("(c p) n -> p c n", p=128)
                dst = Wdst[kq * 128:(kq + 4) * 128, cb * 512:(cb + 1) * 512].rearrange("(c p) n -> p c n", p=128)
                P.dma("sp", lambda e, s=s, src=src: e.dma_start(out=stg[s][:], in_=src), writes=[("cvst", s)])
                cx.copy(cx.conv_eng(), o16[s][:], stg[s][:], [("cvst", s)], [("cvo", s)])
                P.dma("pool", lambda e, s=s, dst=dst: e.dma_start(out=dst, in_=o16[s][:]), reads=[("cvo", s)], writes=[(name, kq // 4, cb)],
                      sem=("cvout", s))
    cx.phase_end()
    cx.phase_begin()
    hT = cx.sb("hT_sb", [128, DC, T], BF16)
    accR = cx.sb("accR", [128, T // 128, D], F32)
    wgt = [cx.sb("wg%d" % i, [128, DC, 128], BF16) for i in range(2)]
    wut = [cx.sb("wu%d" % i, [128, DC, 128], BF16) for i in range(2)]
    wdt = cx.sb("wdt", [128, FG, D], BF16)
    g16 = [cx.sb("g16_%d" % i, [128, FG, T], BF16) for i in range(2)]
    sgl = [cx.sb("sg%d" % i, [128, 512], F32) for i in range(2)]
    orow = [cx.sb("orow%d" % i, [128, D], BF16) for i in range(2)]
    gi = 0
    it = 0
    for ch in range(NCH3):
        for k0 in range(0, DC, 4):
            P.dma("sp", lambda e, k0=k0, ch=ch: e.dma_start(out=hT[:, k0:k0 + 4, :], in_=hcd[ch, :, k0:k0 + 4, :]), writes=["hT"], sem="hT")
        for fg in range(nft // FG):
            gb = g16[fg % 2]
            for j in range(FG):
                ft = fg * FG + j
                s = gi % 2
                gi += 1
                dep = [("Wg16", kq, ft // 4) for kq in range(4)] + [("Wu16", kq, ft // 4) for kq in range(4)]
                srcg = Wg16[:, ft * 128:(ft + 1) * 128].rearrange("(c p) n -> p c n", p=128)
                srcu = Wu16[:, ft * 128:(ft + 1) * 128].rearrange("(c p) n -> p c n", p=128)
                P.dma("sp", lambda e, s=s, srcg=srcg: e.dma_start(out=wgt[s][:], in_=srcg), reads=dep if ch == 0 else [], writes=[("wgt", s)])
                P.dma("sp", lambda e, s=s, srcu=srcu: e.dma_start(out=wut[s][:], in_=srcu), reads=dep if ch == 0 else [], writes=[("wut", s)])
                for half in range(T // 512):
                    hs = slice(half * 512, (half + 1) * 512)
                    psg, pkg = cx.psum()
                    mm_group(cx, psg[:], pkg, [(wgt[s][:, kc, :], hT[:, kc, hs]) for kc in range(DC)], [("wgt", s), "hT"])
                    psu, pku = cx.psum()
                    mm_group(cx, psu[:], pku, [(wut[s][:, kc, :], hT[:, kc, hs]) for kc in range(DC)], [("wut", s), "hT"])
                    s2 = it % 2
                    it += 1
                    P.op("act", lambda e, s2=s2, psg=psg: e.activation(out=sgl[s2][:], in_=psg[:], func=AF.Silu), reads=[pkg], writes=[("sg", s2)])
                    P.op("dve", lambda e, s2=s2, psu=psu, gb=gb, j=j, hs=hs: e.tensor_tensor(out=gb[:, j, hs], in0=sgl[s2][:], in1=psu[:], op=ALU.mult),
                         reads=[("sg", s2), pku], writes=[("g16", fg % 2, j)])
            for j in range(FG):
                ft = fg * FG + j
                dep = [("Wd16", ft // 4, cb) for cb in range(D // 512)]
                P.dma("sp", lambda e, j=j, ft=ft: e.dma_start(out=wdt[:, j, :], in_=Wd16[ft * 128:(ft + 1) * 128, :]),
                      reads=dep if ch == 0 else [], writes=[("wdt", j)])
            for rt in range(T // 128):
                for db in range(D // 512):
                    ps, pk = cx.psum()
                    mm_group(cx, ps[:], pk, [(gb[:, j, rt * 128:(rt + 1) * 128], wdt[:, j, db * 512:(db + 1) * 512]) for j in range(FG)],
                             [("wdt", j) for j in range(FG)] + [("g16", fg % 2, j) for j in range(FG)])
                    dsl = accR[:, rt, db * 512:(db + 1) * 512]
                    if fg == 0:
                        P.op("dve", lambda e, dsl=dsl, ps=ps: e.tensor_copy(out=dsl, in_=ps[:]), reads=[pk], writes=[("accR", rt, db)])
                    else:
                        P.op("dve", lambda e, dsl=dsl, ps=ps: e.tensor_tensor(out=dsl, in0=ps[:], in1=dsl, op=ALU.add),
                             reads=[pk, ("accR", rt, db)], writes=[("accR", rt, db)])
        for rt in range(T // 128):
            s = rt % 2
            row0 = ch * T + rt * 128
            P.op("act", lambda e, s=s, rt=rt: e.activation(out=orow[s][:], in_=accR[:, rt, :], func=AF.Identity),
                 reads=[("accR", rt, db) for db in range(D // 512)], writes=[("orow", s)])
            cx.store(yo[row0:row0 + 128, :], orow[s][:], [("orow", s)], ("yo", ch, rt), sem=("st_orow", s))
    cx.phase_end()
    return cx.finish()


def build_final3():
    cx = Ctx()
    P = cx.P
    NT_ = T // 128
    xd = cx.din("x", [T, D], F32)
    yd = cx.din("yseg", [NEXP, SEG, D], BF16)
    pbd = cx.din("posb", [128, NEXP, T], F32)
    rwd = cx.din("rwt", [128, NT_, NEXP], F32)
    sld = cx.din("slotidx", [128, NR], F32)
    gfd = cx.din("gatef", [128, D], F32)
    gnd = cx.din("gfin", [128, D], F32)
    out = cx.dout("out", [T, D], F32)
    cx.alloc_psum(8)
    acc = cx.sb("acc", [128, NT_, D], F32)
    posb = cx.sb("posb_sb", [128, NEXP, T], F32)
    rwt = cx.sb("rwt_sb", [128, NT_, NEXP], F32)
    slot = cx.sb("slot_sb", [128, NR], F32)
    gf = cx.sb("gf_sb", [128, D], F32)
    gfin = cx.sb("gfin_sb", [128, D], F32)
    cx.load(posb[:].rearrange("p e t -> p (e t)"), pbd.rearrange("p e t -> p (e t)"), "posb")
    cx.load(rwt[:], rwd, "rwt")
    cx.load(slot[:], sld, "slot")
    cx.load(gf[:], gfd, "gf")
    cx.load(gfin[:], gnd, "gfin")
    ye = [cx.sb("ye%d" % i, [128, NR, D], BF16) for i in range(2)]
    PwT = [cx.sb("PwT%d" % i, [128, NR, T], BF16) for i in range(2)]
    for ex in range(NEXP):
        s = ex % 2
        for r in range(NR):
            P.dma("sp", lambda e, s=s, r=r, ex=ex: e.dma_start(out=ye[s][:, r, :], in_=yd[ex, r * 128:(r + 1) * 128, :]), writes=[("ye", s, r)], sem=("ye", s, r))
            P.op("dve" if r % 2 == 0 else "pool", lambda e, s=s, r=r, ex=ex: e.tensor_scalar(
                out=PwT[s][:, r, :], in0=posb[:, ex, :], scalar1=slot[:, r:r + 1], scalar2=None, op0=ALU.is_equal),
                reads=["posb", "slot"], writes=[("PwT", s, r)])
        for tt in range(NT_):
            nr = min(tt, NR - 1) + 1
            for db in range(D // 512):
                ps, pk = cx.psum()
                mm_group(cx, ps[:], pk, [(PwT[s][:, r, tt * 128:(tt + 1) * 128], ye[s][:, r, db * 512:(db + 1) * 512]) for r in range(nr)],
                         [("PwT", s, r) for r in range(nr)] + [("ye", s, r) for r in range(nr)])
                dsl = acc[:, tt, db * 512:(db + 1) * 512]
                if ex == 0:
                    P.op("dve", lambda e, dsl=dsl, ps=ps, tt=tt, ex=ex: e.tensor_scalar(out=dsl, in0=ps[:], scalar1=rwt[:, tt, ex:ex + 1], scalar2=None, op0=ALU.mult),
                         reads=[pk, "rwt"], writes=[("acc", tt, db)])
                else:
                    P.op("dve", lambda e, dsl=dsl, ps=ps, tt=tt, ex=ex: e.scalar_tensor_tensor(out=dsl, in0=ps[:], scalar=rwt[:, tt, ex:ex + 1], in1=dsl,
                                                                                     op0=ALU.mult, op1=ALU.add),
                         reads=[pk, "rwt", ("acc", tt, db)], writes=[("acc", tt, db)])
    xt = [cx.sb("xt%d" % i, [128, D], F32) for i in range(2)]
    sqj = cx.sb("sqj", [128, D], F32)
    ss = [cx.sb("ss%d" % i, [128, 1], F32) for i in range(2)]
    for tt in range(NT_):
        s = tt % 2
        rows = slice(tt * 128, (tt + 1) * 128)
        ak = [("acc", tt, db) for db in range(D // 512)]
        cx.load(xt[s][:], xd[rows, :], ("xt", s))
        P.op("pool", lambda e, tt=tt: e.tensor_tensor(out=acc[:, tt, :], in0=acc[:, tt, :], in1=gf[:], op=ALU.mult), reads=ak + ["gf"], writes=ak)
        P.op("dve", lambda e, s=s, tt=tt: e.tensor_tensor(out=xt[s][:], in0=xt[s][:], in1=acc[:, tt, :], op=ALU.add), reads=ak + [("xt", s)], writes=[("xt", s)])
        P.op("act", lambda e, s=s: e.activation(out=sqj[:], in_=xt[s][:], func=AF.Square, accum_out=ss[s][:]), reads=[("xt", s)], writes=["sqj", ("ss", s)])
        P.op("dve", lambda e, s=s: e.tensor_scalar(out=ss[s][:], in0=ss[s][:], scalar1=1.0 / D, scalar2=EPS, op0=ALU.mult, op1=ALU.add),
             reads=[("ss", s)], writes=[("ss", s)])
        P.op("act", lambda e, s=s: e.activation(out=ss[s][:], in_=ss[s][:], func=AF.Sqrt), reads=[("ss", s)], writes=[("ss", s)])
        P.op("dve", lambda e, s=s: e.reciprocal(out=ss[s][:], in_=ss[s][:]), reads=[("ss", s)], writes=[("ss", s)])
        P.op("dve", lambda e, s=s, tt=tt: e.scalar_tensor_tensor(out=acc[:, tt, :], in0=xt[s][:], scalar=ss[s][:, 0:1], in1=gfin[:], op0=ALU.mult, op1=ALU.mult),
             reads=[("xt", s), ("ss", s), "gfin"] + ak, writes=ak)
        cx.store(out[rows, :], acc[:, tt, :], ak, ("out", tt), sem=("st_out", s))
    return cx.finish()


def route_consts():
    kk = np.arange(128)
    return {"Ltri": (kk[:, None] < kk[None, :]).astype(np.float32), "ident": np.eye(128, dtype=np.float32),
            "iota": np.ascontiguousarray(np.broadcast_to(kk[None, :].astype(np.float32), (128, 128)))}


def rw_layout(rw):
    return np.ascontiguousarray(rw.reshape(T // 128, 128, NEXP).transpose(1, 0, 2))


def run_route(h2s, rws):
    nc = get_nc("route", build_route)
    cst = route_consts()
    ins = [dict(h2T=h2s[i], rw=rw_layout(rws[i]), **cst) for i in range(NCORES)]
    res = run(nc, ins)
    return [res[i]["hc"] for i in range(NCORES)], [res[i]["posm"] for i in range(NCORES)]


def run_moe3(hcs, inp):
    nc = get_nc("moe3", build_moe3)
    ins = []
    for e in range(NCORES):
        hcat = np.concatenate([hcs[i][e] for i in range(NCORES)], axis=2)
        hc = np.ascontiguousarray(hcat.reshape(128, DC, NCH3, T).transpose(2, 0, 1, 3))
        ins.append({"hc": hc, "w_gate": np.ascontiguousarray(inp["moe_w_gate"][0][e]), "w_up": np.ascontiguousarray(inp["moe_w_up"][0][e]),
                    "w_down": np.ascontiguousarray(inp["moe_w_down"][0][e])})
    res = run(nc, ins)
    return [res[e]["y"] for e in range(NCORES)]


def run_final3(xTs, ys, posms, rws, mod_l, gfin):
    nc = get_nc("final3", build_final3)
    ins = []
    slotidx = (np.arange(128)[:, None] + 128 * np.arange(NR)[None, :]).astype(np.float32)
    for i in range(NCORES):
        b = i // 4
        gatef = mod_l[b].reshape(6, D)[5]
        posm = posms[i]
        pos_et = posm.transpose(2, 1, 0).reshape(NEXP, T)
        ins.append({"x": np.ascontiguousarray(xTs[i].transpose(1, 0, 2).reshape(D, T).T),
                    "yseg": np.ascontiguousarray(np.stack([ys[e][i * SEG:(i + 1) * SEG] for e in range(NEXP)])),
                    "posb": np.ascontiguousarray(np.broadcast_to(pos_et[None], (128, NEXP, T))),
                    "rwt": rw_layout(rws[i]), "slotidx": slotidx,
                    "gatef": np.ascontiguousarray(np.broadcast_to(gatef[None, :], (128, D))),
                    "gfin": np.ascontiguousarray(np.broadcast_to(gfin[None, :], (128, D)))})
    res = run(nc, ins)
    out = np.zeros((2, SEQ, D), np.float32)
    for i in range(NCORES):
        b, q = i // 4, i % 4
        out[b, q * T:(q + 1) * T, :] = res[i]["out"]
    return out
```

```python
import contextlib
import numpy as np
import ml_dtypes
import concourse.bass as bass
import concourse.mybir as mybir
from concourse.bass_utils import run_bass_kernel_spmd

F32 = mybir.dt.float32
BF16 = mybir.dt.bfloat16
I32 = mybir.dt.int32
ALU = mybir.AluOpType
AF = mybir.ActivationFunctionType
NPBF = ml_dtypes.bfloat16

ENGS = ("pe", "dve", "act", "pool", "sp")

D = 2048
DC = 16
NCORES = 8
T = 1024
SEQ = 4096
EPS = 1e-6
IN_W = 9728
DFF = 5632
DFFE = 7168
NEXP = 8


class Prog:
    def __init__(self, nc):
        self.nc = nc
        self.ops = {e: [] for e in ENGS}
        self.last_w = {}
        self.readers = {}
        self.ndma = {}

    def _deps(self, reads, writes):
        deps = []
        for k in reads:
            t = self.last_w.get(k)
            if t is not None:
                deps.append(t)
        for k in writes:
            t = self.last_w.get(k)
            if t is not None:
                deps.append(t)
            deps.extend(self.readers.get(k, ()))
        return deps

    def _commit(self, tok, reads, writes):
        for k in reads:
            self.readers.setdefault(k, []).append(tok)
        for k in writes:
            self.last_w[k] = tok
            self.readers[k] = []

    def op(self, eng, fn, reads=(), writes=(), signal=True):
        deps = self._deps(reads, writes)
        tok = ("c", eng, len(self.ops[eng]))
        self.ops[eng].append(dict(kind="c", fn=fn, deps=deps, signal=signal or eng != "pe"))
        self._commit(tok, reads, writes)
        return tok

    def dma(self, q, fn, reads=(), writes=(), inc=16, sem=None):
        if sem is None:
            sem = writes[0]
        deps = self._deps(reads, writes)
        self.ndma[sem] = self.ndma.get(sem, 0) + inc
        tok = ("d", sem, self.ndma[sem])
        self.ops[q].append(dict(kind="d", fn=fn, deps=deps, inc=inc, sem=sem))
        self._commit(tok, reads, writes)
        return tok

    def wait_all_on(self, eng, toks):
        self.ops[eng].append(dict(kind="w", deps=list(toks)))

    def barrier(self):
        toks = []
        for e in ENGS:
            for i in range(len(self.ops[e]) - 1, -1, -1):
                o = self.ops[e][i]
                if o["kind"] == "c" and o["signal"]:
                    toks.append(("c", e, i))
                    break
        for k, v in self.ndma.items():
            toks.append(("d", k, v))
        for e in ENGS:
            self.ops[e].append(dict(kind="w", deps=list(toks)))

    def emit(self, st):
        nc = self.nc
        tick_at = {}
        for e in ENGS:
            n = 0
            ticks = []
            for o in self.ops[e]:
                if o["kind"] == "c" and o["signal"]:
                    n += 1
                ticks.append(n)
            res = [None] * len(ticks)
            nxt = None
            for i in range(len(ticks) - 1, -1, -1):
                o = self.ops[e][i]
                if o["kind"] == "c" and o["signal"]:
                    nxt = ticks[i]
                res[i] = nxt
            tick_at[e] = res
        csem = {e: st.enter_context(nc.semaphore("c_" + e)) for e in ENGS if e != "sp"}
        dkeys = []
        dset = set()
        for e in ENGS:
            for o in self.ops[e]:
                if o["kind"] == "d" and o["sem"] not in dset:
                    dset.add(o["sem"])
                    dkeys.append(o["sem"])
        dsem = {k: st.enter_context(nc.semaphore("d%d" % i)) for i, k in enumerate(dkeys)}
        block = st.enter_context(nc.Block())

        def make(e):
            def body(eng):
                seen = {}
                for o in self.ops[e]:
                    for t in o["deps"]:
                        if t[0] == "c":
                            if t[1] == e and e == "pe":
                                continue
                            v = tick_at[t[1]][t[2]]
                            assert v is not None, ("unsignaled dep", t)
                            key = ("c", t[1])
                            sem = csem[t[1]]
                        else:
                            v = t[2]
                            key = ("d", t[1])
                            sem = dsem[t[1]]
                        if seen.get(key, 0) >= v:
                            continue
                        seen[key] = v
                        eng.wait_ge(sem, v)
                    if o["kind"] == "c":
                        ins = o["fn"](eng)
                        if o["signal"]:
                            ins.then_inc(csem[e], 1)
                    elif o["kind"] == "d":
                        ins = o["fn"](eng)
                        ins.then_inc(dsem[o["sem"]], o["inc"])
            return body

        for e in ENGS:
            if not self.ops[e]:
                continue
            dec = {"pe": block.tensor, "dve": block.vector, "act": block.scalar,
                   "pool": block.gpsimd, "sp": block.sync}[e]
            dec(make(e))


class Ctx:
    def __init__(self):
        self.nc = bass.Bass("TRN2", target_bir_lowering=False)
        self.st = contextlib.ExitStack()
        self.P = Prog(self.nc)
        self.ps = []
        self.ps_i = 0
        self.out_toks = []
        self.cv_i = 0
        self._n = 0
        self.phase_st = None
        self.arena = None
        self.arena_off = 0

    def din(self, name, shape, dt):
        return self.nc.dram_tensor(name, list(shape), dt, kind="ExternalInput").ap()

    def dout(self, name, shape, dt):
        return self.nc.dram_tensor(name, list(shape), dt, kind="ExternalOutput").ap()

    ARENA_BYTES = 160 * 1024

    def sb(self, name, shape, dt):
        if self.phase_st is None:
            return self.st.enter_context(self.nc.sbuf_tensor(name, list(shape), dt))
        esz = mybir.dt.size(dt)
        n = int(np.prod(shape[1:]))
        nbytes = (n * esz + 63) // 64 * 64
        off = self.arena_off
        assert off + nbytes <= self.ARENA_BYTES, ("arena overflow", name, off, nbytes)
        self.arena_off += nbytes
        v = self.arena[0:shape[0], off // 2:(off + n * esz) // 2]
        if dt != BF16:
            v = v.bitcast(dt)
        if len(shape) > 2:
            names = " ".join("d%d" % i for i in range(1, len(shape)))
            v = v.rearrange("p (%s) -> p %s" % (names, names), **{"d%d" % i: shape[i] for i in range(1, len(shape))})
        return v

    def phase_begin(self):
        if self.arena is None:
            self.arena = self.st.enter_context(self.nc.sbuf_tensor("arena", [128, self.ARENA_BYTES // 2], BF16))
        self.phase_st = True
        self.arena_off = 0

    def phase_end(self):
        self.P.barrier()
        self.phase_st = None

    def alloc_psum(self, n=8):
        for i in range(n):
            self.ps.append(self.st.enter_context(self.nc.psum_tensor("ps%d" % i, [128, 512], F32)))

    def psum(self):
        i = self.ps_i % len(self.ps)
        self.ps_i += 1
        return self.ps[i], ("ps", i)

    def load(self, dst_ap, src_ap, key, q="sp"):
        return self.P.dma(q, lambda e: e.dma_start(out=dst_ap, in_=src_ap), writes=[key])

    def store(self, dst_ap, src_ap, rkeys, wkey, q="sp", sem=None):
        t = self.P.dma(q, lambda e: e.dma_start(out=dst_ap, in_=src_ap), reads=rkeys, writes=[wkey],
                       sem=sem if sem is not None else ("st", wkey))
        self.out_toks.append(t)
        return t

    def conv_eng(self):
        e = ("pool", "act", "dve")[self.cv_i % 3]
        self.cv_i += 1
        return e

    def copy(self, eng, out, in_, reads, writes):
        if eng == "act":
            return self.P.op("act", lambda e: e.activation(out=out, in_=in_, func=AF.Identity), reads=reads, writes=writes)
        return self.P.op(eng, lambda e: e.tensor_copy(out=out, in_=in_), reads=reads, writes=writes)

    def finish(self):
        last = {}
        for t in self.out_toks:
            last[t[1]] = t
        self.P.wait_all_on("sp", list(last.values()))
        self.P.emit(self.st)
        self.st.close()
        return self.nc


def mm_group(cx, ps_ap, ps_key, pairs, reads):
    n = len(pairs)
    for i, (l, r) in enumerate(pairs):
        cx.P.op("pe", lambda e, l=l, r=r, i=i: e.matmul(ps_ap, lhsT=l, rhs=r, start=(i == 0), stop=(i == n - 1)),
                reads=reads, writes=[ps_key], signal=(i == n - 1))


class WLoader:
    def __init__(self, cx, name, KC, ncol, nslots=2, nstage=4, kq=4):
        self.cx, self.KC, self.ncol, self.kq = cx, KC, ncol, kq
        self.name = name
        self.wb = [cx.sb("%s_wb%d" % (name, i), [128, KC, ncol], BF16) for i in range(nslots)]
        self.stg = [cx.sb("%s_st%d" % (name, i), [128, kq, ncol], F32) for i in range(nstage)]
        self.si = 0
        self.wi = 0

    def load(self, w_rows_ap, ncol=None):
        cx = self.cx
        ncol = ncol or self.ncol
        slot = self.wi % len(self.wb)
        self.wi += 1
        wb = self.wb[slot]
        key = (self.name, "wb", slot)
        for k0 in range(0, self.KC, self.kq):
            kn = min(self.kq, self.KC - k0)
            s = self.si % len(self.stg)
            self.si += 1
            stg = self.stg[s]
            skey = (self.name, "st", s)
            src = w_rows_ap[k0 * 128:(k0 + kn) * 128, :].rearrange("(c p) n -> p c n", p=128)
            cx.P.dma("sp", lambda e, stg=stg, src=src, kn=kn: e.dma_start(out=stg[:, 0:kn, 0:ncol], in_=src),
                     writes=[skey])
            cx.copy(cx.conv_eng(), wb[:, k0:k0 + kn, 0:ncol], stg[:, 0:kn, 0:ncol], [skey], [(key, k0)])
        return wb, [(key, k0) for k0 in range(0, self.KC, self.kq)]


def build_mod():
    cx = Ctx()
    P = cx.P
    NCOL = 3072
    cT = cx.din("cT", [128, DC, 2], F32)
    W = cx.din("W", [D, NCOL], F32)
    b = cx.din("b", [2, NCOL], F32)
    y = cx.dout("y", [2, NCOL], F32)
    cx.alloc_psum(2)
    c_sb = cx.sb("c_sb", [128, DC, 2], F32)
    cond = cx.sb("cond", [128, DC, 2], F32)
    b_sb = cx.sb("b_sb", [2, NCOL], F32)
    o_sb = cx.sb("o_sb", [2, NCOL], F32)
    wst = [cx.sb("wst%d" % i, [128, DC, 512], F32) for i in range(2)]
    cx.load(c_sb[:], cT, "c_sb")
    cx.load(b_sb[:], b, "b_sb")
    P.op("act", lambda e: e.activation(out=cond[:], in_=c_sb[:], func=AF.Silu), reads=["c_sb"], writes=["cond"])
    for ct in range(NCOL // 512):
        s = ct % 2
        for k0 in range(0, DC, 4):
            src = W[k0 * 128:(k0 + 4) * 128, ct * 512:(ct + 1) * 512].rearrange("(c p) n -> p c n", p=128)
            P.dma("sp", lambda e, s=s, k0=k0, src=src: e.dma_start(out=wst[s][:, k0:k0 + 4, :], in_=src),
                  writes=[("wst", s)])
        ps, pk = cx.psum()
        mm_group(cx, ps[0:2, :], pk, [(cond[:, kc, :], wst[s][:, kc, :]) for kc in range(DC)], ["cond", ("wst", s)])
        P.op("dve", lambda e, ps=ps, ct=ct: e.tensor_tensor(out=o_sb[:, ct * 512:(ct + 1) * 512], in0=ps[0:2, :],
                                                        in1=b_sb[:, ct * 512:(ct + 1) * 512], op=ALU.add),
             reads=[pk, "b_sb"], writes=["o_sb"])
    cx.store(y, o_sb[:], ["o_sb"], "y")
    return cx.finish()


def emit_rmsnorm_mod(cx, xT, xkey, gm, shift, hT, hkey, ones16, tag):
    P = cx.P
    sq = [cx.sb("%s_sq%d" % (tag, i), [128, 512], BF16) for i in range(2)]
    rstd = cx.sb("%s_rstd" % tag, [128, T], F32)
    tmp = [cx.sb("%s_tmp%d" % (tag, i), [128, 512], F32) for i in range(2)]
    for half in range(T // 512):
        hs = slice(half * 512, (half + 1) * 512)
        ps, pk = cx.psum()
        for kc in range(DC):
            s = kc % 2
            P.op("act", lambda e, s=s, kc=kc, hs=hs: e.activation(out=sq[s][:], in_=xT[:, kc, hs], func=AF.Square),
                 reads=[xkey], writes=[(tag, "sq", s)])
            P.op("pe", lambda e, s=s, kc=kc, ps=ps: e.matmul(ps[:], lhsT=ones16[:], rhs=sq[s][:], start=(kc == 0),
                                                          stop=(kc == DC - 1)),
                 reads=[(tag, "sq", s), "ones16"], writes=[pk], signal=True)
        rk = (tag, "rstd", half)
        P.op("dve", lambda e, ps=ps, hs=hs: e.tensor_scalar(out=rstd[:, hs], in0=ps[:], scalar1=1.0 / D, scalar2=EPS,
                                                    op0=ALU.mult, op1=ALU.add), reads=[pk], writes=[rk])
        P.op("act", lambda e, hs=hs: e.activation(out=rstd[:, hs], in_=rstd[:, hs], func=AF.Sqrt), reads=[rk], writes=[rk])
        P.op("dve", lambda e, hs=hs: e.reciprocal(out=rstd[:, hs], in_=rstd[:, hs]), reads=[rk], writes=[rk])
        for kc in range(DC):
            s = kc % 2
            P.op("dve", lambda e, s=s, kc=kc, hs=hs: e.tensor_tensor(out=tmp[s][:], in0=xT[:, kc, hs], in1=rstd[:, hs], op=ALU.mult),
                 reads=[xkey, rk], writes=[(tag, "tmp", s)])
            P.op("act", lambda e, s=s, kc=kc, hs=hs: e.activation(out=hT[:, kc, hs], in_=tmp[s][:], func=AF.Identity,
                                                        scale=gm[:, kc:kc + 1], bias=shift[:, kc:kc + 1]),
                 reads=[(tag, "tmp", s), "modv"], writes=[hkey])
    return rstd


def emit_modvecs(cx, modv, gnorm, gm):
    P = cx.P
    P.op("dve", lambda e: e.tensor_scalar(out=gm[:], in0=modv[:, 1, :], scalar1=1.0, scalar2=None, op0=ALU.add),
         reads=["modv_in"], writes=["gm_tmp"])
    P.op("dve", lambda e: e.tensor_tensor(out=gm[:], in0=gm[:], in1=gnorm[:], op=ALU.mult),
         reads=["gm_tmp", "gnorm"], writes=["modv"])


def emit_linear(cx, wl, W, KC, N, inT, in_keys, evac, ngrp=256):
    for g0 in range(0, N, ngrp):
        gn = min(ngrp, N - g0)
        wb, wkeys = wl.load(W[:, g0:g0 + gn], gn)
        for j in range(gn // 128):
            nt = g0 // 128 + j
            for half in range(T // 512):
                ps, pk = cx.psum()
                mm_group(cx, ps[:], pk,
                         [(wb[:, kc, j * 128:(j + 1) * 128], inT[:, kc, half * 512:(half + 1) * 512]) for kc in range(KC)],
                         list(wkeys) + list(in_keys))
                evac(nt, half, ps, pk)


def build_inproj():
    cx = Ctx()
    P = cx.P
    xTd = cx.din("xT", [128, DC, T], F32)
    modd = cx.din("modv", [128, 6, DC], F32)
    gnd = cx.din("gnorm", [128, DC], F32)
    W = cx.din("W", [D, IN_W], F32)
    out = cx.dout("projT", [IN_W, T], BF16)
    cx.alloc_psum(8)
    xT = cx.sb("xT_sb", [128, DC, T], F32)
    hT = cx.sb("hT_sb", [128, DC, T], BF16)
    modv = cx.sb("modv_sb", [128, 6, DC], F32)
    gnorm = cx.sb("gnorm_sb", [128, DC], F32)
    gm = cx.sb("gm_sb", [128, DC], F32)
    ones16 = cx.sb("ones16", [128, 128], BF16)
    osb = [cx.sb("osb%d" % i, [128, T], BF16) for i in range(3)]
    P.op("pool", lambda e: e.memset(ones16[:], 1.0), writes=["ones16"])
    for k0 in range(0, DC, 4):
        P.dma("sp", lambda e, k0=k0: e.dma_start(out=xT[:, k0:k0 + 4, :], in_=xTd[:, k0:k0 + 4, :]), writes=["xT"], sem="xT")
    cx.load(modv[:], modd, "modv_in")
    cx.load(gnorm[:], gnd, "gnorm")
    emit_modvecs(cx, modv, gnorm, gm)
    emit_rmsnorm_mod(cx, xT, "xT", gm, modv[:, 0, :], hT, "hT", ones16, "n1")
    wl = WLoader(cx, "win", DC, 512, nslots=2, nstage=4)
    cnt = [0]

    def evac(nt, half, ps, pk):
        s = nt % 3
        eng = "act" if (cnt[0] % 2 == 0) else "dve"
        cnt[0] += 1
        cx.copy(eng, osb[s][:, half * 512:(half + 1) * 512], ps[:], [pk], [("osb", s, half)])
        if half == T // 512 - 1:
            cx.store(out[nt * 128:(nt + 1) * 128, :], osb[s][:], [("osb", s, h) for h in range(T // 512)], ("out", nt),
                     sem=("st_osb", s))

    emit_linear(cx, wl, W, DC, IN_W, hT, ["hT"], evac, ngrp=512)
    return cx.finish()


def fm(a2d):
    F, n = a2d.shape
    return np.ascontiguousarray(a2d.reshape(F // 128, 128, n).transpose(1, 0, 2))


def vec_fm(v):
    return np.ascontiguousarray(v.reshape(-1, 128).T)


_cache = {}


def get_nc(name, builder):
    if name not in _cache:
        _cache[name] = builder()
    return _cache[name]


def run(nc, in_maps):
    res = run_bass_kernel_spmd(nc, in_maps, core_ids=list(range(NCORES)))
    return res.results


def run_mod(c, w_mod, b_mod):
    nc = get_nc("mod", build_mod)
    cT = np.ascontiguousarray(c.T.reshape(DC, 128, 2).transpose(1, 0, 2))
    ins = []
    for i in range(NCORES):
        l, q = i // 4, i % 4
        cols = slice(q * 3072, (q + 1) * 3072)
        ins.append({"cT": cT, "W": np.ascontiguousarray(w_mod[l][:, cols]),
                    "b": np.ascontiguousarray(np.broadcast_to(b_mod[l][cols], (2, 3072)))})
    res = run(nc, ins)
    mod = np.zeros((2, 2, 6 * D), np.float32)
    for i in range(NCORES):
        l, q = i // 4, i % 4
        mod[l][:, q * 3072:(q + 1) * 3072] = res[i]["y"]
    return mod


def modv_layout(mod_lb):
    return np.ascontiguousarray(mod_lb.reshape(6, DC, 128).transpose(2, 0, 1))


def x_to_cores(x):
    outs = []
    for i in range(NCORES):
        b, q = i // 4, i % 4
        outs.append(fm(np.ascontiguousarray(x[b, q * T:(q + 1) * T, :].T)))
    return outs


def run_inproj(xTs, mod_l, gnorm, w_in_l):
    nc = get_nc("inproj", build_inproj)
    ins = []
    for i in range(NCORES):
        b = i // 4
        ins.append({"xT": xTs[i], "modv": modv_layout(mod_l[b]), "gnorm": vec_fm(gnorm), "W": w_in_l})
    res = run(nc, ins)
    projT = np.zeros((2, IN_W, SEQ), NPBF)
    for i in range(NCORES):
        b, q = i // 4, i % 4
        projT[b][:, q * T:(q + 1) * T] = res[i]["projT"]
    return projT


DILS = (1, 4, 16)


def build_attn():
    cx = Ctx()
    P = cx.P
    QTd = cx.din("QT", [128, 3, SEQ], BF16)
    KTd = cx.din("KT", [128, 3, SEQ], BF16)
    Vd = cx.din("V", [128, 3, 32, 128], BF16)
    Bmd = cx.din("Bm", [128, 3, 256], F32)
    out = cx.dout("oT", [128, SEQ], BF16)
    cx.alloc_psum(8)
    QT = cx.sb("QT_sb", [128, 3, SEQ], BF16)
    KT = cx.sb("KT_sb", [128, 3, SEQ], BF16)
    V = cx.sb("V_sb", [128, 3, 32, 128], BF16)
    Bm = cx.sb("Bm_sb", [128, 3, 256], F32)
    ones16 = cx.sb("ones16", [128, 128], BF16)
    num = cx.sb("num", [128, SEQ], F32)
    den = cx.sb("den", [128, SEQ], F32)
    o16 = cx.sb("o16", [128, SEQ], BF16)
    lg = [cx.sb("lg%d" % i, [128, 256], F32) for i in range(3)]
    pT = [cx.sb("pT%d" % i, [128, 256], BF16) for i in range(3)]
    P.op("pool", lambda e: e.memset(ones16[:], 1.0), writes=["ones16"])
    Vf = V[:].rearrange("p g t d -> p g (t d)")
    Vdf = Vd.rearrange("p g t d -> p g (t d)")
    for g in range(3):
        cx.load(QT[:, g, :], QTd[:, g, :], ("QT", g))
        cx.load(KT[:, g, :], KTd[:, g, :], ("KT", g))
        cx.load(Vf[:, g, :], Vdf[:, g, :], ("V", g))
    cx.load(Bm[:], Bmd, "Bm")
    scale = 128 ** -0.5
    blk = 0
    import os
    DBG = os.environ.get("ATT_DBG", "")
    if DBG == "loads":
        P.op("pool", lambda e: e.memset(o16[:], 0.0), reads=[("QT", 0), ("QT", 1), ("QT", 2), ("KT", 0), ("KT", 1), ("KT", 2), ("V", 0), ("V", 1), ("V", 2), "Bm"], writes=["o16"])
        cx.store(out, o16[:], ["o16"], "oT")
        return cx.finish()
    for g in range(3 if DBG != "g0" else 1):
        d = DILS[g]
        run = SEQ // d
        numv = num[:].rearrange("p (m d) -> p m d", d=d)
        denv = den[:].rearrange("p (m d) -> p m d", d=d)
        for r in range(d):
            for i in range(run // 128):
                p0 = r * run + 128 * i
                nc_ = 256 if i > 0 else 128
                s = blk % 3
                blk += 1
                ps, pk = cx.psum()
                P.op("pe", lambda e, ps=ps, g=g, p0=p0: e.matmul(ps[:, 0:128], lhsT=KT[:, g, p0:p0 + 128],
                                                               rhs=QT[:, g, p0:p0 + 128], start=True, stop=True),
                     reads=[("KT", g), ("QT", g)], writes=[pk], signal=(i == 0))
                if i > 0:
                    P.op("pe", lambda e, ps=ps, g=g, p0=p0: e.matmul(ps[:, 128:256], lhsT=KT[:, g, p0 - 128:p0],
                                                                   rhs=QT[:, g, p0:p0 + 128], start=True, stop=True),
                         reads=[("KT", g), ("QT", g)], writes=[pk])
                LV = int(os.environ.get("ATT_LV", "9"))
                if LV == 1:
                    P.op("dve", lambda e, ps=ps, s=s, n=nc_: e.tensor_copy(out=lg[s][:, 0:n], in_=ps[:, 0:n]), reads=[pk], writes=[("lg", s)])
                    continue
                P.op("dve", lambda e, ps=ps, g=g, s=s, n=nc_: e.scalar_tensor_tensor(
                    out=lg[s][:, 0:n], in0=ps[:, 0:n], scalar=scale, in1=Bm[:, g, 0:n], op0=ALU.mult, op1=ALU.add),
                    reads=[pk, "Bm"], writes=[("lg", s)])
                if LV == 2:
                    continue
                P.op("act", lambda e, s=s, n=nc_: e.activation(out=pT[s][:, 0:n], in_=lg[s][:, 0:n], func=AF.Exp),
                     reads=[("lg", s)], writes=[("pT", s)])
                if LV == 3:
                    continue
                ps2, pk2 = cx.psum()
                td = p0 // 128
                P.op("pe", lambda e, ps2=ps2, g=g, td=td, s=s, i=i: e.matmul(
                    ps2[:, 0:128], lhsT=V[:, g, td, :], rhs=pT[s][:, 0:128], start=True, stop=(i == 0)),
                    reads=[("V", g), ("pT", s)], writes=[pk2], signal=False)
                if i > 0:
                    P.op("pe", lambda e, ps2=ps2, g=g, td=td, s=s: e.matmul(
                        ps2[:, 0:128], lhsT=V[:, g, td - 1, :], rhs=pT[s][:, 128:256], start=False, stop=True),
                        reads=[("V", g), ("pT", s)], writes=[pk2], signal=False)
                P.op("pe", lambda e, ps2=ps2, s=s, i=i: e.matmul(
                    ps2[:, 128:256], lhsT=ones16[:], rhs=pT[s][:, 0:128], start=True, stop=(i == 0)),
                    reads=["ones16", ("pT", s)], writes=[pk2], signal=(i == 0))
                if i > 0:
                    P.op("pe", lambda e, ps2=ps2, s=s: e.matmul(
                        ps2[:, 128:256], lhsT=ones16[:], rhs=pT[s][:, 128:256], start=False, stop=True),
                        reads=["ones16", ("pT", s)], writes=[pk2])
                if LV == 4:
                    P.op("dve", lambda e, ps2=ps2, s=s: e.tensor_copy(out=lg[s][:, 0:256], in_=ps2[:, 0:256]), reads=[pk2], writes=[("lg", s)])
                    continue
                nv = numv[:, 128 * i:128 * (i + 1), r]
                dv = denv[:, 128 * i:128 * (i + 1), r]
                if g == 0:
                    P.op("dve", lambda e, ps2=ps2, nv=nv: e.tensor_copy(out=nv, in_=ps2[:, 0:128]), reads=[pk2], writes=["num"])
                    P.op("dve", lambda e, ps2=ps2, dv=dv: e.tensor_copy(out=dv, in_=ps2[:, 128:256]), reads=[pk2], writes=["den"])
                else:
                    P.op("dve", lambda e, ps2=ps2, nv=nv: e.tensor_tensor(out=nv, in0=ps2[:, 0:128], in1=nv, op=ALU.add),
                         reads=[pk2, "num"], writes=["num"])
                    P.op("dve", lambda e, ps2=ps2, dv=dv: e.tensor_tensor(out=dv, in0=ps2[:, 128:256], in1=dv, op=ALU.add),
                         reads=[pk2, "den"], writes=["den"])
    if int(os.environ.get("ATT_LV", "9")) < 9:
        P.op("dve", lambda e: e.memset(o16[:], 0.0), reads=[("lg", 0), ("lg", 1), ("lg", 2), ("pT", 0), ("pT", 1), ("pT", 2), "num", "den"], writes=["o16"])
        cx.store(out, o16[:], ["o16"], "oT")
        return cx.finish()
    P.op("dve", lambda e: e.reciprocal(out=den[:], in_=den[:]), reads=["den"], writes=["den"])
    P.op("dve", lambda e: e.tensor_tensor(out=o16[:], in0=num[:], in1=den[:], op=ALU.mult), reads=["num", "den"], writes=["o16"])
    cx.store(out, o16[:], ["o16"], "oT")
    return cx.finish()


def t5_bucket_np(dist):
    dist = np.asarray(dist, np.int32)
    max_exact = 16
    d32 = np.maximum(dist, 1).astype(np.float32)
    large = max_exact + (np.log(d32 / np.float32(max_exact)) / np.float32(np.log(2048 / 16)) * np.float32(16)).astype(np.int32)
    return np.where(dist < max_exact, dist, np.minimum(large, 31))


def attn_bias_mats(rel_bias, slot):
    Bm = np.full((128, 3, 256), -1e30, np.float32)
    ik = np.arange(128)[:, None]
    jq = np.arange(128)[None, :]
    for g, d in enumerate(DILS):
        head = 4 * g + slot
        rel = jq - ik
        bd = rel_bias[t5_bucket_np(np.maximum(rel, 0) * d), head]
        Bm[:, g, 0:128] = np.where(rel >= 0, bd, np.float32(-1e30))
        rel2 = jq - ik + 128
        bo = rel_bias[t5_bucket_np(np.minimum(rel2, 128) * d), head]
        Bm[:, g, 128:256] = np.where(rel2 <= 128, bo, np.float32(-1e30))
    return Bm


def run_attn(projT, rel_bias):
    nc = get_nc("attn", build_attn)
    ins = []
    for i in range(NCORES):
        b, slot = i // 4, i % 4
        QT = np.zeros((128, 3, SEQ), NPBF)
        KT = np.zeros((128, 3, SEQ), NPBF)
        V = np.zeros((128, 3, 32, 128), NPBF)
        for g, d in enumerate(DILS):
            head = 4 * g + slot
            perm = np.arange(SEQ).reshape(SEQ // d, d).T.reshape(-1)
            QT[:, g, :] = projT[b, 1024 + head * 128:1024 + (head + 1) * 128, :][:, perm]
            KT[:, g, :] = projT[b, 2560 + head * 128:2560 + (head + 1) * 128, :][:, perm]
            vt = projT[b, 4096 + head * 128:4096 + (head + 1) * 128, :][:, perm]
            V[:, g, :, :] = vt.T.reshape(32, 128, 128).transpose(1, 0, 2)
        ins.append({"QT": QT, "KT": KT, "V": V, "Bm": attn_bias_mats(rel_bias, slot)})
    res = run(nc, ins)
    yT = np.zeros((2, 512, SEQ), NPBF)
    for i in range(NCORES):
        b, slot = i // 4, i % 4
        yT[b, slot * 128:(slot + 1) * 128, :] = res[i]["oT"]
    return yT


NPW = 33
TWO_PI = 6.283185307179586
C1 = 6.28125
C2 = TWO_PI - C1


def ssm_nvec():
    n = [7 - k for k in range(8)] + [t - 7 for t in range(8)] + [t + 1 for t in range(8)] + [8 * 2 ** j for j in range(9)]
    return np.asarray(n, np.float32)


def build_ssm():
    cx = Ctx()
    P = cx.P
    G = 8
    NCH = 512
    Ud = cx.din("U", [128, G, 2, NCH], BF16)
    prm = cx.din("prm", [128, 3, G], F32)
    Bd = cx.din("Bri", [128, 2, G, 16], F32)
    Cd = cx.din("Cri", [128, 2, G, 16], F32)
    Dd = cx.din("Dcol", [128, G], F32)
    nvd = cx.din("nvec", [128, NPW], F32)
    mkd = cx.din("maskT", [128, 128], F32)
    idd = cx.din("ident", [128, 128], F32)
    out = cx.dout("ypre", [128, G, 2, NCH], F32)
    cx.alloc_psum(8)
    U = cx.sb("U_sb", [128, G, 2, NCH], BF16)
    prm_s = cx.sb("prm_s", [128, 3, G], F32)
    Bri = cx.sb("Bri_s", [128, 2, G, 16], F32)
    Cri = cx.sb("Cri_s", [128, 2, G, 16], F32)
    Dcol = cx.sb("Dcol_s", [128, G], F32)
    nvec = cx.sb("nvec_s", [128, NPW], F32)
    maskT = cx.sb("maskT_s", [128, 128], F32)
    ident = cx.sb("ident_s", [128, 128], F32)
    for g in range(G):
        cx.load(U[:, g, :, :].rearrange("p b c -> p (b c)"), Ud[:, g, :, :].rearrange("p b c -> p (b c)"), ("U", g))
    cx.load(prm_s[:], prm, "prm")
    cx.load(Bri[:].rearrange("p a g h -> p (a g h)"), Bd.rearrange("p a g h -> p (a g h)"), "Bri")
    cx.load(Cri[:].rearrange("p a g h -> p (a g h)"), Cd.rearrange("p a g h -> p (a g h)"), "Cri")
    cx.load(Dcol[:], Dd, "Dcol")
    cx.load(nvec[:], nvd, "nvec")
    cx.load(maskT[:], mkd, "maskT")
    cx.load(ident[:], idd, "ident")

    sm = lambda name, shape: cx.sb(name, shape, F32)
    dt_ = sm("dt_", [128, G]); lr = sm("lr", [128, G]); li = sm("li", [128, G])
    ang = sm("ang", [128, G, NPW]); mag = sm("mag", [128, G, NPW]); kf = sm("kf", [128, G, NPW])
    ki = cx.sb("ki", [128, G, NPW], I32)
    msk = sm("msk", [128, G, NPW]); rs = sm("rs", [128, G, NPW]); rc = sm("rc", [128, G, NPW])
    Pr = sm("Pr", [128, G, NPW]); Pi = sm("Pi", [128, G, NPW]); nPi = sm("nPi", [128, G, NPW])
    K = "ssmprep"

    def dve(fn, reads=(), writes=()):
        P.op("dve", fn, reads=[K] + list(reads), writes=[K] + list(writes))

    def act(fn, reads=(), writes=()):
        P.op("act", fn, reads=[K] + list(reads), writes=[K] + list(writes))

    act(lambda e: e.activation(out=dt_[:], in_=prm_s[:, 2, :], func=AF.Exp), reads=["prm"])
    dve(lambda e: e.tensor_tensor(out=lr[:], in0=prm_s[:, 0, :], in1=dt_[:], op=ALU.mult))
    dve(lambda e: e.tensor_tensor(out=li[:], in0=prm_s[:, 1, :], in1=dt_[:], op=ALU.mult))
    for g in range(G):
        dve(lambda e, g=g: e.tensor_scalar(out=ang[:, g, :], in0=nvec[:], scalar1=li[:, g:g + 1], scalar2=None, op0=ALU.mult),
            reads=["nvec"])
        act(lambda e, g=g: e.activation(out=mag[:, g, :], in_=nvec[:], func=AF.Exp, scale=lr[:, g:g + 1]), reads=["nvec"])

    def reduce_sin(src_fn, dst):
        dve(lambda e: e.tensor_scalar(out=kf[:], in0=src_fn(), scalar1=1.0 / TWO_PI, scalar2=None, op0=ALU.mult))
        dve(lambda e: e.tensor_copy(out=ki[:], in_=kf[:]))
        dve(lambda e: e.tensor_copy(out=kf[:], in_=ki[:]))
        dve(lambda e: e.scalar_tensor_tensor(out=dst[:], in0=kf[:], scalar=-C1, in1=src_fn(), op0=ALU.mult, op1=ALU.add))
        dve(lambda e: e.scalar_tensor_tensor(out=dst[:], in0=kf[:], scalar=-C2, in1=dst[:], op0=ALU.mult, op1=ALU.add))
        dve(lambda e: e.tensor_scalar(out=msk[:], in0=dst[:], scalar1=float(np.pi), scalar2=None, op0=ALU.is_gt))
        dve(lambda e: e.scalar_tensor_tensor(out=dst[:], in0=msk[:], scalar=-TWO_PI, in1=dst[:], op0=ALU.mult, op1=ALU.add))
        dve(lambda e: e.tensor_scalar(out=msk[:], in0=dst[:], scalar1=-float(np.pi), scalar2=None, op0=ALU.is_lt))
        dve(lambda e: e.scalar_tensor_tensor(out=dst[:], in0=msk[:], scalar=TWO_PI, in1=dst[:], op0=ALU.mult, op1=ALU.add))
        dve(lambda e: e.tensor_scalar(out=dst[:], in0=dst[:], scalar1=3.1415925, scalar2=-3.1415925, op0=ALU.min, op1=ALU.max))
        act(lambda e: e.activation(out=dst[:], in_=dst[:], func=AF.Sin))

    reduce_sin(lambda: ang[:], rs)
    dve(lambda e: e.tensor_scalar(out=ang[:], in0=ang[:], scalar1=float(np.pi / 2), scalar2=None, op0=ALU.add))
    reduce_sin(lambda: ang[:], rc)
    dve(lambda e: e.tensor_tensor(out=Pr[:], in0=mag[:], in1=rc[:], op=ALU.mult))
    dve(lambda e: e.tensor_tensor(out=Pi[:], in0=mag[:], in1=rs[:], op=ALU.mult))
    dve(lambda e: e.tensor_scalar(out=nPi[:], in0=Pi[:], scalar1=-1.0, scalar2=None, op0=ALU.mult))

    xr = sm("xr", [128, G]); abi = sm("abi", [128, G]); den_ = sm("den_", [128, G]); t1 = sm("t1", [128, G]); t2 = sm("t2", [128, G])
    cr = sm("cr", [128, G]); ci = sm("ci", [128, G]); nci = sm("nci", [128, G])
    are = prm_s[:, 0, :]
    aim = prm_s[:, 1, :]
    dve(lambda e: e.tensor_scalar(out=xr[:], in0=Pr[:, :, 16], scalar1=-1.0, scalar2=None, op0=ALU.add))
    dve(lambda e: e.tensor_copy(out=abi[:], in_=Pi[:, :, 16]))
    dve(lambda e: e.tensor_tensor(out=t1[:], in0=are, in1=are, op=ALU.mult))
    dve(lambda e: e.tensor_tensor(out=t2[:], in0=aim, in1=aim, op=ALU.mult))
    dve(lambda e: e.tensor_tensor(out=den_[:], in0=t1[:], in1=t2[:], op=ALU.add))
    dve(lambda e: e.reciprocal(out=den_[:], in_=den_[:]))
    dve(lambda e: e.tensor_tensor(out=t1[:], in0=xr[:], in1=are, op=ALU.mult))
    dve(lambda e: e.tensor_tensor(out=t2[:], in0=abi[:], in1=aim, op=ALU.mult))
    dve(lambda e: e.tensor_tensor(out=cr[:], in0=t1[:], in1=t2[:], op=ALU.add))
    dve(lambda e: e.tensor_tensor(out=cr[:], in0=cr[:], in1=den_[:], op=ALU.mult))
    dve(lambda e: e.tensor_tensor(out=t1[:], in0=abi[:], in1=are, op=ALU.mult))
    dve(lambda e: e.tensor_tensor(out=t2[:], in0=xr[:], in1=aim, op=ALU.mult))
    dve(lambda e: e.tensor_tensor(out=ci[:], in0=t1[:], in1=t2[:], op=ALU.subtract))
    dve(lambda e: e.tensor_tensor(out=ci[:], in0=ci[:], in1=den_[:], op=ALU.mult))
    dve(lambda e: e.tensor_scalar(out=nci[:], in0=ci[:], scalar1=-1.0, scalar2=None, op0=ALU.mult))

    Bbr = sm("Bbr", [128, G, 16]); Bbi = sm("Bbi", [128, G, 16])
    BX = sm("BX", [128, G, 16]); BY = sm("BY", [128, G, 16]); nBX = sm("nBX", [128, G, 16])
    CX = sm("CX", [128, G, 16]); CY = sm("CY", [128, G, 16])
    for g in range(G):
        dve(lambda e, g=g: e.tensor_scalar(out=Bbr[:, g, :], in0=Bri[:, 0, g, :], scalar1=cr[:, g:g + 1], scalar2=None, op0=ALU.mult), reads=["Bri"])
        dve(lambda e, g=g: e.scalar_tensor_tensor(out=Bbr[:, g, :], in0=Bri[:, 1, g, :], scalar=nci[:, g:g + 1], in1=Bbr[:, g, :],
                                               op0=ALU.mult, op1=ALU.add))
        dve(lambda e, g=g: e.tensor_scalar(out=Bbi[:, g, :], in0=Bri[:, 1, g, :], scalar1=cr[:, g:g + 1], scalar2=None, op0=ALU.mult))
        dve(lambda e, g=g: e.scalar_tensor_tensor(out=Bbi[:, g, :], in0=Bri[:, 0, g, :], scalar=ci[:, g:g + 1], in1=Bbi[:, g, :],
                                               op0=ALU.mult, op1=ALU.add))
    lo, hi = slice(0, 64), slice(64, 128)
    dve(lambda e: e.tensor_copy(out=BX[lo], in_=Bbr[lo]))
    dve(lambda e: e.tensor_copy(out=BX[hi], in_=Bbi[hi]))
    dve(lambda e: e.tensor_scalar(out=BY[lo], in0=Bbi[lo], scalar1=-1.0, scalar2=None, op0=ALU.mult))
    dve(lambda e: e.tensor_copy(out=BY[hi], in_=Bbr[hi]))
    dve(lambda e: e.tensor_scalar(out=nBX[:], in0=BX[:], scalar1=-1.0, scalar2=None, op0=ALU.mult))
    dve(lambda e: e.tensor_copy(out=CX[lo], in_=Cri[lo, 0, :, :]), reads=["Cri"])
    dve(lambda e: e.tensor_scalar(out=CX[hi], in0=Cri[hi, 1, :, :], scalar1=-1.0, scalar2=None, op0=ALU.mult))
    dve(lambda e: e.tensor_scalar(out=CY[lo], in0=Cri[lo, 1, :, :], scalar1=-1.0, scalar2=None, op0=ALU.mult))
    dve(lambda e: e.tensor_scalar(out=CY[hi], in0=Cri[hi, 0, :, :], scalar1=-1.0, scalar2=None, op0=ALU.mult))

    BcA = sm("BcA", [128, G, 8, 16]); BcB = sm("BcB", [128, G, 8, 16]); Cm = sm("Cm", [128, G, 8, 16]); Cc = sm("Cc", [128, G, 8, 16])
    for g in range(G):
        for j in range(8):
            dve(lambda e, g=g, j=j: e.tensor_scalar(out=BcA[:, g, j, :], in0=BX[:, g, :], scalar1=Pr[:, g, j:j + 1], scalar2=None, op0=ALU.mult))
            dve(lambda e, g=g, j=j: e.scalar_tensor_tensor(out=BcA[:, g, j, :], in0=BY[:, g, :], scalar=Pi[:, g, j:j + 1], in1=BcA[:, g, j, :],
                                                        op0=ALU.mult, op1=ALU.add))
            dve(lambda e, g=g, j=j: e.tensor_scalar(out=BcB[:, g, j, :], in0=BY[:, g, :], scalar1=Pr[:, g, j:j + 1], scalar2=None, op0=ALU.mult))
            dve(lambda e, g=g, j=j: e.scalar_tensor_tensor(out=BcB[:, g, j, :], in0=nBX[:, g, :], scalar=Pi[:, g, j:j + 1], in1=BcB[:, g, j, :],
                                                        op0=ALU.mult, op1=ALU.add))
            for dst, k0 in ((Cm, 8), (Cc, 16)):
                dve(lambda e, g=g, j=j, dst=dst, k0=k0: e.tensor_scalar(out=dst[:, g, j, :], in0=CX[:, g, :], scalar1=Pr[:, g, k0 + j:k0 + j + 1],
                                                                   scalar2=None, op0=ALU.mult))
                dve(lambda e, g=g, j=j, dst=dst, k0=k0: e.scalar_tensor_tensor(out=dst[:, g, j, :], in0=CY[:, g, :], scalar=Pi[:, g, k0 + j:k0 + j + 1],
                                                                          in1=dst[:, g, j, :], op0=ALU.mult, op1=ALU.add))

    MT16 = cx.sb("MT16", [128, G, 128], BF16)
    BaT16 = cx.sb("BaT16", [128, G, 128], BF16)
    BbT16 = cx.sb("BbT16", [128, G, 128], BF16)
    Cc16 = cx.sb("Cc16", [128, G, 128], BF16)
    dve(lambda e: e.tensor_copy(out=Cc16[:].rearrange("p g n -> p (g n)"), in_=Cc[:].rearrange("p g t h -> p (g t h)")), writes=["Cc16"])
    for g in range(G):
        bca = BcA[:, g, :, :].rearrange("p s h -> p (s h)")
        bcb = BcB[:, g, :, :].rearrange("p s h -> p (s h)")
        cm = Cm[:, g, :, :].rearrange("p t h -> p (t h)")
        ps, pk = cx.psum()
        P.op("pe", lambda e, ps=ps, bca=bca, cm=cm: e.matmul(ps[:, 0:128], lhsT=bca, rhs=cm, start=True, stop=True), reads=[K], writes=[pk])
        P.op("dve", lambda e, ps=ps, g=g: e.tensor_tensor(out=MT16[:, g, :], in0=ps[:, 0:128], in1=maskT[:], op=ALU.mult),
             reads=[pk, "maskT"], writes=[("MT16", g)])
        ps, pk = cx.psum()
        P.op("pe", lambda e, ps=ps, bca=bca: e.transpose(ps[:, 0:128], bca, ident[:]), reads=[K, "ident"], writes=[pk])
        P.op("dve", lambda e, ps=ps, g=g: e.tensor_copy(out=BaT16[:, g, :], in_=ps[:, 0:128]), reads=[pk], writes=[("BaT16", g)])
        ps, pk = cx.psum()
        P.op("pe", lambda e, ps=ps, bcb=bcb: e.transpose(ps[:, 0:128], bcb, ident[:]), reads=[K, "ident"], writes=[pk])
        P.op("dve", lambda e, ps=ps, g=g: e.tensor_copy(out=BbT16[:, g, :], in_=ps[:, 0:128]), reads=[pk], writes=[("BbT16", g)])

    SA = [cx.sb("SA%d" % i, [128, 2, NCH], F32) for i in range(2)]
    SB = [cx.sb("SB%d" % i, [128, 2, NCH], F32) for i in range(2)]
    S16 = [cx.sb("S16_%d" % i, [128, 2, NCH], BF16) for i in range(2)]
    ysb = [cx.sb("ysb%d" % i, [128, 2, NCH], F32) for i in range(2)]
    for i in range(2):
        P.op("pool", lambda e, i=i: e.memset(S16[i][:], 0.0), writes=[("S16", i)])
    for g in range(G):
        for (WT, dst, dk) in ((BaT16, SA[0], "SA0"), (BbT16, SB[0], "SB0")):
            for b in range(2):
                ps, pk = cx.psum()
                P.op("pe", lambda e, ps=ps, WT=WT, g=g, b=b: e.matmul(ps[:], lhsT=WT[:, g, :], rhs=U[:, g, b, :], start=True, stop=True),
                     reads=[("BaT16", g), ("BbT16", g), ("U", g)], writes=[pk])
                P.op("dve", lambda e, ps=ps, dst=dst, b=b: e.tensor_copy(out=dst[:, b, :], in_=ps[:]), reads=[pk], writes=[(dk, b)])
        cur = 0
        for j in range(9):
            d = 2 ** j
            nxt = 1 - cur
            oA, oB, nA, nB = SA[cur], SB[cur], SA[nxt], SB[nxt]
            kA = ["SA%d" % cur, ("SA%d" % cur, 0), ("SA%d" % cur, 1)]
            kB = ["SB%d" % cur, ("SB%d" % cur, 0), ("SB%d" % cur, 1)]
            wA = ["SA%d" % nxt, ("SA%d" % nxt, 0), ("SA%d" % nxt, 1)]
            wB = ["SB%d" % nxt, ("SB%d" % nxt, 0), ("SB%d" % nxt, 1)]
            pr = Pr[:, g, 24 + j:25 + j]
            pi = Pi[:, g, 24 + j:25 + j]
            npi = nPi[:, g, 24 + j:25 + j]
            P.op("dve", lambda e, oA=oA, nA=nA, d=d, pr=pr: e.scalar_tensor_tensor(
                out=nA[:, :, d:NCH], in0=oA[:, :, 0:NCH - d], scalar=pr, in1=oA[:, :, d:NCH], op0=ALU.mult, op1=ALU.add),
                reads=kA + [K], writes=wA)
            P.op("dve", lambda e, oB=oB, nA=nA, d=d, pi=pi: e.scalar_tensor_tensor(
                out=nA[:, :, d:NCH], in0=oB[:, :, 0:NCH - d], scalar=pi, in1=nA[:, :, d:NCH], op0=ALU.mult, op1=ALU.add),
                reads=kB + wA, writes=wA)
            P.op("act", lambda e, oA=oA, nA=nA, d=d: e.activation(out=nA[:, :, 0:d], in_=oA[:, :, 0:d], func=AF.Identity), reads=kA, writes=wA)
            P.op("dve", lambda e, oB=oB, nB=nB, d=d, pr=pr: e.scalar_tensor_tensor(
                out=nB[:, :, d:NCH], in0=oB[:, :, 0:NCH - d], scalar=pr, in1=oB[:, :, d:NCH], op0=ALU.mult, op1=ALU.add),
                reads=kB, writes=wB)
            P.op("dve", lambda e, oA=oA, nB=nB, d=d, npi=npi: e.scalar_tensor_tensor(
                out=nB[:, :, d:NCH], in0=oA[:, :, 0:NCH - d], scalar=npi, in1=nB[:, :, d:NCH], op0=ALU.mult, op1=ALU.add),
                reads=kA + wB, writes=wB)
            P.op("act", lambda e, oB=oB, nB=nB, d=d: e.activation(out=nB[:, :, 0:d], in_=oB[:, :, 0:d], func=AF.Identity), reads=kB, writes=wB)
            cur = nxt
        fin = SA[cur]
        s16 = S16[g % 2]
        P.op("act", lambda e, fin=fin, s16=s16: e.activation(out=s16[:, :, 1:NCH], in_=fin[:, :, 0:NCH - 1], func=AF.Identity),
             reads=["SA%d" % cur], writes=[("S16", g % 2)])
        for b in range(2):
            ps, pk = cx.psum()
            P.op("pe", lambda e, ps=ps, g=g, b=b: e.matmul(ps[:], lhsT=MT16[:, g, :], rhs=U[:, g, b, :], start=True, stop=False),
                 reads=[("MT16", g), ("U", g)], writes=[pk], signal=False)
            P.op("pe", lambda e, ps=ps, g=g, b=b, s16=s16: e.matmul(ps[:], lhsT=Cc16[:, g, :], rhs=s16[:, b, :], start=False, stop=True),
                 reads=["Cc16", ("S16", g % 2)], writes=[pk])
            y = ysb[g % 2]
            P.op("dve", lambda e, ps=ps, g=g, b=b, y=y: e.scalar_tensor_tensor(
                out=y[:, b, :], in0=U[:, g, b, :], scalar=Dcol[:, g:g + 1], in1=ps[:], op0=ALU.mult, op1=ALU.add),
                reads=[pk, "Dcol", ("U", g)], writes=[("ysb", g % 2, b)])
        cx.store(out[:, g, :, :].rearrange("p b c -> p (b c)"), ysb[g % 2][:].rearrange("p b c -> p (b c)"),
                 [("ysb", g % 2, 0), ("ysb", g % 2, 1)], ("out", g), sem=("st_ysb", g % 2))
    return cx.finish()


def ssm_inputs(projT, l, inp, core):
    G = 8
    gs = slice(core * G, (core + 1) * G)
    u = projT[:, core * 128:(core + 1) * 128, :]
    U = u.reshape(2, G, 16, 512, 8).transpose(4, 2, 1, 0, 3).reshape(128, G, 2, 512)
    dup = lambda a: np.concatenate([a, a], axis=0)
    are = inp["ssm_a_re"][l][gs].T
    aim = inp["ssm_a_im"][l][gs].T
    ldt = np.broadcast_to(inp["ssm_log_dt"][l][gs][None, :], (64, G))
    prm = dup(np.stack([are, aim, ldt], axis=1))
    Bri = dup(np.stack([inp["ssm_b_re"][l][gs].transpose(1, 0, 2), inp["ssm_b_im"][l][gs].transpose(1, 0, 2)], axis=1))
    Cri = dup(np.stack([inp["ssm_c_re"][l][gs].transpose(2, 0, 1), inp["ssm_c_im"][l][gs].transpose(2, 0, 1)], axis=1))
    dsk = inp["ssm_d"][l][core * 128:(core + 1) * 128].reshape(G, 16)
    Dcol = np.tile(dsk.T, (8, 1))
    sidx = np.arange(128) // 16
    maskT = (sidx[:, None] <= sidx[None, :]).astype(np.float32)
    return {"U": np.ascontiguousarray(U), "prm": np.ascontiguousarray(prm, dtype=np.float32),
            "Bri": np.ascontiguousarray(Bri, dtype=np.float32), "Cri": np.ascontiguousarray(Cri, dtype=np.float32),
            "Dcol": np.ascontiguousarray(Dcol, dtype=np.float32),
            "nvec": np.ascontiguousarray(np.broadcast_to(ssm_nvec()[None, :], (128, NPW))),
            "maskT": maskT, "ident": np.eye(128, dtype=np.float32)}


def run_ssm(projT, l, inp):
    nc = get_nc("ssm", build_ssm)
    ins = [ssm_inputs(projT, l, inp, i) for i in range(NCORES)]
    res = run(nc, ins)
    yT = np.zeros((2, 1024, SEQ), np.float32)
    for i in range(NCORES):
        y = res[i]["ypre"]
        yT[:, i * 128:(i + 1) * 128, :] = y.reshape(8, 16, 8, 2, 512).transpose(3, 2, 1, 4, 0).reshape(2, 128, SEQ)
    return yT


def build_post(moe):
    cx = Ctx()
    P = cx.P
    xTd = cx.din("xT", [128, DC, T], F32)
    ypd = cx.din("ypreT", [128, 8, T], F32)
    yad = cx.din("yattT", [128, 4, T], BF16)
    gsd = cx.din("gsT", [128, DC, T], BF16)
    gad = cx.din("gaT", [128, DC, T], BF16)
    modd = cx.din("modv", [128, 6, DC], F32)
    gnd = cx.din("gnorm", [128, DC], F32)
    bgd = cx.din("bglu", [128, 8], F32)
    Wglu = cx.din("w_glu", [1024, 1024], F32)
    Wbs = cx.din("w_bs", [1024, D], F32)
    Wba = cx.din("w_ba", [512, D], F32)
    Wout = cx.din("w_out", [D, D], F32)
    xo = cx.dout("xT_out", [128, DC, T], F32)
    ho = cx.dout("h2T", [128, DC, T], BF16)
    if moe:
        wrd = cx.din("w_router", [128, DC, NEXP], F32)
        rwo = cx.dout("rw", [T // 128, 128, NEXP], F32)
    cx.alloc_psum(8)
    xT = cx.sb("xT_sb", [128, DC, T], F32)
    bufA = cx.sb("bufA", [128, DC, T], BF16)
    y32 = bufA[:].bitcast(F32).rearrange("p (k t) -> p k t", k=8) if False else None
    y32t = cx.sb("y32", [128, 8, T // 2], F32) if False else None
    y16 = cx.sb("y16", [128, 8, T], BF16)
    yg16 = cx.sb("yg16", [128, 8, T], BF16)
    ya16 = cx.sb("ya16", [128, 4, T], BF16)
    modv = cx.sb("modv_sb", [128, 6, DC], F32)
    gnorm = cx.sb("gnorm_sb", [128, DC], F32)
    gm = cx.sb("gm_sb", [128, DC], F32)
    bglu = cx.sb("bglu_sb", [128, 8], F32)
    ones16 = cx.sb("ones16", [128, 128], BF16)
    P.op("pool", lambda e: e.memset(ones16[:], 1.0), writes=["ones16"])
    for k0 in range(0, DC, 4):
        P.dma("sp", lambda e, k0=k0: e.dma_start(out=xT[:, k0:k0 + 4, :], in_=xTd[:, k0:k0 + 4, :]), writes=["xT"], sem="xT")
    cx.load(ya16[:], yad, "ya16")
    cx.load(modv[:], modd, "modv_in")
    cx.load(gnorm[:], gnd, "gnorm")
    cx.load(bglu[:], bgd, "bglu")
    wl = WLoader(cx, "wl", DC, 128, nslots=4, nstage=4)

    y32 = bufA[:].rearrange("p k t -> p (k t)").bitcast(F32).rearrange("p (k t) -> p k t", k=8)
    yp = [cx.sb("yp%d" % i, [128, T], F32) for i in range(2)]
    ta = [cx.sb("ta%d" % i, [128, T], F32) for i in range(2)]
    for kc in range(8):
        s = kc % 2
        cx.load(yp[s][:], ypd[:, kc, :], ("yp", s))
        P.op("act", lambda e, s=s: e.activation(out=ta[s][:], in_=yp[s][:], func=AF.Square), reads=[("yp", s)], writes=[("ta", s)])
        P.op("dve", lambda e, s=s: e.tensor_scalar(out=ta[s][:], in0=ta[s][:], scalar1=0.044715, scalar2=1.0, op0=ALU.mult, op1=ALU.add),
             reads=[("ta", s)], writes=[("ta", s)])
        P.op("dve", lambda e, s=s: e.tensor_tensor(out=ta[s][:], in0=ta[s][:], in1=yp[s][:], op=ALU.mult), reads=[("ta", s), ("yp", s)], writes=[("ta", s)])
        P.op("act", lambda e, s=s: e.activation(out=ta[s][:], in_=ta[s][:], func=AF.Sigmoid, scale=1.5957691216057308),
             reads=[("ta", s)], writes=[("ta", s)])
        P.op("dve", lambda e, s=s, kc=kc: e.tensor_tensor(out=y32[:, kc, :], in0=ta[s][:], in1=yp[s][:], op=ALU.mult),
             reads=[("ta", s), ("yp", s)], writes=["bufA"])
        P.op("pool", lambda e, kc=kc: e.tensor_copy(out=y16[:, kc, :], in_=y32[:, kc, :]), reads=["bufA"], writes=["y16"])

    sg = [cx.sb("sg%d" % i, [128, 512], F32) for i in range(2)]
    cnt = [0]

    def evac_glu(nt, half, ps, pk):
        s = cnt[0] % 2
        cnt[0] += 1
        hs = slice(half * 512, (half + 1) * 512)
        P.op("act", lambda e: e.activation(out=sg[s][:], in_=ps[:], func=AF.Sigmoid, bias=bglu[:, nt:nt + 1]),
             reads=[pk, "bglu"], writes=[("sg", s)])
        P.op("dve", lambda e: e.tensor_tensor(out=yg16[:, nt, hs], in0=sg[s][:], in1=y32[:, nt, hs], op=ALU.mult),
             reads=[("sg", s), "bufA"], writes=["yg16"])

    wl.KC = 8
    emit_linear(cx, wl, Wglu, 8, 1024, y16, ["y16"], evac_glu, ngrp=128)

    gts = [cx.sb("gts%d" % i, [128, 512], BF16) for i in range(2)]
    gta = [cx.sb("gta%d" % i, [128, 512], BF16) for i in range(2)]
    m1 = [cx.sb("m1_%d" % i, [128, 512], F32) for i in range(2)]
    m2 = [cx.sb("m2_%d" % i, [128, 512], F32) for i in range(2)]
    it = 0
    for nt in range(DC):
        wl.KC = 8
        wbs, kbs = wl.load(Wbs[:, nt * 128:(nt + 1) * 128], 128)
        wl.KC = 4
        wba, kba = wl.load(Wba[:, nt * 128:(nt + 1) * 128], 128)
        for half in range(2):
            s = it % 2
            it += 1
            hs = slice(half * 512, (half + 1) * 512)
            cx.load(gts[s][:], gsd[:, nt, hs], ("gts", s))
            cx.load(gta[s][:], gad[:, nt, hs], ("gta", s))
            ps1, pk1 = cx.psum()
            mm_group(cx, ps1[:], pk1, [(wbs[:, kc, 0:128], yg16[:, kc, hs]) for kc in range(8)], list(kbs) + ["yg16"])
            ps2, pk2 = cx.psum()
            mm_group(cx, ps2[:], pk2, [(wba[:, kc, 0:128], ya16[:, kc, hs]) for kc in range(4)], list(kba) + ["ya16"])
            P.op("act", lambda e, s=s: e.activation(out=m1[s][:], in_=gts[s][:], func=AF.Sigmoid), reads=[("gts", s)], writes=[("m1", s)])
            P.op("act", lambda e, s=s: e.activation(out=m2[s][:], in_=gta[s][:], func=AF.Sigmoid), reads=[("gta", s)], writes=[("m2", s)])
            P.op("dve", lambda e, s=s, ps1=ps1: e.tensor_tensor(out=m1[s][:], in0=m1[s][:], in1=ps1[:], op=ALU.mult), reads=[("m1", s), pk1], writes=[("m1", s)])
            P.op("dve", lambda e, s=s, ps2=ps2: e.tensor_tensor(out=m2[s][:], in0=m2[s][:], in1=ps2[:], op=ALU.mult), reads=[("m2", s), pk2], writes=[("m2", s)])
            P.op("pool", lambda e, s=s, nt=nt, hs=hs: e.tensor_tensor(out=bufA[:, nt, hs], in0=m1[s][:], in1=m2[s][:], op=ALU.add),
                 reads=[("m1", s), ("m2", s)], writes=["bufA"])

    def evac_out(nt, half, ps, pk):
        hs = slice(half * 512, (half + 1) * 512)
        P.op("dve", lambda e: e.scalar_tensor_tensor(out=xT[:, nt, hs], in0=ps[:], scalar=modv[:, 2, nt:nt + 1], in1=xT[:, nt, hs],
                                                    op0=ALU.mult, op1=ALU.add), reads=[pk, "modv_in", "xT"], writes=["xT"])

    wl.KC = DC
    emit_linear(cx, wl, Wout, DC, D, bufA, ["bufA"], evac_out, ngrp=128)
    for k0 in range(0, DC, 4):
        cx.store(xo[:, k0:k0 + 4, :], xT[:, k0:k0 + 4, :], ["xT"], ("xo", k0), sem="st_x")

    P.op("dve", lambda e: e.tensor_scalar(out=gm[:], in0=modv[:, 4, :], scalar1=1.0, scalar2=None, op0=ALU.add), reads=["modv_in"], writes=["gm_tmp"])
    P.op("dve", lambda e: e.tensor_tensor(out=gm[:], in0=gm[:], in1=gnorm[:], op=ALU.mult), reads=["gm_tmp", "gnorm"], writes=["modv"])
    rstd = emit_rmsnorm_mod(cx, xT, "xT", gm, modv[:, 3, :], bufA, "bufA", ones16, "n2")
    for k0 in range(0, DC, 4):
        cx.store(ho[:, k0:k0 + 4, :], bufA[:, k0:k0 + 4, :], ["bufA"], ("ho", k0), sem="st_h")

    if moe:
        wr = cx.sb("wr_sb", [128, DC, NEXP], F32)
        cx.load(wr[:], wrd, "wr")
        h32 = [cx.sb("h32_%d" % i, [128, 128], F32) for i in range(3)]
        lgt = cx.sb("lgt", [128, NEXP], F32)
        mx8 = cx.sb("mx8", [128, 8], F32)
        nm1 = cx.sb("nm1", [128, 1], F32)
        sel = cx.sb("sel", [128, NEXP], F32)
        ex = cx.sb("ex", [128, NEXP], F32)
        ssum = cx.sb("ssum", [128, 1], F32)
        rwt = [cx.sb("rwt%d" % i, [128, NEXP], F32) for i in range(2)]
        R = "router"
        for tt in range(T // 128):
            ts_ = slice(tt * 128, (tt + 1) * 128)
            ps, pk = cx.psum()
            for kc in range(DC):
                s = kc % 3
                P.op("dve", lambda e, s=s, kc=kc, ts_=ts_: e.tensor_tensor(out=h32[s][:], in0=xT[:, kc, ts_], in1=rstd[:, ts_], op=ALU.mult),
                     reads=["xT", ("n2", "rstd", tt // 4)], writes=[("h32", s)])
                P.op("act", lambda e, s=s, kc=kc: e.activation(out=h32[s][:], in_=h32[s][:], func=AF.Identity, scale=gm[:, kc:kc + 1],
                                                            bias=modv[:, 3, kc:kc + 1]), reads=[("h32", s), "modv"], writes=[("h32", s)])
                P.op("pe", lambda e, s=s, kc=kc, ps=ps: e.matmul(ps[:, 0:NEXP], lhsT=h32[s][:], rhs=wr[:, kc, :], start=(kc == 0), stop=(kc == DC - 1)),
                     reads=[("h32", s), "wr"], writes=[pk], signal=True)
            P.op("dve", lambda e, ps=ps: e.tensor_copy(out=lgt[:], in_=ps[:, 0:NEXP]), reads=[pk, R], writes=[R])
            P.op("dve", lambda e: e.max(out=mx8[:], in_=lgt[:]), reads=[R], writes=[R])
            P.op("dve", lambda e: e.tensor_scalar(out=nm1[:], in0=mx8[:, 0:1], scalar1=-1.0, scalar2=None, op0=ALU.mult), reads=[R], writes=[R])
            P.op("dve", lambda e: e.tensor_scalar(out=sel[:], in0=lgt[:], scalar1=mx8[:, 1:2], scalar2=None, op0=ALU.is_ge), reads=[R], writes=[R])
            P.op("act", lambda e: e.activation(out=ex[:], in_=lgt[:], func=AF.Exp, bias=nm1[:, 0:1]), reads=[R], writes=[R])
            P.op("dve", lambda e: e.tensor_tensor(out=ex[:], in0=ex[:], in1=sel[:], op=ALU.mult), reads=[R], writes=[R])
            P.op("dve", lambda e: e.tensor_reduce(out=ssum[:], in_=ex[:], axis=mybir.AxisListType.X, op=ALU.add), reads=[R], writes=[R])
            P.op("dve", lambda e: e.reciprocal(out=ssum[:], in_=ssum[:]), reads=[R], writes=[R])
            o = rwt[tt % 2]
            P.op("dve", lambda e, o=o: e.tensor_scalar(out=o[:], in0=ex[:], scalar1=ssum[:, 0:1], scalar2=None, op0=ALU.mult),
                 reads=[R], writes=[R, ("rwt", tt % 2)])
            cx.store(rwo[tt], o[:], [("rwt", tt % 2)], ("rwo", tt), sem=("st_rw", tt % 2))
    return cx.finish()


def post_inputs(i, xTs, ypreT, yattT, projT, mod_l, l, inp, moe):
    b, q = i // 4, i % 4
    ts_ = slice(q * T, (q + 1) * T)
    m = {"xT": xTs[i], "ypreT": fm(ypreT[b][:, ts_]), "yattT": fm(yattT[b][:, ts_]),
         "gsT": fm(projT[b, 5632:7680, ts_]), "gaT": fm(projT[b, 7680:9728, ts_]),
         "modv": modv_layout(mod_l[b]), "gnorm": vec_fm(inp["norm_ffn_g"][l]), "bglu": vec_fm(inp["b_glu"][l]),
         "w_glu": np.ascontiguousarray(inp["w_glu"][l]), "w_bs": np.ascontiguousarray(inp["w_branch_ssm"][l]),
         "w_ba": np.ascontiguousarray(inp["w_branch_att"][l]), "w_out": np.ascontiguousarray(inp["w_out"][l])}
    if moe:
        m["w_router"] = np.ascontiguousarray(inp["moe_router"][l // 2].reshape(DC, 128, NEXP).transpose(1, 0, 2))
    return m


def run_post(xTs, ypreT, yattT, projT, mod_l, l, inp, moe):
    nc = get_nc("post%d" % int(moe), lambda: build_post(moe))
    ins = [post_inputs(i, xTs, ypreT, yattT, projT, mod_l, l, inp, moe) for i in range(NCORES)]
    res = run(nc, ins)
    xo = [res[i]["xT_out"] for i in range(NCORES)]
    h2 = [res[i]["h2T"] for i in range(NCORES)]
    rw = [res[i]["rw"].reshape(T, NEXP) for i in range(NCORES)] if moe else None
    return xo, h2, rw


FG = 4


def ffn_bufs(cx, tag, with_rw):
    g16 = [cx.sb("%s_g16_%d" % (tag, i), [128, FG, T], BF16) for i in range(2)]
    sgl = [cx.sb("%s_sg%d" % (tag, i), [128, 512], F32) for i in range(2)]
    tt = [cx.sb("%s_tt%d" % (tag, i), [128, 512], F32) for i in range(2)] if with_rw else None
    return g16, sgl, tt


def emit_ffn(cx, bufs, hT, hkeys, nft, get_gu, get_d, out_evac, tag, rwb=None, rwkey=None):
    P = cx.P
    g16, sgl, tt = bufs
    it = 0
    ngroups = (nft + FG - 1) // FG
    for fg in range(ngroups):
        gb = g16[fg % 2]
        nj = min(FG, nft - fg * FG)
        for j in range(nj):
            ft = fg * FG + j
            gu = get_gu(ft)
            wg, wu, wkeys = gu[0], gu[1], gu[2]
            co = gu[3] if len(gu) > 3 else 0
            for half in range(T // 512):
                hs = slice(half * 512, (half + 1) * 512)
                psg, pkg = cx.psum()
                mm_group(cx, psg[:], pkg, [(wg[:, kc, co:co + 128], hT[:, kc, hs]) for kc in range(DC)], list(wkeys) + list(hkeys))
                psu, pku = cx.psum()
                mm_group(cx, psu[:], pku, [(wu[:, kc, co:co + 128], hT[:, kc, hs]) for kc in range(DC)], list(wkeys) + list(hkeys))
                s = it % 2
                it += 1
                P.op("act", lambda e, s=s, psg=psg: e.activation(out=sgl[s][:], in_=psg[:], func=AF.Silu), reads=[pkg], writes=[(tag, "sg", s)])
                if rwb is None:
                    P.op("dve", lambda e, s=s, psu=psu, gb=gb, j=j, hs=hs: e.tensor_tensor(out=gb[:, j, hs], in0=sgl[s][:], in1=psu[:], op=ALU.mult),
                         reads=[(tag, "sg", s), pku], writes=[(tag, "g16", fg % 2, j)])
                else:
                    P.op("dve", lambda e, s=s, psu=psu: e.tensor_tensor(out=tt[s][:], in0=sgl[s][:], in1=psu[:], op=ALU.mult),
                         reads=[(tag, "sg", s), pku], writes=[(tag, "tt", s)])
                    P.op("pool", lambda e, s=s, gb=gb, j=j, hs=hs: e.tensor_tensor(out=gb[:, j, hs], in0=tt[s][:], in1=rwb[:, hs], op=ALU.mult),
                         reads=[(tag, "tt", s), rwkey], writes=[(tag, "g16", fg % 2, j)])
        wd, dkeys = get_d(fg)
        for dc in range(DC):
            for half in range(T // 512):
                hs = slice(half * 512, (half + 1) * 512)
                ps, pk = cx.psum()
                mm_group(cx, ps[:], pk, [(wd[:, j, dc * 128:(dc + 1) * 128], gb[:, j, hs]) for j in range(nj)],
                         list(dkeys) + [(tag, "g16", fg % 2, j) for j in range(nj)])
                out_evac(fg, dc, half, ps, pk)


class DLoader:
    def __init__(self, cx, name):
        self.cx = cx
        self.name = name
        self.wd = cx.sb(name + "_wd", [128, FG, D], BF16)
        self.stg = [cx.sb("%s_st%d" % (name, i), [128, D], F32) for i in range(2)]
        self.si = 0

    def load(self, Wd, fg, nj):
        cx = self.cx
        keys = []
        for j in range(nj):
            ft = fg * FG + j
            s = self.si % 2
            self.si += 1
            stg = self.stg[s]
            skey = (self.name, "st", s)
            cx.P.dma("sp", lambda e, stg=stg, ft=ft: e.dma_start(out=stg[:], in_=Wd[ft * 128:(ft + 1) * 128, :]), writes=[skey])
            key = (self.name, "wd", j)
            cx.copy(cx.conv_eng(), self.wd[:, j, :], stg[:], [skey], [key])
            keys.append(key)
        return self.wd, keys


def build_ffn():
    cx = Ctx()
    P = cx.P
    xTd = cx.din("xT", [128, DC, T], F32)
    hTd = cx.din("h2T", [128, DC, T], BF16)
    gfd = cx.din("gatef", [128, DC], F32)
    Wg = cx.din("w_gate", [D, DFF], F32)
    Wu = cx.din("w_up", [D, DFF], F32)
    Wd = cx.din("w_down", [DFF, D], F32)
    xo = cx.dout("xT_out", [128, DC, T], F32)
    cx.alloc_psum(8)
    xT = cx.sb("xT_sb", [128, DC, T], F32)
    hT = cx.sb("hT_sb", [128, DC, T], BF16)
    gf = cx.sb("gf_sb", [128, DC], F32)
    for k0 in range(0, DC, 4):
        P.dma("sp", lambda e, k0=k0: e.dma_start(out=hT[:, k0:k0 + 4, :], in_=hTd[:, k0:k0 + 4, :]), writes=["hT"], sem="hT")
    for k0 in range(0, DC, 4):
        P.dma("sp", lambda e, k0=k0: e.dma_start(out=xT[:, k0:k0 + 4, :], in_=xTd[:, k0:k0 + 4, :]), writes=["xT"], sem="xT")
    cx.load(gf[:], gfd, "gf")
    wl = WLoader(cx, "wgu", DC, 256, nslots=4, nstage=4)
    dl = DLoader(cx, "wdl")
    nft = DFF // 128
    cache = {}

    def get_gu(ft):
        base = ft - ft % 2
        if base not in cache:
            cache.clear()
            wg, k1 = wl.load(Wg[:, base * 128:base * 128 + 256], 256)
            wu, k2 = wl.load(Wu[:, base * 128:base * 128 + 256], 256)
            cache[base] = (wg, wu, list(k1) + list(k2))
        wg, wu, keys = cache[base]
        return wg, wu, keys, (ft % 2) * 128

    def get_d(fg):
        return dl.load(Wd, fg, min(FG, nft - fg * FG))

    def out_evac(fg, dc, half, ps, pk):
        hs = slice(half * 512, (half + 1) * 512)
        P.op("dve", lambda e: e.scalar_tensor_tensor(out=xT[:, dc, hs], in0=ps[:], scalar=gf[:, dc:dc + 1], in1=xT[:, dc, hs],
                                                    op0=ALU.mult, op1=ALU.add), reads=[pk, "gf", "xT"], writes=["xT"])

    emit_ffn(cx, ffn_bufs(cx, "ffn", False), hT, ["hT"], nft, get_gu, get_d, out_evac, "ffn")
    for k0 in range(0, DC, 4):
        cx.store(xo[:, k0:k0 + 4, :], xT[:, k0:k0 + 4, :], ["xT"], ("xo", k0), sem="st_x")
    return cx.finish()


NCHUNK = SEQ * 2 // T


def build_moe():
    cx = Ctx()
    P = cx.P
    nc = cx.nc
    hTd = cx.din("h2T", [NCHUNK, 128, DC, T], BF16)
    rwd = cx.din("rwb", [NCHUNK, 128, T], F32)
    Wg = cx.din("w_gate", [D, DFFE], F32)
    Wu = cx.din("w_up", [D, DFFE], F32)
    Wd = cx.din("w_down", [DFFE, D], F32)
    po = cx.dout("partial", [NCHUNK, 128, DC, T], BF16)
    Wg16 = nc.dram_tensor("Wg16", [D, DFFE], BF16, kind="Internal").ap()
    Wu16 = nc.dram_tensor("Wu16", [D, DFFE], BF16, kind="Internal").ap()
    Wd16 = nc.dram_tensor("Wd16", [DFFE, D], BF16, kind="Internal").ap()
    cx.alloc_psum(8)
    nft = DFFE // 128
    stg = [cx.sb("cv_st%d" % i, [128, 4, 512], F32) for i in range(3)]
    o16 = [cx.sb("cv_o%d" % i, [128, 4, 512], BF16) for i in range(3)]
    ci = 0
    for (Wsrc, Wdst, name, KC_, ncols) in ((Wg, Wg16, "Wg16", DC, DFFE), (Wu, Wu16, "Wu16", DC, DFFE), (Wd, Wd16, "Wd16", nft, D)):
        for kq in range(0, KC_, 4):
            for cb in range(ncols // 512):
                s = ci % 3
                ci += 1
                src = Wsrc[kq * 128:(kq + 4) * 128, cb * 512:(cb + 1) * 512].rearrange("(c p) n -> p c n", p=128)
                dst = Wdst[kq * 128:(kq + 4) * 128, cb * 512:(cb + 1) * 512].rearrange("(c p) n -> p c n", p=128)
                P.dma("sp", lambda e, s=s, src=src: e.dma_start(out=stg[s][:], in_=src), writes=[("cvst", s)])
                cx.copy(cx.conv_eng(), o16[s][:], stg[s][:], [("cvst", s)], [("cvo", s)])
                P.dma("pool", lambda e, s=s, dst=dst: e.dma_start(out=dst, in_=o16[s][:]), reads=[("cvo", s)], writes=[(name, kq // 4, cb)],
                      sem=("cvout", s))
    hT = cx.sb("hT_sb", [128, DC, T], BF16)
    rwb = cx.sb("rwb_sb", [128, T], F32)
    acc = cx.sb("acc", [128, DC, T], F32)
    wgt = [cx.sb("wg%d" % i, [128, DC, 128], BF16) for i in range(2)]
    wut = [cx.sb("wu%d" % i, [128, DC, 128], BF16) for i in range(2)]
    wdt = cx.sb("wdt", [128, FG, D], BF16)
    o16c = [cx.sb("oc%d" % i, [128, T], BF16) for i in range(2)]
    gi = [0]
    bufs = ffn_bufs(cx, "moe", True)
    for ch in range(NCHUNK):
        for k0 in range(0, DC, 4):
            P.dma("sp", lambda e, k0=k0, ch=ch: e.dma_start(out=hT[:, k0:k0 + 4, :], in_=hTd[ch, :, k0:k0 + 4, :]), writes=["hT"], sem="hT")
        cx.load(rwb[:], rwd[ch], "rwb")

        def get_gu(ft):
            s = gi[0] % 2
            gi[0] += 1
            dep = [("Wg16", kq, ft // 4) for kq in range(4)] + [("Wu16", kq, ft // 4) for kq in range(4)]
            srcg = Wg16[:, ft * 128:(ft + 1) * 128].rearrange("(c p) n -> p c n", p=128)
            srcu = Wu16[:, ft * 128:(ft + 1) * 128].rearrange("(c p) n -> p c n", p=128)
            P.dma("sp", lambda e, s=s, srcg=srcg: e.dma_start(out=wgt[s][:], in_=srcg), reads=dep, writes=[("wgt", s)])
            P.dma("sp", lambda e, s=s, srcu=srcu: e.dma_start(out=wut[s][:], in_=srcu), reads=dep, writes=[("wut", s)])
            return wgt[s], wut[s], [("wgt", s), ("wut", s)]

        def get_d(fg):
            keys = []
            for j in range(FG):
                ft = fg * FG + j
                dep = [("Wd16", ft // 4, cb) for cb in range(D // 512)]
                P.dma("sp", lambda e, j=j, ft=ft: e.dma_start(out=wdt[:, j, :], in_=Wd16[ft * 128:(ft + 1) * 128, :]), reads=dep, writes=[("wdt", j)])
                keys.append(("wdt", j))
            return wdt, keys

        def out_evac(fg, dc, half, ps, pk):
            hs = slice(half * 512, (half + 1) * 512)
            if fg == 0:
                P.op("dve", lambda e: e.tensor_copy(out=acc[:, dc, hs], in_=ps[:]), reads=[pk], writes=[("acc", dc, half)])
            else:
                P.op("dve", lambda e: e.tensor_tensor(out=acc[:, dc, hs], in0=ps[:], in1=acc[:, dc, hs], op=ALU.add),
                     reads=[pk, ("acc", dc, half)], writes=[("acc", dc, half)])

        emit_ffn(cx, bufs, hT, ["hT"], nft, get_gu, get_d, out_evac, "moe", rwb=rwb, rwkey="rwb")
        for dc in range(DC):
            s = dc % 2
            P.op("act", lambda e, s=s, dc=dc: e.activation(out=o16c[s][:], in_=acc[:, dc, :], func=AF.Identity),
                 reads=[("acc", dc, 0), ("acc", dc, 1)], writes=[("oc", s)])
            cx.store(po[ch, :, dc, :], o16c[s][:], [("oc", s)], ("po", ch, dc), sem=("st_oc", s))
    return cx.finish()


def build_final():
    cx = Ctx()
    P = cx.P
    xTd = cx.din("xT", [128, DC, T], F32)
    pd = cx.din("partials", [NEXP, 128, DC, T], BF16)
    gfd = cx.din("gatef", [128, DC], F32)
    gnd = cx.din("gfin", [128, DC], F32)
    out = cx.dout("outT", [128, DC, T], F32)
    cx.alloc_psum(4)
    xT = cx.sb("xT_sb", [128, DC, T], F32)
    gf = cx.sb("gf_sb", [128, DC], F32)
    gfin = cx.sb("gfin_sb", [128, DC], F32)
    zero = cx.sb("zero_sb", [128, DC], F32)
    ones16 = cx.sb("ones16", [128, 128], BF16)
    P.op("pool", lambda e: e.memset(ones16[:], 1.0), writes=["ones16"])
    P.op("pool", lambda e: e.memset(zero[:], 0.0), writes=["modv"])
    for k0 in range(0, DC, 4):
        P.dma("sp", lambda e, k0=k0: e.dma_start(out=xT[:, k0:k0 + 4, :], in_=xTd[:, k0:k0 + 4, :]), writes=["xT_in"], sem="xT")
    cx.load(gf[:], gfd, "gf")
    cx.load(gfin[:], gnd, "gfin")
    pb = [cx.sb("pb%d" % i, [128, NEXP, T], BF16) for i in range(2)]
    acc = [cx.sb("facc%d" % i, [128, T], F32) for i in range(2)]
    for kc in range(DC):
        s = kc % 2
        for e_ in range(NEXP):
            P.dma("sp", lambda e, s=s, e_=e_, kc=kc: e.dma_start(out=pb[s][:, e_, :], in_=pd[e_, :, kc, :]), writes=[("pb", s)], sem=("pb", s))
        P.op("dve", lambda e, s=s: e.tensor_tensor(out=acc[s][:], in0=pb[s][:, 0, :], in1=pb[s][:, 1, :], op=ALU.add), reads=[("pb", s)], writes=[("facc", s)])
        for e_ in range(2, NEXP):
            P.op("dve", lambda e, s=s, e_=e_: e.tensor_tensor(out=acc[s][:], in0=acc[s][:], in1=pb[s][:, e_, :], op=ALU.add),
                 reads=[("pb", s), ("facc", s)], writes=[("facc", s)])
        P.op("dve", lambda e, s=s, kc=kc: e.scalar_tensor_tensor(out=xT[:, kc, :], in0=acc[s][:], scalar=gf[:, kc:kc + 1], in1=xT[:, kc, :],
                                                              op0=ALU.mult, op1=ALU.add), reads=[("facc", s), "gf", "xT_in"], writes=["xT"])
    oT = cx.sb("oT_sb", [128, DC // 2, T], F32)
    P.op("dve", lambda e: e.tensor_copy(out=gfin[:], in_=gfin[:]), reads=["gfin"], writes=["modv"])
    rstd = emit_rmsnorm_stats(cx, xT, "xT", ones16, "nf")
    tmpf = [cx.sb("nf_t%d" % i, [128, T], F32) for i in range(2)]
    for kc in range(DC):
        s = kc % 2
        P.op("dve", lambda e, s=s, kc=kc: e.tensor_tensor(out=tmpf[s][:], in0=xT[:, kc, :], in1=rstd[:], op=ALU.mult),
             reads=["xT", "nf_rstd"], writes=[("nft", s)])
        P.op("act", lambda e, s=s, kc=kc: e.activation(out=tmpf[s][:], in_=tmpf[s][:], func=AF.Identity, scale=gfin[:, kc:kc + 1]),
             reads=[("nft", s), "modv"], writes=[("nft", s)])
        cx.store(out[:, kc, :], tmpf[s][:], [("nft", s)], ("out", kc), sem=("st_nft", s))
    return cx.finish()


def emit_rmsnorm_stats(cx, xT, xkey, ones16, tag):
    P = cx.P
    sq = [cx.sb("%s_sq%d" % (tag, i), [128, 512], BF16) for i in range(2)]
    rstd = cx.sb("%s_rstd" % tag, [128, T], F32)
    for half in range(T // 512):
        hs = slice(half * 512, (half + 1) * 512)
        ps, pk = cx.psum()
        for kc in range(DC):
            s = kc % 2
            P.op("act", lambda e, s=s, kc=kc, hs=hs: e.activation(out=sq[s][:], in_=xT[:, kc, hs], func=AF.Square),
                 reads=[xkey], writes=[(tag, "sq", s)])
            P.op("pe", lambda e, s=s, kc=kc, ps=ps: e.matmul(ps[:], lhsT=ones16[:], rhs=sq[s][:], start=(kc == 0), stop=(kc == DC - 1)),
                 reads=[(tag, "sq", s), "ones16"], writes=[pk], signal=True)
        rk = tag + "_rstd"
        P.op("dve", lambda e, ps=ps, hs=hs: e.tensor_scalar(out=rstd[:, hs], in0=ps[:], scalar1=1.0 / D, scalar2=EPS, op0=ALU.mult, op1=ALU.add),
             reads=[pk, rk], writes=[rk])
        P.op("act", lambda e, hs=hs: e.activation(out=rstd[:, hs], in_=rstd[:, hs], func=AF.Sqrt), reads=[rk], writes=[rk])
        P.op("dve", lambda e, hs=hs: e.reciprocal(out=rstd[:, hs], in_=rstd[:, hs]), reads=[rk], writes=[rk])
    return rstd


def run_ffn(xTs, h2s, mod_l, inp):
    nc = get_nc("ffn", build_ffn)
    wg = np.ascontiguousarray(inp["ffn_w_gate"][0]); wu = np.ascontiguousarray(inp["ffn_w_up"][0]); wd = np.ascontiguousarray(inp["ffn_w_down"][0])
    ins = []
    for i in range(NCORES):
        b = i // 4
        ins.append({"xT": xTs[i], "h2T": h2s[i], "gatef": np.ascontiguousarray(modv_layout(mod_l[b])[:, 5, :]),
                    "w_gate": wg, "w_up": wu, "w_down": wd})
    res = run(nc, ins)
    return [res[i]["xT_out"] for i in range(NCORES)]


def run_moe(h2s, rws, inp):
    nc = get_nc("moe", build_moe)
    h2all = np.ascontiguousarray(np.stack(h2s))
    rwall = np.stack(rws)
    ins = []
    for e in range(NCORES):
        rwb = np.ascontiguousarray(np.broadcast_to(rwall[:, None, :, e], (NCHUNK, 128, T)))
        ins.append({"h2T": h2all, "rwb": rwb, "w_gate": np.ascontiguousarray(inp["moe_w_gate"][0][e]),
                    "w_up": np.ascontiguousarray(inp["moe_w_up"][0][e]), "w_down": np.ascontiguousarray(inp["moe_w_down"][0][e])})
    res = run(nc, ins)
    return [res[e]["partial"] for e in range(NCORES)]


def run_final(xTs, partials, mod_l, gfin):
    nc = get_nc("final", build_final)
    ins = []
    for i in range(NCORES):
        b = i // 4
        ins.append({"xT": xTs[i], "partials": np.ascontiguousarray(np.stack([partials[e][i] for e in range(NEXP)])),
                    "gatef": np.ascontiguousarray(modv_layout(mod_l[b])[:, 5, :]), "gfin": vec_fm(gfin)})
    res = run(nc, ins)
    out = np.zeros((2, SEQ, D), np.float32)
    for i in range(NCORES):
        b, q = i // 4, i % 4
        out[b, q * T:(q + 1) * T, :] = res[i]["outT"].transpose(1, 0, 2).reshape(D, T).T
    return out


def kernel(**inputs):
    inp = {k: np.asarray(v) for k, v in inputs.items()}
    mod = run_mod(inp["c"], inp["w_mod"], inp["b_mod"])
    xTs = x_to_cores(inp["x"])
    out = None
    for l in range(2):
        projT = run_inproj(xTs, mod[l], inp["norm_mix_g"][l], np.ascontiguousarray(inp["w_in"][l]))
        ypreT = run_ssm(projT, l, inp)
        yattT = run_attn(projT, inp["rel_bias"])
        moe = (l % 2 == 1)
        xTs, h2s, rws = run_post(xTs, ypreT, yattT, projT, mod[l], l, inp, moe)
        if not moe:
            xTs = run_ffn(xTs, h2s, mod[l], inp)
        else:
            hcs, posms, overflow = run_route(h2s, rws)
            if not overflow:
                ys = run_moe3(hcs, inp)
                out = run_final3(xTs, ys, posms, rws, mod[l], inp["final_norm_g"])
            else:
                partials = run_moe(h2s, rws, inp)
                out = run_final(xTs, partials, mod[l], inp["final_norm_g"])
    return out


CAP = 4096
NTOK = 8192
NTT = NTOK // 128


def build_moe2():
    cx = Ctx()
    P = cx.P
    nc = cx.nc
    h2d = cx.din("h2tm", [NTOK, D], BF16)
    rwd = cx.din("rwc", [128, NTT], F32)
    Ld = cx.din("Ltri", [128, 128], F32)
    idd = cx.din("ident", [128, 128], F32)
    Wg = cx.din("w_gate", [D, DFFE], F32)
    Wu = cx.din("w_up", [D, DFFE], F32)
    Wd = cx.din("w_down", [DFFE, D], F32)
    po = cx.dout("partial", [NTOK, D], BF16)
    Wg16 = nc.dram_tensor("Wg16", [D, DFFE], BF16, kind="Internal").ap()
    Wu16 = nc.dram_tensor("Wu16", [D, DFFE], BF16, kind="Internal").ap()
    Wd16 = nc.dram_tensor("Wd16", [DFFE, D], BF16, kind="Internal").ap()
    Xc = nc.dram_tensor("Xc", [CAP, D], BF16, kind="Internal").ap()
    Yc = nc.dram_tensor("Yc", [CAP, D], F32, kind="Internal").ap()
    for i in range(6):
        cx.ps.append(cx.st.enter_context(nc.psum_tensor("ps%d" % i, [128, 512], F32)))
    psb = [cx.st.enter_context(nc.psum_tensor("psb%d" % i, [128, 1024], BF16)) for i in range(2)]
    nft = DFFE // 128
    rwc = cx.sb("rwc_sb", [128, NTT], F32)
    posi = cx.sb("posi", [128, NTT], I32)
    identb = cx.sb("identb", [128, 128], BF16)
    cx.load(rwc[:], rwd, "rwc")

    cx.phase_begin()
    stg = [cx.sb("cv_st%d" % i, [128, 4, 512], F32) for i in range(3)]
    o16 = [cx.sb("cv_o%d" % i, [128, 4, 512], BF16) for i in range(3)]
    Ltri = cx.sb("Ltri_sb", [128, 128], F32)
    identf = cx.sb("identf", [128, 128], F32)
    ones32 = cx.sb("ones32", [128, 128], F32)
    onesr = cx.sb("onesr", [128, NTT], F32)
    m = cx.sb("m_sb", [128, NTT], F32)
    S = cx.sb("S_sb", [128, NTT], F32)
    incl = cx.sb("incl", [128, NTT], F32)
    posf = cx.sb("posf", [128, NTT], F32)
    z16 = cx.sb("z16", [128, D], BF16)
    hrow = [cx.sb("hrow%d" % i, [128, D], BF16) for i in range(4)]
    cx.load(Ltri[:], Ld, "Ltri")
    cx.load(identf[:], idd, "identf")
    P.op("dve", lambda e: e.tensor_copy(out=identb[:], in_=identf[:]), reads=["identf"], writes=["identb"])
    P.op("pool", lambda e: e.memset(ones32[:], 1.0), writes=["ones32"])
    P.op("pool", lambda e: e.memset(onesr[:], 1.0), writes=["onesr"])
    P.op("pool", lambda e: e.memset(z16[:], 0.0), writes=["z16"])
    C = "cmp"
    P.op("dve", lambda e: e.tensor_scalar(out=m[:], in0=rwc[:], scalar1=0.0, scalar2=None, op0=ALU.is_gt), reads=["rwc"], writes=[C])
    ps, pk = cx.psum()
    P.op("pe", lambda e: e.matmul(ps[:, 0:NTT], lhsT=Ltri[:], rhs=m[:], start=True, stop=True), reads=[C, "Ltri"], writes=[pk])
    P.op("pe", lambda e: e.matmul(ps[:, NTT:2 * NTT], lhsT=ones32[:], rhs=m[:], start=True, stop=True), reads=[C, "ones32"], writes=[pk])
    P.op("dve", lambda e: e.tensor_copy(out=S[:], in_=ps[:, NTT:2 * NTT]), reads=[pk, C], writes=[C])
    P.op("dve", lambda e: e.tensor_tensor_scan(out=incl[:], data0=onesr[:], data1=S[:], initial=0.0, op0=ALU.mult, op1=ALU.add),
         reads=[C, "onesr"], writes=[C])
    P.op("dve", lambda e: e.tensor_tensor(out=incl[:], in0=incl[:], in1=S[:], op=ALU.subtract), reads=[C], writes=[C])
    P.op("dve", lambda e: e.tensor_tensor(out=posf[:], in0=ps[:, 0:NTT], in1=incl[:], op=ALU.add), reads=[pk, C], writes=[C])
    P.op("dve", lambda e: e.tensor_scalar(out=m[:], in0=m[:], scalar1=-1.0e6, scalar2=1.0e6, op0=ALU.mult, op1=ALU.add), reads=[C], writes=[C])
    P.op("dve", lambda e: e.tensor_tensor(out=posf[:], in0=posf[:], in1=m[:], op=ALU.add), reads=[C], writes=[C])
    P.op("dve", lambda e: e.tensor_copy(out=posi[:], in_=posf[:]), reads=[C], writes=["posi"])
    for r in range(CAP // 128):
        P.dma("sp", lambda e, r=r: e.dma_start(out=Xc[r * 128:(r + 1) * 128, :], in_=z16[:]), reads=["z16"], writes=[("Xcz", r)], sem="xcz")
    xcz = [("Xcz", r) for r in range(CAP // 128)]
    for j in range(NTT):
        s = j % 4
        cx.load(hrow[s][:], h2d[j * 128:(j + 1) * 128, :], ("hrow", s))
        P.dma("pool", lambda e, j=j, s=s: e.indirect_dma_start(
            out=Xc[:, :], out_offset=bass.IndirectOffsetOnAxis(ap=posi[:, j:j + 1], axis=0), in_=hrow[s][:, :], in_offset=None,
            bounds_check=CAP - 1, oob_is_err=False), reads=[("hrow", s), "posi"] + (xcz if j < 4 else []), writes=[("Xcs", j)], sem=("sc", s))
    xcs = [("Xcs", j) for j in range(NTT)]
    ci = 0
    for (Wsrc, Wdst, name, KC_, ncols) in ((Wg, Wg16, "Wg16", DC, DFFE), (Wu, Wu16, "Wu16", DC, DFFE), (Wd, Wd16, "Wd16", nft, D)):
        for kq in range(0, KC_, 4):
            for cb in range(ncols // 512):
                s = ci % 3
                ci += 1
                src = Wsrc[kq * 128:(kq + 4) * 128, cb * 512:(cb + 1) * 512].rearrange("(c p) n -> p c n", p=128)
                dst = Wdst[kq * 128:(kq + 4) * 128, cb * 512:(cb + 1) * 512].rearrange("(c p) n -> p c n", p=128)
                P.dma("sp", lambda e, s=s, src=src: e.dma_start(out=stg[s][:], in_=src), writes=[("cvst", s)])
                cx.copy(cx.conv_eng(), o16[s][:], stg[s][:], [("cvst", s)], [("cvo", s)])
                P.dma("act", lambda e, s=s, dst=dst: e.dma_start(out=dst, in_=o16[s][:]), reads=[("cvo", s)], writes=[(name, kq // 4, cb)],
                      sem=("cvout", s))
    cx.phase_end()

    cx.phase_begin()
    hT = cx.sb("hT_sb", [128, DC, T], BF16)
    accR = cx.sb("accR", [128, T // 128, D], F32)
    wgt = [cx.sb("wg%d" % i, [128, DC, 128], BF16) for i in range(2)]
    wut = [cx.sb("wu%d" % i, [128, DC, 128], BF16) for i in range(2)]
    wdt = cx.sb("wdt", [128, FG, D], BF16)
    xrow = [cx.sb("xrow%d" % i, [128, D], BF16) for i in range(2)]
    g16 = [cx.sb("g16_%d" % i, [128, FG, T], BF16) for i in range(2)]
    sgl = [cx.sb("sg%d" % i, [128, 512], F32) for i in range(2)]
    gi = 0
    it = 0
    ti = 0
    for ch in range(CAP // T):
        for rt in range(T // 128):
            s = (ch * 8 + rt) % 2
            row0 = ch * T + rt * 128
            cx.P.dma("sp", lambda e, s=s, row0=row0: e.dma_start(out=xrow[s][:], in_=Xc[row0:row0 + 128, :]),
                     reads=(xcs + xcz) if (ch == 0 and rt < 2) else [], writes=[("xrow", s)])
            for kq in range(DC // 4):
                pb = psb[ti % 2]
                pbk = ("psb", ti % 2)
                ti += 1
                for q in range(4):
                    kc = kq * 4 + q
                    P.op("pe", lambda e, pb=pb, q=q, kc=kc, s=s: e.transpose(pb[:, q * 128:(q + 1) * 128], xrow[s][:, kc * 128:(kc + 1) * 128], identb[:]),
                         reads=[("xrow", s), "identb"], writes=[pbk], signal=(q == 3))
                eng = "act" if (ti % 2 == 0) else "dve"
                dst = hT[:, kq * 4:(kq + 1) * 4, rt * 128:(rt + 1) * 128]
                srcv = pb[:, 0:512].rearrange("p (k n) -> p k n", k=4)
                cx.copy(eng, dst, srcv, [pbk], ["hT"])
        for fg in range(nft // FG):
            gb = g16[fg % 2]
            for j in range(FG):
                ft = fg * FG + j
                s = gi % 2
                gi += 1
                dep = [("Wg16", kq, ft // 4) for kq in range(4)] + [("Wu16", kq, ft // 4) for kq in range(4)]
                srcg = Wg16[:, ft * 128:(ft + 1) * 128].rearrange("(c p) n -> p c n", p=128)
                srcu = Wu16[:, ft * 128:(ft + 1) * 128].rearrange("(c p) n -> p c n", p=128)
                P.dma("sp", lambda e, s=s, srcg=srcg: e.dma_start(out=wgt[s][:], in_=srcg), reads=dep if ch == 0 else [], writes=[("wgt", s)])
                P.dma("sp", lambda e, s=s, srcu=srcu: e.dma_start(out=wut[s][:], in_=srcu), reads=dep if ch == 0 else [], writes=[("wut", s)])
                for half in range(T // 512):
                    hs = slice(half * 512, (half + 1) * 512)
                    psg, pkg = cx.psum()
                    mm_group(cx, psg[:], pkg, [(wgt[s][:, kc, :], hT[:, kc, hs]) for kc in range(DC)], [("wgt", s), "hT"])
                    psu, pku = cx.psum()
                    mm_group(cx, psu[:], pku, [(wut[s][:, kc, :], hT[:, kc, hs]) for kc in range(DC)], [("wut", s), "hT"])
                    s2 = it % 2
                    it += 1
                    P.op("act", lambda e, s2=s2, psg=psg: e.activation(out=sgl[s2][:], in_=psg[:], func=AF.Silu), reads=[pkg], writes=[("sg", s2)])
                    P.op("dve", lambda e, s2=s2, psu=psu, gb=gb, j=j, hs=hs: e.tensor_tensor(out=gb[:, j, hs], in0=sgl[s2][:], in1=psu[:], op=ALU.mult),
                         reads=[("sg", s2), pku], writes=[("g16", fg % 2, j)])
            for j in range(FG):
                ft = fg * FG + j
                dep = [("Wd16", ft // 4, cb) for cb in range(D // 512)]
                P.dma("sp", lambda e, j=j, ft=ft: e.dma_start(out=wdt[:, j, :], in_=Wd16[ft * 128:(ft + 1) * 128, :]),
                      reads=dep if ch == 0 else [], writes=[("wdt", j)])
            di = 0
            for rt in range(T // 128):
                for db in range(D // 512):
                    ps, pk = cx.psum()
                    mm_group(cx, ps[:], pk, [(gb[:, j, rt * 128:(rt + 1) * 128], wdt[:, j, db * 512:(db + 1) * 512]) for j in range(FG)],
                             [("wdt", j) for j in range(FG)] + [("g16", fg % 2, j) for j in range(FG)])
                    dsl = accR[:, rt, db * 512:(db + 1) * 512]
                    eng = "dve" if (di % 4 != 3) else "pool"
                    di += 1
                    if fg == 0:
                        P.op("dve", lambda e, dsl=dsl, ps=ps: e.tensor_copy(out=dsl, in_=ps[:]), reads=[pk], writes=[("accR", rt, db)])
                    else:
                        P.op("dve", lambda e, dsl=dsl, ps=ps: e.tensor_tensor(out=dsl, in0=ps[:], in1=dsl, op=ALU.add),
                             reads=[pk, ("accR", rt, db)], writes=[("accR", rt, db)])
        for rt in range(T // 128):
            row0 = ch * T + rt * 128
            P.dma("sp", lambda e, rt=rt, row0=row0: e.dma_start(out=Yc[row0:row0 + 128, :], in_=accR[:, rt, :]),
                  reads=[("accR", rt, db) for db in range(D // 512)], writes=[("Yc", ch, rt)], sem=("st_acc", rt % 2))
    ycs = [("Yc", ch, rt) for ch in range(CAP // T) for rt in range(T // 128)]
    cx.phase_end()

    cx.phase_begin()
    zt = [cx.sb("zt%d" % i, [128, D], F32) for i in range(3)]
    ot = [cx.sb("ot%d" % i, [128, D], BF16) for i in range(3)]
    for i in range(3):
        P.op("pool", lambda e, i=i: e.memset(zt[i][:], 0.0), writes=[("zt", i)])
    for j in range(NTT):
        s = j % 3
        P.dma("pool", lambda e, j=j, s=s: e.indirect_dma_start(
            out=zt[s][:, :], out_offset=None, in_=Yc[:, :], in_offset=bass.IndirectOffsetOnAxis(ap=posi[:, j:j + 1], axis=0),
            bounds_check=CAP - 1, oob_is_err=False), reads=["posi", ("zt", s)], writes=[("zt", s)], sem=("ga", s))
        if j % 2 == 0:
            P.op("dve", lambda e, j=j, s=s: e.tensor_scalar(out=ot[s][:], in0=zt[s][:], scalar1=rwc[:, j:j + 1], scalar2=None, op0=ALU.mult),
                 reads=[("zt", s), "rwc"], writes=[("ot", s)])
        else:
            P.op("act", lambda e, j=j, s=s: e.activation(out=ot[s][:], in_=zt[s][:], func=AF.Identity, scale=rwc[:, j:j + 1]),
                 reads=[("zt", s), "rwc"], writes=[("ot", s)])
        cx.store(po[j * 128:(j + 1) * 128, :], ot[s][:], [("ot", s)], ("po", j), sem=("st_ot", s))
    cx.phase_end()
    return cx.finish()


def build_final2():
    cx = Ctx()
    P = cx.P
    NT_ = T // 128
    xd = cx.din("x", [T, D], F32)
    pd = cx.din("partials", [NEXP, T, D], BF16)
    gfd = cx.din("gatef", [128, D], F32)
    gnd = cx.din("gfin", [128, D], F32)
    out = cx.dout("out", [T, D], F32)
    gf = cx.sb("gf_sb", [128, D], F32)
    gfin = cx.sb("gfin_sb", [128, D], F32)
    cx.load(gf[:], gfd, "gf")
    cx.load(gfin[:], gnd, "gfin")
    xt = [cx.sb("xt%d" % i, [128, D], F32) for i in range(2)]
    pb = [cx.sb("pb%d" % i, [128, NEXP, D], BF16) for i in range(2)]
    acc = [cx.sb("acc%d" % i, [128, D], F32) for i in range(2)]
    sqj = cx.sb("sqj", [128, D], F32)
    ss = [cx.sb("ss%d" % i, [128, 1], F32) for i in range(2)]
    for tt in range(NT_):
        s = tt % 2
        rows = slice(tt * 128, (tt + 1) * 128)
        cx.load(xt[s][:], xd[rows, :], ("xt", s))
        for e_ in range(NEXP):
            P.dma("sp", lambda e, s=s, e_=e_, rows=rows: e.dma_start(out=pb[s][:, e_, :], in_=pd[e_, rows, :]), writes=[("pb", s, e_)], sem=("pb", s))
        pbk = [("pb", s, e_) for e_ in range(NEXP)]
        P.op("dve", lambda e, s=s: e.tensor_tensor(out=acc[s][:], in0=pb[s][:, 0, :], in1=pb[s][:, 1, :], op=ALU.add), reads=pbk, writes=[("acc", s)])
        for e_ in range(2, NEXP):
            eng = "dve" if e_ % 2 == 0 else "pool"
            P.op(eng, lambda e, s=s, e_=e_: e.tensor_tensor(out=acc[s][:], in0=acc[s][:], in1=pb[s][:, e_, :], op=ALU.add),
                 reads=pbk + [("acc", s)], writes=[("acc", s)])
        P.op("dve", lambda e, s=s: e.tensor_tensor(out=acc[s][:], in0=acc[s][:], in1=gf[:], op=ALU.mult), reads=[("acc", s), "gf"], writes=[("acc", s)])
        P.op("dve", lambda e, s=s: e.tensor_tensor(out=xt[s][:], in0=xt[s][:], in1=acc[s][:], op=ALU.add), reads=[("acc", s), ("xt", s)], writes=[("xt", s)])
        P.op("act", lambda e, s=s: e.activation(out=sqj[:], in_=xt[s][:], func=AF.Square, accum_out=ss[s][:]), reads=[("xt", s)], writes=["sqj", ("ss", s)])
        P.op("dve", lambda e, s=s: e.tensor_scalar(out=ss[s][:], in0=ss[s][:], scalar1=1.0 / D, scalar2=EPS, op0=ALU.mult, op1=ALU.add),
             reads=[("ss", s)], writes=[("ss", s)])
        P.op("act", lambda e, s=s: e.activation(out=ss[s][:], in_=ss[s][:], func=AF.Sqrt), reads=[("ss", s)], writes=[("ss", s)])
        P.op("dve", lambda e, s=s: e.reciprocal(out=ss[s][:], in_=ss[s][:]), reads=[("ss", s)], writes=[("ss", s)])
        P.op("dve", lambda e, s=s: e.scalar_tensor_tensor(out=acc[s][:], in0=xt[s][:], scalar=ss[s][:, 0:1], in1=gfin[:], op0=ALU.mult, op1=ALU.mult),
             reads=[("xt", s), ("ss", s), "gfin", ("acc", s)], writes=[("acc", s)])
        cx.store(out[rows, :], acc[s][:], [("acc", s)], ("out", tt), sem=("st_acc", s))
    return cx.finish()


def run_moe2(h2s, rws, inp):
    nc = get_nc("moe2", build_moe2)
    h2tm = np.ascontiguousarray(np.concatenate([h.transpose(1, 0, 2).reshape(D, T).T for h in h2s], axis=0))
    rwall = np.concatenate(rws, axis=0)
    kk = np.arange(128)
    Ltri = (kk[:, None] < kk[None, :]).astype(np.float32)
    ins = []
    for e in range(NCORES):
        ins.append({"h2tm": h2tm, "rwc": np.ascontiguousarray(rwall[:, e].reshape(NTT, 128).T), "Ltri": Ltri,
                    "ident": np.eye(128, dtype=np.float32),
                    "w_gate": np.ascontiguousarray(inp["moe_w_gate"][0][e]), "w_up": np.ascontiguousarray(inp["moe_w_up"][0][e]),
                    "w_down": np.ascontiguousarray(inp["moe_w_down"][0][e])})
    res = run(nc, ins)
    return [res[e]["partial"] for e in range(NCORES)]


def run_final2(xTs, partials, mod_l, gfin):
    nc = get_nc("final2", build_final2)
    ins = []
    for i in range(NCORES):
        b = i // 4
        gatef = mod_l[b].reshape(6, D)[5]
        ins.append({"x": np.ascontiguousarray(xTs[i].transpose(1, 0, 2).reshape(D, T).T),
                    "partials": np.ascontiguousarray(np.stack([partials[e][i * T:(i + 1) * T] for e in range(NEXP)])),
                    "gatef": np.ascontiguousarray(np.broadcast_to(gatef[None, :], (128, D))),
                    "gfin": np.ascontiguousarray(np.broadcast_to(gfin[None, :], (128, D)))})
    res = run(nc, ins)
    out = np.zeros((2, SEQ, D), np.float32)
    for i in range(NCORES):
        b, q = i // 4, i % 4
        out[b, q * T:(q + 1) * T, :] = res[i]["out"]
    return out


SEG = 512
NR = SEG // 128


def build_route():
    cx = Ctx()
    P = cx.P
    nc = cx.nc
    NT_ = T // 128
    hTd = cx.din("h2T", [128, DC, T], BF16)
    rwd = cx.din("rw", [128, NT_, NEXP], F32)
    Ld = cx.din("Ltri", [128, 128], F32)
    idd = cx.din("ident", [128, 128], F32)
    iod = cx.din("iota", [128, 128], F32)
    hco = cx.dout("hc", [NEXP, 128, DC, SEG], BF16)
    pso = cx.dout("posm", [128, NT_, NEXP], F32)
    ovo = cx.dout("ovf", [128, 1], F32)
    for i in range(6):
        cx.ps.append(cx.st.enter_context(nc.psum_tensor("ps%d" % i, [128, 512], F32)))
    psb = [cx.st.enter_context(nc.psum_tensor("psb%d" % i, [128, 1024], BF16)) for i in range(2)]
    hT = cx.sb("hT_sb", [128, DC, T], BF16)
    htm = cx.sb("htm", [128, NT_, D], BF16)
    ovf = cx.sb("ovf_sb", [128, 1], F32)
    rw = cx.sb("rw_sb", [128, NT_, NEXP], F32)
    Ltri = cx.sb("Ltri_sb", [128, 128], F32)
    identf = cx.sb("identf", [128, 128], F32)
    identb = cx.sb("identb", [128, 128], BF16)
    iota = cx.sb("iota_sb", [128, 128], F32)
    ones32 = cx.sb("ones32", [128, 128], F32)
    for k0 in range(0, DC, 4):
        P.dma("sp", lambda e, k0=k0: e.dma_start(out=hT[:, k0:k0 + 4, :], in_=hTd[:, k0:k0 + 4, :]), writes=["hT"], sem="hT")
    cx.load(rw[:], rwd, "rw")
    cx.load(Ltri[:], Ld, "Ltri")
    cx.load(identf[:], idd, "identf")
    cx.load(iota[:], iod, "iota")
    P.op("dve", lambda e: e.tensor_copy(out=identb[:], in_=identf[:]), reads=["identf"], writes=["identb"])
    P.op("pool", lambda e: e.memset(ones32[:], 1.0), writes=["ones32"])
    ti = 0
    for tt in range(NT_):
        for kq in range(DC // 4):
            pb = psb[ti % 2]
            pbk = ("psb", ti % 2)
            ti += 1
            for q in range(4):
                kc = kq * 4 + q
                P.op("pe", lambda e, pb=pb, q=q, kc=kc, tt=tt: e.transpose(pb[:, q * 128:(q + 1) * 128], hT[:, kc, tt * 128:(tt + 1) * 128], identb[:]),
                     reads=["hT", "identb"], writes=[pbk], signal=(q == 3))
            cx.copy("act" if ti % 2 else "dve", htm[:, tt, kq * 512:(kq + 1) * 512], pb[:, 0:512], [pbk], [("htm", tt)])
    C = "cmp"
    NF = NT_ * NEXP
    m = cx.sb("m_sb", [128, NT_, NEXP], F32)
    S = cx.sb("S_sb", [128, NT_, NEXP], F32)
    offs = cx.sb("offs", [128, NT_, NEXP], F32)
    pos = cx.sb("pos", [128, NT_, NEXP], F32)
    posr = cx.sb("posr", [128, NR, NT_, NEXP], F32)
    mf = m[:].rearrange("p t e -> p (t e)")
    P.op("dve", lambda e: e.tensor_scalar(out=m[:], in0=rw[:], scalar1=0.0, scalar2=None, op0=ALU.is_gt), reads=["rw"], writes=[C])
    ps, pk = cx.psum()
    P.op("pe", lambda e: e.matmul(ps[:, 0:NF], lhsT=Ltri[:], rhs=mf, start=True, stop=True), reads=[C, "Ltri"], writes=[pk])
    P.op("pe", lambda e: e.matmul(ps[:, NF:2 * NF], lhsT=ones32[:], rhs=mf, start=True, stop=True), reads=[C, "ones32"], writes=[pk])
    P.op("dve", lambda e: e.tensor_copy(out=S[:].rearrange("p t e -> p (t e)"), in_=ps[:, NF:2 * NF]), reads=[pk, C], writes=[C])
    P.op("pool", lambda e: e.memset(offs[:, 0, :], 0.0), reads=[C], writes=[C])
    for tt in range(1, NT_):
        P.op("dve", lambda e, tt=tt: e.tensor_tensor(out=offs[:, tt, :], in0=offs[:, tt - 1, :], in1=S[:, tt - 1, :], op=ALU.add), reads=[C], writes=[C])
    P.op("dve", lambda e: e.tensor_tensor(out=pos[:].rearrange("p t e -> p (t e)"), in0=ps[:, 0:NF], in1=offs[:].rearrange("p t e -> p (t e)"), op=ALU.add),
         reads=[pk, C], writes=[C])
    P.op("dve", lambda e: e.tensor_scalar(out=pos[:], in0=pos[:], scalar1=1.0e6, scalar2=None, op0=ALU.add), reads=[C], writes=[C])
    P.op("dve", lambda e: e.tensor_tensor(out=pos[:], in0=pos[:], in1=m[:], op=ALU.mult), reads=[C], writes=[C])
    P.op("dve", lambda e: e.tensor_scalar(out=pos[:], in0=pos[:], scalar1=-1.0e6, scalar2=None, op0=ALU.add), reads=[C], writes=[C])
    cx.store(pso, pos[:], [C], "pso")
    P.op("dve", lambda e: e.tensor_reduce(out=ovf[:], in_=pos[:].rearrange("p t e -> p (t e)"), axis=mybir.AxisListType.X, op=ALU.max), reads=[C], writes=["ovf"])
    P.op("dve", lambda e: e.tensor_scalar(out=ovf[:], in0=ovf[:], scalar1=float(SEG), scalar2=None, op0=ALU.is_ge), reads=["ovf"], writes=["ovf"])
    cx.store(ovo, ovf[:], ["ovf"], "ovo")
    for r in range(NR):
        P.op("dve", lambda e, r=r: e.tensor_scalar(out=posr[:, r, :, :], in0=pos[:], scalar1=-128.0 * r, scalar2=None, op0=ALU.add), reads=[C], writes=[C])
    Pm = [cx.sb("Pm%d" % i, [128, NT_, NR, 128], BF16) for i in range(2)]
    hcb = [cx.sb("hcb%d" % i, [128, DC, SEG], BF16) for i in range(2)]
    htk = [("htm", tt) for tt in range(NT_)]
    for ex in range(NEXP):
        s = ex % 2
        for tt in range(NT_):
            for r in range(min(tt, NR - 1) + 1):
                eng = "dve" if (tt + r) % 2 == 0 else "pool"
                P.op(eng, lambda e, s=s, tt=tt, r=r, ex=ex: e.tensor_scalar(out=Pm[s][:, tt, r, :], in0=iota[:], scalar1=posr[:, r, tt, ex:ex + 1],
                                                                        scalar2=None, op0=ALU.is_equal), reads=[C, "iota"], writes=[("Pm", s)])
        for r in range(NR):
            for kq in range(DC // 4):
                ps, pk = cx.psum()
                for q in range(4):
                    for tt in range(r, NT_):
                        kc = kq * 4 + q
                        P.op("pe", lambda e, ps=ps, q=q, kc=kc, tt=tt, r=r, s=s: e.matmul(
                            ps[:, q * 128:(q + 1) * 128], lhsT=htm[:, tt, kc * 128:(kc + 1) * 128], rhs=Pm[s][:, tt, r, :],
                            start=(tt == r), stop=(tt == NT_ - 1)), reads=htk + [("Pm", s)], writes=[pk], signal=(tt == NT_ - 1 and q == 3))
                cx.copy("act" if (kq % 2) else "dve", hcb[s][:, kq * 4:(kq + 1) * 4, r * 128:(r + 1) * 128],
                        ps[:].rearrange("p (k n) -> p k n", k=4), [pk], [("hcb", s)])
        cx.store(hco[ex].rearrange("p k n -> p (k n)"), hcb[s][:].rearrange("p k n -> p (k n)"), [("hcb", s)], ("hco", ex), sem=("st_hcb", s))
    return cx.finish()


NCH3 = CAP // T


def build_moe3():
    cx = Ctx()
    P = cx.P
    nc = cx.nc
    hcd = cx.din("hc", [NCH3, 128, DC, T], BF16)
    Wg = cx.din("w_gate", [D, DFFE], F32)
    Wu = cx.din("w_up", [D, DFFE], F32)
    Wd = cx.din("w_down", [DFFE, D], F32)
    yo = cx.dout("y", [CAP, D], BF16)
    Wg16 = nc.dram_tensor("Wg16", [D, DFFE], BF16, kind="Internal").ap()
    Wu16 = nc.dram_tensor("Wu16", [D, DFFE], BF16, kind="Internal").ap()
    Wd16 = nc.dram_tensor("Wd16", [DFFE, D], BF16, kind="Internal").ap()
    cx.alloc_psum(8)
    nft = DFFE // 128
    cx.phase_begin()
    stg = [cx.sb("cv_st%d" % i, [128, 4, 512], F32) for i in range(3)]
    o16 = [cx.sb("cv_o%d" % i, [128, 4, 512], BF16) for i in range(3)]
    ci = 0
    for (Wsrc, Wdst, name, KC_, ncols) in ((Wg, Wg16, "Wg16", DC, DFFE), (Wu, Wu16, "Wu16", DC, DFFE), (Wd, Wd16, "Wd16", nft, D)):
        for kq in range(0, KC_, 4):
            for cb in range(ncols // 512):
                s = ci % 3
                ci += 1
                src = Wsrc[kq * 128:(kq + 4) * 128, cb * 512:(cb + 1) * 512].rearrange("(c p) n -> p c n", p=128)
                dst = Wdst[kq * 128:(kq + 4) * 128, cb * 512:(cb + 1) * 512].rearrange("(c p) n -> p c n", p=128)
                P.dma("sp", lambda e, s=s, src=src: e.dma_start(out=stg[s][:], in_=src), writes=[("cvst", s)])
                cx.copy(cx.conv_eng(), o16[s][:], stg[s][:], [("cvst", s)], [("cvo", s)])
                P.dma("pool", lambda e, s=s, dst=dst: e.dma_start(out=dst, in_=o16[s][:]), reads=[("cvo", s)], writes=[(name, kq // 4, cb)],
                      sem=("cvout", s))
    cx.phase_end()
    cx.phase_begin()
    hT = cx.sb("hT_sb", [128, DC, T], BF16)
    accR = cx.sb("accR", [128, T // 128, D], F32)
    wgt = [cx.sb("wg%d" % i, [128, DC, 128], BF16) for i in range(2)]
    wut = [cx.sb("wu%d" % i, [128, DC, 128], BF16) for i in range(2)]
    wdt = cx.sb("wdt", [128, FG, D], BF16)
    g16 = [cx.sb("g16_%d" % i, [128, FG, T], BF16) for i in range(2)]
    sgl = [cx.sb("sg%d" % i, [128, 512], F32) for i in range(2)]
    orow = [cx.sb("orow%d" % i, [128, D], BF16) for i in range(2)]
    gi = 0
    it = 0
    for ch in range(NCH3):
        for k0 in range(0, DC, 4):
            P.dma("sp", lambda e, k0=k0, ch=ch: e.dma_start(out=hT[:, k0:k0 + 4, :], in_=hcd[ch, :, k0:k0 + 4, :]), writes=["hT"], sem="hT")
        for fg in range(nft // FG):
            gb = g16[fg % 2]
            for j in range(FG):
                ft = fg * FG + j
                s = gi % 2
                gi += 1
                dep = [("Wg16", kq, ft // 4) for kq in range(4)] + [("Wu16", kq, ft // 4) for kq in range(4)]
                srcg = Wg16[:, ft * 128:(ft + 1) * 128].rearrange("(c p) n -> p c n", p=128)
                srcu = Wu16[:, ft * 128:(ft + 1) * 128].rearrange("(c p) n -> p c n", p=128)
                P.dma("sp", lambda e, s=s, srcg=srcg: e.dma_start(out=wgt[s][:], in_=srcg), reads=dep if ch == 0 else [], writes=[("wgt", s)])
                P.dma("sp", lambda e, s=s, srcu=srcu: e.dma_start(out=wut[s][:], in_=srcu), reads=dep if ch == 0 else [], writes=[("wut", s)])
                for half in range(T // 512):
                    hs = slice(half * 512, (half + 1) * 512)
                    psg, pkg = cx.psum()
                    mm_group(cx, psg[:], pkg, [(wgt[s][:, kc, :], hT[:, kc, hs]) for kc in range(DC)], [("wgt", s), "hT"])
                    psu, pku = cx.psum()
                    mm_group(cx, psu[:], pku, [(wut[s][:, kc, :], hT[:, kc, hs]) for kc in range(DC)], [("wut", s), "hT"])
                    s2 = it % 2
                    it += 1
                    P.op("act", lambda e, s2=s2, psg=psg: e.activation(out=sgl[s2][:], in_=psg[:], func=AF.Silu), reads=[pkg], writes=[("sg", s2)])
                    P.op("dve", lambda e, s2=s2, psu=psu, gb=gb, j=j, hs=hs: e.tensor_tensor(out=gb[:, j, hs], in0=sgl[s2][:], in1=psu[:], op=ALU.mult),
                         reads=[("sg", s2), pku], writes=[("g16", fg % 2, j)])
            for j in range(FG):
                ft = fg * FG + j
                dep = [("Wd16", ft // 4, cb) for cb in range(D // 512)]
                P.dma("sp", lambda e, j=j, ft=ft: e.dma_start(out=wdt[:, j, :], in_=Wd16[ft * 128:(ft + 1) * 128, :]),
                      reads=dep if ch == 0 else [], writes=[("wdt", j)])
            for rt in range(T // 128):
                for db in range(D // 512):
                    ps, pk = cx.psum()
                    mm_group(cx, ps[:], pk, [(gb[:, j, rt * 128:(rt + 1) * 128], wdt[:, j, db * 512:(db + 1) * 512]) for j in range(FG)],
                             [("wdt", j) for j in range(FG)] + [("g16", fg % 2, j) for j in range(FG)])
                    dsl = accR[:, rt, db * 512:(db + 1) * 512]
                    if fg == 0:
                        P.op("dve", lambda e, dsl=dsl, ps=ps: e.tensor_copy(out=dsl, in_=ps[:]), reads=[pk], writes=[("accR", rt, db)])
                    else:
                        P.op("dve", lambda e, dsl=dsl, ps=ps: e.tensor_tensor(out=dsl, in0=ps[:], in1=dsl, op=ALU.add),
                             reads=[pk, ("accR", rt, db)], writes=[("accR", rt, db)])
        for rt in range(T // 128):
            s = rt % 2
            row0 = ch * T + rt * 128
            P.op("act", lambda e, s=s, rt=rt: e.activation(out=orow[s][:], in_=accR[:, rt, :], func=AF.Identity),
                 reads=[("accR", rt, db) for db in range(D // 512)], writes=[("orow", s)])
            cx.store(yo[row0:row0 + 128, :], orow[s][:], [("orow", s)], ("yo", ch, rt), sem=("st_orow", s))
    cx.phase_end()
    return cx.finish()


def build_final3():
    cx = Ctx()
    P = cx.P
    NT_ = T // 128
    xd = cx.din("x", [T, D], F32)
    yd = cx.din("yseg", [NEXP, SEG, D], BF16)
    pbd = cx.din("posb", [128, NEXP, T], F32)
    rwd = cx.din("rwt", [128, NT_, NEXP], F32)
    sld = cx.din("slotidx", [128, NR], F32)
    gfd = cx.din("gatef", [128, D], F32)
    gnd = cx.din("gfin", [128, D], F32)
    out = cx.dout("out", [T, D], F32)
    cx.alloc_psum(8)
    acc = cx.sb("acc", [128, NT_, D], F32)
    posb = cx.sb("posb_sb", [128, NEXP, T], F32)
    rwt = cx.sb("rwt_sb", [128, NT_, NEXP], F32)
    slot = cx.sb("slot_sb", [128, NR], F32)
    gf = cx.sb("gf_sb", [128, D], F32)
    gfin = cx.sb("gfin_sb", [128, D], F32)
    cx.load(posb[:].rearrange("p e t -> p (e t)"), pbd.rearrange("p e t -> p (e t)"), "posb")
    cx.load(rwt[:], rwd, "rwt")
    cx.load(slot[:], sld, "slot")
    cx.load(gf[:], gfd, "gf")
    cx.load(gfin[:], gnd, "gfin")
    ye = [cx.sb("ye%d" % i, [128, NR, D], BF16) for i in range(2)]
    PwT = [cx.sb("PwT%d" % i, [128, NR, T], BF16) for i in range(2)]
    for ex in range(NEXP):
        s = ex % 2
        for r in range(NR):
            P.dma("sp", lambda e, s=s, r=r, ex=ex: e.dma_start(out=ye[s][:, r, :], in_=yd[ex, r * 128:(r + 1) * 128, :]), writes=[("ye", s, r)], sem=("ye", s, r))
            P.op("dve" if r % 2 == 0 else "pool", lambda e, s=s, r=r, ex=ex: e.tensor_scalar(
                out=PwT[s][:, r, :], in0=posb[:, ex, :], scalar1=slot[:, r:r + 1], scalar2=None, op0=ALU.is_equal),
                reads=["posb", "slot"], writes=[("PwT", s, r)])
        for tt in range(NT_):
            nr = min(tt, NR - 1) + 1
            for db in range(D // 512):
                ps, pk = cx.psum()
                mm_group(cx, ps[:], pk, [(PwT[s][:, r, tt * 128:(tt + 1) * 128], ye[s][:, r, db * 512:(db + 1) * 512]) for r in range(nr)],
                         [("PwT", s, r) for r in range(nr)] + [("ye", s, r) for r in range(nr)])
                dsl = acc[:, tt, db * 512:(db + 1) * 512]
                if ex == 0:
                    P.op("dve", lambda e, dsl=dsl, ps=ps, tt=tt, ex=ex: e.tensor_scalar(out=dsl, in0=ps[:], scalar1=rwt[:, tt, ex:ex + 1], scalar2=None, op0=ALU.mult),
                         reads=[pk, "rwt"], writes=[("acc", tt, db)])
                else:
                    P.op("dve", lambda e, dsl=dsl, ps=ps, tt=tt, ex=ex: e.scalar_tensor_tensor(out=dsl, in0=ps[:], scalar=rwt[:, tt, ex:ex + 1], in1=dsl,
                                                                                     op0=ALU.mult, op1=ALU.add),
                         reads=[pk, "rwt", ("acc", tt, db)], writes=[("acc", tt, db)])
    xt = [cx.sb("xt%d" % i, [128, D], F32) for i in range(2)]
    sqj = cx.sb("sqj", [128, D], F32)
    ss = [cx.sb("ss%d" % i, [128, 1], F32) for i in range(2)]
    for tt in range(NT_):
        s = tt % 2
        rows = slice(tt * 128, (tt + 1) * 128)
        ak = [("acc", tt, db) for db in range(D // 512)]
        cx.load(xt[s][:], xd[rows, :], ("xt", s))
        P.op("pool", lambda e, tt=tt: e.tensor_tensor(out=acc[:, tt, :], in0=acc[:, tt, :], in1=gf[:], op=ALU.mult), reads=ak + ["gf"], writes=ak)
        P.op("dve", lambda e, s=s, tt=tt: e.tensor_tensor(out=xt[s][:], in0=xt[s][:], in1=acc[:, tt, :], op=ALU.add), reads=ak + [("xt", s)], writes=[("xt", s)])
        P.op("act", lambda e, s=s: e.activation(out=sqj[:], in_=xt[s][:], func=AF.Square, accum_out=ss[s][:]), reads=[("xt", s)], writes=["sqj", ("ss", s)])
        P.op("dve", lambda e, s=s: e.tensor_scalar(out=ss[s][:], in0=ss[s][:], scalar1=1.0 / D, scalar2=EPS, op0=ALU.mult, op1=ALU.add),
             reads=[("ss", s)], writes=[("ss", s)])
        P.op("act", lambda e, s=s: e.activation(out=ss[s][:], in_=ss[s][:], func=AF.Sqrt), reads=[("ss", s)], writes=[("ss", s)])
        P.op("dve", lambda e, s=s: e.reciprocal(out=ss[s][:], in_=ss[s][:]), reads=[("ss", s)], writes=[("ss", s)])
        P.op("dve", lambda e, s=s, tt=tt: e.scalar_tensor_tensor(out=acc[:, tt, :], in0=xt[s][:], scalar=ss[s][:, 0:1], in1=gfin[:], op0=ALU.mult, op1=ALU.mult),
             reads=[("xt", s), ("ss", s), "gfin"] + ak, writes=ak)
        cx.store(out[rows, :], acc[:, tt, :], ak, ("out", tt), sem=("st_out", s))
    return cx.finish()


def route_consts():
    kk = np.arange(128)
    return {"Ltri": (kk[:, None] < kk[None, :]).astype(np.float32), "ident": np.eye(128, dtype=np.float32),
            "iota": np.ascontiguousarray(np.broadcast_to(kk[None, :].astype(np.float32), (128, 128)))}


def rw_layout(rw):
    return np.ascontiguousarray(rw.reshape(T // 128, 128, NEXP).transpose(1, 0, 2))


def run_route(h2s, rws):
    nc = get_nc("route", build_route)
    cst = route_consts()
    ins = [dict(h2T=h2s[i], rw=rw_layout(rws[i]), **cst) for i in range(NCORES)]
    res = run(nc, ins)
    overflow = any(bool(np.any(res[i]["ovf"] > 0.5)) for i in range(NCORES))
    return [res[i]["hc"] for i in range(NCORES)], [res[i]["posm"] for i in range(NCORES)], overflow


def run_moe3(hcs, inp):
    nc = get_nc("moe3", build_moe3)
    ins = []
    for e in range(NCORES):
        hcat = np.concatenate([hcs[i][e] for i in range(NCORES)], axis=2)
        hc = np.ascontiguousarray(hcat.reshape(128, DC, NCH3, T).transpose(2, 0, 1, 3))
        ins.append({"hc": hc, "w_gate": np.ascontiguousarray(inp["moe_w_gate"][0][e]), "w_up": np.ascontiguousarray(inp["moe_w_up"][0][e]),
                    "w_down": np.ascontiguousarray(inp["moe_w_down"][0][e])})
    res = run(nc, ins)
    return [res[e]["y"] for e in range(NCORES)]


def run_final3(xTs, ys, posms, rws, mod_l, gfin):
    nc = get_nc("final3", build_final3)
    ins = []
    slotidx = (np.arange(128)[:, None] + 128 * np.arange(NR)[None, :]).astype(np.float32)
    for i in range(NCORES):
        b = i // 4
        gatef = mod_l[b].reshape(6, D)[5]
        posm = posms[i]
        pos_et = posm.transpose(2, 1, 0).reshape(NEXP, T)
        ins.append({"x": np.ascontiguousarray(xTs[i].transpose(1, 0, 2).reshape(D, T).T),
                    "yseg": np.ascontiguousarray(np.stack([ys[e][i * SEG:(i + 1) * SEG] for e in range(NEXP)])),
                    "posb": np.ascontiguousarray(np.broadcast_to(pos_et[None], (128, NEXP, T))),
                    "rwt": rw_layout(rws[i]), "slotidx": slotidx,
                    "gatef": np.ascontiguousarray(np.broadcast_to(gatef[None, :], (128, D))),
                    "gfin": np.ascontiguousarray(np.broadcast_to(gfin[None, :], (128, D)))})
    res = run(nc, ins)
    out = np.zeros((2, SEQ, D), np.float32)
    for i in range(NCORES):
        b, q = i // 4, i % 4
        out[b, q * T:(q + 1) * T, :] = res[i]["out"]
    return out
```

```python
import contextlib
import numpy as np
import ml_dtypes
import concourse.bass as bass
import concourse.mybir as mybir
from concourse.bass_utils import run_bass_kernel_spmd

F32 = mybir.dt.float32
BF16 = mybir.dt.bfloat16
I32 = mybir.dt.int32
ALU = mybir.AluOpType
AF = mybir.ActivationFunctionType
NPBF = ml_dtypes.bfloat16

ENGS = ("pe", "dve", "act", "pool", "sp")

D = 2048
DC = 16
NCORES = 8
T = 1024
SEQ = 4096
EPS = 1e-6
IN_W = 9728
DFF = 5632
DFFE = 7168
NEXP = 8


class Prog:
    def __init__(self, nc):
        self.nc = nc
        self.ops = {e: [] for e in ENGS}
        self.last_w = {}
        self.readers = {}
        self.ndma = {}

    def _deps(self, reads, writes):
        deps = []
        for k in reads:
            t = self.last_w.get(k)
            if t is not None:
                deps.append(t)
        for k in writes:
            t = self.last_w.get(k)
            if t is not None:
                deps.append(t)
            deps.extend(self.readers.get(k, ()))
        return deps

    def _commit(self, tok, reads, writes):
        for k in reads:
            self.readers.setdefault(k, []).append(tok)
        for k in writes:
            self.last_w[k] = tok
            self.readers[k] = []

    def op(self, eng, fn, reads=(), writes=(), signal=True):
        deps = self._deps(reads, writes)
        tok = ("c", eng, len(self.ops[eng]))
        self.ops[eng].append(dict(kind="c", fn=fn, deps=deps, signal=signal or eng != "pe"))
        self._commit(tok, reads, writes)
        return tok

    def dma(self, q, fn, reads=(), writes=(), inc=16, sem=None):
        if sem is None:
            sem = writes[0]
        deps = self._deps(reads, writes)
        self.ndma[sem] = self.ndma.get(sem, 0) + inc
        tok = ("d", sem, self.ndma[sem])
        self.ops[q].append(dict(kind="d", fn=fn, deps=deps, inc=inc, sem=sem))
        self._commit(tok, reads, writes)
        return tok

    def wait_all_on(self, eng, toks):
        self.ops[eng].append(dict(kind="w", deps=list(toks)))

    def barrier(self):
        toks = []
        for e in ENGS:
            for i in range(len(self.ops[e]) - 1, -1, -1):
                o = self.ops[e][i]
                if o["kind"] == "c" and o["signal"]:
                    toks.append(("c", e, i))
                    break
        for k, v in self.ndma.items():
            toks.append(("d", k, v))
        for e in ENGS:
            self.ops[e].append(dict(kind="w", deps=list(toks)))

    def emit(self, st):
        nc = self.nc
        tick_at = {}
        for e in ENGS:
            n = 0
            ticks = []
            for o in self.ops[e]:
                if o["kind"] == "c" and o["signal"]:
                    n += 1
                ticks.append(n)
            res = [None] * len(ticks)
            nxt = None
            for i in range(len(ticks) - 1, -1, -1):
                o = self.ops[e][i]
                if o["kind"] == "c" and o["signal"]:
                    nxt = ticks[i]
                res[i] = nxt
            tick_at[e] = res
        csem = {e: st.enter_context(nc.semaphore("c_" + e)) for e in ENGS if e != "sp"}
        dkeys = []
        dset = set()
        for e in ENGS:
            for o in self.ops[e]:
                if o["kind"] == "d" and o["sem"] not in dset:
                    dset.add(o["sem"])
                    dkeys.append(o["sem"])
        dsem = {k: st.enter_context(nc.semaphore("d%d" % i)) for i, k in enumerate(dkeys)}
        block = st.enter_context(nc.Block())

        def make(e):
            def body(eng):
                seen = {}
                for o in self.ops[e]:
                    for t in o["deps"]:
                        if t[0] == "c":
                            if t[1] == e and e == "pe":
                                continue
                            v = tick_at[t[1]][t[2]]
                            assert v is not None, ("unsignaled dep", t)
                            key = ("c", t[1])
                            sem = csem[t[1]]
                        else:
                            v = t[2]
                            key = ("d", t[1])
                            sem = dsem[t[1]]
                        if seen.get(key, 0) >= v:
                            continue
                        seen[key] = v
                        eng.wait_ge(sem, v)
                    if o["kind"] == "c":
                        ins = o["fn"](eng)
                        if o["signal"]:
                            ins.then_inc(csem[e], 1)
                    elif o["kind"] == "d":
                        ins = o["fn"](eng)
                        ins.then_inc(dsem[o["sem"]], o["inc"])
            return body

        for e in ENGS:
            if not self.ops[e]:
                continue
            dec = {"pe": block.tensor, "dve": block.vector, "act": block.scalar,
                   "pool": block.gpsimd, "sp": block.sync}[e]
            dec(make(e))


class Ctx:
    def __init__(self):
        self.nc = bass.Bass("TRN2", target_bir_lowering=False)
        self.st = contextlib.ExitStack()
        self.P = Prog(self.nc)
        self.ps = []
        self.ps_i = 0
        self.out_toks = []
        self.cv_i = 0
        self._n = 0
        self.phase_st = None
        self.arena = None
        self.arena_off = 0

    def din(self, name, shape, dt):
        return self.nc.dram_tensor(name, list(shape), dt, kind="ExternalInput").ap()

    def dout(self, name, shape, dt):
        return self.nc.dram_tensor(name, list(shape), dt, kind="ExternalOutput").ap()

    ARENA_BYTES = 160 * 1024

    def sb(self, name, shape, dt):
        if self.phase_st is None:
            return self.st.enter_context(self.nc.sbuf_tensor(name, list(shape), dt))
        esz = mybir.dt.size(dt)
        n = int(np.prod(shape[1:]))
        nbytes = (n * esz + 63) // 64 * 64
        off = self.arena_off
        assert off + nbytes <= self.ARENA_BYTES, ("arena overflow", name, off, nbytes)
        self.arena_off += nbytes
        v = self.arena[0:shape[0], off // 2:(off + n * esz) // 2]
        if dt != BF16:
            v = v.bitcast(dt)
        if len(shape) > 2:
            names = " ".join("d%d" % i for i in range(1, len(shape)))
            v = v.rearrange("p (%s) -> p %s" % (names, names), **{"d%d" % i: shape[i] for i in range(1, len(shape))})
        return v

    def phase_begin(self):
        if self.arena is None:
            self.arena = self.st.enter_context(self.nc.sbuf_tensor("arena", [128, self.ARENA_BYTES // 2], BF16))
        self.phase_st = True
        self.arena_off = 0

    def phase_end(self):
        self.P.barrier()
        self.phase_st = None

    def alloc_psum(self, n=8):
        for i in range(n):
            self.ps.append(self.st.enter_context(self.nc.psum_tensor("ps%d" % i, [128, 512], F32)))

    def psum(self):
        i = self.ps_i % len(self.ps)
        self.ps_i += 1
        return self.ps[i], ("ps", i)

    def load(self, dst_ap, src_ap, key, q="sp"):
        return self.P.dma(q, lambda e: e.dma_start(out=dst_ap, in_=src_ap), writes=[key])

    def store(self, dst_ap, src_ap, rkeys, wkey, q="sp", sem=None):
        t = self.P.dma(q, lambda e: e.dma_start(out=dst_ap, in_=src_ap), reads=rkeys, writes=[wkey],
                       sem=sem if sem is not None else ("st", wkey))
        self.out_toks.append(t)
        return t

    def conv_eng(self):
        e = ("act", "dve")[self.cv_i % 2]
        self.cv_i += 1
        return e

    def copy(self, eng, out, in_, reads, writes):
        if eng == "act":
            return self.P.op("act", lambda e: e.activation(out=out, in_=in_, func=AF.Identity), reads=reads, writes=writes)
        return self.P.op(eng, lambda e: e.tensor_copy(out=out, in_=in_), reads=reads, writes=writes)

    def finish(self):
        last = {}
        for t in self.out_toks:
            last[t[1]] = t
        self.P.wait_all_on("sp", list(last.values()))
        self.P.emit(self.st)
        self.st.close()
        return self.nc


def mm_group(cx, ps_ap, ps_key, pairs, reads):
    n = len(pairs)
    for i, (l, r) in enumerate(pairs):
        cx.P.op("pe", lambda e, l=l, r=r, i=i: e.matmul(ps_ap, lhsT=l, rhs=r, start=(i == 0), stop=(i == n - 1)),
                reads=reads, writes=[ps_key], signal=(i == n - 1))


class WLoader:
    def __init__(self, cx, name, KC, ncol, nslots=2, nstage=4, kq=4):
        self.cx, self.KC, self.ncol, self.kq = cx, KC, ncol, kq
        self.name = name
        self.wb = [cx.sb("%s_wb%d" % (name, i), [128, KC, ncol], BF16) for i in range(nslots)]
        self.stg = [cx.sb("%s_st%d" % (name, i), [128, kq, ncol], F32) for i in range(nstage)]
        self.si = 0
        self.wi = 0

    def load(self, w_rows_ap, ncol=None):
        cx = self.cx
        ncol = ncol or self.ncol
        slot = self.wi % len(self.wb)
        self.wi += 1
        wb = self.wb[slot]
        key = (self.name, "wb", slot)
        for k0 in range(0, self.KC, self.kq):
            kn = min(self.kq, self.KC - k0)
            s = self.si % len(self.stg)
            self.si += 1
            stg = self.stg[s]
            skey = (self.name, "st", s)
            src = w_rows_ap[k0 * 128:(k0 + kn) * 128, :].rearrange("(c p) n -> p c n", p=128)
            import os
            q = "sp"
            if os.environ.get("VAR", "") == "v1" and (self.si % 2 == 0):
                q = "act"
            cx.P.dma(q, lambda e, stg=stg, src=src, kn=kn: e.dma_start(out=stg[:, 0:kn, 0:ncol], in_=src),
                     writes=[skey])
            ce = cx.conv_eng()
            if os.environ.get("VAR", "") == "v3" and ce == "pool":
                ce = "dve" if (self.si % 2 == 0) else "act"
            cx.copy(ce, wb[:, k0:k0 + kn, 0:ncol], stg[:, 0:kn, 0:ncol], [skey], [(key, k0)])
        return wb, [(key, k0) for k0 in range(0, self.KC, self.kq)]


def build_mod():
    cx = Ctx()
    P = cx.P
    NCOL = 3072
    cT = cx.din("cT", [128, DC, 2], F32)
    W = cx.din("W", [D, NCOL], F32)
    b = cx.din("b", [2, NCOL], F32)
    y = cx.dout("y", [2, NCOL], F32)
    cx.alloc_psum(2)
    c_sb = cx.sb("c_sb", [128, DC, 2], F32)
    cond = cx.sb("cond", [128, DC, 2], F32)
    b_sb = cx.sb("b_sb", [2, NCOL], F32)
    o_sb = cx.sb("o_sb", [2, NCOL], F32)
    wst = [cx.sb("wst%d" % i, [128, DC, 512], F32) for i in range(2)]
    cx.load(c_sb[:], cT, "c_sb")
    cx.load(b_sb[:], b, "b_sb")
    P.op("act", lambda e: e.activation(out=cond[:], in_=c_sb[:], func=AF.Silu), reads=["c_sb"], writes=["cond"])
    for ct in range(NCOL // 512):
        s = ct % 2
        for k0 in range(0, DC, 4):
            src = W[k0 * 128:(k0 + 4) * 128, ct * 512:(ct + 1) * 512].rearrange("(c p) n -> p c n", p=128)
            P.dma("sp", lambda e, s=s, k0=k0, src=src: e.dma_start(out=wst[s][:, k0:k0 + 4, :], in_=src),
                  writes=[("wst", s)])
        ps, pk = cx.psum()
        mm_group(cx, ps[0:2, :], pk, [(cond[:, kc, :], wst[s][:, kc, :]) for kc in range(DC)], ["cond", ("wst", s)])
        P.op("dve", lambda e, ps=ps, ct=ct: e.tensor_tensor(out=o_sb[:, ct * 512:(ct + 1) * 512], in0=ps[0:2, :],
                                                        in1=b_sb[:, ct * 512:(ct + 1) * 512], op=ALU.add),
             reads=[pk, "b_sb"], writes=["o_sb"])
    cx.store(y, o_sb[:], ["o_sb"], "y")
    return cx.finish()


def emit_rmsnorm_mod(cx, xT, xkey, gm, shift, hT, hkey, ones16, tag):
    P = cx.P
    sq = [cx.sb("%s_sq%d" % (tag, i), [128, 512], BF16) for i in range(2)]
    rstd = cx.sb("%s_rstd" % tag, [128, T], F32)
    tmp = [cx.sb("%s_tmp%d" % (tag, i), [128, 512], F32) for i in range(2)]
    for half in range(T // 512):
        hs = slice(half * 512, (half + 1) * 512)
        ps, pk = cx.psum()
        for kc in range(DC):
            s = kc % 2
            P.op("act", lambda e, s=s, kc=kc, hs=hs: e.activation(out=sq[s][:], in_=xT[:, kc, hs], func=AF.Square),
                 reads=[xkey], writes=[(tag, "sq", s)])
            P.op("pe", lambda e, s=s, kc=kc, ps=ps: e.matmul(ps[:], lhsT=ones16[:], rhs=sq[s][:], start=(kc == 0),
                                                          stop=(kc == DC - 1)),
                 reads=[(tag, "sq", s), "ones16"], writes=[pk], signal=True)
        rk = (tag, "rstd", half)
        P.op("dve", lambda e, ps=ps, hs=hs: e.tensor_scalar(out=rstd[:, hs], in0=ps[:], scalar1=1.0 / D, scalar2=EPS,
                                                    op0=ALU.mult, op1=ALU.add), reads=[pk], writes=[rk])
        P.op("act", lambda e, hs=hs: e.activation(out=rstd[:, hs], in_=rstd[:, hs], func=AF.Sqrt), reads=[rk], writes=[rk])
        P.op("dve", lambda e, hs=hs: e.reciprocal(out=rstd[:, hs], in_=rstd[:, hs]), reads=[rk], writes=[rk])
        for kc in range(DC):
            s = kc % 2
            P.op("dve", lambda e, s=s, kc=kc, hs=hs: e.tensor_tensor(out=tmp[s][:], in0=xT[:, kc, hs], in1=rstd[:, hs], op=ALU.mult),
                 reads=[xkey, rk], writes=[(tag, "tmp", s)])
            P.op("act", lambda e, s=s, kc=kc, hs=hs: e.activation(out=hT[:, kc, hs], in_=tmp[s][:], func=AF.Identity,
                                                        scale=gm[:, kc:kc + 1], bias=shift[:, kc:kc + 1]),
                 reads=[(tag, "tmp", s), "modv"], writes=[hkey])
    return rstd


def emit_modvecs(cx, modv, gnorm, gm):
    P = cx.P
    P.op("dve", lambda e: e.tensor_scalar(out=gm[:], in0=modv[:, 1, :], scalar1=1.0, scalar2=None, op0=ALU.add),
         reads=["modv_in"], writes=["gm_tmp"])
    P.op("dve", lambda e: e.tensor_tensor(out=gm[:], in0=gm[:], in1=gnorm[:], op=ALU.mult),
         reads=["gm_tmp", "gnorm"], writes=["modv"])


def emit_linear(cx, wl, W, KC, N, inT, in_keys, evac, ngrp=256):
    groups = [(g0, min(ngrp, N - g0)) for g0 in range(0, N, ngrp)]
    nxt = wl.load(W[:, groups[0][0]:groups[0][0] + groups[0][1]], groups[0][1])
    for gi_, (g0, gn) in enumerate(groups):
        wb, wkeys = nxt
        if gi_ + 1 < len(groups):
            n0, nn = groups[gi_ + 1]
            nxt = wl.load(W[:, n0:n0 + nn], nn)
        for j in range(gn // 128):
            nt = g0 // 128 + j
            for half in range(T // 512):
                ps, pk = cx.psum()
                mm_group(cx, ps[:], pk,
                         [(wb[:, kc, j * 128:(j + 1) * 128], inT[:, kc, half * 512:(half + 1) * 512]) for kc in range(KC)],
                         list(wkeys) + list(in_keys))
                evac(nt, half, ps, pk)


def build_inproj():
    cx = Ctx()
    P = cx.P
    xTd = cx.din("xT", [128, DC, T], F32)
    modd = cx.din("modv", [128, 6, DC], F32)
    gnd = cx.din("gnorm", [128, DC], F32)
    W = cx.din("W", [D, IN_W], F32)
    out = cx.dout("projT", [IN_W, T], BF16)
    cx.alloc_psum(8)
    xT = cx.sb("xT_sb", [128, DC, T], F32)
    hT = cx.sb("hT_sb", [128, DC, T], BF16)
    modv = cx.sb("modv_sb", [128, 6, DC], F32)
    gnorm = cx.sb("gnorm_sb", [128, DC], F32)
    gm = cx.sb("gm_sb", [128, DC], F32)
    ones16 = cx.sb("ones16", [128, 128], BF16)
    osb = [cx.sb("osb%d" % i, [128, T], BF16) for i in range(3)]
    P.op("pool", lambda e: e.memset(ones16[:], 1.0), writes=["ones16"])
    for k0 in range(0, DC, 4):
        P.dma("sp", lambda e, k0=k0: e.dma_start(out=xT[:, k0:k0 + 4, :], in_=xTd[:, k0:k0 + 4, :]), writes=["xT"], sem="xT")
    cx.load(modv[:], modd, "modv_in")
    cx.load(gnorm[:], gnd, "gnorm")
    emit_modvecs(cx, modv, gnorm, gm)
    emit_rmsnorm_mod(cx, xT, "xT", gm, modv[:, 0, :], hT, "hT", ones16, "n1")
    import os
    if os.environ.get("VAR", "") == "v4":
        wl = WLoader(cx, "win", DC, 512, nslots=2, nstage=16, kq=1)
    elif os.environ.get("VAR", "") == "v5":
        wl = WLoader(cx, "win", DC, 512, nslots=2, nstage=8, kq=2)
    else:
        wl = WLoader(cx, "win", DC, 512, nslots=2, nstage=4)
    cnt = [0]

    def evac(nt, half, ps, pk):
        s = nt % 3
        eng = "act" if (cnt[0] % 2 == 0) else "dve"
        cnt[0] += 1
        cx.copy(eng, osb[s][:, half * 512:(half + 1) * 512], ps[:], [pk], [("osb", s, half)])
        if half == T // 512 - 1:
            cx.store(out[nt * 128:(nt + 1) * 128, :], osb[s][:], [("osb", s, h) for h in range(T // 512)], ("out", nt),
                     sem=("st_osb", s))

    emit_linear(cx, wl, W, DC, IN_W, hT, ["hT"], evac, ngrp=512)
    return cx.finish()


def fm(a2d):
    F, n = a2d.shape
    return np.ascontiguousarray(a2d.reshape(F // 128, 128, n).transpose(1, 0, 2))


def vec_fm(v):
    return np.ascontiguousarray(v.reshape(-1, 128).T)


_cache = {}


def get_nc(name, builder):
    if name not in _cache:
        _cache[name] = builder()
    return _cache[name]


def run(nc, in_maps):
    res = run_bass_kernel_spmd(nc, in_maps, core_ids=list(range(NCORES)))
    return res.results


def run_mod(c, w_mod, b_mod):
    nc = get_nc("mod", build_mod)
    cT = np.ascontiguousarray(c.T.reshape(DC, 128, 2).transpose(1, 0, 2))
    ins = []
    for i in range(NCORES):
        l, q = i // 4, i % 4
        cols = slice(q * 3072, (q + 1) * 3072)
        ins.append({"cT": cT, "W": np.ascontiguousarray(w_mod[l][:, cols]),
                    "b": np.ascontiguousarray(np.broadcast_to(b_mod[l][cols], (2, 3072)))})
    res = run(nc, ins)
    mod = np.zeros((2, 2, 6 * D), np.float32)
    for i in range(NCORES):
        l, q = i // 4, i % 4
        mod[l][:, q * 3072:(q + 1) * 3072] = res[i]["y"]
    return mod


def modv_layout(mod_lb):
    return np.ascontiguousarray(mod_lb.reshape(6, DC, 128).transpose(2, 0, 1))


def x_to_cores(x):
    outs = []
    for i in range(NCORES):
        b, q = i // 4, i % 4
        outs.append(fm(np.ascontiguousarray(x[b, q * T:(q + 1) * T, :].T)))
    return outs


def run_inproj(xTs, mod_l, gnorm, w_in_l):
    nc = get_nc("inproj", build_inproj)
    ins = []
    for i in range(NCORES):
        b = i // 4
        ins.append({"xT": xTs[i], "modv": modv_layout(mod_l[b]), "gnorm": vec_fm(gnorm), "W": w_in_l})
    res = run(nc, ins)
    projT = np.zeros((2, IN_W, SEQ), NPBF)
    for i in range(NCORES):
        b, q = i // 4, i % 4
        projT[b][:, q * T:(q + 1) * T] = res[i]["projT"]
    return projT


DILS = (1, 4, 16)


def build_attn():
    cx = Ctx()
    P = cx.P
    QTd = cx.din("QT", [128, 3, SEQ], BF16)
    KTd = cx.din("KT", [128, 3, SEQ], BF16)
    Vd = cx.din("V", [128, 3, 32, 128], BF16)
    Bmd = cx.din("Bm", [128, 3, 256], F32)
    out = cx.dout("oT", [128, SEQ], BF16)
    cx.alloc_psum(8)
    QT = cx.sb("QT_sb", [128, 3, SEQ], BF16)
    KT = cx.sb("KT_sb", [128, 3, SEQ], BF16)
    V = cx.sb("V_sb", [128, 3, 32, 128], BF16)
    Bm = cx.sb("Bm_sb", [128, 3, 256], F32)
    ones16 = cx.sb("ones16", [128, 128], BF16)
    num = cx.sb("num", [128, SEQ], F32)
    den = cx.sb("den", [128, SEQ], F32)
    o16 = cx.sb("o16", [128, SEQ], BF16)
    lg = [cx.sb("lg%d" % i, [128, 256], F32) for i in range(3)]
    pT = [cx.sb("pT%d" % i, [128, 256], BF16) for i in range(3)]
    P.op("pool", lambda e: e.memset(ones16[:], 1.0), writes=["ones16"])
    Vf = V[:].rearrange("p g t d -> p g (t d)")
    Vdf = Vd.rearrange("p g t d -> p g (t d)")
    for g in range(3):
        cx.load(QT[:, g, :], QTd[:, g, :], ("QT", g))
        cx.load(KT[:, g, :], KTd[:, g, :], ("KT", g))
        cx.load(Vf[:, g, :], Vdf[:, g, :], ("V", g))
    cx.load(Bm[:], Bmd, "Bm")
    scale = 128 ** -0.5
    blk = 0
    import os
    DBG = os.environ.get("ATT_DBG", "")
    if DBG == "loads":
        P.op("pool", lambda e: e.memset(o16[:], 0.0), reads=[("QT", 0), ("QT", 1), ("QT", 2), ("KT", 0), ("KT", 1), ("KT", 2), ("V", 0), ("V", 1), ("V", 2), "Bm"], writes=["o16"])
        cx.store(out, o16[:], ["o16"], "oT")
        return cx.finish()
    for g in range(3 if DBG != "g0" else 1):
        d = DILS[g]
        run = SEQ // d
        numv = num[:].rearrange("p (m d) -> p m d", d=d)
        denv = den[:].rearrange("p (m d) -> p m d", d=d)
        for r in range(d):
            for i in range(run // 128):
                p0 = r * run + 128 * i
                nc_ = 256 if i > 0 else 128
                s = blk % 3
                blk += 1
                ps, pk = cx.psum()
                P.op("pe", lambda e, ps=ps, g=g, p0=p0: e.matmul(ps[:, 0:128], lhsT=KT[:, g, p0:p0 + 128],
                                                               rhs=QT[:, g, p0:p0 + 128], start=True, stop=True),
                     reads=[("KT", g), ("QT", g)], writes=[pk], signal=(i == 0))
                if i > 0:
                    P.op("pe", lambda e, ps=ps, g=g, p0=p0: e.matmul(ps[:, 128:256], lhsT=KT[:, g, p0 - 128:p0],
                                                                   rhs=QT[:, g, p0:p0 + 128], start=True, stop=True),
                         reads=[("KT", g), ("QT", g)], writes=[pk])
                LV = int(os.environ.get("ATT_LV", "9"))
                if LV == 1:
                    P.op("dve", lambda e, ps=ps, s=s, n=nc_: e.tensor_copy(out=lg[s][:, 0:n], in_=ps[:, 0:n]), reads=[pk], writes=[("lg", s)])
                    continue
                P.op("dve", lambda e, ps=ps, g=g, s=s, n=nc_: e.scalar_tensor_tensor(
                    out=lg[s][:, 0:n], in0=ps[:, 0:n], scalar=scale, in1=Bm[:, g, 0:n], op0=ALU.mult, op1=ALU.add),
                    reads=[pk, "Bm"], writes=[("lg", s)])
                if LV == 2:
                    continue
                P.op("act", lambda e, s=s, n=nc_: e.activation(out=pT[s][:, 0:n], in_=lg[s][:, 0:n], func=AF.Exp),
                     reads=[("lg", s)], writes=[("pT", s)])
                if LV == 3:
                    continue
                ps2, pk2 = cx.psum()
                td = p0 // 128
                P.op("pe", lambda e, ps2=ps2, g=g, td=td, s=s, i=i: e.matmul(
                    ps2[:, 0:128], lhsT=V[:, g, td, :], rhs=pT[s][:, 0:128], start=True, stop=(i == 0)),
                    reads=[("V", g), ("pT", s)], writes=[pk2], signal=False)
                if i > 0:
                    P.op("pe", lambda e, ps2=ps2, g=g, td=td, s=s: e.matmul(
                        ps2[:, 0:128], lhsT=V[:, g, td - 1, :], rhs=pT[s][:, 128:256], start=False, stop=True),
                        reads=[("V", g), ("pT", s)], writes=[pk2], signal=False)
                P.op("pe", lambda e, ps2=ps2, s=s, i=i: e.matmul(
                    ps2[:, 128:256], lhsT=ones16[:], rhs=pT[s][:, 0:128], start=True, stop=(i == 0)),
                    reads=["ones16", ("pT", s)], writes=[pk2], signal=(i == 0))
                if i > 0:
                    P.op("pe", lambda e, ps2=ps2, s=s: e.matmul(
                        ps2[:, 128:256], lhsT=ones16[:], rhs=pT[s][:, 128:256], start=False, stop=True),
                        reads=["ones16", ("pT", s)], writes=[pk2])
                if LV == 4:
                    P.op("dve", lambda e, ps2=ps2, s=s: e.tensor_copy(out=lg[s][:, 0:256], in_=ps2[:, 0:256]), reads=[pk2], writes=[("lg", s)])
                    continue
                nv = numv[:, 128 * i:128 * (i + 1), r]
                dv = denv[:, 128 * i:128 * (i + 1), r]
                if g == 0:
                    P.op("dve", lambda e, ps2=ps2, nv=nv: e.tensor_copy(out=nv, in_=ps2[:, 0:128]), reads=[pk2], writes=["num"])
                    P.op("dve", lambda e, ps2=ps2, dv=dv: e.tensor_copy(out=dv, in_=ps2[:, 128:256]), reads=[pk2], writes=["den"])
                else:
                    P.op("dve", lambda e, ps2=ps2, nv=nv: e.tensor_tensor(out=nv, in0=ps2[:, 0:128], in1=nv, op=ALU.add),
                         reads=[pk2, "num"], writes=["num"])
                    P.op("dve", lambda e, ps2=ps2, dv=dv: e.tensor_tensor(out=dv, in0=ps2[:, 128:256], in1=dv, op=ALU.add),
                         reads=[pk2, "den"], writes=["den"])
    if int(os.environ.get("ATT_LV", "9")) < 9:
        P.op("dve", lambda e: e.memset(o16[:], 0.0), reads=[("lg", 0), ("lg", 1), ("lg", 2), ("pT", 0), ("pT", 1), ("pT", 2), "num", "den"], writes=["o16"])
        cx.store(out, o16[:], ["o16"], "oT")
        return cx.finish()
    P.op("dve", lambda e: e.reciprocal(out=den[:], in_=den[:]), reads=["den"], writes=["den"])
    P.op("dve", lambda e: e.tensor_tensor(out=o16[:], in0=num[:], in1=den[:], op=ALU.mult), reads=["num", "den"], writes=["o16"])
    cx.store(out, o16[:], ["o16"], "oT")
    return cx.finish()


def t5_bucket_np(dist):
    dist = np.asarray(dist, np.int32)
    max_exact = 16
    d32 = np.maximum(dist, 1).astype(np.float32)
    large = max_exact + (np.log(d32 / np.float32(max_exact)) / np.float32(np.log(2048 / 16)) * np.float32(16)).astype(np.int32)
    return np.where(dist < max_exact, dist, np.minimum(large, 31))


def attn_bias_mats(rel_bias, slot):
    Bm = np.full((128, 3, 256), -1e30, np.float32)
    ik = np.arange(128)[:, None]
    jq = np.arange(128)[None, :]
    for g, d in enumerate(DILS):
        head = 4 * g + slot
        rel = jq - ik
        bd = rel_bias[t5_bucket_np(np.maximum(rel, 0) * d), head]
        Bm[:, g, 0:128] = np.where(rel >= 0, bd, np.float32(-1e30))
        rel2 = jq - ik + 128
        bo = rel_bias[t5_bucket_np(np.minimum(rel2, 128) * d), head]
        Bm[:, g, 128:256] = np.where(rel2 <= 128, bo, np.float32(-1e30))
    return Bm


def run_attn(projT, rel_bias):
    nc = get_nc("attn", build_attn)
    ins = []
    for i in range(NCORES):
        b, slot = i // 4, i % 4
        QT = np.zeros((128, 3, SEQ), NPBF)
        KT = np.zeros((128, 3, SEQ), NPBF)
        V = np.zeros((128, 3, 32, 128), NPBF)
        for g, d in enumerate(DILS):
            head = 4 * g + slot
            perm = np.arange(SEQ).reshape(SEQ // d, d).T.reshape(-1)
            QT[:, g, :] = projT[b, 1024 + head * 128:1024 + (head + 1) * 128, :][:, perm]
            KT[:, g, :] = projT[b, 2560 + head * 128:2560 + (head + 1) * 128, :][:, perm]
            vt = projT[b, 4096 + head * 128:4096 + (head + 1) * 128, :][:, perm]
            V[:, g, :, :] = vt.T.reshape(32, 128, 128).transpose(1, 0, 2)
        ins.append({"QT": QT, "KT": KT, "V": V, "Bm": attn_bias_mats(rel_bias, slot)})
    res = run(nc, ins)
    yT = np.zeros((2, 512, SEQ), NPBF)
    for i in range(NCORES):
        b, slot = i // 4, i % 4
        yT[b, slot * 128:(slot + 1) * 128, :] = res[i]["oT"]
    return yT


NPW = 33
TWO_PI = 6.283185307179586
C1 = 6.28125
C2 = TWO_PI - C1


def ssm_nvec():
    n = [7 - k for k in range(8)] + [t - 7 for t in range(8)] + [t + 1 for t in range(8)] + [8 * 2 ** j for j in range(9)]
    return np.asarray(n, np.float32)


def build_ssm():
    cx = Ctx()
    P = cx.P
    G = 8
    NCH = 512
    Ud = cx.din("U", [128, G, 2, NCH], BF16)
    prm = cx.din("prm", [128, 3, G], F32)
    Bd = cx.din("Bri", [128, 2, G, 16], F32)
    Cd = cx.din("Cri", [128, 2, G, 16], F32)
    Dd = cx.din("Dcol", [128, G], F32)
    nvd = cx.din("nvec", [128, NPW], F32)
    mkd = cx.din("maskT", [128, 128], F32)
    idd = cx.din("ident", [128, 128], F32)
    out = cx.dout("ypre", [128, G, 2, NCH], F32)
    cx.alloc_psum(8)
    U = cx.sb("U_sb", [128, G, 2, NCH], BF16)
    prm_s = cx.sb("prm_s", [128, 3, G], F32)
    Bri = cx.sb("Bri_s", [128, 2, G, 16], F32)
    Cri = cx.sb("Cri_s", [128, 2, G, 16], F32)
    Dcol = cx.sb("Dcol_s", [128, G], F32)
    nvec = cx.sb("nvec_s", [128, NPW], F32)
    maskT = cx.sb("maskT_s", [128, 128], F32)
    ident = cx.sb("ident_s", [128, 128], F32)
    for g in range(G):
        cx.load(U[:, g, :, :].rearrange("p b c -> p (b c)"), Ud[:, g, :, :].rearrange("p b c -> p (b c)"), ("U", g))
    cx.load(prm_s[:], prm, "prm")
    cx.load(Bri[:].rearrange("p a g h -> p (a g h)"), Bd.rearrange("p a g h -> p (a g h)"), "Bri")
    cx.load(Cri[:].rearrange("p a g h -> p (a g h)"), Cd.rearrange("p a g h -> p (a g h)"), "Cri")
    cx.load(Dcol[:], Dd, "Dcol")
    cx.load(nvec[:], nvd, "nvec")
    cx.load(maskT[:], mkd, "maskT")
    cx.load(ident[:], idd, "ident")

    sm = lambda name, shape: cx.sb(name, shape, F32)
    dt_ = sm("dt_", [128, G]); lr = sm("lr", [128, G]); li = sm("li", [128, G])
    ang = sm("ang", [128, G, NPW]); mag = sm("mag", [128, G, NPW]); kf = sm("kf", [128, G, NPW])
    ki = cx.sb("ki", [128, G, NPW], I32)
    msk = sm("msk", [128, G, NPW]); rs = sm("rs", [128, G, NPW]); rc = sm("rc", [128, G, NPW])
    Pr = sm("Pr", [128, G, NPW]); Pi = sm("Pi", [128, G, NPW]); nPi = sm("nPi", [128, G, NPW])
    K = "ssmprep"

    def dve(fn, reads=(), writes=()):
        P.op("dve", fn, reads=[K] + list(reads), writes=[K] + list(writes))

    def act(fn, reads=(), writes=()):
        P.op("act", fn, reads=[K] + list(reads), writes=[K] + list(writes))

    act(lambda e: e.activation(out=dt_[:], in_=prm_s[:, 2, :], func=AF.Exp), reads=["prm"])
    dve(lambda e: e.tensor_tensor(out=lr[:], in0=prm_s[:, 0, :], in1=dt_[:], op=ALU.mult))
    dve(lambda e: e.tensor_tensor(out=li[:], in0=prm_s[:, 1, :], in1=dt_[:], op=ALU.mult))
    for g in range(G):
        dve(lambda e, g=g: e.tensor_scalar(out=ang[:, g, :], in0=nvec[:], scalar1=li[:, g:g + 1], scalar2=None, op0=ALU.mult),
            reads=["nvec"])
        act(lambda e, g=g: e.activation(out=mag[:, g, :], in_=nvec[:], func=AF.Exp, scale=lr[:, g:g + 1]), reads=["nvec"])

    def reduce_sin(src_fn, dst):
        dve(lambda e: e.tensor_scalar(out=kf[:], in0=src_fn(), scalar1=1.0 / TWO_PI, scalar2=None, op0=ALU.mult))
        dve(lambda e: e.tensor_copy(out=ki[:], in_=kf[:]))
        dve(lambda e: e.tensor_copy(out=kf[:], in_=ki[:]))
        dve(lambda e: e.scalar_tensor_tensor(out=dst[:], in0=kf[:], scalar=-C1, in1=src_fn(), op0=ALU.mult, op1=ALU.add))
        dve(lambda e: e.scalar_tensor_tensor(out=dst[:], in0=kf[:], scalar=-C2, in1=dst[:], op0=ALU.mult, op1=ALU.add))
        dve(lambda e: e.tensor_scalar(out=msk[:], in0=dst[:], scalar1=float(np.pi), scalar2=None, op0=ALU.is_gt))
        dve(lambda e: e.scalar_tensor_tensor(out=dst[:], in0=msk[:], scalar=-TWO_PI, in1=dst[:], op0=ALU.mult, op1=ALU.add))
        dve(lambda e: e.tensor_scalar(out=msk[:], in0=dst[:], scalar1=-float(np.pi), scalar2=None, op0=ALU.is_lt))
        dve(lambda e: e.scalar_tensor_tensor(out=dst[:], in0=msk[:], scalar=TWO_PI, in1=dst[:], op0=ALU.mult, op1=ALU.add))
        dve(lambda e: e.tensor_scalar(out=dst[:], in0=dst[:], scalar1=3.1415925, scalar2=-3.1415925, op0=ALU.min, op1=ALU.max))
        act(lambda e: e.activation(out=dst[:], in_=dst[:], func=AF.Sin))

    reduce_sin(lambda: ang[:], rs)
    dve(lambda e: e.tensor_scalar(out=ang[:], in0=ang[:], scalar1=float(np.pi / 2), scalar2=None, op0=ALU.add))
    reduce_sin(lambda: ang[:], rc)
    dve(lambda e: e.tensor_tensor(out=Pr[:], in0=mag[:], in1=rc[:], op=ALU.mult))
    dve(lambda e: e.tensor_tensor(out=Pi[:], in0=mag[:], in1=rs[:], op=ALU.mult))
    dve(lambda e: e.tensor_scalar(out=nPi[:], in0=Pi[:], scalar1=-1.0, scalar2=None, op0=ALU.mult))

    xr = sm("xr", [128, G]); abi = sm("abi", [128, G]); den_ = sm("den_", [128, G]); t1 = sm("t1", [128, G]); t2 = sm("t2", [128, G])
    cr = sm("cr", [128, G]); ci = sm("ci", [128, G]); nci = sm("nci", [128, G])
    are = prm_s[:, 0, :]
    aim = prm_s[:, 1, :]
    dve(lambda e: e.tensor_scalar(out=xr[:], in0=Pr[:, :, 16], scalar1=-1.0, scalar2=None, op0=ALU.add))
    dve(lambda e: e.tensor_copy(out=abi[:], in_=Pi[:, :, 16]))
    dve(lambda e: e.tensor_tensor(out=t1[:], in0=are, in1=are, op=ALU.mult))
    dve(lambda e: e.tensor_tensor(out=t2[:], in0=aim, in1=aim, op=ALU.mult))
    dve(lambda e: e.tensor_tensor(out=den_[:], in0=t1[:], in1=t2[:], op=ALU.add))
    dve(lambda e: e.reciprocal(out=den_[:], in_=den_[:]))
    dve(lambda e: e.tensor_tensor(out=t1[:], in0=xr[:], in1=are, op=ALU.mult))
    dve(lambda e: e.tensor_tensor(out=t2[:], in0=abi[:], in1=aim, op=ALU.mult))
    dve(lambda e: e.tensor_tensor(out=cr[:], in0=t1[:], in1=t2[:], op=ALU.add))
    dve(lambda e: e.tensor_tensor(out=cr[:], in0=cr[:], in1=den_[:], op=ALU.mult))
    dve(lambda e: e.tensor_tensor(out=t1[:], in0=abi[:], in1=are, op=ALU.mult))
    dve(lambda e: e.tensor_tensor(out=t2[:], in0=xr[:], in1=aim, op=ALU.mult))
    dve(lambda e: e.tensor_tensor(out=ci[:], in0=t1[:], in1=t2[:], op=ALU.subtract))
    dve(lambda e: e.tensor_tensor(out=ci[:], in0=ci[:], in1=den_[:], op=ALU.mult))
    dve(lambda e: e.tensor_scalar(out=nci[:], in0=ci[:], scalar1=-1.0, scalar2=None, op0=ALU.mult))

    Bbr = sm("Bbr", [128, G, 16]); Bbi = sm("Bbi", [128, G, 16])
    BX = sm("BX", [128, G, 16]); BY = sm("BY", [128, G, 16]); nBX = sm("nBX", [128, G, 16])
    CX = sm("CX", [128, G, 16]); CY = sm("CY", [128, G, 16])
    for g in range(G):
        dve(lambda e, g=g: e.tensor_scalar(out=Bbr[:, g, :], in0=Bri[:, 0, g, :], scalar1=cr[:, g:g + 1], scalar2=None, op0=ALU.mult), reads=["Bri"])
        dve(lambda e, g=g: e.scalar_tensor_tensor(out=Bbr[:, g, :], in0=Bri[:, 1, g, :], scalar=nci[:, g:g + 1], in1=Bbr[:, g, :],
                                               op0=ALU.mult, op1=ALU.add))
        dve(lambda e, g=g: e.tensor_scalar(out=Bbi[:, g, :], in0=Bri[:, 1, g, :], scalar1=cr[:, g:g + 1], scalar2=None, op0=ALU.mult))
        dve(lambda e, g=g: e.scalar_tensor_tensor(out=Bbi[:, g, :], in0=Bri[:, 0, g, :], scalar=ci[:, g:g + 1], in1=Bbi[:, g, :],
                                               op0=ALU.mult, op1=ALU.add))
    lo, hi = slice(0, 64), slice(64, 128)
    dve(lambda e: e.tensor_copy(out=BX[lo], in_=Bbr[lo]))
    dve(lambda e: e.tensor_copy(out=BX[hi], in_=Bbi[hi]))
    dve(lambda e: e.tensor_scalar(out=BY[lo], in0=Bbi[lo], scalar1=-1.0, scalar2=None, op0=ALU.mult))
    dve(lambda e: e.tensor_copy(out=BY[hi], in_=Bbr[hi]))
    dve(lambda e: e.tensor_scalar(out=nBX[:], in0=BX[:], scalar1=-1.0, scalar2=None, op0=ALU.mult))
    dve(lambda e: e.tensor_copy(out=CX[lo], in_=Cri[lo, 0, :, :]), reads=["Cri"])
    dve(lambda e: e.tensor_scalar(out=CX[hi], in0=Cri[hi, 1, :, :], scalar1=-1.0, scalar2=None, op0=ALU.mult))
    dve(lambda e: e.tensor_scalar(out=CY[lo], in0=Cri[lo, 1, :, :], scalar1=-1.0, scalar2=None, op0=ALU.mult))
    dve(lambda e: e.tensor_scalar(out=CY[hi], in0=Cri[hi, 0, :, :], scalar1=-1.0, scalar2=None, op0=ALU.mult))

    BcA = sm("BcA", [128, G, 8, 16]); BcB = sm("BcB", [128, G, 8, 16]); Cm = sm("Cm", [128, G, 8, 16]); Cc = sm("Cc", [128, G, 8, 16])
    for g in range(G):
        for j in range(8):
            dve(lambda e, g=g, j=j: e.tensor_scalar(out=BcA[:, g, j, :], in0=BX[:, g, :], scalar1=Pr[:, g, j:j + 1], scalar2=None, op0=ALU.mult))
            dve(lambda e, g=g, j=j: e.scalar_tensor_tensor(out=BcA[:, g, j, :], in0=BY[:, g, :], scalar=Pi[:, g, j:j + 1], in1=BcA[:, g, j, :],
                                                        op0=ALU.mult, op1=ALU.add))
            dve(lambda e, g=g, j=j: e.tensor_scalar(out=BcB[:, g, j, :], in0=BY[:, g, :], scalar1=Pr[:, g, j:j + 1], scalar2=None, op0=ALU.mult))
            dve(lambda e, g=g, j=j: e.scalar_tensor_tensor(out=BcB[:, g, j, :], in0=nBX[:, g, :], scalar=Pi[:, g, j:j + 1], in1=BcB[:, g, j, :],
                                                        op0=ALU.mult, op1=ALU.add))
            for dst, k0 in ((Cm, 8), (Cc, 16)):
                dve(lambda e, g=g, j=j, dst=dst, k0=k0: e.tensor_scalar(out=dst[:, g, j, :], in0=CX[:, g, :], scalar1=Pr[:, g, k0 + j:k0 + j + 1],
                                                                   scalar2=None, op0=ALU.mult))
                dve(lambda e, g=g, j=j, dst=dst, k0=k0: e.scalar_tensor_tensor(out=dst[:, g, j, :], in0=CY[:, g, :], scalar=Pi[:, g, k0 + j:k0 + j + 1],
                                                                          in1=dst[:, g, j, :], op0=ALU.mult, op1=ALU.add))

    MT16 = cx.sb("MT16", [128, G, 128], BF16)
    BaT16 = cx.sb("BaT16", [128, G, 128], BF16)
    BbT16 = cx.sb("BbT16", [128, G, 128], BF16)
    Cc16 = cx.sb("Cc16", [128, G, 128], BF16)
    dve(lambda e: e.tensor_copy(out=Cc16[:].rearrange("p g n -> p (g n)"), in_=Cc[:].rearrange("p g t h -> p (g t h)")), writes=["Cc16"])
    for g in range(G):
        bca = BcA[:, g, :, :].rearrange("p s h -> p (s h)")
        bcb = BcB[:, g, :, :].rearrange("p s h -> p (s h)")
        cm = Cm[:, g, :, :].rearrange("p t h -> p (t h)")
        ps, pk = cx.psum()
        P.op("pe", lambda e, ps=ps, bca=bca, cm=cm: e.matmul(ps[:, 0:128], lhsT=bca, rhs=cm, start=True, stop=True), reads=[K], writes=[pk])
        P.op("dve", lambda e, ps=ps, g=g: e.tensor_tensor(out=MT16[:, g, :], in0=ps[:, 0:128], in1=maskT[:], op=ALU.mult),
             reads=[pk, "maskT"], writes=[("MT16", g)])
        ps, pk = cx.psum()
        P.op("pe", lambda e, ps=ps, bca=bca: e.transpose(ps[:, 0:128], bca, ident[:]), reads=[K, "ident"], writes=[pk])
        P.op("dve", lambda e, ps=ps, g=g: e.tensor_copy(out=BaT16[:, g, :], in_=ps[:, 0:128]), reads=[pk], writes=[("BaT16", g)])
        ps, pk = cx.psum()
        P.op("pe", lambda e, ps=ps, bcb=bcb: e.transpose(ps[:, 0:128], bcb, ident[:]), reads=[K, "ident"], writes=[pk])
        P.op("dve", lambda e, ps=ps, g=g: e.tensor_copy(out=BbT16[:, g, :], in_=ps[:, 0:128]), reads=[pk], writes=[("BbT16", g)])

    SA = [cx.sb("SA%d" % i, [128, 2, NCH], F32) for i in range(2)]
    SB = [cx.sb("SB%d" % i, [128, 2, NCH], F32) for i in range(2)]
    S16 = [cx.sb("S16_%d" % i, [128, 2, NCH], BF16) for i in range(2)]
    ysb = [cx.sb("ysb%d" % i, [128, 2, NCH], F32) for i in range(2)]
    for i in range(2):
        P.op("pool", lambda e, i=i: e.memset(S16[i][:], 0.0), writes=[("S16", i)])
    for g in range(G):
        for (WT, dst, dk) in ((BaT16, SA[0], "SA0"), (BbT16, SB[0], "SB0")):
            for b in range(2):
                ps, pk = cx.psum()
                P.op("pe", lambda e, ps=ps, WT=WT, g=g, b=b: e.matmul(ps[:], lhsT=WT[:, g, :], rhs=U[:, g, b, :], start=True, stop=True),
                     reads=[("BaT16", g), ("BbT16", g), ("U", g)], writes=[pk])
                P.op("dve", lambda e, ps=ps, dst=dst, b=b: e.tensor_copy(out=dst[:, b, :], in_=ps[:]), reads=[pk], writes=[(dk, b)])
        cur = 0
        for j in range(9):
            d = 2 ** j
            nxt = 1 - cur
            oA, oB, nA, nB = SA[cur], SB[cur], SA[nxt], SB[nxt]
            kA = ["SA%d" % cur, ("SA%d" % cur, 0), ("SA%d" % cur, 1)]
            kB = ["SB%d" % cur, ("SB%d" % cur, 0), ("SB%d" % cur, 1)]
            wA = ["SA%d" % nxt, ("SA%d" % nxt, 0), ("SA%d" % nxt, 1)]
            wB = ["SB%d" % nxt, ("SB%d" % nxt, 0), ("SB%d" % nxt, 1)]
            pr = Pr[:, g, 24 + j:25 + j]
            pi = Pi[:, g, 24 + j:25 + j]
            npi = nPi[:, g, 24 + j:25 + j]
            P.op("dve", lambda e, oA=oA, nA=nA, d=d, pr=pr: e.scalar_tensor_tensor(
                out=nA[:, :, d:NCH], in0=oA[:, :, 0:NCH - d], scalar=pr, in1=oA[:, :, d:NCH], op0=ALU.mult, op1=ALU.add),
                reads=kA + [K], writes=wA)
            P.op("dve", lambda e, oB=oB, nA=nA, d=d, pi=pi: e.scalar_tensor_tensor(
                out=nA[:, :, d:NCH], in0=oB[:, :, 0:NCH - d], scalar=pi, in1=nA[:, :, d:NCH], op0=ALU.mult, op1=ALU.add),
                reads=kB + wA, writes=wA)
            P.op("act", lambda e, oA=oA, nA=nA, d=d: e.activation(out=nA[:, :, 0:d], in_=oA[:, :, 0:d], func=AF.Identity), reads=kA, writes=wA)
            P.op("dve", lambda e, oB=oB, nB=nB, d=d, pr=pr: e.scalar_tensor_tensor(
                out=nB[:, :, d:NCH], in0=oB[:, :, 0:NCH - d], scalar=pr, in1=oB[:, :, d:NCH], op0=ALU.mult, op1=ALU.add),
                reads=kB, writes=wB)
            P.op("dve", lambda e, oA=oA, nB=nB, d=d, npi=npi: e.scalar_tensor_tensor(
                out=nB[:, :, d:NCH], in0=oA[:, :, 0:NCH - d], scalar=npi, in1=nB[:, :, d:NCH], op0=ALU.mult, op1=ALU.add),
                reads=kA + wB, writes=wB)
            P.op("act", lambda e, oB=oB, nB=nB, d=d: e.activation(out=nB[:, :, 0:d], in_=oB[:, :, 0:d], func=AF.Identity), reads=kB, writes=wB)
            cur = nxt
        fin = SA[cur]
        s16 = S16[g % 2]
        P.op("act", lambda e, fin=fin, s16=s16: e.activation(out=s16[:, :, 1:NCH], in_=fin[:, :, 0:NCH - 1], func=AF.Identity),
             reads=["SA%d" % cur], writes=[("S16", g % 2)])
        for b in range(2):
            ps, pk = cx.psum()
            P.op("pe", lambda e, ps=ps, g=g, b=b: e.matmul(ps[:], lhsT=MT16[:, g, :], rhs=U[:, g, b, :], start=True, stop=False),
                 reads=[("MT16", g), ("U", g)], writes=[pk], signal=False)
            P.op("pe", lambda e, ps=ps, g=g, b=b, s16=s16: e.matmul(ps[:], lhsT=Cc16[:, g, :], rhs=s16[:, b, :], start=False, stop=True),
                 reads=["Cc16", ("S16", g % 2)], writes=[pk])
            y = ysb[g % 2]
            P.op("dve", lambda e, ps=ps, g=g, b=b, y=y: e.scalar_tensor_tensor(
                out=y[:, b, :], in0=U[:, g, b, :], scalar=Dcol[:, g:g + 1], in1=ps[:], op0=ALU.mult, op1=ALU.add),
                reads=[pk, "Dcol", ("U", g)], writes=[("ysb", g % 2, b)])
        cx.store(out[:, g, :, :].rearrange("p b c -> p (b c)"), ysb[g % 2][:].rearrange("p b c -> p (b c)"),
                 [("ysb", g % 2, 0), ("ysb", g % 2, 1)], ("out", g), sem=("st_ysb", g % 2))
    return cx.finish()


def ssm_inputs(projT, l, inp, core):
    G = 8
    gs = slice(core * G, (core + 1) * G)
    u = projT[:, core * 128:(core + 1) * 128, :]
    U = u.reshape(2, G, 16, 512, 8).transpose(4, 2, 1, 0, 3).reshape(128, G, 2, 512)
    dup = lambda a: np.concatenate([a, a], axis=0)
    are = inp["ssm_a_re"][l][gs].T
    aim = inp["ssm_a_im"][l][gs].T
    ldt = np.broadcast_to(inp["ssm_log_dt"][l][gs][None, :], (64, G))
    prm = dup(np.stack([are, aim, ldt], axis=1))
    Bri = dup(np.stack([inp["ssm_b_re"][l][gs].transpose(1, 0, 2), inp["ssm_b_im"][l][gs].transpose(1, 0, 2)], axis=1))
    Cri = dup(np.stack([inp["ssm_c_re"][l][gs].transpose(2, 0, 1), inp["ssm_c_im"][l][gs].transpose(2, 0, 1)], axis=1))
    dsk = inp["ssm_d"][l][core * 128:(core + 1) * 128].reshape(G, 16)
    Dcol = np.tile(dsk.T, (8, 1))
    sidx = np.arange(128) // 16
    maskT = (sidx[:, None] <= sidx[None, :]).astype(np.float32)
    return {"U": np.ascontiguousarray(U), "prm": np.ascontiguousarray(prm, dtype=np.float32),
            "Bri": np.ascontiguousarray(Bri, dtype=np.float32), "Cri": np.ascontiguousarray(Cri, dtype=np.float32),
            "Dcol": np.ascontiguousarray(Dcol, dtype=np.float32),
            "nvec": np.ascontiguousarray(np.broadcast_to(ssm_nvec()[None, :], (128, NPW))),
            "maskT": maskT, "ident": np.eye(128, dtype=np.float32)}


def run_ssm(projT, l, inp):
    nc = get_nc("ssm", build_ssm)
    ins = [ssm_inputs(projT, l, inp, i) for i in range(NCORES)]
    res = run(nc, ins)
    yT = np.zeros((2, 1024, SEQ), np.float32)
    for i in range(NCORES):
        y = res[i]["ypre"]
        yT[:, i * 128:(i + 1) * 128, :] = y.reshape(8, 16, 8, 2, 512).transpose(3, 2, 1, 4, 0).reshape(2, 128, SEQ)
    return yT


def build_post(moe):
    cx = Ctx()
    P = cx.P
    xTd = cx.din("xT", [128, DC, T], F32)
    ypd = cx.din("ypreT", [128, 8, T], F32)
    yad = cx.din("yattT", [128, 4, T], BF16)
    gsd = cx.din("gsT", [128, DC, T], BF16)
    gad = cx.din("gaT", [128, DC, T], BF16)
    modd = cx.din("modv", [128, 6, DC], F32)
    gnd = cx.din("gnorm", [128, DC], F32)
    bgd = cx.din("bglu", [128, 8], F32)
    Wglu = cx.din("w_glu", [1024, 1024], F32)
    Wbs = cx.din("w_bs", [1024, D], F32)
    Wba = cx.din("w_ba", [512, D], F32)
    Wout = cx.din("w_out", [D, D], F32)
    xo = cx.dout("xT_out", [128, DC, T], F32)
    ho = cx.dout("h2T", [128, DC, T], BF16)
    if moe:
        wrd = cx.din("w_router", [128, DC, NEXP], F32)
        rwo = cx.dout("rw", [T // 128, 128, NEXP], F32)
    cx.alloc_psum(8)
    xT = cx.sb("xT_sb", [128, DC, T], F32)
    bufA = cx.sb("bufA", [128, DC, T], BF16)
    y32 = bufA[:].bitcast(F32).rearrange("p (k t) -> p k t", k=8) if False else None
    y32t = cx.sb("y32", [128, 8, T // 2], F32) if False else None
    y16 = cx.sb("y16", [128, 8, T], BF16)
    yg16 = cx.sb("yg16", [128, 8, T], BF16)
    ya16 = cx.sb("ya16", [128, 4, T], BF16)
    modv = cx.sb("modv_sb", [128, 6, DC], F32)
    gnorm = cx.sb("gnorm_sb", [128, DC], F32)
    gm = cx.sb("gm_sb", [128, DC], F32)
    bglu = cx.sb("bglu_sb", [128, 8], F32)
    ones16 = cx.sb("ones16", [128, 128], BF16)
    P.op("pool", lambda e: e.memset(ones16[:], 1.0), writes=["ones16"])
    for k0 in range(0, DC, 4):
        P.dma("sp", lambda e, k0=k0: e.dma_start(out=xT[:, k0:k0 + 4, :], in_=xTd[:, k0:k0 + 4, :]), writes=["xT"], sem="xT")
    cx.load(ya16[:], yad, "ya16")
    cx.load(modv[:], modd, "modv_in")
    cx.load(gnorm[:], gnd, "gnorm")
    cx.load(bglu[:], bgd, "bglu")
    wl = WLoader(cx, "wl", DC, 128, nslots=4, nstage=4)

    y32 = bufA[:].rearrange("p k t -> p (k t)").bitcast(F32).rearrange("p (k t) -> p k t", k=8)
    yp = [cx.sb("yp%d" % i, [128, T], F32) for i in range(2)]
    ta = [cx.sb("ta%d" % i, [128, T], F32) for i in range(2)]
    for kc in range(8):
        s = kc % 2
        cx.load(yp[s][:], ypd[:, kc, :], ("yp", s))
        P.op("act", lambda e, s=s: e.activation(out=ta[s][:], in_=yp[s][:], func=AF.Square), reads=[("yp", s)], writes=[("ta", s)])
        P.op("dve", lambda e, s=s: e.tensor_scalar(out=ta[s][:], in0=ta[s][:], scalar1=0.044715, scalar2=1.0, op0=ALU.mult, op1=ALU.add),
             reads=[("ta", s)], writes=[("ta", s)])
        P.op("dve", lambda e, s=s: e.tensor_tensor(out=ta[s][:], in0=ta[s][:], in1=yp[s][:], op=ALU.mult), reads=[("ta", s), ("yp", s)], writes=[("ta", s)])
        P.op("act", lambda e, s=s: e.activation(out=ta[s][:], in_=ta[s][:], func=AF.Sigmoid, scale=1.5957691216057308),
             reads=[("ta", s)], writes=[("ta", s)])
        P.op("dve", lambda e, s=s, kc=kc: e.tensor_tensor(out=y32[:, kc, :], in0=ta[s][:], in1=yp[s][:], op=ALU.mult),
             reads=[("ta", s), ("yp", s)], writes=["bufA"])
        P.op("pool", lambda e, kc=kc: e.tensor_copy(out=y16[:, kc, :], in_=y32[:, kc, :]), reads=["bufA"], writes=["y16"])

    sg = [cx.sb("sg%d" % i, [128, 512], F32) for i in range(2)]
    cnt = [0]

    def evac_glu(nt, half, ps, pk):
        s = cnt[0] % 2
        cnt[0] += 1
        hs = slice(half * 512, (half + 1) * 512)
        P.op("act", lambda e: e.activation(out=sg[s][:], in_=ps[:], func=AF.Sigmoid, bias=bglu[:, nt:nt + 1]),
             reads=[pk, "bglu"], writes=[("sg", s)])
        P.op("dve", lambda e: e.tensor_tensor(out=yg16[:, nt, hs], in0=sg[s][:], in1=y32[:, nt, hs], op=ALU.mult),
             reads=[("sg", s), "bufA"], writes=["yg16"])

    wl.KC = 8
    emit_linear(cx, wl, Wglu, 8, 1024, y16, ["y16"], evac_glu, ngrp=128)

    gts = [cx.sb("gts%d" % i, [128, 512], BF16) for i in range(2)]
    gta = [cx.sb("gta%d" % i, [128, 512], BF16) for i in range(2)]
    m1 = [cx.sb("m1_%d" % i, [128, 512], F32) for i in range(2)]
    m2 = [cx.sb("m2_%d" % i, [128, 512], F32) for i in range(2)]
    it = 0
    def load_c(nt_):
        wl.KC = 8
        a_ = wl.load(Wbs[:, nt_ * 128:(nt_ + 1) * 128], 128)
        wl.KC = 4
        b_ = wl.load(Wba[:, nt_ * 128:(nt_ + 1) * 128], 128)
        return a_, b_

    nxtc = load_c(0)
    for nt in range(DC):
        (wbs, kbs), (wba, kba) = nxtc
        if nt + 1 < DC:
            nxtc = load_c(nt + 1)
        for half in range(2):
            s = it % 2
            it += 1
            hs = slice(half * 512, (half + 1) * 512)
            cx.load(gts[s][:], gsd[:, nt, hs], ("gts", s))
            cx.load(gta[s][:], gad[:, nt, hs], ("gta", s))
            ps1, pk1 = cx.psum()
            mm_group(cx, ps1[:], pk1, [(wbs[:, kc, 0:128], yg16[:, kc, hs]) for kc in range(8)], list(kbs) + ["yg16"])
            ps2, pk2 = cx.psum()
            mm_group(cx, ps2[:], pk2, [(wba[:, kc, 0:128], ya16[:, kc, hs]) for kc in range(4)], list(kba) + ["ya16"])
            P.op("act", lambda e, s=s: e.activation(out=m1[s][:], in_=gts[s][:], func=AF.Sigmoid), reads=[("gts", s)], writes=[("m1", s)])
            P.op("act", lambda e, s=s: e.activation(out=m2[s][:], in_=gta[s][:], func=AF.Sigmoid), reads=[("gta", s)], writes=[("m2", s)])
            P.op("dve", lambda e, s=s, ps1=ps1: e.tensor_tensor(out=m1[s][:], in0=m1[s][:], in1=ps1[:], op=ALU.mult), reads=[("m1", s), pk1], writes=[("m1", s)])
            P.op("dve", lambda e, s=s, ps2=ps2: e.tensor_tensor(out=m2[s][:], in0=m2[s][:], in1=ps2[:], op=ALU.mult), reads=[("m2", s), pk2], writes=[("m2", s)])
            P.op("pool", lambda e, s=s, nt=nt, hs=hs: e.tensor_tensor(out=bufA[:, nt, hs], in0=m1[s][:], in1=m2[s][:], op=ALU.add),
                 reads=[("m1", s), ("m2", s)], writes=["bufA"])

    def evac_out(nt, half, ps, pk):
        hs = slice(half * 512, (half + 1) * 512)
        P.op("dve", lambda e: e.scalar_tensor_tensor(out=xT[:, nt, hs], in0=ps[:], scalar=modv[:, 2, nt:nt + 1], in1=xT[:, nt, hs],
                                                    op0=ALU.mult, op1=ALU.add), reads=[pk, "modv_in", "xT"], writes=["xT"])

    wl.KC = DC
    emit_linear(cx, wl, Wout, DC, D, bufA, ["bufA"], evac_out, ngrp=128)
    for k0 in range(0, DC, 4):
        cx.store(xo[:, k0:k0 + 4, :], xT[:, k0:k0 + 4, :], ["xT"], ("xo", k0), sem="st_x")

    P.op("dve", lambda e: e.tensor_scalar(out=gm[:], in0=modv[:, 4, :], scalar1=1.0, scalar2=None, op0=ALU.add), reads=["modv_in"], writes=["gm_tmp"])
    P.op("dve", lambda e: e.tensor_tensor(out=gm[:], in0=gm[:], in1=gnorm[:], op=ALU.mult), reads=["gm_tmp", "gnorm"], writes=["modv"])
    rstd = emit_rmsnorm_mod(cx, xT, "xT", gm, modv[:, 3, :], bufA, "bufA", ones16, "n2")
    for k0 in range(0, DC, 4):
        cx.store(ho[:, k0:k0 + 4, :], bufA[:, k0:k0 + 4, :], ["bufA"], ("ho", k0), sem="st_h")

    if moe:
        wr = cx.sb("wr_sb", [128, DC, NEXP], F32)
        cx.load(wr[:], wrd, "wr")
        h32 = [cx.sb("h32_%d" % i, [128, 128], F32) for i in range(3)]
        lgt = cx.sb("lgt", [128, NEXP], F32)
        mx8 = cx.sb("mx8", [128, 8], F32)
        nm1 = cx.sb("nm1", [128, 1], F32)
        sel = cx.sb("sel", [128, NEXP], F32)
        ex = cx.sb("ex", [128, NEXP], F32)
        ssum = cx.sb("ssum", [128, 1], F32)
        rwt = [cx.sb("rwt%d" % i, [128, NEXP], F32) for i in range(2)]
        R = "router"
        for tt in range(T // 128):
            ts_ = slice(tt * 128, (tt + 1) * 128)
            ps, pk = cx.psum()
            for kc in range(DC):
                s = kc % 3
                P.op("dve", lambda e, s=s, kc=kc, ts_=ts_: e.tensor_tensor(out=h32[s][:], in0=xT[:, kc, ts_], in1=rstd[:, ts_], op=ALU.mult),
                     reads=["xT", ("n2", "rstd", tt // 4)], writes=[("h32", s)])
                P.op("act", lambda e, s=s, kc=kc: e.activation(out=h32[s][:], in_=h32[s][:], func=AF.Identity, scale=gm[:, kc:kc + 1],
                                                            bias=modv[:, 3, kc:kc + 1]), reads=[("h32", s), "modv"], writes=[("h32", s)])
                P.op("pe", lambda e, s=s, kc=kc, ps=ps: e.matmul(ps[:, 0:NEXP], lhsT=h32[s][:], rhs=wr[:, kc, :], start=(kc == 0), stop=(kc == DC - 1)),
                     reads=[("h32", s), "wr"], writes=[pk], signal=True)
            P.op("dve", lambda e, ps=ps: e.tensor_copy(out=lgt[:], in_=ps[:, 0:NEXP]), reads=[pk, R], writes=[R])
            P.op("dve", lambda e: e.max(out=mx8[:], in_=lgt[:]), reads=[R], writes=[R])
            P.op("dve", lambda e: e.tensor_scalar(out=nm1[:], in0=mx8[:, 0:1], scalar1=-1.0, scalar2=None, op0=ALU.mult), reads=[R], writes=[R])
            P.op("dve", lambda e: e.tensor_scalar(out=sel[:], in0=lgt[:], scalar1=mx8[:, 1:2], scalar2=None, op0=ALU.is_ge), reads=[R], writes=[R])
            P.op("act", lambda e: e.activation(out=ex[:], in_=lgt[:], func=AF.Exp, bias=nm1[:, 0:1]), reads=[R], writes=[R])
            P.op("dve", lambda e: e.tensor_tensor(out=ex[:], in0=ex[:], in1=sel[:], op=ALU.mult), reads=[R], writes=[R])
            P.op("dve", lambda e: e.tensor_reduce(out=ssum[:], in_=ex[:], axis=mybir.AxisListType.X, op=ALU.add), reads=[R], writes=[R])
            P.op("dve", lambda e: e.reciprocal(out=ssum[:], in_=ssum[:]), reads=[R], writes=[R])
            o = rwt[tt % 2]
            P.op("dve", lambda e, o=o: e.tensor_scalar(out=o[:], in0=ex[:], scalar1=ssum[:, 0:1], scalar2=None, op0=ALU.mult),
                 reads=[R], writes=[R, ("rwt", tt % 2)])
            cx.store(rwo[tt], o[:], [("rwt", tt % 2)], ("rwo", tt), sem=("st_rw", tt % 2))
    return cx.finish()


def post_inputs(i, xTs, ypreT, yattT, projT, mod_l, l, inp, moe):
    b, q = i // 4, i % 4
    ts_ = slice(q * T, (q + 1) * T)
    m = {"xT": xTs[i], "ypreT": fm(ypreT[b][:, ts_]), "yattT": fm(yattT[b][:, ts_]),
         "gsT": fm(projT[b, 5632:7680, ts_]), "gaT": fm(projT[b, 7680:9728, ts_]),
         "modv": modv_layout(mod_l[b]), "gnorm": vec_fm(inp["norm_ffn_g"][l]), "bglu": vec_fm(inp["b_glu"][l]),
         "w_glu": np.ascontiguousarray(inp["w_glu"][l]), "w_bs": np.ascontiguousarray(inp["w_branch_ssm"][l]),
         "w_ba": np.ascontiguousarray(inp["w_branch_att"][l]), "w_out": np.ascontiguousarray(inp["w_out"][l])}
    if moe:
        m["w_router"] = np.ascontiguousarray(inp["moe_router"][l // 2].reshape(DC, 128, NEXP).transpose(1, 0, 2))
    return m


def run_post(xTs, ypreT, yattT, projT, mod_l, l, inp, moe):
    nc = get_nc("post%d" % int(moe), lambda: build_post(moe))
    ins = [post_inputs(i, xTs, ypreT, yattT, projT, mod_l, l, inp, moe) for i in range(NCORES)]
    res = run(nc, ins)
    xo = [res[i]["xT_out"] for i in range(NCORES)]
    h2 = [res[i]["h2T"] for i in range(NCORES)]
    rw = [res[i]["rw"].reshape(T, NEXP) for i in range(NCORES)] if moe else None
    return xo, h2, rw


FG = 4


def ffn_bufs(cx, tag, with_rw):
    g16 = [cx.sb("%s_g16_%d" % (tag, i), [128, FG, T], BF16) for i in range(2)]
    sgl = [cx.sb("%s_sg%d" % (tag, i), [128, 512], F32) for i in range(2)]
    tt = [cx.sb("%s_tt%d" % (tag, i), [128, 512], F32) for i in range(2)] if with_rw else None
    return g16, sgl, tt


def emit_ffn(cx, bufs, hT, hkeys, nft, get_gu, get_d, out_evac, tag, rwb=None, rwkey=None):
    P = cx.P
    g16, sgl, tt = bufs
    it = 0
    ngroups = (nft + FG - 1) // FG
    for fg in range(ngroups):
        gb = g16[fg % 2]
        nj = min(FG, nft - fg * FG)
        for j in range(nj):
            ft = fg * FG + j
            gu = get_gu(ft)
            wg, wu, wkeys = gu[0], gu[1], gu[2]
            co = gu[3] if len(gu) > 3 else 0
            for half in range(T // 512):
                hs = slice(half * 512, (half + 1) * 512)
                psg, pkg = cx.psum()
                mm_group(cx, psg[:], pkg, [(wg[:, kc, co:co + 128], hT[:, kc, hs]) for kc in range(DC)], list(wkeys) + list(hkeys))
                psu, pku = cx.psum()
                mm_group(cx, psu[:], pku, [(wu[:, kc, co:co + 128], hT[:, kc, hs]) for kc in range(DC)], list(wkeys) + list(hkeys))
                s = it % 2
                it += 1
                P.op("act", lambda e, s=s, psg=psg: e.activation(out=sgl[s][:], in_=psg[:], func=AF.Silu), reads=[pkg], writes=[(tag, "sg", s)])
                if rwb is None:
                    P.op("dve", lambda e, s=s, psu=psu, gb=gb, j=j, hs=hs: e.tensor_tensor(out=gb[:, j, hs], in0=sgl[s][:], in1=psu[:], op=ALU.mult),
                         reads=[(tag, "sg", s), pku], writes=[(tag, "g16", fg % 2, j)])
                else:
                    P.op("dve", lambda e, s=s, psu=psu: e.tensor_tensor(out=tt[s][:], in0=sgl[s][:], in1=psu[:], op=ALU.mult),
                         reads=[(tag, "sg", s), pku], writes=[(tag, "tt", s)])
                    P.op("pool", lambda e, s=s, gb=gb, j=j, hs=hs: e.tensor_tensor(out=gb[:, j, hs], in0=tt[s][:], in1=rwb[:, hs], op=ALU.mult),
                         reads=[(tag, "tt", s), rwkey], writes=[(tag, "g16", fg % 2, j)])
        wd, dkeys = get_d(fg)
        for dc in range(DC):
            for half in range(T // 512):
                hs = slice(half * 512, (half + 1) * 512)
                ps, pk = cx.psum()
                mm_group(cx, ps[:], pk, [(wd[:, j, dc * 128:(dc + 1) * 128], gb[:, j, hs]) for j in range(nj)],
                         list(dkeys) + [(tag, "g16", fg % 2, j) for j in range(nj)])
                out_evac(fg, dc, half, ps, pk)


class DLoader:
    def __init__(self, cx, name):
        self.cx = cx
        self.name = name
        self.wd = cx.sb(name + "_wd", [128, FG, D], BF16)
        self.stg = [cx.sb("%s_st%d" % (name, i), [128, D], F32) for i in range(2)]
        self.si = 0

    def load(self, Wd, fg, nj):
        cx = self.cx
        keys = []
        for j in range(nj):
            ft = fg * FG + j
            s = self.si % 2
            self.si += 1
            stg = self.stg[s]
            skey = (self.name, "st", s)
            cx.P.dma("sp", lambda e, stg=stg, ft=ft: e.dma_start(out=stg[:], in_=Wd[ft * 128:(ft + 1) * 128, :]), writes=[skey])
            key = (self.name, "wd", j)
            cx.copy(cx.conv_eng(), self.wd[:, j, :], stg[:], [skey], [key])
            keys.append(key)
        return self.wd, keys


def build_ffn():
    cx = Ctx()
    P = cx.P
    xTd = cx.din("xT", [128, DC, T], F32)
    hTd = cx.din("h2T", [128, DC, T], BF16)
    gfd = cx.din("gatef", [128, DC], F32)
    Wg = cx.din("w_gate", [D, DFF], F32)
    Wu = cx.din("w_up", [D, DFF], F32)
    Wd = cx.din("w_down", [DFF, D], F32)
    xo = cx.dout("xT_out", [128, DC, T], F32)
    cx.alloc_psum(8)
    xT = cx.sb("xT_sb", [128, DC, T], F32)
    hT = cx.sb("hT_sb", [128, DC, T], BF16)
    gf = cx.sb("gf_sb", [128, DC], F32)
    for k0 in range(0, DC, 4):
        P.dma("sp", lambda e, k0=k0: e.dma_start(out=hT[:, k0:k0 + 4, :], in_=hTd[:, k0:k0 + 4, :]), writes=["hT"], sem="hT")
    for k0 in range(0, DC, 4):
        P.dma("sp", lambda e, k0=k0: e.dma_start(out=xT[:, k0:k0 + 4, :], in_=xTd[:, k0:k0 + 4, :]), writes=["xT"], sem="xT")
    cx.load(gf[:], gfd, "gf")
    wl = WLoader(cx, "wgu", DC, 256, nslots=4, nstage=4)
    dl = DLoader(cx, "wdl")
    nft = DFF // 128
    cache = {}

    dcache = {}

    def load_pair(base):
        if base not in cache and base < nft:
            wg, k1 = wl.load(Wg[:, base * 128:base * 128 + 256], 256)
            wu, k2 = wl.load(Wu[:, base * 128:base * 128 + 256], 256)
            cache[base] = (wg, wu, list(k1) + list(k2))

    def get_gu(ft):
        base = ft - ft % 2
        if ft % FG == 0 and (ft // FG) not in dcache:
            dcache[ft // FG] = dl.load(Wd, ft // FG, min(FG, nft - ft))
        load_pair(base)
        if ft % 2 == 0:
            load_pair(base + 2)
            for k in [k for k in cache if k < base]:
                del cache[k]
        wg, wu, keys = cache[base]
        return wg, wu, keys, (ft % 2) * 128

    def get_d(fg):
        return dcache[fg]

    def out_evac(fg, dc, half, ps, pk):
        hs = slice(half * 512, (half + 1) * 512)
        P.op("dve", lambda e: e.scalar_tensor_tensor(out=xT[:, dc, hs], in0=ps[:], scalar=gf[:, dc:dc + 1], in1=xT[:, dc, hs],
                                                    op0=ALU.mult, op1=ALU.add), reads=[pk, "gf", "xT"], writes=["xT"])

    emit_ffn(cx, ffn_bufs(cx, "ffn", False), hT, ["hT"], nft, get_gu, get_d, out_evac, "ffn")
    for k0 in range(0, DC, 4):
        cx.store(xo[:, k0:k0 + 4, :], xT[:, k0:k0 + 4, :], ["xT"], ("xo", k0), sem="st_x")
    return cx.finish()


NCHUNK = SEQ * 2 // T


def build_moe():
    cx = Ctx()
    P = cx.P
    nc = cx.nc
    hTd = cx.din("h2T", [NCHUNK, 128, DC, T], BF16)
    rwd = cx.din("rwb", [NCHUNK, 128, T], F32)
    Wg = cx.din("w_gate", [D, DFFE], F32)
    Wu = cx.din("w_up", [D, DFFE], F32)
    Wd = cx.din("w_down", [DFFE, D], F32)
    po = cx.dout("partial", [NCHUNK, 128, DC, T], BF16)
    Wg16 = nc.dram_tensor("Wg16", [D, DFFE], BF16, kind="Internal").ap()
    Wu16 = nc.dram_tensor("Wu16", [D, DFFE], BF16, kind="Internal").ap()
    Wd16 = nc.dram_tensor("Wd16", [DFFE, D], BF16, kind="Internal").ap()
    cx.alloc_psum(8)
    nft = DFFE // 128
    stg = [cx.sb("cv_st%d" % i, [128, 4, 512], F32) for i in range(3)]
    o16 = [cx.sb("cv_o%d" % i, [128, 4, 512], BF16) for i in range(3)]
    ci = 0
    for (Wsrc, Wdst, name, KC_, ncols) in ((Wg, Wg16, "Wg16", DC, DFFE), (Wu, Wu16, "Wu16", DC, DFFE), (Wd, Wd16, "Wd16", nft, D)):
        for kq in range(0, KC_, 4):
            for cb in range(ncols // 512):
                s = ci % 3
                ci += 1
                src = Wsrc[kq * 128:(kq + 4) * 128, cb * 512:(cb + 1) * 512].rearrange("(c p) n -> p c n", p=128)
                dst = Wdst[kq * 128:(kq + 4) * 128, cb * 512:(cb + 1) * 512].rearrange("(c p) n -> p c n", p=128)
                P.dma("sp", lambda e, s=s, src=src: e.dma_start(out=stg[s][:], in_=src), writes=[("cvst", s)])
                cx.copy(cx.conv_eng(), o16[s][:], stg[s][:], [("cvst", s)], [("cvo", s)])
                P.dma("pool", lambda e, s=s, dst=dst: e.dma_start(out=dst, in_=o16[s][:]), reads=[("cvo", s)], writes=[(name, kq // 4, cb)],
                      sem=("cvout", s))
    hT = cx.sb("hT_sb", [128, DC, T], BF16)
    rwb = cx.sb("rwb_sb", [128, T], F32)
    acc = cx.sb("acc", [128, DC, T], F32)
    wgt = [cx.sb("wg%d" % i, [128, DC, 128], BF16) for i in range(2)]
    wut = [cx.sb("wu%d" % i, [128, DC, 128], BF16) for i in range(2)]
    wdt = cx.sb("wdt", [128, FG, D], BF16)
    o16c = [cx.sb("oc%d" % i, [128, T], BF16) for i in range(2)]
    gi = [0]
    bufs = ffn_bufs(cx, "moe", True)
    for ch in range(NCHUNK):
        for k0 in range(0, DC, 4):
            P.dma("sp", lambda e, k0=k0, ch=ch: e.dma_start(out=hT[:, k0:k0 + 4, :], in_=hTd[ch, :, k0:k0 + 4, :]), writes=["hT"], sem="hT")
        cx.load(rwb[:], rwd[ch], "rwb")

        def get_gu(ft):
            s = gi[0] % 2
            gi[0] += 1
            dep = [("Wg16", kq, ft // 4) for kq in range(4)] + [("Wu16", kq, ft // 4) for kq in range(4)]
            srcg = Wg16[:, ft * 128:(ft + 1) * 128].rearrange("(c p) n -> p c n", p=128)
            srcu = Wu16[:, ft * 128:(ft + 1) * 128].rearrange("(c p) n -> p c n", p=128)
            P.dma("sp", lambda e, s=s, srcg=srcg: e.dma_start(out=wgt[s][:], in_=srcg), reads=dep, writes=[("wgt", s)])
            P.dma("sp", lambda e, s=s, srcu=srcu: e.dma_start(out=wut[s][:], in_=srcu), reads=dep, writes=[("wut", s)])
            return wgt[s], wut[s], [("wgt", s), ("wut", s)]

        def get_d(fg):
            keys = []
            for j in range(FG):
                ft = fg * FG + j
                dep = [("Wd16", ft // 4, cb) for cb in range(D // 512)]
                P.dma("sp", lambda e, j=j, ft=ft: e.dma_start(out=wdt[:, j, :], in_=Wd16[ft * 128:(ft + 1) * 128, :]), reads=dep, writes=[("wdt", j)])
                keys.append(("wdt", j))
            return wdt, keys

        def out_evac(fg, dc, half, ps, pk):
            hs = slice(half * 512, (half + 1) * 512)
            if fg == 0:
                P.op("dve", lambda e: e.tensor_copy(out=acc[:, dc, hs], in_=ps[:]), reads=[pk], writes=[("acc", dc, half)])
            else:
                P.op("dve", lambda e: e.tensor_tensor(out=acc[:, dc, hs], in0=ps[:], in1=acc[:, dc, hs], op=ALU.add),
                     reads=[pk, ("acc", dc, half)], writes=[("acc", dc, half)])

        emit_ffn(cx, bufs, hT, ["hT"], nft, get_gu, get_d, out_evac, "moe", rwb=rwb, rwkey="rwb")
        for dc in range(DC):
            s = dc % 2
            P.op("act", lambda e, s=s, dc=dc: e.activation(out=o16c[s][:], in_=acc[:, dc, :], func=AF.Identity),
                 reads=[("acc", dc, 0), ("acc", dc, 1)], writes=[("oc", s)])
            cx.store(po[ch, :, dc, :], o16c[s][:], [("oc", s)], ("po", ch, dc), sem=("st_oc", s))
    return cx.finish()


def build_final():
    cx = Ctx()
    P = cx.P
    xTd = cx.din("xT", [128, DC, T], F32)
    pd = cx.din("partials", [NEXP, 128, DC, T], BF16)
    gfd = cx.din("gatef", [128, DC], F32)
    gnd = cx.din("gfin", [128, DC], F32)
    out = cx.dout("outT", [128, DC, T], F32)
    cx.alloc_psum(4)
    xT = cx.sb("xT_sb", [128, DC, T], F32)
    gf = cx.sb("gf_sb", [128, DC], F32)
    gfin = cx.sb("gfin_sb", [128, DC], F32)
    zero = cx.sb("zero_sb", [128, DC], F32)
    ones16 = cx.sb("ones16", [128, 128], BF16)
    P.op("pool", lambda e: e.memset(ones16[:], 1.0), writes=["ones16"])
    P.op("pool", lambda e: e.memset(zero[:], 0.0), writes=["modv"])
    for k0 in range(0, DC, 4):
        P.dma("sp", lambda e, k0=k0: e.dma_start(out=xT[:, k0:k0 + 4, :], in_=xTd[:, k0:k0 + 4, :]), writes=["xT_in"], sem="xT")
    cx.load(gf[:], gfd, "gf")
    cx.load(gfin[:], gnd, "gfin")
    pb = [cx.sb("pb%d" % i, [128, NEXP, T], BF16) for i in range(2)]
    acc = [cx.sb("facc%d" % i, [128, T], F32) for i in range(2)]
    for kc in range(DC):
        s = kc % 2
        for e_ in range(NEXP):
            P.dma("sp", lambda e, s=s, e_=e_, kc=kc: e.dma_start(out=pb[s][:, e_, :], in_=pd[e_, :, kc, :]), writes=[("pb", s)], sem=("pb", s))
        P.op("dve", lambda e, s=s: e.tensor_tensor(out=acc[s][:], in0=pb[s][:, 0, :], in1=pb[s][:, 1, :], op=ALU.add), reads=[("pb", s)], writes=[("facc", s)])
        for e_ in range(2, NEXP):
            P.op("dve", lambda e, s=s, e_=e_: e.tensor_tensor(out=acc[s][:], in0=acc[s][:], in1=pb[s][:, e_, :], op=ALU.add),
                 reads=[("pb", s), ("facc", s)], writes=[("facc", s)])
        P.op("dve", lambda e, s=s, kc=kc: e.scalar_tensor_tensor(out=xT[:, kc, :], in0=acc[s][:], scalar=gf[:, kc:kc + 1], in1=xT[:, kc, :],
                                                              op0=ALU.mult, op1=ALU.add), reads=[("facc", s), "gf", "xT_in"], writes=["xT"])
    oT = cx.sb("oT_sb", [128, DC // 2, T], F32)
    P.op("dve", lambda e: e.tensor_copy(out=gfin[:], in_=gfin[:]), reads=["gfin"], writes=["modv"])
    rstd = emit_rmsnorm_stats(cx, xT, "xT", ones16, "nf")
    tmpf = [cx.sb("nf_t%d" % i, [128, T], F32) for i in range(2)]
    for kc in range(DC):
        s = kc % 2
        P.op("dve", lambda e, s=s, kc=kc: e.tensor_tensor(out=tmpf[s][:], in0=xT[:, kc, :], in1=rstd[:], op=ALU.mult),
             reads=["xT", "nf_rstd"], writes=[("nft", s)])
        P.op("act", lambda e, s=s, kc=kc: e.activation(out=tmpf[s][:], in_=tmpf[s][:], func=AF.Identity, scale=gfin[:, kc:kc + 1]),
             reads=[("nft", s), "modv"], writes=[("nft", s)])
        cx.store(out[:, kc, :], tmpf[s][:], [("nft", s)], ("out", kc), sem=("st_nft", s))
    return cx.finish()


def emit_rmsnorm_stats(cx, xT, xkey, ones16, tag):
    P = cx.P
    sq = [cx.sb("%s_sq%d" % (tag, i), [128, 512], BF16) for i in range(2)]
    rstd = cx.sb("%s_rstd" % tag, [128, T], F32)
    for half in range(T // 512):
        hs = slice(half * 512, (half + 1) * 512)
        ps, pk = cx.psum()
        for kc in range(DC):
            s = kc % 2
            P.op("act", lambda e, s=s, kc=kc, hs=hs: e.activation(out=sq[s][:], in_=xT[:, kc, hs], func=AF.Square),
                 reads=[xkey], writes=[(tag, "sq", s)])
            P.op("pe", lambda e, s=s, kc=kc, ps=ps: e.matmul(ps[:], lhsT=ones16[:], rhs=sq[s][:], start=(kc == 0), stop=(kc == DC - 1)),
                 reads=[(tag, "sq", s), "ones16"], writes=[pk], signal=True)
        rk = tag + "_rstd"
        P.op("dve", lambda e, ps=ps, hs=hs: e.tensor_scalar(out=rstd[:, hs], in0=ps[:], scalar1=1.0 / D, scalar2=EPS, op0=ALU.mult, op1=ALU.add),
             reads=[pk, rk], writes=[rk])
        P.op("act", lambda e, hs=hs: e.activation(out=rstd[:, hs], in_=rstd[:, hs], func=AF.Sqrt), reads=[rk], writes=[rk])
        P.op("dve", lambda e, hs=hs: e.reciprocal(out=rstd[:, hs], in_=rstd[:, hs]), reads=[rk], writes=[rk])
    return rstd


def run_ffn(xTs, h2s, mod_l, inp):
    nc = get_nc("ffn", build_ffn)
    wg = np.ascontiguousarray(inp["ffn_w_gate"][0]); wu = np.ascontiguousarray(inp["ffn_w_up"][0]); wd = np.ascontiguousarray(inp["ffn_w_down"][0])
    ins = []
    for i in range(NCORES):
        b = i // 4
        ins.append({"xT": xTs[i], "h2T": h2s[i], "gatef": np.ascontiguousarray(modv_layout(mod_l[b])[:, 5, :]),
                    "w_gate": wg, "w_up": wu, "w_down": wd})
    res = run(nc, ins)
    return [res[i]["xT_out"] for i in range(NCORES)]


def run_moe(h2s, rws, inp):
    nc = get_nc("moe", build_moe)
    h2all = np.ascontiguousarray(np.stack(h2s))
    rwall = np.stack(rws)
    ins = []
    for e in range(NCORES):
        rwb = np.ascontiguousarray(np.broadcast_to(rwall[:, None, :, e], (NCHUNK, 128, T)))
        ins.append({"h2T": h2all, "rwb": rwb, "w_gate": np.ascontiguousarray(inp["moe_w_gate"][0][e]),
                    "w_up": np.ascontiguousarray(inp["moe_w_up"][0][e]), "w_down": np.ascontiguousarray(inp["moe_w_down"][0][e])})
    res = run(nc, ins)
    return [res[e]["partial"] for e in range(NCORES)]


def run_final(xTs, partials, mod_l, gfin):
    nc = get_nc("final", build_final)
    ins = []
    for i in range(NCORES):
        b = i // 4
        ins.append({"xT": xTs[i], "partials": np.ascontiguousarray(np.stack([partials[e][i] for e in range(NEXP)])),
                    "gatef": np.ascontiguousarray(modv_layout(mod_l[b])[:, 5, :]), "gfin": vec_fm(gfin)})
    res = run(nc, ins)
    out = np.zeros((2, SEQ, D), np.float32)
    for i in range(NCORES):
        b, q = i // 4, i % 4
        out[b, q * T:(q + 1) * T, :] = res[i]["outT"].transpose(1, 0, 2).reshape(D, T).T
    return out


def kernel(**inputs):
    inp = {k: np.asarray(v) for k, v in inputs.items()}
    mod = run_mod(inp["c"], inp["w_mod"], inp["b_mod"])
    xTs = x_to_cores(inp["x"])
    out = None
    for l in range(2):
        projT = run_inproj(xTs, mod[l], inp["norm_mix_g"][l], np.ascontiguousarray(inp["w_in"][l]))
        ypreT = run_ssm(projT, l, inp)
        yattT = run_attn(projT, inp["rel_bias"])
        moe = (l % 2 == 1)
        xTs, h2s, rws = run_post(xTs, ypreT, yattT, projT, mod[l], l, inp, moe)
        if not moe:
            xTs = run_ffn(xTs, h2s, mod[l], inp)
        else:
            hcs, posms, overflow = run_route(h2s, rws)
            if not overflow:
                ys = run_moe3(hcs, inp)
                out = run_final3(xTs, ys, posms, rws, mod[l], inp["final_norm_g"])
            else:
                partials = run_moe(h2s, rws, inp)
                out = run_final(xTs, partials, mod[l], inp["final_norm_g"])
    return out


CAP = 4096
NTOK = 8192
NTT = NTOK // 128


def build_moe2():
    cx = Ctx()
    P = cx.P
    nc = cx.nc
    h2d = cx.din("h2tm", [NTOK, D], BF16)
    rwd = cx.din("rwc", [128, NTT], F32)
    Ld = cx.din("Ltri", [128, 128], F32)
    idd = cx.din("ident", [128, 128], F32)
    Wg = cx.din("w_gate", [D, DFFE], F32)
    Wu = cx.din("w_up", [D, DFFE], F32)
    Wd = cx.din("w_down", [DFFE, D], F32)
    po = cx.dout("partial", [NTOK, D], BF16)
    Wg16 = nc.dram_tensor("Wg16", [D, DFFE], BF16, kind="Internal").ap()
    Wu16 = nc.dram_tensor("Wu16", [D, DFFE], BF16, kind="Internal").ap()
    Wd16 = nc.dram_tensor("Wd16", [DFFE, D], BF16, kind="Internal").ap()
    Xc = nc.dram_tensor("Xc", [CAP, D], BF16, kind="Internal").ap()
    Yc = nc.dram_tensor("Yc", [CAP, D], F32, kind="Internal").ap()
    for i in range(6):
        cx.ps.append(cx.st.enter_context(nc.psum_tensor("ps%d" % i, [128, 512], F32)))
    psb = [cx.st.enter_context(nc.psum_tensor("psb%d" % i, [128, 1024], BF16)) for i in range(2)]
    nft = DFFE // 128
    rwc = cx.sb("rwc_sb", [128, NTT], F32)
    posi = cx.sb("posi", [128, NTT], I32)
    identb = cx.sb("identb", [128, 128], BF16)
    cx.load(rwc[:], rwd, "rwc")

    cx.phase_begin()
    stg = [cx.sb("cv_st%d" % i, [128, 4, 512], F32) for i in range(3)]
    o16 = [cx.sb("cv_o%d" % i, [128, 4, 512], BF16) for i in range(3)]
    Ltri = cx.sb("Ltri_sb", [128, 128], F32)
    identf = cx.sb("identf", [128, 128], F32)
    ones32 = cx.sb("ones32", [128, 128], F32)
    onesr = cx.sb("onesr", [128, NTT], F32)
    m = cx.sb("m_sb", [128, NTT], F32)
    S = cx.sb("S_sb", [128, NTT], F32)
    incl = cx.sb("incl", [128, NTT], F32)
    posf = cx.sb("posf", [128, NTT], F32)
    z16 = cx.sb("z16", [128, D], BF16)
    hrow = [cx.sb("hrow%d" % i, [128, D], BF16) for i in range(4)]
    cx.load(Ltri[:], Ld, "Ltri")
    cx.load(identf[:], idd, "identf")
    P.op("dve", lambda e: e.tensor_copy(out=identb[:], in_=identf[:]), reads=["identf"], writes=["identb"])
    P.op("pool", lambda e: e.memset(ones32[:], 1.0), writes=["ones32"])
    P.op("pool", lambda e: e.memset(onesr[:], 1.0), writes=["onesr"])
    P.op("pool", lambda e: e.memset(z16[:], 0.0), writes=["z16"])
    C = "cmp"
    P.op("dve", lambda e: e.tensor_scalar(out=m[:], in0=rwc[:], scalar1=0.0, scalar2=None, op0=ALU.is_gt), reads=["rwc"], writes=[C])
    ps, pk = cx.psum()
    P.op("pe", lambda e: e.matmul(ps[:, 0:NTT], lhsT=Ltri[:], rhs=m[:], start=True, stop=True), reads=[C, "Ltri"], writes=[pk])
    P.op("pe", lambda e: e.matmul(ps[:, NTT:2 * NTT], lhsT=ones32[:], rhs=m[:], start=True, stop=True), reads=[C, "ones32"], writes=[pk])
    P.op("dve", lambda e: e.tensor_copy(out=S[:], in_=ps[:, NTT:2 * NTT]), reads=[pk, C], writes=[C])
    P.op("dve", lambda e: e.tensor_tensor_scan(out=incl[:], data0=onesr[:], data1=S[:], initial=0.0, op0=ALU.mult, op1=ALU.add),
         reads=[C, "onesr"], writes=[C])
    P.op("dve", lambda e: e.tensor_tensor(out=incl[:], in0=incl[:], in1=S[:], op=ALU.subtract), reads=[C], writes=[C])
    P.op("dve", lambda e: e.tensor_tensor(out=posf[:], in0=ps[:, 0:NTT], in1=incl[:], op=ALU.add), reads=[pk, C], writes=[C])
    P.op("dve", lambda e: e.tensor_scalar(out=m[:], in0=m[:], scalar1=-1.0e6, scalar2=1.0e6, op0=ALU.mult, op1=ALU.add), reads=[C], writes=[C])
    P.op("dve", lambda e: e.tensor_tensor(out=posf[:], in0=posf[:], in1=m[:], op=ALU.add), reads=[C], writes=[C])
    P.op("dve", lambda e: e.tensor_copy(out=posi[:], in_=posf[:]), reads=[C], writes=["posi"])
    for r in range(CAP // 128):
        P.dma("sp", lambda e, r=r: e.dma_start(out=Xc[r * 128:(r + 1) * 128, :], in_=z16[:]), reads=["z16"], writes=[("Xcz", r)], sem="xcz")
    xcz = [("Xcz", r) for r in range(CAP // 128)]
    for j in range(NTT):
        s = j % 4
        cx.load(hrow[s][:], h2d[j * 128:(j + 1) * 128, :], ("hrow", s))
        P.dma("pool", lambda e, j=j, s=s: e.indirect_dma_start(
            out=Xc[:, :], out_offset=bass.IndirectOffsetOnAxis(ap=posi[:, j:j + 1], axis=0), in_=hrow[s][:, :], in_offset=None,
            bounds_check=CAP - 1, oob_is_err=False), reads=[("hrow", s), "posi"] + (xcz if j < 4 else []), writes=[("Xcs", j)], sem=("sc", s))
    xcs = [("Xcs", j) for j in range(NTT)]
    ci = 0
    for (Wsrc, Wdst, name, KC_, ncols) in ((Wg, Wg16, "Wg16", DC, DFFE), (Wu, Wu16, "Wu16", DC, DFFE), (Wd, Wd16, "Wd16", nft, D)):
        for kq in range(0, KC_, 4):
            for cb in range(ncols // 512):
                s = ci % 3
                ci += 1
                src = Wsrc[kq * 128:(kq + 4) * 128, cb * 512:(cb + 1) * 512].rearrange("(c p) n -> p c n", p=128)
                dst = Wdst[kq * 128:(kq + 4) * 128, cb * 512:(cb + 1) * 512].rearrange("(c p) n -> p c n", p=128)
                P.dma("sp", lambda e, s=s, src=src: e.dma_start(out=stg[s][:], in_=src), writes=[("cvst", s)])
                cx.copy(cx.conv_eng(), o16[s][:], stg[s][:], [("cvst", s)], [("cvo", s)])
                P.dma("act", lambda e, s=s, dst=dst: e.dma_start(out=dst, in_=o16[s][:]), reads=[("cvo", s)], writes=[(name, kq // 4, cb)],
                      sem=("cvout", s))
    cx.phase_end()

    cx.phase_begin()
    hT = cx.sb("hT_sb", [128, DC, T], BF16)
    accR = cx.sb("accR", [128, T // 128, D], F32)
    wgt = [cx.sb("wg%d" % i, [128, DC, 128], BF16) for i in range(2)]
    wut = [cx.sb("wu%d" % i, [128, DC, 128], BF16) for i in range(2)]
    wdt = cx.sb("wdt", [128, FG, D], BF16)
    xrow = [cx.sb("xrow%d" % i, [128, D], BF16) for i in range(2)]
    g16 = [cx.sb("g16_%d" % i, [128, FG, T], BF16) for i in range(2)]
    sgl = [cx.sb("sg%d" % i, [128, 512], F32) for i in range(2)]
    gi = 0
    it = 0
    ti = 0
    for ch in range(CAP // T):
        for rt in range(T // 128):
            s = (ch * 8 + rt) % 2
            row0 = ch * T + rt * 128
            cx.P.dma("sp", lambda e, s=s, row0=row0: e.dma_start(out=xrow[s][:], in_=Xc[row0:row0 + 128, :]),
                     reads=(xcs + xcz) if (ch == 0 and rt < 2) else [], writes=[("xrow", s)])
            for kq in range(DC // 4):
                pb = psb[ti % 2]
                pbk = ("psb", ti % 2)
                ti += 1
                for q in range(4):
                    kc = kq * 4 + q
                    P.op("pe", lambda e, pb=pb, q=q, kc=kc, s=s: e.transpose(pb[:, q * 128:(q + 1) * 128], xrow[s][:, kc * 128:(kc + 1) * 128], identb[:]),
                         reads=[("xrow", s), "identb"], writes=[pbk], signal=(q == 3))
                eng = "act" if (ti % 2 == 0) else "dve"
                dst = hT[:, kq * 4:(kq + 1) * 4, rt * 128:(rt + 1) * 128]
                srcv = pb[:, 0:512].rearrange("p (k n) -> p k n", k=4)
                cx.copy(eng, dst, srcv, [pbk], ["hT"])
        for fg in range(nft // FG):
            gb = g16[fg % 2]
            for j in range(FG):
                ft = fg * FG + j
                s = gi % 2
                gi += 1
                dep = [("Wg16", kq, ft // 4) for kq in range(4)] + [("Wu16", kq, ft // 4) for kq in range(4)]
                srcg = Wg16[:, ft * 128:(ft + 1) * 128].rearrange("(c p) n -> p c n", p=128)
                srcu = Wu16[:, ft * 128:(ft + 1) * 128].rearrange("(c p) n -> p c n", p=128)
                P.dma("sp", lambda e, s=s, srcg=srcg: e.dma_start(out=wgt[s][:], in_=srcg), reads=dep if ch == 0 else [], writes=[("wgt", s)])
                P.dma("sp", lambda e, s=s, srcu=srcu: e.dma_start(out=wut[s][:], in_=srcu), reads=dep if ch == 0 else [], writes=[("wut", s)])
                for half in range(T // 512):
                    hs = slice(half * 512, (half + 1) * 512)
                    psg, pkg = cx.psum()
                    mm_group(cx, psg[:], pkg, [(wgt[s][:, kc, :], hT[:, kc, hs]) for kc in range(DC)], [("wgt", s), "hT"])
                    psu, pku = cx.psum()
                    mm_group(cx, psu[:], pku, [(wut[s][:, kc, :], hT[:, kc, hs]) for kc in range(DC)], [("wut", s), "hT"])
                    s2 = it % 2
                    it += 1
                    P.op("act", lambda e, s2=s2, psg=psg: e.activation(out=sgl[s2][:], in_=psg[:], func=AF.Silu), reads=[pkg], writes=[("sg", s2)])
                    P.op("dve", lambda e, s2=s2, psu=psu, gb=gb, j=j, hs=hs: e.tensor_tensor(out=gb[:, j, hs], in0=sgl[s2][:], in1=psu[:], op=ALU.mult),
                         reads=[("sg", s2), pku], writes=[("g16", fg % 2, j)])
            for j in range(FG):
                ft = fg * FG + j
                dep = [("Wd16", ft // 4, cb) for cb in range(D // 512)]
                P.dma("sp", lambda e, j=j, ft=ft: e.dma_start(out=wdt[:, j, :], in_=Wd16[ft * 128:(ft + 1) * 128, :]),
                      reads=dep if ch == 0 else [], writes=[("wdt", j)])
            di = 0
            for rt in range(T // 128):
                for db in range(D // 512):
                    ps, pk = cx.psum()
                    mm_group(cx, ps[:], pk, [(gb[:, j, rt * 128:(rt + 1) * 128], wdt[:, j, db * 512:(db + 1) * 512]) for j in range(FG)],
                             [("wdt", j) for j in range(FG)] + [("g16", fg % 2, j) for j in range(FG)])
                    dsl = accR[:, rt, db * 512:(db + 1) * 512]
                    eng = "dve" if (di % 4 != 3) else "pool"
                    di += 1
                    if fg == 0:
                        P.op("dve", lambda e, dsl=dsl, ps=ps: e.tensor_copy(out=dsl, in_=ps[:]), reads=[pk], writes=[("accR", rt, db)])
                    else:
                        P.op("dve", lambda e, dsl=dsl, ps=ps: e.tensor_tensor(out=dsl, in0=ps[:], in1=dsl, op=ALU.add),
                             reads=[pk, ("accR", rt, db)], writes=[("accR", rt, db)])
        for rt in range(T // 128):
            row0 = ch * T + rt * 128
            P.dma("sp", lambda e, rt=rt, row0=row0: e.dma_start(out=Yc[row0:row0 + 128, :], in_=accR[:, rt, :]),
                  reads=[("accR", rt, db) for db in range(D // 512)], writes=[("Yc", ch, rt)], sem=("st_acc", rt % 2))
    ycs = [("Yc", ch, rt) for ch in range(CAP // T) for rt in range(T // 128)]
    cx.phase_end()

    cx.phase_begin()
    zt = [cx.sb("zt%d" % i, [128, D], F32) for i in range(3)]
    ot = [cx.sb("ot%d" % i, [128, D], BF16) for i in range(3)]
    for i in range(3):
        P.op("pool", lambda e, i=i: e.memset(zt[i][:], 0.0), writes=[("zt", i)])
    for j in range(NTT):
        s = j % 3
        P.dma("pool", lambda e, j=j, s=s: e.indirect_dma_start(
            out=zt[s][:, :], out_offset=None, in_=Yc[:, :], in_offset=bass.IndirectOffsetOnAxis(ap=posi[:, j:j + 1], axis=0),
            bounds_check=CAP - 1, oob_is_err=False), reads=["posi", ("zt", s)], writes=[("zt", s)], sem=("ga", s))
        if j % 2 == 0:
            P.op("dve", lambda e, j=j, s=s: e.tensor_scalar(out=ot[s][:], in0=zt[s][:], scalar1=rwc[:, j:j + 1], scalar2=None, op0=ALU.mult),
                 reads=[("zt", s), "rwc"], writes=[("ot", s)])
        else:
            P.op("act", lambda e, j=j, s=s: e.activation(out=ot[s][:], in_=zt[s][:], func=AF.Identity, scale=rwc[:, j:j + 1]),
                 reads=[("zt", s), "rwc"], writes=[("ot", s)])
        cx.store(po[j * 128:(j + 1) * 128, :], ot[s][:], [("ot", s)], ("po", j), sem=("st_ot", s))
    cx.phase_end()
    return cx.finish()


def build_final2():
    cx = Ctx()
    P = cx.P
    NT_ = T // 128
    xd = cx.din("x", [T, D], F32)
    pd = cx.din("partials", [NEXP, T, D], BF16)
    gfd = cx.din("gatef", [128, D], F32)
    gnd = cx.din("gfin", [128, D], F32)
    out = cx.dout("out", [T, D], F32)
    gf = cx.sb("gf_sb", [128, D], F32)
    gfin = cx.sb("gfin_sb", [128, D], F32)
    cx.load(gf[:], gfd, "gf")
    cx.load(gfin[:], gnd, "gfin")
    xt = [cx.sb("xt%d" % i, [128, D], F32) for i in range(2)]
    pb = [cx.sb("pb%d" % i, [128, NEXP, D], BF16) for i in range(2)]
    acc = [cx.sb("acc%d" % i, [128, D], F32) for i in range(2)]
    sqj = cx.sb("sqj", [128, D], F32)
    ss = [cx.sb("ss%d" % i, [128, 1], F32) for i in range(2)]
    for tt in range(NT_):
        s = tt % 2
        rows = slice(tt * 128, (tt + 1) * 128)
        cx.load(xt[s][:], xd[rows, :], ("xt", s))
        for e_ in range(NEXP):
            P.dma("sp", lambda e, s=s, e_=e_, rows=rows: e.dma_start(out=pb[s][:, e_, :], in_=pd[e_, rows, :]), writes=[("pb", s, e_)], sem=("pb", s))
        pbk = [("pb", s, e_) for e_ in range(NEXP)]
        P.op("dve", lambda e, s=s: e.tensor_tensor(out=acc[s][:], in0=pb[s][:, 0, :], in1=pb[s][:, 1, :], op=ALU.add), reads=pbk, writes=[("acc", s)])
        for e_ in range(2, NEXP):
            eng = "dve" if e_ % 2 == 0 else "pool"
            P.op(eng, lambda e, s=s, e_=e_: e.tensor_tensor(out=acc[s][:], in0=acc[s][:], in1=pb[s][:, e_, :], op=ALU.add),
                 reads=pbk + [("acc", s)], writes=[("acc", s)])
        P.op("dve", lambda e, s=s: e.tensor_tensor(out=acc[s][:], in0=acc[s][:], in1=gf[:], op=ALU.mult), reads=[("acc", s), "gf"], writes=[("acc", s)])
        P.op("dve", lambda e, s=s: e.tensor_tensor(out=xt[s][:], in0=xt[s][:], in1=acc[s][:], op=ALU.add), reads=[("acc", s), ("xt", s)], writes=[("xt", s)])
        P.op("act", lambda e, s=s: e.activation(out=sqj[:], in_=xt[s][:], func=AF.Square, accum_out=ss[s][:]), reads=[("xt", s)], writes=["sqj", ("ss", s)])
        P.op("dve", lambda e, s=s: e.tensor_scalar(out=ss[s][:], in0=ss[s][:], scalar1=1.0 / D, scalar2=EPS, op0=ALU.mult, op1=ALU.add),
             reads=[("ss", s)], writes=[("ss", s)])
        P.op("act", lambda e, s=s: e.activation(out=ss[s][:], in_=ss[s][:], func=AF.Sqrt), reads=[("ss", s)], writes=[("ss", s)])
        P.op("dve", lambda e, s=s: e.reciprocal(out=ss[s][:], in_=ss[s][:]), reads=[("ss", s)], writes=[("ss", s)])
        P.op("dve", lambda e, s=s: e.scalar_tensor_tensor(out=acc[s][:], in0=xt[s][:], scalar=ss[s][:, 0:1], in1=gfin[:], op0=ALU.mult, op1=ALU.mult),
             reads=[("xt", s), ("ss", s), "gfin", ("acc", s)], writes=[("acc", s)])
        cx.store(out[rows, :], acc[s][:], [("acc", s)], ("out", tt), sem=("st_acc", s))
    return cx.finish()


def run_moe2(h2s, rws, inp):
    nc = get_nc("moe2", build_moe2)
    h2tm = np.ascontiguousarray(np.concatenate([h.transpose(1, 0, 2).reshape(D, T).T for h in h2s], axis=0))
    rwall = np.concatenate(rws, axis=0)
    kk = np.arange(128)
    Ltri = (kk[:, None] < kk[None, :]).astype(np.float32)
    ins = []
    for e in range(NCORES):
        ins.append({"h2tm": h2tm, "rwc": np.ascontiguousarray(rwall[:, e].reshape(NTT, 128).T), "Ltri": Ltri,
                    "ident": np.eye(128, dtype=np.float32),
                    "w_gate": np.ascontiguousarray(inp["moe_w_gate"][0][e]), "w_up": np.ascontiguousarray(inp["moe_w_up"][0][e]),
                    "w_down": np.ascontiguousarray(inp["moe_w_down"][0][e])})
    res = run(nc, ins)
    return [res[e]["partial"] for e in range(NCORES)]


def run_final2(xTs, partials, mod_l, gfin):
    nc = get_nc("final2", build_final2)
    ins = []
    for i in range(NCORES):
        b = i // 4
        gatef = mod_l[b].reshape(6, D)[5]
        ins.append({"x": np.ascontiguousarray(xTs[i].transpose(1, 0, 2).reshape(D, T).T),
                    "partials": np.ascontiguousarray(np.stack([partials[e][i * T:(i + 1) * T] for e in range(NEXP)])),
                    "gatef": np.ascontiguousarray(np.broadcast_to(gatef[None, :], (128, D))),
                    "gfin": np.ascontiguousarray(np.broadcast_to(gfin[None, :], (128, D)))})
    res = run(nc, ins)
    out = np.zeros((2, SEQ, D), np.float32)
    for i in range(NCORES):
        b, q = i // 4, i % 4
        out[b, q * T:(q + 1) * T, :] = res[i]["out"]
    return out


SEG = 512
NR = SEG // 128


def build_route():
    cx = Ctx()
    P = cx.P
    nc = cx.nc
    NT_ = T // 128
    hTd = cx.din("h2T", [128, DC, T], BF16)
    rwd = cx.din("rw", [128, NT_, NEXP], F32)
    Ld = cx.din("Ltri", [128, 128], F32)
    idd = cx.din("ident", [128, 128], F32)
    iod = cx.din("iota", [128, 128], F32)
    hco = cx.dout("hc", [NEXP, 128, DC, SEG], BF16)
    pso = cx.dout("posm", [128, NT_, NEXP], F32)
    ovo = cx.dout("ovf", [128, 1], F32)
    for i in range(6):
        cx.ps.append(cx.st.enter_context(nc.psum_tensor("ps%d" % i, [128, 512], F32)))
    psb = [cx.st.enter_context(nc.psum_tensor("psb%d" % i, [128, 1024], BF16)) for i in range(2)]
    hT = cx.sb("hT_sb", [128, DC, T], BF16)
    htm = cx.sb("htm", [128, NT_, D], BF16)
    ovf = cx.sb("ovf_sb", [128, 1], F32)
    rw = cx.sb("rw_sb", [128, NT_, NEXP], F32)
    Ltri = cx.sb("Ltri_sb", [128, 128], F32)
    identf = cx.sb("identf", [128, 128], F32)
    identb = cx.sb("identb", [128, 128], BF16)
    iota = cx.sb("iota_sb", [128, 128], F32)
    ones32 = cx.sb("ones32", [128, 128], F32)
    for k0 in range(0, DC, 4):
        P.dma("sp", lambda e, k0=k0: e.dma_start(out=hT[:, k0:k0 + 4, :], in_=hTd[:, k0:k0 + 4, :]), writes=["hT"], sem="hT")
    cx.load(rw[:], rwd, "rw")
    cx.load(Ltri[:], Ld, "Ltri")
    cx.load(identf[:], idd, "identf")
    cx.load(iota[:], iod, "iota")
    P.op("dve", lambda e: e.tensor_copy(out=identb[:], in_=identf[:]), reads=["identf"], writes=["identb"])
    P.op("pool", lambda e: e.memset(ones32[:], 1.0), writes=["ones32"])
    ti = 0
    for tt in range(NT_):
        for kq in range(DC // 4):
            pb = psb[ti % 2]
            pbk = ("psb", ti % 2)
            ti += 1
            for q in range(4):
                kc = kq * 4 + q
                P.op("pe", lambda e, pb=pb, q=q, kc=kc, tt=tt: e.transpose(pb[:, q * 128:(q + 1) * 128], hT[:, kc, tt * 128:(tt + 1) * 128], identb[:]),
                     reads=["hT", "identb"], writes=[pbk], signal=(q == 3))
            cx.copy("act" if ti % 2 else "dve", htm[:, tt, kq * 512:(kq + 1) * 512], pb[:, 0:512], [pbk], [("htm", tt)])
    C = "cmp"
    NF = NT_ * NEXP
    m = cx.sb("m_sb", [128, NT_, NEXP], F32)
    S = cx.sb("S_sb", [128, NT_, NEXP], F32)
    offs = cx.sb("offs", [128, NT_, NEXP], F32)
    pos = cx.sb("pos", [128, NT_, NEXP], F32)
    posr = cx.sb("posr", [128, NR, NT_, NEXP], F32)
    mf = m[:].rearrange("p t e -> p (t e)")
    P.op("dve", lambda e: e.tensor_scalar(out=m[:], in0=rw[:], scalar1=0.0, scalar2=None, op0=ALU.is_gt), reads=["rw"], writes=[C])
    ps, pk = cx.psum()
    P.op("pe", lambda e: e.matmul(ps[:, 0:NF], lhsT=Ltri[:], rhs=mf, start=True, stop=True), reads=[C, "Ltri"], writes=[pk])
    P.op("pe", lambda e: e.matmul(ps[:, NF:2 * NF], lhsT=ones32[:], rhs=mf, start=True, stop=True), reads=[C, "ones32"], writes=[pk])
    P.op("dve", lambda e: e.tensor_copy(out=S[:].rearrange("p t e -> p (t e)"), in_=ps[:, NF:2 * NF]), reads=[pk, C], writes=[C])
    P.op("pool", lambda e: e.memset(offs[:, 0, :], 0.0), reads=[C], writes=[C])
    for tt in range(1, NT_):
        P.op("dve", lambda e, tt=tt: e.tensor_tensor(out=offs[:, tt, :], in0=offs[:, tt - 1, :], in1=S[:, tt - 1, :], op=ALU.add), reads=[C], writes=[C])
    P.op("dve", lambda e: e.tensor_tensor(out=pos[:].rearrange("p t e -> p (t e)"), in0=ps[:, 0:NF], in1=offs[:].rearrange("p t e -> p (t e)"), op=ALU.add),
         reads=[pk, C], writes=[C])
    P.op("dve", lambda e: e.tensor_scalar(out=pos[:], in0=pos[:], scalar1=1.0e6, scalar2=None, op0=ALU.add), reads=[C], writes=[C])
    P.op("dve", lambda e: e.tensor_tensor(out=pos[:], in0=pos[:], in1=m[:], op=ALU.mult), reads=[C], writes=[C])
    P.op("dve", lambda e: e.tensor_scalar(out=pos[:], in0=pos[:], scalar1=-1.0e6, scalar2=None, op0=ALU.add), reads=[C], writes=[C])
    cx.store(pso, pos[:], [C], "pso")
    P.op("dve", lambda e: e.tensor_reduce(out=ovf[:], in_=pos[:].rearrange("p t e -> p (t e)"), axis=mybir.AxisListType.X, op=ALU.max), reads=[C], writes=["ovf"])
    P.op("dve", lambda e: e.tensor_scalar(out=ovf[:], in0=ovf[:], scalar1=float(SEG), scalar2=None, op0=ALU.is_ge), reads=["ovf"], writes=["ovf"])
    cx.store(ovo, ovf[:], ["ovf"], "ovo")
    for r in range(NR):
        P.op("dve", lambda e, r=r: e.tensor_scalar(out=posr[:, r, :, :], in0=pos[:], scalar1=-128.0 * r, scalar2=None, op0=ALU.add), reads=[C], writes=[C])
    Pm = [cx.sb("Pm%d" % i, [128, NT_, NR, 128], BF16) for i in range(2)]
    hcb = [cx.sb("hcb%d" % i, [128, DC, SEG], BF16) for i in range(2)]
    htk = [("htm", tt) for tt in range(NT_)]
    for ex in range(NEXP):
        s = ex % 2
        for tt in range(NT_):
            for r in range(min(tt, NR - 1) + 1):
                eng = "dve" if (tt + r) % 2 == 0 else "pool"
                P.op(eng, lambda e, s=s, tt=tt, r=r, ex=ex: e.tensor_scalar(out=Pm[s][:, tt, r, :], in0=iota[:], scalar1=posr[:, r, tt, ex:ex + 1],
                                                                        scalar2=None, op0=ALU.is_equal), reads=[C, "iota"], writes=[("Pm", s)])
        for r in range(NR):
            for kq in range(DC // 4):
                ps, pk = cx.psum()
                for q in range(4):
                    for tt in range(r, NT_):
                        kc = kq * 4 + q
                        P.op("pe", lambda e, ps=ps, q=q, kc=kc, tt=tt, r=r, s=s: e.matmul(
                            ps[:, q * 128:(q + 1) * 128], lhsT=htm[:, tt, kc * 128:(kc + 1) * 128], rhs=Pm[s][:, tt, r, :],
                            start=(tt == r), stop=(tt == NT_ - 1)), reads=htk + [("Pm", s)], writes=[pk], signal=(tt == NT_ - 1 and q == 3))
                cx.copy("act" if (kq % 2) else "dve", hcb[s][:, kq * 4:(kq + 1) * 4, r * 128:(r + 1) * 128],
                        ps[:].rearrange("p (k n) -> p k n", k=4), [pk], [("hcb", s)])
        cx.store(hco[ex].rearrange("p k n -> p (k n)"), hcb[s][:].rearrange("p k n -> p (k n)"), [("hcb", s)], ("hco", ex), sem=("st_hcb", s))
    return cx.finish()


NCH3 = CAP // T


def build_moe3():
    cx = Ctx()
    P = cx.P
    nc = cx.nc
    hcd = cx.din("hc", [NCH3, 128, DC, T], BF16)
    Wg = cx.din("w_gate", [D, DFFE], F32)
    Wu = cx.din("w_up", [D, DFFE], F32)
    Wd = cx.din("w_down", [DFFE, D], F32)
    yo = cx.dout("y", [CAP, D], BF16)
    Wg16 = nc.dram_tensor("Wg16", [D, DFFE], BF16, kind="Internal").ap()
    Wu16 = nc.dram_tensor("Wu16", [D, DFFE], BF16, kind="Internal").ap()
    Wd16 = nc.dram_tensor("Wd16", [DFFE, D], BF16, kind="Internal").ap()
    cx.alloc_psum(8)
    nft = DFFE // 128
    cx.phase_begin()
    stg = [cx.sb("cv_st%d" % i, [128, 4, 512], F32) for i in range(3)]
    o16 = [cx.sb("cv_o%d" % i, [128, 4, 512], BF16) for i in range(3)]
    ci = 0
    for (Wsrc, Wdst, name, KC_, ncols) in ((Wg, Wg16, "Wg16", DC, DFFE), (Wu, Wu16, "Wu16", DC, DFFE), (Wd, Wd16, "Wd16", nft, D)):
        for kq in range(0, KC_, 4):
            for cb in range(ncols // 512):
                s = ci % 3
                ci += 1
                src = Wsrc[kq * 128:(kq + 4) * 128, cb * 512:(cb + 1) * 512].rearrange("(c p) n -> p c n", p=128)
                dst = Wdst[kq * 128:(kq + 4) * 128, cb * 512:(cb + 1) * 512].rearrange("(c p) n -> p c n", p=128)
                P.dma("sp", lambda e, s=s, src=src: e.dma_start(out=stg[s][:], in_=src), writes=[("cvst", s)])
                cx.copy(cx.conv_eng(), o16[s][:], stg[s][:], [("cvst", s)], [("cvo", s)])
                P.dma("pool", lambda e, s=s, dst=dst: e.dma_start(out=dst, in_=o16[s][:]), reads=[("cvo", s)], writes=[(name, kq // 4, cb)],
                      sem=("cvout", s))
    cx.phase_end()
    cx.phase_begin()
    hT = cx.sb("hT_sb", [128, DC, T], BF16)
    accR = cx.sb("accR", [128, T // 128, D], F32)
    wgt = [cx.sb("wg%d" % i, [128, DC, 128], BF16) for i in range(2)]
    wut = [cx.sb("wu%d" % i, [128, DC, 128], BF16) for i in range(2)]
    wdt = cx.sb("wdt", [128, FG, D], BF16)
    g16 = [cx.sb("g16_%d" % i, [128, FG, T], BF16) for i in range(2)]
    sgl = [cx.sb("sg%d" % i, [128, 512], F32) for i in range(2)]
    orow = [cx.sb("orow%d" % i, [128, D], BF16) for i in range(2)]
    gi = 0
    it = 0
    for ch in range(NCH3):
        for k0 in range(0, DC, 4):
            P.dma("sp", lambda e, k0=k0, ch=ch: e.dma_start(out=hT[:, k0:k0 + 4, :], in_=hcd[ch, :, k0:k0 + 4, :]), writes=["hT"], sem="hT")
        for fg in range(nft // FG):
            gb = g16[fg % 2]
            for j in range(FG):
                ft = fg * FG + j
                s = gi % 2
                gi += 1
                dep = [("Wg16", kq, ft // 4) for kq in range(4)] + [("Wu16", kq, ft // 4) for kq in range(4)]
                srcg = Wg16[:, ft * 128:(ft + 1) * 128].rearrange("(c p) n -> p c n", p=128)
                srcu = Wu16[:, ft * 128:(ft + 1) * 128].rearrange("(c p) n -> p c n", p=128)
                P.dma("sp", lambda e, s=s, srcg=srcg: e.dma_start(out=wgt[s][:], in_=srcg), reads=dep if ch == 0 else [], writes=[("wgt", s)])
                P.dma("sp", lambda e, s=s, srcu=srcu: e.dma_start(out=wut[s][:], in_=srcu), reads=dep if ch == 0 else [], writes=[("wut", s)])
                for half in range(T // 512):
                    hs = slice(half * 512, (half + 1) * 512)
                    psg, pkg = cx.psum()
                    mm_group(cx, psg[:], pkg, [(wgt[s][:, kc, :], hT[:, kc, hs]) for kc in range(DC)], [("wgt", s), "hT"])
                    psu, pku = cx.psum()
                    mm_group(cx, psu[:], pku, [(wut[s][:, kc, :], hT[:, kc, hs]) for kc in range(DC)], [("wut", s), "hT"])
                    s2 = it % 2
                    it += 1
                    P.op("act", lambda e, s2=s2, psg=psg: e.activation(out=sgl[s2][:], in_=psg[:], func=AF.Silu), reads=[pkg], writes=[("sg", s2)])
                    P.op("dve", lambda e, s2=s2, psu=psu, gb=gb, j=j, hs=hs: e.tensor_tensor(out=gb[:, j, hs], in0=sgl[s2][:], in1=psu[:], op=ALU.mult),
                         reads=[("sg", s2), pku], writes=[("g16", fg % 2, j)])
            for j in range(FG):
                ft = fg * FG + j
                dep = [("Wd16", ft // 4, cb) for cb in range(D // 512)]
                P.dma("sp", lambda e, j=j, ft=ft: e.dma_start(out=wdt[:, j, :], in_=Wd16[ft * 128:(ft + 1) * 128, :]),
                      reads=dep if ch == 0 else [], writes=[("wdt", j)])
            for rt in range(T // 128):
                for db in range(D // 512):
                    ps, pk = cx.psum()
                    mm_group(cx, ps[:], pk, [(gb[:, j, rt * 128:(rt + 1) * 128], wdt[:, j, db * 512:(db + 1) * 512]) for j in range(FG)],
                             [("wdt", j) for j in range(FG)] + [("g16", fg % 2, j) for j in range(FG)])
                    dsl = accR[:, rt, db * 512:(db + 1) * 512]
                    if fg == 0:
                        P.op("dve", lambda e, dsl=dsl, ps=ps: e.tensor_copy(out=dsl, in_=ps[:]), reads=[pk], writes=[("accR", rt, db)])
                    else:
                        P.op("dve", lambda e, dsl=dsl, ps=ps: e.tensor_tensor(out=dsl, in0=ps[:], in1=dsl, op=ALU.add),
                             reads=[pk, ("accR", rt, db)], writes=[("accR", rt, db)])
        for rt in range(T // 128):
            s = rt % 2
            row0 = ch * T + rt * 128
            P.op("act", lambda e, s=s, rt=rt: e.activation(out=orow[s][:], in_=accR[:, rt, :], func=AF.Identity),
                 reads=[("accR", rt, db) for db in range(D // 512)], writes=[("orow", s)])
            cx.store(yo[row0:row0 + 128, :], orow[s][:], [("orow", s)], ("yo", ch, rt), sem=("st_orow", s))
    cx.phase_end()
    return cx.finish()


def build_final3():
    cx = Ctx()
    P = cx.P
    NT_ = T // 128
    xd = cx.din("x", [T, D], F32)
    yd = cx.din("yseg", [NEXP, SEG, D], BF16)
    pbd = cx.din("posb", [128, NEXP, T], F32)
    rwd = cx.din("rwt", [128, NT_, NEXP], F32)
    sld = cx.din("slotidx", [128, NR], F32)
    gfd = cx.din("gatef", [128, D], F32)
    gnd = cx.din("gfin", [128, D], F32)
    out = cx.dout("out", [T, D], F32)
    cx.alloc_psum(8)
    acc = cx.sb("acc", [128, NT_, D], F32)
    posb = cx.sb("posb_sb", [128, NEXP, T], F32)
    rwt = cx.sb("rwt_sb", [128, NT_, NEXP], F32)
    slot = cx.sb("slot_sb", [128, NR], F32)
    gf = cx.sb("gf_sb", [128, D], F32)
    gfin = cx.sb("gfin_sb", [128, D], F32)
    cx.load(posb[:].rearrange("p e t -> p (e t)"), pbd.rearrange("p e t -> p (e t)"), "posb")
    cx.load(rwt[:], rwd, "rwt")
    cx.load(slot[:], sld, "slot")
    cx.load(gf[:], gfd, "gf")
    cx.load(gfin[:], gnd, "gfin")
    ye = [cx.sb("ye%d" % i, [128, NR, D], BF16) for i in range(2)]
    PwT = [cx.sb("PwT%d" % i, [128, NR, T], BF16) for i in range(2)]
    for ex in range(NEXP):
        s = ex % 2
        for r in range(NR):
            P.dma("sp", lambda e, s=s, r=r, ex=ex: e.dma_start(out=ye[s][:, r, :], in_=yd[ex, r * 128:(r + 1) * 128, :]), writes=[("ye", s, r)], sem=("ye", s, r))
            P.op("dve" if r % 2 == 0 else "pool", lambda e, s=s, r=r, ex=ex: e.tensor_scalar(
                out=PwT[s][:, r, :], in0=posb[:, ex, :], scalar1=slot[:, r:r + 1], scalar2=None, op0=ALU.is_equal),
                reads=["posb", "slot"], writes=[("PwT", s, r)])
        for tt in range(NT_):
            nr = min(tt, NR - 1) + 1
            for db in range(D // 512):
                ps, pk = cx.psum()
                mm_group(cx, ps[:], pk, [(PwT[s][:, r, tt * 128:(tt + 1) * 128], ye[s][:, r, db * 512:(db + 1) * 512]) for r in range(nr)],
                         [("PwT", s, r) for r in range(nr)] + [("ye", s, r) for r in range(nr)])
                dsl = acc[:, tt, db * 512:(db + 1) * 512]
                if ex == 0:
                    P.op("dve", lambda e, dsl=dsl, ps=ps, tt=tt, ex=ex: e.tensor_scalar(out=dsl, in0=ps[:], scalar1=rwt[:, tt, ex:ex + 1], scalar2=None, op0=ALU.mult),
                         reads=[pk, "rwt"], writes=[("acc", tt, db)])
                else:
                    P.op("dve", lambda e, dsl=dsl, ps=ps, tt=tt, ex=ex: e.scalar_tensor_tensor(out=dsl, in0=ps[:], scalar=rwt[:, tt, ex:ex + 1], in1=dsl,
                                                                                     op0=ALU.mult, op1=ALU.add),
                         reads=[pk, "rwt", ("acc", tt, db)], writes=[("acc", tt, db)])
    xt = [cx.sb("xt%d" % i, [128, D], F32) for i in range(2)]
    sqj = cx.sb("sqj", [128, D], F32)
    ss = [cx.sb("ss%d" % i, [128, 1], F32) for i in range(2)]
    for tt in range(NT_):
        s = tt % 2
        rows = slice(tt * 128, (tt + 1) * 128)
        ak = [("acc", tt, db) for db in range(D // 512)]
        cx.load(xt[s][:], xd[rows, :], ("xt", s))
        P.op("pool", lambda e, tt=tt: e.tensor_tensor(out=acc[:, tt, :], in0=acc[:, tt, :], in1=gf[:], op=ALU.mult), reads=ak + ["gf"], writes=ak)
        P.op("dve", lambda e, s=s, tt=tt: e.tensor_tensor(out=xt[s][:], in0=xt[s][:], in1=acc[:, tt, :], op=ALU.add), reads=ak + [("xt", s)], writes=[("xt", s)])
        P.op("act", lambda e, s=s: e.activation(out=sqj[:], in_=xt[s][:], func=AF.Square, accum_out=ss[s][:]), reads=[("xt", s)], writes=["sqj", ("ss", s)])
        P.op("dve", lambda e, s=s: e.tensor_scalar(out=ss[s][:], in0=ss[s][:], scalar1=1.0 / D, scalar2=EPS, op0=ALU.mult, op1=ALU.add),
             reads=[("ss", s)], writes=[("ss", s)])
        P.op("act", lambda e, s=s: e.activation(out=ss[s][:], in_=ss[s][:], func=AF.Sqrt), reads=[("ss", s)], writes=[("ss", s)])
        P.op("dve", lambda e, s=s: e.reciprocal(out=ss[s][:], in_=ss[s][:]), reads=[("ss", s)], writes=[("ss", s)])
        P.op("dve", lambda e, s=s, tt=tt: e.scalar_tensor_tensor(out=acc[:, tt, :], in0=xt[s][:], scalar=ss[s][:, 0:1], in1=gfin[:], op0=ALU.mult, op1=ALU.mult),
             reads=[("xt", s), ("ss", s), "gfin"] + ak, writes=ak)
        cx.store(out[rows, :], acc[:, tt, :], ak, ("out", tt), sem=("st_out", s))
    return cx.finish()


def route_consts():
    kk = np.arange(128)
    return {"Ltri": (kk[:, None] < kk[None, :]).astype(np.float32), "ident": np.eye(128, dtype=np.float32),
            "iota": np.ascontiguousarray(np.broadcast_to(kk[None, :].astype(np.float32), (128, 128)))}


def rw_layout(rw):
    return np.ascontiguousarray(rw.reshape(T // 128, 128, NEXP).transpose(1, 0, 2))


def run_route(h2s, rws):
    nc = get_nc("route", build_route)
    cst = route_consts()
    ins = [dict(h2T=h2s[i], rw=rw_layout(rws[i]), **cst) for i in range(NCORES)]
    res = run(nc, ins)
    overflow = any(bool(np.any(res[i]["ovf"] > 0.5)) for i in range(NCORES))
    return [res[i]["hc"] for i in range(NCORES)], [res[i]["posm"] for i in range(NCORES)], overflow


def run_moe3(hcs, inp):
    nc = get_nc("moe3", build_moe3)
    ins = []
    for e in range(NCORES):
        hcat = np.concatenate([hcs[i][e] for i in range(NCORES)], axis=2)
        hc = np.ascontiguousarray(hcat.reshape(128, DC, NCH3, T).transpose(2, 0, 1, 3))
        ins.append({"hc": hc, "w_gate": np.ascontiguousarray(inp["moe_w_gate"][0][e]), "w_up": np.ascontiguousarray(inp["moe_w_up"][0][e]),
                    "w_down": np.ascontiguousarray(inp["moe_w_down"][0][e])})
    res = run(nc, ins)
    return [res[e]["y"] for e in range(NCORES)]


def run_final3(xTs, ys, posms, rws, mod_l, gfin):
    nc = get_nc("final3", build_final3)
    ins = []
    slotidx = (np.arange(128)[:, None] + 128 * np.arange(NR)[None, :]).astype(np.float32)
    for i in range(NCORES):
        b = i // 4
        gatef = mod_l[b].reshape(6, D)[5]
        posm = posms[i]
        pos_et = posm.transpose(2, 1, 0).reshape(NEXP, T)
        ins.append({"x": np.ascontiguousarray(xTs[i].transpose(1, 0, 2).reshape(D, T).T),
                    "yseg": np.ascontiguousarray(np.stack([ys[e][i * SEG:(i + 1) * SEG] for e in range(NEXP)])),
                    "posb": np.ascontiguousarray(np.broadcast_to(pos_et[None], (128, NEXP, T))),
                    "rwt": rw_layout(rws[i]), "slotidx": slotidx,
                    "gatef": np.ascontiguousarray(np.broadcast_to(gatef[None, :], (128, D))),
                    "gfin": np.ascontiguousarray(np.broadcast_to(gfin[None, :], (128, D)))})
    res = run(nc, ins)
    out = np.zeros((2, SEQ, D), np.float32)
    for i in range(NCORES):
        b, q = i // 4, i % 4
        out[b, q * T:(q + 1) * T, :] = res[i]["out"]
    return out
```

```python
import contextlib
import numpy as np
import ml_dtypes
import concourse.bass as bass
import concourse.mybir as mybir
from concourse.bass_utils import run_bass_kernel_spmd

F32 = mybir.dt.float32
BF16 = mybir.dt.bfloat16
I32 = mybir.dt.int32
ALU = mybir.AluOpType
AF = mybir.ActivationFunctionType
NPBF = ml_dtypes.bfloat16

ENGS = ("pe", "dve", "act", "pool", "sp")

D = 2048
DC = 16
NCORES = 8
T = 1024
SEQ = 4096
EPS = 1e-6
IN_W = 9728
DFF = 5632
DFFE = 7168
NEXP = 8


class Prog:
    def __init__(self, nc):
        self.nc = nc
        self.ops = {e: [] for e in ENGS}
        self.last_w = {}
        self.readers = {}
        self.ndma = {}

    def _deps(self, reads, writes):
        deps = []
        for k in reads:
            t = self.last_w.get(k)
            if t is not None:
                deps.append(t)
        for k in writes:
            t = self.last_w.get(k)
            if t is not None:
                deps.append(t)
            deps.extend(self.readers.get(k, ()))
        return deps

    def _commit(self, tok, reads, writes):
        for k in reads:
            self.readers.setdefault(k, []).append(tok)
        for k in writes:
            self.last_w[k] = tok
            self.readers[k] = []

    def op(self, eng, fn, reads=(), writes=(), signal=True):
        deps = self._deps(reads, writes)
        tok = ("c", eng, len(self.ops[eng]))
        self.ops[eng].append(dict(kind="c", fn=fn, deps=deps, signal=signal or eng != "pe"))
        self._commit(tok, reads, writes)
        return tok

    def dma(self, q, fn, reads=(), writes=(), inc=16, sem=None):
        if sem is None:
            sem = writes[0]
        deps = self._deps(reads, writes)
        self.ndma[sem] = self.ndma.get(sem, 0) + inc
        tok = ("d", sem, self.ndma[sem])
        self.ops[q].append(dict(kind="d", fn=fn, deps=deps, inc=inc, sem=sem))
        self._commit(tok, reads, writes)
        return tok

    def wait_all_on(self, eng, toks):
        self.ops[eng].append(dict(kind="w", deps=list(toks)))

    def barrier(self):
        toks = []
        for e in ENGS:
            for i in range(len(self.ops[e]) - 1, -1, -1):
                o = self.ops[e][i]
                if o["kind"] == "c" and o["signal"]:
                    toks.append(("c", e, i))
                    break
        for k, v in self.ndma.items():
            toks.append(("d", k, v))
        for e in ENGS:
            self.ops[e].append(dict(kind="w", deps=list(toks)))

    def emit(self, st):
        nc = self.nc
        tick_at = {}
        for e in ENGS:
            n = 0
            ticks = []
            for o in self.ops[e]:
                if o["kind"] == "c" and o["signal"]:
                    n += 1
                ticks.append(n)
            res = [None] * len(ticks)
            nxt = None
            for i in range(len(ticks) - 1, -1, -1):
                o = self.ops[e][i]
                if o["kind"] == "c" and o["signal"]:
                    nxt = ticks[i]
                res[i] = nxt
            tick_at[e] = res
        csem = {e: st.enter_context(nc.semaphore("c_" + e)) for e in ENGS if e != "sp"}
        dkeys = []
        dset = set()
        for e in ENGS:
            for o in self.ops[e]:
                if o["kind"] == "d" and o["sem"] not in dset:
                    dset.add(o["sem"])
                    dkeys.append(o["sem"])
        dsem = {k: st.enter_context(nc.semaphore("d%d" % i)) for i, k in enumerate(dkeys)}
        block = st.enter_context(nc.Block())

        def make(e):
            def body(eng):
                seen = {}
                for o in self.ops[e]:
                    for t in o["deps"]:
                        if t[0] == "c":
                            if t[1] == e and e == "pe":
                                continue
                            v = tick_at[t[1]][t[2]]
                            assert v is not None, ("unsignaled dep", t)
                            key = ("c", t[1])
                            sem = csem[t[1]]
                        else:
                            v = t[2]
                            key = ("d", t[1])
                            sem = dsem[t[1]]
                        if seen.get(key, 0) >= v:
                            continue
                        seen[key] = v
                        eng.wait_ge(sem, v)
                    if o["kind"] == "c":
                        ins = o["fn"](eng)
                        if o["signal"]:
                            ins.then_inc(csem[e], 1)
                    elif o["kind"] == "d":
                        ins = o["fn"](eng)
                        ins.then_inc(dsem[o["sem"]], o["inc"])
            return body

        for e in ENGS:
            if not self.ops[e]:
                continue
            dec = {"pe": block.tensor, "dve": block.vector, "act": block.scalar,
                   "pool": block.gpsimd, "sp": block.sync}[e]
            dec(make(e))


class Ctx:
    def __init__(self):
        self.nc = bass.Bass("TRN2", target_bir_lowering=False)
        self.st = contextlib.ExitStack()
        self.P = Prog(self.nc)
        self.ps = []
        self.ps_i = 0
        self.out_toks = []
        self.cv_i = 0
        self._n = 0
        self.phase_st = None
        self.arena = None
        self.arena_off = 0

    def din(self, name, shape, dt):
        return self.nc.dram_tensor(name, list(shape), dt, kind="ExternalInput").ap()

    def dout(self, name, shape, dt):
        return self.nc.dram_tensor(name, list(shape), dt, kind="ExternalOutput").ap()

    ARENA_BYTES = 160 * 1024

    def sb(self, name, shape, dt):
        if self.phase_st is None:
            return self.st.enter_context(self.nc.sbuf_tensor(name, list(shape), dt))
        esz = mybir.dt.size(dt)
        n = int(np.prod(shape[1:]))
        nbytes = (n * esz + 63) // 64 * 64
        off = self.arena_off
        assert off + nbytes <= self.ARENA_BYTES, ("arena overflow", name, off, nbytes)
        self.arena_off += nbytes
        v = self.arena[0:shape[0], off // 2:(off + n * esz) // 2]
        if dt != BF16:
            v = v.bitcast(dt)
        if len(shape) > 2:
            names = " ".join("d%d" % i for i in range(1, len(shape)))
            v = v.rearrange("p (%s) -> p %s" % (names, names), **{"d%d" % i: shape[i] for i in range(1, len(shape))})
        return v

    def phase_begin(self):
        if self.arena is None:
            self.arena = self.st.enter_context(self.nc.sbuf_tensor("arena", [128, self.ARENA_BYTES // 2], BF16))
        self.phase_st = True
        self.arena_off = 0

    def phase_end(self):
        self.P.barrier()
        self.phase_st = None

    def alloc_psum(self, n=8):
        for i in range(n):
            self.ps.append(self.st.enter_context(self.nc.psum_tensor("ps%d" % i, [128, 512], F32)))

    def psum(self):
        i = self.ps_i % len(self.ps)
        self.ps_i += 1
        return self.ps[i], ("ps", i)

    def load(self, dst_ap, src_ap, key, q="sp"):
        return self.P.dma(q, lambda e: e.dma_start(out=dst_ap, in_=src_ap), writes=[key])

    def store(self, dst_ap, src_ap, rkeys, wkey, q="sp", sem=None):
        t = self.P.dma(q, lambda e: e.dma_start(out=dst_ap, in_=src_ap), reads=rkeys, writes=[wkey],
                       sem=sem if sem is not None else ("st", wkey))
        self.out_toks.append(t)
        return t

    def conv_eng(self):
        e = ("act", "dve")[self.cv_i % 2]
        self.cv_i += 1
        return e

    def copy(self, eng, out, in_, reads, writes):
        if eng == "act":
            return self.P.op("act", lambda e: e.activation(out=out, in_=in_, func=AF.Identity), reads=reads, writes=writes)
        return self.P.op(eng, lambda e: e.tensor_copy(out=out, in_=in_), reads=reads, writes=writes)

    def finish(self):
        last = {}
        for t in self.out_toks:
            last[t[1]] = t
        self.P.wait_all_on("sp", list(last.values()))
        self.P.emit(self.st)
        self.st.close()
        return self.nc


def mm_group(cx, ps_ap, ps_key, pairs, reads):
    n = len(pairs)
    for i, (l, r) in enumerate(pairs):
        cx.P.op("pe", lambda e, l=l, r=r, i=i: e.matmul(ps_ap, lhsT=l, rhs=r, start=(i == 0), stop=(i == n - 1)),
                reads=reads, writes=[ps_key], signal=(i == n - 1))


class WLoader:
    def __init__(self, cx, name, KC, ncol, nslots=2, nstage=4, kq=4):
        self.cx, self.KC, self.ncol, self.kq = cx, KC, ncol, kq
        self.name = name
        self.wb = [cx.sb("%s_wb%d" % (name, i), [128, KC, ncol], BF16) for i in range(nslots)]
        self.stg = [cx.sb("%s_st%d" % (name, i), [128, kq, ncol], F32) for i in range(nstage)]
        self.si = 0
        self.wi = 0

    def load(self, w_rows_ap, ncol=None):
        cx = self.cx
        ncol = ncol or self.ncol
        slot = self.wi % len(self.wb)
        self.wi += 1
        wb = self.wb[slot]
        key = (self.name, "wb", slot)
        for k0 in range(0, self.KC, self.kq):
            kn = min(self.kq, self.KC - k0)
            s = self.si % len(self.stg)
            self.si += 1
            stg = self.stg[s]
            skey = (self.name, "st", s)
            src = w_rows_ap[k0 * 128:(k0 + kn) * 128, :].rearrange("(c p) n -> p c n", p=128)
            import os
            q = "sp"
            if os.environ.get("VAR", "") == "v1" and (self.si % 2 == 0):
                q = "act"
            cx.P.dma(q, lambda e, stg=stg, src=src, kn=kn: e.dma_start(out=stg[:, 0:kn, 0:ncol], in_=src),
                     writes=[skey])
            ce = cx.conv_eng()
            if os.environ.get("VAR", "") == "v3" and ce == "pool":
                ce = "dve" if (self.si % 2 == 0) else "act"
            cx.copy(ce, wb[:, k0:k0 + kn, 0:ncol], stg[:, 0:kn, 0:ncol], [skey], [(key, k0)])
        return wb, [(key, k0) for k0 in range(0, self.KC, self.kq)]


def build_mod():
    cx = Ctx()
    P = cx.P
    NCOL = 3072
    cT = cx.din("cT", [128, DC, 2], F32)
    W = cx.din("W", [D, NCOL], F32)
    b = cx.din("b", [2, NCOL], F32)
    y = cx.dout("y", [2, NCOL], F32)
    cx.alloc_psum(2)
    c_sb = cx.sb("c_sb", [128, DC, 2], F32)
    cond = cx.sb("cond", [128, DC, 2], F32)
    b_sb = cx.sb("b_sb", [2, NCOL], F32)
    o_sb = cx.sb("o_sb", [2, NCOL], F32)
    wst = [cx.sb("wst%d" % i, [128, DC, 512], F32) for i in range(2)]
    cx.load(c_sb[:], cT, "c_sb")
    cx.load(b_sb[:], b, "b_sb")
    P.op("act", lambda e: e.activation(out=cond[:], in_=c_sb[:], func=AF.Silu), reads=["c_sb"], writes=["cond"])
    for ct in range(NCOL // 512):
        s = ct % 2
        for k0 in range(0, DC, 4):
            src = W[k0 * 128:(k0 + 4) * 128, ct * 512:(ct + 1) * 512].rearrange("(c p) n -> p c n", p=128)
            P.dma("sp", lambda e, s=s, k0=k0, src=src: e.dma_start(out=wst[s][:, k0:k0 + 4, :], in_=src),
                  writes=[("wst", s)])
        ps, pk = cx.psum()
        mm_group(cx, ps[0:2, :], pk, [(cond[:, kc, :], wst[s][:, kc, :]) for kc in range(DC)], ["cond", ("wst", s)])
        P.op("dve", lambda e, ps=ps, ct=ct: e.tensor_tensor(out=o_sb[:, ct * 512:(ct + 1) * 512], in0=ps[0:2, :],
                                                        in1=b_sb[:, ct * 512:(ct + 1) * 512], op=ALU.add),
             reads=[pk, "b_sb"], writes=["o_sb"])
    cx.store(y, o_sb[:], ["o_sb"], "y")
    return cx.finish()


def emit_rmsnorm_mod(cx, xT, xkey, gm, shift, hT, hkey, ones16, tag):
    P = cx.P
    sq = [cx.sb("%s_sq%d" % (tag, i), [128, 512], BF16) for i in range(2)]
    rstd = cx.sb("%s_rstd" % tag, [128, T], F32)
    tmp = [cx.sb("%s_tmp%d" % (tag, i), [128, 512], F32) for i in range(2)]
    for half in range(T // 512):
        hs = slice(half * 512, (half + 1) * 512)
        ps, pk = cx.psum()
        for kc in range(DC):
            s = kc % 2
            P.op("act", lambda e, s=s, kc=kc, hs=hs: e.activation(out=sq[s][:], in_=xT[:, kc, hs], func=AF.Square),
                 reads=[xkey], writes=[(tag, "sq", s)])
            P.op("pe", lambda e, s=s, kc=kc, ps=ps: e.matmul(ps[:], lhsT=ones16[:], rhs=sq[s][:], start=(kc == 0),
                                                          stop=(kc == DC - 1)),
                 reads=[(tag, "sq", s), "ones16"], writes=[pk], signal=True)
        rk = (tag, "rstd", half)
        P.op("dve", lambda e, ps=ps, hs=hs: e.tensor_scalar(out=rstd[:, hs], in0=ps[:], scalar1=1.0 / D, scalar2=EPS,
                                                    op0=ALU.mult, op1=ALU.add), reads=[pk], writes=[rk])
        P.op("act", lambda e, hs=hs: e.activation(out=rstd[:, hs], in_=rstd[:, hs], func=AF.Sqrt), reads=[rk], writes=[rk])
        P.op("dve", lambda e, hs=hs: e.reciprocal(out=rstd[:, hs], in_=rstd[:, hs]), reads=[rk], writes=[rk])
        for kc in range(DC):
            s = kc % 2
            P.op("dve", lambda e, s=s, kc=kc, hs=hs: e.tensor_tensor(out=tmp[s][:], in0=xT[:, kc, hs], in1=rstd[:, hs], op=ALU.mult),
                 reads=[xkey, rk], writes=[(tag, "tmp", s)])
            P.op("act", lambda e, s=s, kc=kc, hs=hs: e.activation(out=hT[:, kc, hs], in_=tmp[s][:], func=AF.Identity,
                                                        scale=gm[:, kc:kc + 1], bias=shift[:, kc:kc + 1]),
                 reads=[(tag, "tmp", s), "modv"], writes=[hkey])
    return rstd


def emit_modvecs(cx, modv, gnorm, gm):
    P = cx.P
    P.op("dve", lambda e: e.tensor_scalar(out=gm[:], in0=modv[:, 1, :], scalar1=1.0, scalar2=None, op0=ALU.add),
         reads=["modv_in"], writes=["gm_tmp"])
    P.op("dve", lambda e: e.tensor_tensor(out=gm[:], in0=gm[:], in1=gnorm[:], op=ALU.mult),
         reads=["gm_tmp", "gnorm"], writes=["modv"])


def emit_linear(cx, wl, W, KC, N, inT, in_keys, evac, ngrp=256):
    groups = [(g0, min(ngrp, N - g0)) for g0 in range(0, N, ngrp)]
    nxt = wl.load(W[:, groups[0][0]:groups[0][0] + groups[0][1]], groups[0][1])
    for gi_, (g0, gn) in enumerate(groups):
        wb, wkeys = nxt
        if gi_ + 1 < len(groups):
            n0, nn = groups[gi_ + 1]
            nxt = wl.load(W[:, n0:n0 + nn], nn)
        for j in range(gn // 128):
            nt = g0 // 128 + j
            for half in range(T // 512):
                ps, pk = cx.psum()
                mm_group(cx, ps[:], pk,
                         [(wb[:, kc, j * 128:(j + 1) * 128], inT[:, kc, half * 512:(half + 1) * 512]) for kc in range(KC)],
                         list(wkeys) + list(in_keys))
                evac(nt, half, ps, pk)


def build_inproj():
    cx = Ctx()
    P = cx.P
    xTd = cx.din("xT", [128, DC, T], F32)
    modd = cx.din("modv", [128, 6, DC], F32)
    gnd = cx.din("gnorm", [128, DC], F32)
    W = cx.din("W", [D, IN_W], F32)
    out = cx.dout("projT", [IN_W, T], BF16)
    cx.alloc_psum(8)
    xT = cx.sb("xT_sb", [128, DC, T], F32)
    hT = cx.sb("hT_sb", [128, DC, T], BF16)
    modv = cx.sb("modv_sb", [128, 6, DC], F32)
    gnorm = cx.sb("gnorm_sb", [128, DC], F32)
    gm = cx.sb("gm_sb", [128, DC], F32)
    ones16 = cx.sb("ones16", [128, 128], BF16)
    osb = [cx.sb("osb%d" % i, [128, T], BF16) for i in range(3)]
    P.op("pool", lambda e: e.memset(ones16[:], 1.0), writes=["ones16"])
    for k0 in range(0, DC, 4):
        P.dma("sp", lambda e, k0=k0: e.dma_start(out=xT[:, k0:k0 + 4, :], in_=xTd[:, k0:k0 + 4, :]), writes=["xT"], sem="xT")
    cx.load(modv[:], modd, "modv_in")
    cx.load(gnorm[:], gnd, "gnorm")
    emit_modvecs(cx, modv, gnorm, gm)
    emit_rmsnorm_mod(cx, xT, "xT", gm, modv[:, 0, :], hT, "hT", ones16, "n1")
    import os
    if os.environ.get("VAR", "") == "v4":
        wl = WLoader(cx, "win", DC, 512, nslots=2, nstage=16, kq=1)
    elif os.environ.get("VAR", "") == "v5":
        wl = WLoader(cx, "win", DC, 512, nslots=2, nstage=8, kq=2)
    else:
        wl = WLoader(cx, "win", DC, 512, nslots=2, nstage=4)
    cnt = [0]

    def evac(nt, half, ps, pk):
        s = nt % 3
        eng = "act" if (cnt[0] % 2 == 0) else "dve"
        cnt[0] += 1
        cx.copy(eng, osb[s][:, half * 512:(half + 1) * 512], ps[:], [pk], [("osb", s, half)])
        if half == T // 512 - 1:
            cx.store(out[nt * 128:(nt + 1) * 128, :], osb[s][:], [("osb", s, h) for h in range(T // 512)], ("out", nt),
                     sem=("st_osb", s))

    emit_linear(cx, wl, W, DC, IN_W, hT, ["hT"], evac, ngrp=512)
    return cx.finish()


def fm(a2d):
    F, n = a2d.shape
    return np.ascontiguousarray(a2d.reshape(F // 128, 128, n).transpose(1, 0, 2))


def vec_fm(v):
    return np.ascontiguousarray(v.reshape(-1, 128).T)


_cache = {}


def get_nc(name, builder):
    if name not in _cache:
        _cache[name] = builder()
    return _cache[name]


def run(nc, in_maps):
    res = run_bass_kernel_spmd(nc, in_maps, core_ids=list(range(NCORES)))
    return res.results


def run_mod(c, w_mod, b_mod):
    nc = get_nc("mod", build_mod)
    cT = np.ascontiguousarray(c.T.reshape(DC, 128, 2).transpose(1, 0, 2))
    ins = []
    for i in range(NCORES):
        l, q = i // 4, i % 4
        cols = slice(q * 3072, (q + 1) * 3072)
        ins.append({"cT": cT, "W": np.ascontiguousarray(w_mod[l][:, cols]),
                    "b": np.ascontiguousarray(np.broadcast_to(b_mod[l][cols], (2, 3072)))})
    res = run(nc, ins)
    mod = np.zeros((2, 2, 6 * D), np.float32)
    for i in range(NCORES):
        l, q = i // 4, i % 4
        mod[l][:, q * 3072:(q + 1) * 3072] = res[i]["y"]
    return mod


def modv_layout(mod_lb):
    return np.ascontiguousarray(mod_lb.reshape(6, DC, 128).transpose(2, 0, 1))


def x_to_cores(x):
    outs = []
    for i in range(NCORES):
        b, q = i // 4, i % 4
        outs.append(fm(np.ascontiguousarray(x[b, q * T:(q + 1) * T, :].T)))
    return outs


def run_inproj(xTs, mod_l, gnorm, w_in_l):
    nc = get_nc("inproj", build_inproj)
    ins = []
    for i in range(NCORES):
        b = i // 4
        ins.append({"xT": xTs[i], "modv": modv_layout(mod_l[b]), "gnorm": vec_fm(gnorm), "W": w_in_l})
    res = run(nc, ins)
    projT = np.zeros((2, IN_W, SEQ), NPBF)
    for i in range(NCORES):
        b, q = i // 4, i % 4
        projT[b][:, q * T:(q + 1) * T] = res[i]["projT"]
    return projT


DILS = (1, 4, 16)


def build_attn():
    cx = Ctx()
    P = cx.P
    QTd = cx.din("QT", [128, 3, SEQ], BF16)
    KTd = cx.din("KT", [128, 3, SEQ], BF16)
    Vd = cx.din("V", [128, 3, 32, 128], BF16)
    Bmd = cx.din("Bm", [128, 3, 256], F32)
    out = cx.dout("oT", [128, SEQ], BF16)
    cx.alloc_psum(8)
    QT = cx.sb("QT_sb", [128, 3, SEQ], BF16)
    KT = cx.sb("KT_sb", [128, 3, SEQ], BF16)
    V = cx.sb("V_sb", [128, 3, 32, 128], BF16)
    Bm = cx.sb("Bm_sb", [128, 3, 256], F32)
    ones16 = cx.sb("ones16", [128, 128], BF16)
    num = cx.sb("num", [128, SEQ], F32)
    den = cx.sb("den", [128, SEQ], F32)
    o16 = cx.sb("o16", [128, SEQ], BF16)
    lg = [cx.sb("lg%d" % i, [128, 256], F32) for i in range(3)]
    pT = [cx.sb("pT%d" % i, [128, 256], BF16) for i in range(3)]
    P.op("pool", lambda e: e.memset(ones16[:], 1.0), writes=["ones16"])
    Vf = V[:].rearrange("p g t d -> p g (t d)")
    Vdf = Vd.rearrange("p g t d -> p g (t d)")
    for g in range(3):
        cx.load(QT[:, g, :], QTd[:, g, :], ("QT", g))
        cx.load(KT[:, g, :], KTd[:, g, :], ("KT", g))
        cx.load(Vf[:, g, :], Vdf[:, g, :], ("V", g))
    cx.load(Bm[:], Bmd, "Bm")
    scale = 128 ** -0.5
    blk = 0
    import os
    DBG = os.environ.get("ATT_DBG", "")
    if DBG == "loads":
        P.op("pool", lambda e: e.memset(o16[:], 0.0), reads=[("QT", 0), ("QT", 1), ("QT", 2), ("KT", 0), ("KT", 1), ("KT", 2), ("V", 0), ("V", 1), ("V", 2), "Bm"], writes=["o16"])
        cx.store(out, o16[:], ["o16"], "oT")
        return cx.finish()
    for g in range(3 if DBG != "g0" else 1):
        d = DILS[g]
        run = SEQ // d
        numv = num[:].rearrange("p (m d) -> p m d", d=d)
        denv = den[:].rearrange("p (m d) -> p m d", d=d)
        for r in range(d):
            for i in range(run // 128):
                p0 = r * run + 128 * i
                nc_ = 256 if i > 0 else 128
                s = blk % 3
                blk += 1
                ps, pk = cx.psum()
                P.op("pe", lambda e, ps=ps, g=g, p0=p0: e.matmul(ps[:, 0:128], lhsT=KT[:, g, p0:p0 + 128],
                                                               rhs=QT[:, g, p0:p0 + 128], start=True, stop=True),
                     reads=[("KT", g), ("QT", g)], writes=[pk], signal=(i == 0))
                if i > 0:
                    P.op("pe", lambda e, ps=ps, g=g, p0=p0: e.matmul(ps[:, 128:256], lhsT=KT[:, g, p0 - 128:p0],
                                                                   rhs=QT[:, g, p0:p0 + 128], start=True, stop=True),
                         reads=[("KT", g), ("QT", g)], writes=[pk])
                LV = int(os.environ.get("ATT_LV", "9"))
                if LV == 1:
                    P.op("dve", lambda e, ps=ps, s=s, n=nc_: e.tensor_copy(out=lg[s][:, 0:n], in_=ps[:, 0:n]), reads=[pk], writes=[("lg", s)])
                    continue
                P.op("dve", lambda e, ps=ps, g=g, s=s, n=nc_: e.scalar_tensor_tensor(
                    out=lg[s][:, 0:n], in0=ps[:, 0:n], scalar=scale, in1=Bm[:, g, 0:n], op0=ALU.mult, op1=ALU.add),
                    reads=[pk, "Bm"], writes=[("lg", s)])
                if LV == 2:
                    continue
                P.op("act", lambda e, s=s, n=nc_: e.activation(out=pT[s][:, 0:n], in_=lg[s][:, 0:n], func=AF.Exp),
                     reads=[("lg", s)], writes=[("pT", s)])
                if LV == 3:
                    continue
                ps2, pk2 = cx.psum()
                td = p0 // 128
                P.op("pe", lambda e, ps2=ps2, g=g, td=td, s=s, i=i: e.matmul(
                    ps2[:, 0:128], lhsT=V[:, g, td, :], rhs=pT[s][:, 0:128], start=True, stop=(i == 0)),
                    reads=[("V", g), ("pT", s)], writes=[pk2], signal=False)
                if i > 0:
                    P.op("pe", lambda e, ps2=ps2, g=g, td=td, s=s: e.matmul(
                        ps2[:, 0:128], lhsT=V[:, g, td - 1, :], rhs=pT[s][:, 128:256], start=False, stop=True),
                        reads=[("V", g), ("pT", s)], writes=[pk2], signal=False)
                P.op("pe", lambda e, ps2=ps2, s=s, i=i: e.matmul(
                    ps2[:, 128:256], lhsT=ones16[:], rhs=pT[s][:, 0:128], start=True, stop=(i == 0)),
                    reads=["ones16", ("pT", s)], writes=[pk2], signal=(i == 0))
                if i > 0:
                    P.op("pe", lambda e, ps2=ps2, s=s: e.matmul(
                        ps2[:, 128:256], lhsT=ones16[:], rhs=pT[s][:, 128:256], start=False, stop=True),
                        reads=["ones16", ("pT", s)], writes=[pk2])
                if LV == 4:
                    P.op("dve", lambda e, ps2=ps2, s=s: e.tensor_copy(out=lg[s][:, 0:256], in_=ps2[:, 0:256]), reads=[pk2], writes=[("lg", s)])
                    continue
                nv = numv[:, 128 * i:128 * (i + 1), r]
                dv = denv[:, 128 * i:128 * (i + 1), r]
                if g == 0:
                    P.op("dve", lambda e, ps2=ps2, nv=nv: e.tensor_copy(out=nv, in_=ps2[:, 0:128]), reads=[pk2], writes=["num"])
                    P.op("dve", lambda e, ps2=ps2, dv=dv: e.tensor_copy(out=dv, in_=ps2[:, 128:256]), reads=[pk2], writes=["den"])
                else:
                    P.op("dve", lambda e, ps2=ps2, nv=nv: e.tensor_tensor(out=nv, in0=ps2[:, 0:128], in1=nv, op=ALU.add),
                         reads=[pk2, "num"], writes=["num"])
                    P.op("dve", lambda e, ps2=ps2, dv=dv: e.tensor_tensor(out=dv, in0=ps2[:, 128:256], in1=dv, op=ALU.add),
                         reads=[pk2, "den"], writes=["den"])
    if int(os.environ.get("ATT_LV", "9")) < 9:
        P.op("dve", lambda e: e.memset(o16[:], 0.0), reads=[("lg", 0), ("lg", 1), ("lg", 2), ("pT", 0), ("pT", 1), ("pT", 2), "num", "den"], writes=["o16"])
        cx.store(out, o16[:], ["o16"], "oT")
        return cx.finish()
    P.op("dve", lambda e: e.reciprocal(out=den[:], in_=den[:]), reads=["den"], writes=["den"])
    P.op("dve", lambda e: e.tensor_tensor(out=o16[:], in0=num[:], in1=den[:], op=ALU.mult), reads=["num", "den"], writes=["o16"])
    cx.store(out, o16[:], ["o16"], "oT")
    return cx.finish()


def t5_bucket_np(dist):
    dist = np.asarray(dist, np.int32)
    max_exact = 16
    d32 = np.maximum(dist, 1).astype(np.float32)
    large = max_exact + (np.log(d32 / np.float32(max_exact)) / np.float32(np.log(2048 / 16)) * np.float32(16)).astype(np.int32)
    return np.where(dist < max_exact, dist, np.minimum(large, 31))


def attn_bias_mats(rel_bias, slot):
    Bm = np.full((128, 3, 256), -1e30, np.float32)
    ik = np.arange(128)[:, None]
    jq = np.arange(128)[None, :]
    for g, d in enumerate(DILS):
        head = 4 * g + slot
        rel = jq - ik
        bd = rel_bias[t5_bucket_np(np.maximum(rel, 0) * d), head]
        Bm[:, g, 0:128] = np.where(rel >= 0, bd, np.float32(-1e30))
        rel2 = jq - ik + 128
        bo = rel_bias[t5_bucket_np(np.minimum(rel2, 128) * d), head]
        Bm[:, g, 128:256] = np.where(rel2 <= 128, bo, np.float32(-1e30))
    return Bm


def run_attn(projT, rel_bias):
    nc = get_nc("attn", build_attn)
    ins = []
    for i in range(NCORES):
        b, slot = i // 4, i % 4
        QT = np.zeros((128, 3, SEQ), NPBF)
        KT = np.zeros((128, 3, SEQ), NPBF)
        V = np.zeros((128, 3, 32, 128), NPBF)
        for g, d in enumerate(DILS):
            head = 4 * g + slot
            perm = np.arange(SEQ).reshape(SEQ // d, d).T.reshape(-1)
            QT[:, g, :] = projT[b, 1024 + head * 128:1024 + (head + 1) * 128, :][:, perm]
            KT[:, g, :] = projT[b, 2560 + head * 128:2560 + (head + 1) * 128, :][:, perm]
            vt = projT[b, 4096 + head * 128:4096 + (head + 1) * 128, :][:, perm]
            V[:, g, :, :] = vt.T.reshape(32, 128, 128).transpose(1, 0, 2)
        ins.append({"QT": QT, "KT": KT, "V": V, "Bm": attn_bias_mats(rel_bias, slot)})
    res = run(nc, ins)
    yT = np.zeros((2, 512, SEQ), NPBF)
    for i in range(NCORES):
        b, slot = i // 4, i % 4
        yT[b, slot * 128:(slot + 1) * 128, :] = res[i]["oT"]
    return yT


NPW = 33
TWO_PI = 6.283185307179586
C1 = 6.28125
C2 = TWO_PI - C1


def ssm_nvec():
    n = [7 - k for k in range(8)] + [t - 7 for t in range(8)] + [t + 1 for t in range(8)] + [8 * 2 ** j for j in range(9)]
    return np.asarray(n, np.float32)


def build_ssm():
    cx = Ctx()
    P = cx.P
    G = 8
    NCH = 512
    Ud = cx.din("U", [128, G, 2, NCH], BF16)
    prm = cx.din("prm", [128, 3, G], F32)
    Bd = cx.din("Bri", [128, 2, G, 16], F32)
    Cd = cx.din("Cri", [128, 2, G, 16], F32)
    Dd = cx.din("Dcol", [128, G], F32)
    nvd = cx.din("nvec", [128, NPW], F32)
    mkd = cx.din("maskT", [128, 128], F32)
    idd = cx.din("ident", [128, 128], F32)
    out = cx.dout("ypre", [128, G, 2, NCH], F32)
    cx.alloc_psum(8)
    U = cx.sb("U_sb", [128, G, 2, NCH], BF16)
    prm_s = cx.sb("prm_s", [128, 3, G], F32)
    Bri = cx.sb("Bri_s", [128, 2, G, 16], F32)
    Cri = cx.sb("Cri_s", [128, 2, G, 16], F32)
    Dcol = cx.sb("Dcol_s", [128, G], F32)
    nvec = cx.sb("nvec_s", [128, NPW], F32)
    maskT = cx.sb("maskT_s", [128, 128], F32)
    ident = cx.sb("ident_s", [128, 128], F32)
    for g in range(G):
        cx.load(U[:, g, :, :].rearrange("p b c -> p (b c)"), Ud[:, g, :, :].rearrange("p b c -> p (b c)"), ("U", g))
    cx.load(prm_s[:], prm, "prm")
    cx.load(Bri[:].rearrange("p a g h -> p (a g h)"), Bd.rearrange("p a g h -> p (a g h)"), "Bri")
    cx.load(Cri[:].rearrange("p a g h -> p (a g h)"), Cd.rearrange("p a g h -> p (a g h)"), "Cri")
    cx.load(Dcol[:], Dd, "Dcol")
    cx.load(nvec[:], nvd, "nvec")
    cx.load(maskT[:], mkd, "maskT")
    cx.load(ident[:], idd, "ident")

    sm = lambda name, shape: cx.sb(name, shape, F32)
    dt_ = sm("dt_", [128, G]); lr = sm("lr", [128, G]); li = sm("li", [128, G])
    ang = sm("ang", [128, G, NPW]); mag = sm("mag", [128, G, NPW]); kf = sm("kf", [128, G, NPW])
    ki = cx.sb("ki", [128, G, NPW], I32)
    msk = sm("msk", [128, G, NPW]); rs = sm("rs", [128, G, NPW]); rc = sm("rc", [128, G, NPW])
    Pr = sm("Pr", [128, G, NPW]); Pi = sm("Pi", [128, G, NPW]); nPi = sm("nPi", [128, G, NPW])
    K = "ssmprep"

    def dve(fn, reads=(), writes=()):
        P.op("dve", fn, reads=[K] + list(reads), writes=[K] + list(writes))

    def act(fn, reads=(), writes=()):
        P.op("act", fn, reads=[K] + list(reads), writes=[K] + list(writes))

    act(lambda e: e.activation(out=dt_[:], in_=prm_s[:, 2, :], func=AF.Exp), reads=["prm"])
    dve(lambda e: e.tensor_tensor(out=lr[:], in0=prm_s[:, 0, :], in1=dt_[:], op=ALU.mult))
    dve(lambda e: e.tensor_tensor(out=li[:], in0=prm_s[:, 1, :], in1=dt_[:], op=ALU.mult))
    for g in range(G):
        dve(lambda e, g=g: e.tensor_scalar(out=ang[:, g, :], in0=nvec[:], scalar1=li[:, g:g + 1], scalar2=None, op0=ALU.mult),
            reads=["nvec"])
        act(lambda e, g=g: e.activation(out=mag[:, g, :], in_=nvec[:], func=AF.Exp, scale=lr[:, g:g + 1]), reads=["nvec"])

    def reduce_sin(src_fn, dst):
        dve(lambda e: e.tensor_scalar(out=kf[:], in0=src_fn(), scalar1=1.0 / TWO_PI, scalar2=None, op0=ALU.mult))
        dve(lambda e: e.tensor_copy(out=ki[:], in_=kf[:]))
        dve(lambda e: e.tensor_copy(out=kf[:], in_=ki[:]))
        dve(lambda e: e.scalar_tensor_tensor(out=dst[:], in0=kf[:], scalar=-C1, in1=src_fn(), op0=ALU.mult, op1=ALU.add))
        dve(lambda e: e.scalar_tensor_tensor(out=dst[:], in0=kf[:], scalar=-C2, in1=dst[:], op0=ALU.mult, op1=ALU.add))
        dve(lambda e: e.tensor_scalar(out=msk[:], in0=dst[:], scalar1=float(np.pi), scalar2=None, op0=ALU.is_gt))
        dve(lambda e: e.scalar_tensor_tensor(out=dst[:], in0=msk[:], scalar=-TWO_PI, in1=dst[:], op0=ALU.mult, op1=ALU.add))
        dve(lambda e: e.tensor_scalar(out=msk[:], in0=dst[:], scalar1=-float(np.pi), scalar2=None, op0=ALU.is_lt))
        dve(lambda e: e.scalar_tensor_tensor(out=dst[:], in0=msk[:], scalar=TWO_PI, in1=dst[:], op0=ALU.mult, op1=ALU.add))
        dve(lambda e: e.tensor_scalar(out=dst[:], in0=dst[:], scalar1=3.1415925, scalar2=-3.1415925, op0=ALU.min, op1=ALU.max))
        act(lambda e: e.activation(out=dst[:], in_=dst[:], func=AF.Sin))

    reduce_sin(lambda: ang[:], rs)
    dve(lambda e: e.tensor_scalar(out=ang[:], in0=ang[:], scalar1=float(np.pi / 2), scalar2=None, op0=ALU.add))
    reduce_sin(lambda: ang[:], rc)
    dve(lambda e: e.tensor_tensor(out=Pr[:], in0=mag[:], in1=rc[:], op=ALU.mult))
    dve(lambda e: e.tensor_tensor(out=Pi[:], in0=mag[:], in1=rs[:], op=ALU.mult))
    dve(lambda e: e.tensor_scalar(out=nPi[:], in0=Pi[:], scalar1=-1.0, scalar2=None, op0=ALU.mult))

    xr = sm("xr", [128, G]); abi = sm("abi", [128, G]); den_ = sm("den_", [128, G]); t1 = sm("t1", [128, G]); t2 = sm("t2", [128, G])
    cr = sm("cr", [128, G]); ci = sm("ci", [128, G]); nci = sm("nci", [128, G])
    are = prm_s[:, 0, :]
    aim = prm_s[:, 1, :]
    dve(lambda e: e.tensor_scalar(out=xr[:], in0=Pr[:, :, 16], scalar1=-1.0, scalar2=None, op0=ALU.add))
    dve(lambda e: e.tensor_copy(out=abi[:], in_=Pi[:, :, 16]))
    dve(lambda e: e.tensor_tensor(out=t1[:], in0=are, in1=are, op=ALU.mult))
    dve(lambda e: e.tensor_tensor(out=t2[:], in0=aim, in1=aim, op=ALU.mult))
    dve(lambda e: e.tensor_tensor(out=den_[:], in0=t1[:], in1=t2[:], op=ALU.add))
    dve(lambda e: e.reciprocal(out=den_[:], in_=den_[:]))
    dve(lambda e: e.tensor_tensor(out=t1[:], in0=xr[:], in1=are, op=ALU.mult))
    dve(lambda e: e.tensor_tensor(out=t2[:], in0=abi[:], in1=aim, op=ALU.mult))
    dve(lambda e: e.tensor_tensor(out=cr[:], in0=t1[:], in1=t2[:], op=ALU.add))
    dve(lambda e: e.tensor_tensor(out=cr[:], in0=cr[:], in1=den_[:], op=ALU.mult))
    dve(lambda e: e.tensor_tensor(out=t1[:], in0=abi[:], in1=are, op=ALU.mult))
    dve(lambda e: e.tensor_tensor(out=t2[:], in0=xr[:], in1=aim, op=ALU.mult))
    dve(lambda e: e.tensor_tensor(out=ci[:], in0=t1[:], in1=t2[:], op=ALU.subtract))
    dve(lambda e: e.tensor_tensor(out=ci[:], in0=ci[:], in1=den_[:], op=ALU.mult))
    dve(lambda e: e.tensor_scalar(out=nci[:], in0=ci[:], scalar1=-1.0, scalar2=None, op0=ALU.mult))

    Bbr = sm("Bbr", [128, G, 16]); Bbi = sm("Bbi", [128, G, 16])
    BX = sm("BX", [128, G, 16]); BY = sm("BY", [128, G, 16]); nBX = sm("nBX", [128, G, 16])
    CX = sm("CX", [128, G, 16]); CY = sm("CY", [128, G, 16])
    for g in range(G):
        dve(lambda e, g=g: e.tensor_scalar(out=Bbr[:, g, :], in0=Bri[:, 0, g, :], scalar1=cr[:, g:g + 1], scalar2=None, op0=ALU.mult), reads=["Bri"])
        dve(lambda e, g=g: e.scalar_tensor_tensor(out=Bbr[:, g, :], in0=Bri[:, 1, g, :], scalar=nci[:, g:g + 1], in1=Bbr[:, g, :],
                                               op0=ALU.mult, op1=ALU.add))
        dve(lambda e, g=g: e.tensor_scalar(out=Bbi[:, g, :], in0=Bri[:, 1, g, :], scalar1=cr[:, g:g + 1], scalar2=None, op0=ALU.mult))
        dve(lambda e, g=g: e.scalar_tensor_tensor(out=Bbi[:, g, :], in0=Bri[:, 0, g, :], scalar=ci[:, g:g + 1], in1=Bbi[:, g, :],
                                               op0=ALU.mult, op1=ALU.add))
    lo, hi = slice(0, 64), slice(64, 128)
    dve(lambda e: e.tensor_copy(out=BX[lo], in_=Bbr[lo]))
    dve(lambda e: e.tensor_copy(out=BX[hi], in_=Bbi[hi]))
    dve(lambda e: e.tensor_scalar(out=BY[lo], in0=Bbi[lo], scalar1=-1.0, scalar2=None, op0=ALU.mult))
    dve(lambda e: e.tensor_copy(out=BY[hi], in_=Bbr[hi]))
    dve(lambda e: e.tensor_scalar(out=nBX[:], in0=BX[:], scalar1=-1.0, scalar2=None, op0=ALU.mult))
    dve(lambda e: e.tensor_copy(out=CX[lo], in_=Cri[lo, 0, :, :]), reads=["Cri"])
    dve(lambda e: e.tensor_scalar(out=CX[hi], in0=Cri[hi, 1, :, :], scalar1=-1.0, scalar2=None, op0=ALU.mult))
    dve(lambda e: e.tensor_scalar(out=CY[lo], in0=Cri[lo, 1, :, :], scalar1=-1.0, scalar2=None, op0=ALU.mult))
    dve(lambda e: e.tensor_scalar(out=CY[hi], in0=Cri[hi, 0, :, :], scalar1=-1.0, scalar2=None, op0=ALU.mult))

    BcA = sm("BcA", [128, G, 8, 16]); BcB = sm("BcB", [128, G, 8, 16]); Cm = sm("Cm", [128, G, 8, 16]); Cc = sm("Cc", [128, G, 8, 16])
    for g in range(G):
        for j in range(8):
            dve(lambda e, g=g, j=j: e.tensor_scalar(out=BcA[:, g, j, :], in0=BX[:, g, :], scalar1=Pr[:, g, j:j + 1], scalar2=None, op0=ALU.mult))
            dve(lambda e, g=g, j=j: e.scalar_tensor_tensor(out=BcA[:, g, j, :], in0=BY[:, g, :], scalar=Pi[:, g, j:j + 1], in1=BcA[:, g, j, :],
                                                        op0=ALU.mult, op1=ALU.add))
            dve(lambda e, g=g, j=j: e.tensor_scalar(out=BcB[:, g, j, :], in0=BY[:, g, :], scalar1=Pr[:, g, j:j + 1], scalar2=None, op0=ALU.mult))
            dve(lambda e, g=g, j=j: e.scalar_tensor_tensor(out=BcB[:, g, j, :], in0=nBX[:, g, :], scalar=Pi[:, g, j:j + 1], in1=BcB[:, g, j, :],
                                                        op0=ALU.mult, op1=ALU.add))
            for dst, k0 in ((Cm, 8), (Cc, 16)):
                dve(lambda e, g=g, j=j, dst=dst, k0=k0: e.tensor_scalar(out=dst[:, g, j, :], in0=CX[:, g, :], scalar1=Pr[:, g, k0 + j:k0 + j + 1],
                                                                   scalar2=None, op0=ALU.mult))
                dve(lambda e, g=g, j=j, dst=dst, k0=k0: e.scalar_tensor_tensor(out=dst[:, g, j, :], in0=CY[:, g, :], scalar=Pi[:, g, k0 + j:k0 + j + 1],
                                                                          in1=dst[:, g, j, :], op0=ALU.mult, op1=ALU.add))

    MT16 = cx.sb("MT16", [128, G, 128], BF16)
    BaT16 = cx.sb("BaT16", [128, G, 128], BF16)
    BbT16 = cx.sb("BbT16", [128, G, 128], BF16)
    Cc16 = cx.sb("Cc16", [128, G, 128], BF16)
    dve(lambda e: e.tensor_copy(out=Cc16[:].rearrange("p g n -> p (g n)"), in_=Cc[:].rearrange("p g t h -> p (g t h)")), writes=["Cc16"])
    for g in range(G):
        bca = BcA[:, g, :, :].rearrange("p s h -> p (s h)")
        bcb = BcB[:, g, :, :].rearrange("p s h -> p (s h)")
        cm = Cm[:, g, :, :].rearrange("p t h -> p (t h)")
        ps, pk = cx.psum()
        P.op("pe", lambda e, ps=ps, bca=bca, cm=cm: e.matmul(ps[:, 0:128], lhsT=bca, rhs=cm, start=True, stop=True), reads=[K], writes=[pk])
        P.op("dve", lambda e, ps=ps, g=g: e.tensor_tensor(out=MT16[:, g, :], in0=ps[:, 0:128], in1=maskT[:], op=ALU.mult),
             reads=[pk, "maskT"], writes=[("MT16", g)])
        ps, pk = cx.psum()
        P.op("pe", lambda e, ps=ps, bca=bca: e.transpose(ps[:, 0:128], bca, ident[:]), reads=[K, "ident"], writes=[pk])
        P.op("dve", lambda e, ps=ps, g=g: e.tensor_copy(out=BaT16[:, g, :], in_=ps[:, 0:128]), reads=[pk], writes=[("BaT16", g)])
        ps, pk = cx.psum()
        P.op("pe", lambda e, ps=ps, bcb=bcb: e.transpose(ps[:, 0:128], bcb, ident[:]), reads=[K, "ident"], writes=[pk])
        P.op("dve", lambda e, ps=ps, g=g: e.tensor_copy(out=BbT16[:, g, :], in_=ps[:, 0:128]), reads=[pk], writes=[("BbT16", g)])

    SA = [cx.sb("SA%d" % i, [128, 2, NCH], F32) for i in range(2)]
    SB = [cx.sb("SB%d" % i, [128, 2, NCH], F32) for i in range(2)]
    S16 = [cx.sb("S16_%d" % i, [128, 2, NCH], BF16) for i in range(2)]
    ysb = [cx.sb("ysb%d" % i, [128, 2, NCH], F32) for i in range(2)]
    for i in range(2):
        P.op("pool", lambda e, i=i: e.memset(S16[i][:], 0.0), writes=[("S16", i)])
    for g in range(G):
        for (WT, dst, dk) in ((BaT16, SA[0], "SA0"), (BbT16, SB[0], "SB0")):
            for b in range(2):
                ps, pk = cx.psum()
                P.op("pe", lambda e, ps=ps, WT=WT, g=g, b=b: e.matmul(ps[:], lhsT=WT[:, g, :], rhs=U[:, g, b, :], start=True, stop=True),
                     reads=[("BaT16", g), ("BbT16", g), ("U", g)], writes=[pk])
                P.op("dve", lambda e, ps=ps, dst=dst, b=b: e.tensor_copy(out=dst[:, b, :], in_=ps[:]), reads=[pk], writes=[(dk, b)])
        cur = 0
        for j in range(9):
            d = 2 ** j
            nxt = 1 - cur
            oA, oB, nA, nB = SA[cur], SB[cur], SA[nxt], SB[nxt]
            kA = ["SA%d" % cur, ("SA%d" % cur, 0), ("SA%d" % cur, 1)]
            kB = ["SB%d" % cur, ("SB%d" % cur, 0), ("SB%d" % cur, 1)]
            wA = ["SA%d" % nxt, ("SA%d" % nxt, 0), ("SA%d" % nxt, 1)]
            wB = ["SB%d" % nxt, ("SB%d" % nxt, 0), ("SB%d" % nxt, 1)]
            pr = Pr[:, g, 24 + j:25 + j]
            pi = Pi[:, g, 24 + j:25 + j]
            npi = nPi[:, g, 24 + j:25 + j]
            P.op("dve", lambda e, oA=oA, nA=nA, d=d, pr=pr: e.scalar_tensor_tensor(
                out=nA[:, :, d:NCH], in0=oA[:, :, 0:NCH - d], scalar=pr, in1=oA[:, :, d:NCH], op0=ALU.mult, op1=ALU.add),
                reads=kA + [K], writes=wA)
            P.op("dve", lambda e, oB=oB, nA=nA, d=d, pi=pi: e.scalar_tensor_tensor(
                out=nA[:, :, d:NCH], in0=oB[:, :, 0:NCH - d], scalar=pi, in1=nA[:, :, d:NCH], op0=ALU.mult, op1=ALU.add),
                reads=kB + wA, writes=wA)
            P.op("act", lambda e, oA=oA, nA=nA, d=d: e.activation(out=nA[:, :, 0:d], in_=oA[:, :, 0:d], func=AF.Identity), reads=kA, writes=wA)
            P.op("dve", lambda e, oB=oB, nB=nB, d=d, pr=pr: e.scalar_tensor_tensor(
                out=nB[:, :, d:NCH], in0=oB[:, :, 0:NCH - d], scalar=pr, in1=oB[:, :, d:NCH], op0=ALU.mult, op1=ALU.add),
                reads=kB, writes=wB)
            P.op("dve", lambda e, oA=oA, nB=nB, d=d, npi=npi: e.scalar_tensor_tensor(
                out=nB[:, :, d:NCH], in0=oA[:, :, 0:NCH - d], scalar=npi, in1=nB[:, :, d:NCH], op0=ALU.mult, op1=ALU.add),
                reads=kA + wB, writes=wB)
            P.op("act", lambda e, oB=oB, nB=nB, d=d: e.activation(out=nB[:, :, 0:d], in_=oB[:, :, 0:d], func=AF.Identity), reads=kB, writes=wB)
            cur = nxt
        fin = SA[cur]
        s16 = S16[g % 2]
        P.op("act", lambda e, fin=fin, s16=s16: e.activation(out=s16[:, :, 1:NCH], in_=fin[:, :, 0:NCH - 1], func=AF.Identity),
             reads=["SA%d" % cur], writes=[("S16", g % 2)])
        for b in range(2):
            ps, pk = cx.psum()
            P.op("pe", lambda e, ps=ps, g=g, b=b: e.matmul(ps[:], lhsT=MT16[:, g, :], rhs=U[:, g, b, :], start=True, stop=False),
                 reads=[("MT16", g), ("U", g)], writes=[pk], signal=False)
            P.op("pe", lambda e, ps=ps, g=g, b=b, s16=s16: e.matmul(ps[:], lhsT=Cc16[:, g, :], rhs=s16[:, b, :], start=False, stop=True),
                 reads=["Cc16", ("S16", g % 2)], writes=[pk])
            y = ysb[g % 2]
            P.op("dve", lambda e, ps=ps, g=g, b=b, y=y: e.scalar_tensor_tensor(
                out=y[:, b, :], in0=U[:, g, b, :], scalar=Dcol[:, g:g + 1], in1=ps[:], op0=ALU.mult, op1=ALU.add),
                reads=[pk, "Dcol", ("U", g)], writes=[("ysb", g % 2, b)])
        cx.store(out[:, g, :, :].rearrange("p b c -> p (b c)"), ysb[g % 2][:].rearrange("p b c -> p (b c)"),
                 [("ysb", g % 2, 0), ("ysb", g % 2, 1)], ("out", g), sem=("st_ysb", g % 2))
    return cx.finish()


def ssm_inputs(projT, l, inp, core):
    G = 8
    gs = slice(core * G, (core + 1) * G)
    u = projT[:, core * 128:(core + 1) * 128, :]
    U = u.reshape(2, G, 16, 512, 8).transpose(4, 2, 1, 0, 3).reshape(128, G, 2, 512)
    dup = lambda a: np.concatenate([a, a], axis=0)
    are = inp["ssm_a_re"][l][gs].T
    aim = inp["ssm_a_im"][l][gs].T
    ldt = np.broadcast_to(inp["ssm_log_dt"][l][gs][None, :], (64, G))
    prm = dup(np.stack([are, aim, ldt], axis=1))
    Bri = dup(np.stack([inp["ssm_b_re"][l][gs].transpose(1, 0, 2), inp["ssm_b_im"][l][gs].transpose(1, 0, 2)], axis=1))
    Cri = dup(np.stack([inp["ssm_c_re"][l][gs].transpose(2, 0, 1), inp["ssm_c_im"][l][gs].transpose(2, 0, 1)], axis=1))
    dsk = inp["ssm_d"][l][core * 128:(core + 1) * 128].reshape(G, 16)
    Dcol = np.tile(dsk.T, (8, 1))
    sidx = np.arange(128) // 16
    maskT = (sidx[:, None] <= sidx[None, :]).astype(np.float32)
    return {"U": np.ascontiguousarray(U), "prm": np.ascontiguousarray(prm, dtype=np.float32),
            "Bri": np.ascontiguousarray(Bri, dtype=np.float32), "Cri": np.ascontiguousarray(Cri, dtype=np.float32),
            "Dcol": np.ascontiguousarray(Dcol, dtype=np.float32),
            "nvec": np.ascontiguousarray(np.broadcast_to(ssm_nvec()[None, :], (128, NPW))),
            "maskT": maskT, "ident": np.eye(128, dtype=np.float32)}


def run_ssm(projT, l, inp):
    nc = get_nc("ssm", build_ssm)
    ins = [ssm_inputs(projT, l, inp, i) for i in range(NCORES)]
    res = run(nc, ins)
    yT = np.zeros((2, 1024, SEQ), np.float32)
    for i in range(NCORES):
        y = res[i]["ypre"]
        yT[:, i * 128:(i + 1) * 128, :] = y.reshape(8, 16, 8, 2, 512).transpose(3, 2, 1, 4, 0).reshape(2, 128, SEQ)
    return yT


def build_post(moe):
    cx = Ctx()
    P = cx.P
    xTd = cx.din("xT", [128, DC, T], F32)
    ypd = cx.din("ypreT", [128, 8, T], F32)
    yad = cx.din("yattT", [128, 4, T], BF16)
    gsd = cx.din("gsT", [128, DC, T], BF16)
    gad = cx.din("gaT", [128, DC, T], BF16)
    modd = cx.din("modv", [128, 6, DC], F32)
    gnd = cx.din("gnorm", [128, DC], F32)
    bgd = cx.din("bglu", [128, 8], F32)
    Wglu = cx.din("w_glu", [1024, 1024], F32)
    Wbs = cx.din("w_bs", [1024, D], F32)
    Wba = cx.din("w_ba", [512, D], F32)
    Wout = cx.din("w_out", [D, D], F32)
    xo = cx.dout("xT_out", [128, DC, T], F32)
    ho = cx.dout("h2T", [128, DC, T], BF16)
    if moe:
        wrd = cx.din("w_router", [128, DC, NEXP], F32)
        rwo = cx.dout("rw", [T // 128, 128, NEXP], F32)
    cx.alloc_psum(8)
    xT = cx.sb("xT_sb", [128, DC, T], F32)
    bufA = cx.sb("bufA", [128, DC, T], BF16)
    y32 = bufA[:].bitcast(F32).rearrange("p (k t) -> p k t", k=8) if False else None
    y32t = cx.sb("y32", [128, 8, T // 2], F32) if False else None
    y16 = cx.sb("y16", [128, 8, T], BF16)
    yg16 = cx.sb("yg16", [128, 8, T], BF16)
    ya16 = cx.sb("ya16", [128, 4, T], BF16)
    modv = cx.sb("modv_sb", [128, 6, DC], F32)
    gnorm = cx.sb("gnorm_sb", [128, DC], F32)
    gm = cx.sb("gm_sb", [128, DC], F32)
    bglu = cx.sb("bglu_sb", [128, 8], F32)
    ones16 = cx.sb("ones16", [128, 128], BF16)
    P.op("pool", lambda e: e.memset(ones16[:], 1.0), writes=["ones16"])
    for k0 in range(0, DC, 4):
        P.dma("sp", lambda e, k0=k0: e.dma_start(out=xT[:, k0:k0 + 4, :], in_=xTd[:, k0:k0 + 4, :]), writes=["xT"], sem="xT")
    cx.load(ya16[:], yad, "ya16")
    cx.load(modv[:], modd, "modv_in")
    cx.load(gnorm[:], gnd, "gnorm")
    cx.load(bglu[:], bgd, "bglu")
    wl = WLoader(cx, "wl", DC, 128, nslots=4, nstage=4)

    y32 = bufA[:].rearrange("p k t -> p (k t)").bitcast(F32).rearrange("p (k t) -> p k t", k=8)
    yp = [cx.sb("yp%d" % i, [128, T], F32) for i in range(2)]
    ta = [cx.sb("ta%d" % i, [128, T], F32) for i in range(2)]
    for kc in range(8):
        s = kc % 2
        cx.load(yp[s][:], ypd[:, kc, :], ("yp", s))
        P.op("act", lambda e, s=s: e.activation(out=ta[s][:], in_=yp[s][:], func=AF.Square), reads=[("yp", s)], writes=[("ta", s)])
        P.op("dve", lambda e, s=s: e.tensor_scalar(out=ta[s][:], in0=ta[s][:], scalar1=0.044715, scalar2=1.0, op0=ALU.mult, op1=ALU.add),
             reads=[("ta", s)], writes=[("ta", s)])
        P.op("dve", lambda e, s=s: e.tensor_tensor(out=ta[s][:], in0=ta[s][:], in1=yp[s][:], op=ALU.mult), reads=[("ta", s), ("yp", s)], writes=[("ta", s)])
        P.op("act", lambda e, s=s: e.activation(out=ta[s][:], in_=ta[s][:], func=AF.Sigmoid, scale=1.5957691216057308),
             reads=[("ta", s)], writes=[("ta", s)])
        P.op("dve", lambda e, s=s, kc=kc: e.tensor_tensor(out=y32[:, kc, :], in0=ta[s][:], in1=yp[s][:], op=ALU.mult),
             reads=[("ta", s), ("yp", s)], writes=["bufA"])
        P.op("pool", lambda e, kc=kc: e.tensor_copy(out=y16[:, kc, :], in_=y32[:, kc, :]), reads=["bufA"], writes=["y16"])

    sg = [cx.sb("sg%d" % i, [128, 512], F32) for i in range(2)]
    cnt = [0]

    def evac_glu(nt, half, ps, pk):
        s = cnt[0] % 2
        cnt[0] += 1
        hs = slice(half * 512, (half + 1) * 512)
        P.op("act", lambda e: e.activation(out=sg[s][:], in_=ps[:], func=AF.Sigmoid, bias=bglu[:, nt:nt + 1]),
             reads=[pk, "bglu"], writes=[("sg", s)])
        P.op("dve", lambda e: e.tensor_tensor(out=yg16[:, nt, hs], in0=sg[s][:], in1=y32[:, nt, hs], op=ALU.mult),
             reads=[("sg", s), "bufA"], writes=["yg16"])

    wl.KC = 8
    emit_linear(cx, wl, Wglu, 8, 1024, y16, ["y16"], evac_glu, ngrp=128)

    gts = [cx.sb("gts%d" % i, [128, 512], BF16) for i in range(2)]
    gta = [cx.sb("gta%d" % i, [128, 512], BF16) for i in range(2)]
    m1 = [cx.sb("m1_%d" % i, [128, 512], F32) for i in range(2)]
    m2 = [cx.sb("m2_%d" % i, [128, 512], F32) for i in range(2)]
    it = 0
    def load_c(nt_):
        wl.KC = 8
        a_ = wl.load(Wbs[:, nt_ * 128:(nt_ + 1) * 128], 128)
        wl.KC = 4
        b_ = wl.load(Wba[:, nt_ * 128:(nt_ + 1) * 128], 128)
        return a_, b_

    nxtc = load_c(0)
    for nt in range(DC):
        (wbs, kbs), (wba, kba) = nxtc
        if nt + 1 < DC:
            nxtc = load_c(nt + 1)
        for half in range(2):
            s = it % 2
            it += 1
            hs = slice(half * 512, (half + 1) * 512)
            cx.load(gts[s][:], gsd[:, nt, hs], ("gts", s))
            cx.load(gta[s][:], gad[:, nt, hs], ("gta", s))
            ps1, pk1 = cx.psum()
            mm_group(cx, ps1[:], pk1, [(wbs[:, kc, 0:128], yg16[:, kc, hs]) for kc in range(8)], list(kbs) + ["yg16"])
            ps2, pk2 = cx.psum()
            mm_group(cx, ps2[:], pk2, [(wba[:, kc, 0:128], ya16[:, kc, hs]) for kc in range(4)], list(kba) + ["ya16"])
            P.op("act", lambda e, s=s: e.activation(out=m1[s][:], in_=gts[s][:], func=AF.Sigmoid), reads=[("gts", s)], writes=[("m1", s)])
            P.op("act", lambda e, s=s: e.activation(out=m2[s][:], in_=gta[s][:], func=AF.Sigmoid), reads=[("gta", s)], writes=[("m2", s)])
            P.op("dve", lambda e, s=s, ps1=ps1: e.tensor_tensor(out=m1[s][:], in0=m1[s][:], in1=ps1[:], op=ALU.mult), reads=[("m1", s), pk1], writes=[("m1", s)])
            P.op("dve", lambda e, s=s, ps2=ps2: e.tensor_tensor(out=m2[s][:], in0=m2[s][:], in1=ps2[:], op=ALU.mult), reads=[("m2", s), pk2], writes=[("m2", s)])
            P.op("pool", lambda e, s=s, nt=nt, hs=hs: e.tensor_tensor(out=bufA[:, nt, hs], in0=m1[s][:], in1=m2[s][:], op=ALU.add),
                 reads=[("m1", s), ("m2", s)], writes=["bufA"])

    def evac_out(nt, half, ps, pk):
        hs = slice(half * 512, (half + 1) * 512)
        P.op("dve", lambda e: e.scalar_tensor_tensor(out=xT[:, nt, hs], in0=ps[:], scalar=modv[:, 2, nt:nt + 1], in1=xT[:, nt, hs],
                                                    op0=ALU.mult, op1=ALU.add), reads=[pk, "modv_in", "xT"], writes=["xT"])

    wl.KC = DC
    emit_linear(cx, wl, Wout, DC, D, bufA, ["bufA"], evac_out, ngrp=128)
    for k0 in range(0, DC, 4):
        cx.store(xo[:, k0:k0 + 4, :], xT[:, k0:k0 + 4, :], ["xT"], ("xo", k0), sem="st_x")

    P.op("dve", lambda e: e.tensor_scalar(out=gm[:], in0=modv[:, 4, :], scalar1=1.0, scalar2=None, op0=ALU.add), reads=["modv_in"], writes=["gm_tmp"])
    P.op("dve", lambda e: e.tensor_tensor(out=gm[:], in0=gm[:], in1=gnorm[:], op=ALU.mult), reads=["gm_tmp", "gnorm"], writes=["modv"])
    rstd = emit_rmsnorm_mod(cx, xT, "xT", gm, modv[:, 3, :], bufA, "bufA", ones16, "n2")
    for k0 in range(0, DC, 4):
        cx.store(ho[:, k0:k0 + 4, :], bufA[:, k0:k0 + 4, :], ["bufA"], ("ho", k0), sem="st_h")

    if moe:
        wr = cx.sb("wr_sb", [128, DC, NEXP], F32)
        cx.load(wr[:], wrd, "wr")
        h32 = [cx.sb("h32_%d" % i, [128, 128], F32) for i in range(3)]
        lgt = cx.sb("lgt", [128, NEXP], F32)
        mx8 = cx.sb("mx8", [128, 8], F32)
        nm1 = cx.sb("nm1", [128, 1], F32)
        sel = cx.sb("sel", [128, NEXP], F32)
        ex = cx.sb("ex", [128, NEXP], F32)
        ssum = cx.sb("ssum", [128, 1], F32)
        rwt = [cx.sb("rwt%d" % i, [128, NEXP], F32) for i in range(2)]
        R = "router"
        for tt in range(T // 128):
            ts_ = slice(tt * 128, (tt + 1) * 128)
            ps, pk = cx.psum()
            for kc in range(DC):
                s = kc % 3
                P.op("dve", lambda e, s=s, kc=kc, ts_=ts_: e.tensor_tensor(out=h32[s][:], in0=xT[:, kc, ts_], in1=rstd[:, ts_], op=ALU.mult),
                     reads=["xT", ("n2", "rstd", tt // 4)], writes=[("h32", s)])
                P.op("act", lambda e, s=s, kc=kc: e.activation(out=h32[s][:], in_=h32[s][:], func=AF.Identity, scale=gm[:, kc:kc + 1],
                                                            bias=modv[:, 3, kc:kc + 1]), reads=[("h32", s), "modv"], writes=[("h32", s)])
                P.op("pe", lambda e, s=s, kc=kc, ps=ps: e.matmul(ps[:, 0:NEXP], lhsT=h32[s][:], rhs=wr[:, kc, :], start=(kc == 0), stop=(kc == DC - 1)),
                     reads=[("h32", s), "wr"], writes=[pk], signal=True)
            P.op("dve", lambda e, ps=ps: e.tensor_copy(out=lgt[:], in_=ps[:, 0:NEXP]), reads=[pk, R], writes=[R])
            P.op("dve", lambda e: e.max(out=mx8[:], in_=lgt[:]), reads=[R], writes=[R])
            P.op("dve", lambda e: e.tensor_scalar(out=nm1[:], in0=mx8[:, 0:1], scalar1=-1.0, scalar2=None, op0=ALU.mult), reads=[R], writes=[R])
            P.op("dve", lambda e: e.tensor_scalar(out=sel[:], in0=lgt[:], scalar1=mx8[:, 1:2], scalar2=None, op0=ALU.is_ge), reads=[R], writes=[R])
            P.op("act", lambda e: e.activation(out=ex[:], in_=lgt[:], func=AF.Exp, bias=nm1[:, 0:1]), reads=[R], writes=[R])
            P.op("dve", lambda e: e.tensor_tensor(out=ex[:], in0=ex[:], in1=sel[:], op=ALU.mult), reads=[R], writes=[R])
            P.op("dve", lambda e: e.tensor_reduce(out=ssum[:], in_=ex[:], axis=mybir.AxisListType.X, op=ALU.add), reads=[R], writes=[R])
            P.op("dve", lambda e: e.reciprocal(out=ssum[:], in_=ssum[:]), reads=[R], writes=[R])
            o = rwt[tt % 2]
            P.op("dve", lambda e, o=o: e.tensor_scalar(out=o[:], in0=ex[:], scalar1=ssum[:, 0:1], scalar2=None, op0=ALU.mult),
                 reads=[R], writes=[R, ("rwt", tt % 2)])
            cx.store(rwo[tt], o[:], [("rwt", tt % 2)], ("rwo", tt), sem=("st_rw", tt % 2))
    return cx.finish()


def post_inputs(i, xTs, ypreT, yattT, projT, mod_l, l, inp, moe):
    b, q = i // 4, i % 4
    ts_ = slice(q * T, (q + 1) * T)
    m = {"xT": xTs[i], "ypreT": fm(ypreT[b][:, ts_]), "yattT": fm(yattT[b][:, ts_]),
         "gsT": fm(projT[b, 5632:7680, ts_]), "gaT": fm(projT[b, 7680:9728, ts_]),
         "modv": modv_layout(mod_l[b]), "gnorm": vec_fm(inp["norm_ffn_g"][l]), "bglu": vec_fm(inp["b_glu"][l]),
         "w_glu": np.ascontiguousarray(inp["w_glu"][l]), "w_bs": np.ascontiguousarray(inp["w_branch_ssm"][l]),
         "w_ba": np.ascontiguousarray(inp["w_branch_att"][l]), "w_out": np.ascontiguousarray(inp["w_out"][l])}
    if moe:
        m["w_router"] = np.ascontiguousarray(inp["moe_router"][l // 2].reshape(DC, 128, NEXP).transpose(1, 0, 2))
    return m


def run_post(xTs, ypreT, yattT, projT, mod_l, l, inp, moe):
    nc = get_nc("post%d" % int(moe), lambda: build_post(moe))
    ins = [post_inputs(i, xTs, ypreT, yattT, projT, mod_l, l, inp, moe) for i in range(NCORES)]
    res = run(nc, ins)
    xo = [res[i]["xT_out"] for i in range(NCORES)]
    h2 = [res[i]["h2T"] for i in range(NCORES)]
    rw = [res[i]["rw"].reshape(T, NEXP) for i in range(NCORES)] if moe else None
    return xo, h2, rw


FG = 4


def ffn_bufs(cx, tag, with_rw):
    g16 = [cx.sb("%s_g16_%d" % (tag, i), [128, FG, T], BF16) for i in range(2)]
    sgl = [cx.sb("%s_sg%d" % (tag, i), [128, 512], F32) for i in range(2)]
    tt = [cx.sb("%s_tt%d" % (tag, i), [128, 512], F32) for i in range(2)] if with_rw else None
    return g16, sgl, tt


def emit_ffn(cx, bufs, hT, hkeys, nft, get_gu, get_d, out_evac, tag, rwb=None, rwkey=None):
    P = cx.P
    g16, sgl, tt = bufs
    it = 0
    ngroups = (nft + FG - 1) // FG
    for fg in range(ngroups):
        gb = g16[fg % 2]
        nj = min(FG, nft - fg * FG)
        for j in range(nj):
            ft = fg * FG + j
            gu = get_gu(ft)
            wg, wu, wkeys = gu[0], gu[1], gu[2]
            co = gu[3] if len(gu) > 3 else 0
            for half in range(T // 512):
                hs = slice(half * 512, (half + 1) * 512)
                psg, pkg = cx.psum()
                mm_group(cx, psg[:], pkg, [(wg[:, kc, co:co + 128], hT[:, kc, hs]) for kc in range(DC)], list(wkeys) + list(hkeys))
                psu, pku = cx.psum()
                mm_group(cx, psu[:], pku, [(wu[:, kc, co:co + 128], hT[:, kc, hs]) for kc in range(DC)], list(wkeys) + list(hkeys))
                s = it % 2
                it += 1
                P.op("act", lambda e, s=s, psg=psg: e.activation(out=sgl[s][:], in_=psg[:], func=AF.Silu), reads=[pkg], writes=[(tag, "sg", s)])
                if rwb is None:
                    P.op("dve", lambda e, s=s, psu=psu, gb=gb, j=j, hs=hs: e.tensor_tensor(out=gb[:, j, hs], in0=sgl[s][:], in1=psu[:], op=ALU.mult),
                         reads=[(tag, "sg", s), pku], writes=[(tag, "g16", fg % 2, j)])
                else:
                    P.op("dve", lambda e, s=s, psu=psu: e.tensor_tensor(out=tt[s][:], in0=sgl[s][:], in1=psu[:], op=ALU.mult),
                         reads=[(tag, "sg", s), pku], writes=[(tag, "tt", s)])
                    P.op("pool", lambda e, s=s, gb=gb, j=j, hs=hs: e.tensor_tensor(out=gb[:, j, hs], in0=tt[s][:], in1=rwb[:, hs], op=ALU.mult),
                         reads=[(tag, "tt", s), rwkey], writes=[(tag, "g16", fg % 2, j)])
        wd, dkeys = get_d(fg)
        for dc in range(DC):
            for half in range(T // 512):
                hs = slice(half * 512, (half + 1) * 512)
                ps, pk = cx.psum()
                mm_group(cx, ps[:], pk, [(wd[:, j, dc * 128:(dc + 1) * 128], gb[:, j, hs]) for j in range(nj)],
                         list(dkeys) + [(tag, "g16", fg % 2, j) for j in range(nj)])
                out_evac(fg, dc, half, ps, pk)


class DLoader:
    def __init__(self, cx, name):
        self.cx = cx
        self.name = name
        self.wd = cx.sb(name + "_wd", [128, FG, D], BF16)
        self.stg = [cx.sb("%s_st%d" % (name, i), [128, D], F32) for i in range(2)]
        self.si = 0

    def load(self, Wd, fg, nj):
        cx = self.cx
        keys = []
        for j in range(nj):
            ft = fg * FG + j
            s = self.si % 2
            self.si += 1
            stg = self.stg[s]
            skey = (self.name, "st", s)
            cx.P.dma("sp", lambda e, stg=stg, ft=ft: e.dma_start(out=stg[:], in_=Wd[ft * 128:(ft + 1) * 128, :]), writes=[skey])
            key = (self.name, "wd", j)
            cx.copy(cx.conv_eng(), self.wd[:, j, :], stg[:], [skey], [key])
            keys.append(key)
        return self.wd, keys


def build_ffn():
    cx = Ctx()
    P = cx.P
    xTd = cx.din("xT", [128, DC, T], F32)
    hTd = cx.din("h2T", [128, DC, T], BF16)
    gfd = cx.din("gatef", [128, DC], F32)
    Wg = cx.din("w_gate", [D, DFF], F32)
    Wu = cx.din("w_up", [D, DFF], F32)
    Wd = cx.din("w_down", [DFF, D], F32)
    xo = cx.dout("xT_out", [128, DC, T], F32)
    cx.alloc_psum(8)
    xT = cx.sb("xT_sb", [128, DC, T], F32)
    hT = cx.sb("hT_sb", [128, DC, T], BF16)
    gf = cx.sb("gf_sb", [128, DC], F32)
    for k0 in range(0, DC, 4):
        P.dma("sp", lambda e, k0=k0: e.dma_start(out=hT[:, k0:k0 + 4, :], in_=hTd[:, k0:k0 + 4, :]), writes=["hT"], sem="hT")
    for k0 in range(0, DC, 4):
        P.dma("sp", lambda e, k0=k0: e.dma_start(out=xT[:, k0:k0 + 4, :], in_=xTd[:, k0:k0 + 4, :]), writes=["xT"], sem="xT")
    cx.load(gf[:], gfd, "gf")
    wl = WLoader(cx, "wgu", DC, 256, nslots=4, nstage=4)
    dl = DLoader(cx, "wdl")
    nft = DFF // 128
    cache = {}

    dcache = {}

    def load_pair(base):
        if base not in cache and base < nft:
            wg, k1 = wl.load(Wg[:, base * 128:base * 128 + 256], 256)
            wu, k2 = wl.load(Wu[:, base * 128:base * 128 + 256], 256)
            cache[base] = (wg, wu, list(k1) + list(k2))

    def get_gu(ft):
        base = ft - ft % 2
        if ft % FG == 0 and (ft // FG) not in dcache:
            dcache[ft // FG] = dl.load(Wd, ft // FG, min(FG, nft - ft))
        load_pair(base)
        if ft % 2 == 0:
            load_pair(base + 2)
            for k in [k for k in cache if k < base]:
                del cache[k]
        wg, wu, keys = cache[base]
        return wg, wu, keys, (ft % 2) * 128

    def get_d(fg):
        return dcache[fg]

    def out_evac(fg, dc, half, ps, pk):
        hs = slice(half * 512, (half + 1) * 512)
        P.op("dve", lambda e: e.scalar_tensor_tensor(out=xT[:, dc, hs], in0=ps[:], scalar=gf[:, dc:dc + 1], in1=xT[:, dc, hs],
                                                    op0=ALU.mult, op1=ALU.add), reads=[pk, "gf", "xT"], writes=["xT"])

    emit_ffn(cx, ffn_bufs(cx, "ffn", False), hT, ["hT"], nft, get_gu, get_d, out_evac, "ffn")
    for k0 in range(0, DC, 4):
        cx.store(xo[:, k0:k0 + 4, :], xT[:, k0:k0 + 4, :], ["xT"], ("xo", k0), sem="st_x")
    return cx.finish()


NCHUNK = SEQ * 2 // T


def build_moe():
    cx = Ctx()
    P = cx.P
    nc = cx.nc
    hTd = cx.din("h2T", [NCHUNK, 128, DC, T], BF16)
    rwd = cx.din("rwb", [NCHUNK, 128, T], F32)
    Wg = cx.din("w_gate", [D, DFFE], F32)
    Wu = cx.din("w_up", [D, DFFE], F32)
    Wd = cx.din("w_down", [DFFE, D], F32)
    po = cx.dout("partial", [NCHUNK, 128, DC, T], BF16)
    Wg16 = nc.dram_tensor("Wg16", [D, DFFE], BF16, kind="Internal").ap()
    Wu16 = nc.dram_tensor("Wu16", [D, DFFE], BF16, kind="Internal").ap()
    Wd16 = nc.dram_tensor("Wd16", [DFFE, D], BF16, kind="Internal").ap()
    cx.alloc_psum(8)
    nft = DFFE // 128
    stg = [cx.sb("cv_st%d" % i, [128, 4, 512], F32) for i in range(3)]
    o16 = [cx.sb("cv_o%d" % i, [128, 4, 512], BF16) for i in range(3)]
    ci = 0
    for (Wsrc, Wdst, name, KC_, ncols) in ((Wg, Wg16, "Wg16", DC, DFFE), (Wu, Wu16, "Wu16", DC, DFFE), (Wd, Wd16, "Wd16", nft, D)):
        for kq in range(0, KC_, 4):
            for cb in range(ncols // 512):
                s = ci % 3
                ci += 1
                src = Wsrc[kq * 128:(kq + 4) * 128, cb * 512:(cb + 1) * 512].rearrange("(c p) n -> p c n", p=128)
                dst = Wdst[kq * 128:(kq + 4) * 128, cb * 512:(cb + 1) * 512].rearrange("(c p) n -> p c n", p=128)
                P.dma("sp", lambda e, s=s, src=src: e.dma_start(out=stg[s][:], in_=src), writes=[("cvst", s)])
                cx.copy(cx.conv_eng(), o16[s][:], stg[s][:], [("cvst", s)], [("cvo", s)])
                P.dma("pool", lambda e, s=s, dst=dst: e.dma_start(out=dst, in_=o16[s][:]), reads=[("cvo", s)], writes=[(name, kq // 4, cb)],
                      sem=("cvout", s))
    hT = cx.sb("hT_sb", [128, DC, T], BF16)
    rwb = cx.sb("rwb_sb", [128, T], F32)
    acc = cx.sb("acc", [128, DC, T], F32)
    wgt = [cx.sb("wg%d" % i, [128, DC, 128], BF16) for i in range(2)]
    wut = [cx.sb("wu%d" % i, [128, DC, 128], BF16) for i in range(2)]
    wdt = cx.sb("wdt", [128, FG, D], BF16)
    o16c = [cx.sb("oc%d" % i, [128, T], BF16) for i in range(2)]
    gi = [0]
    bufs = ffn_bufs(cx, "moe", True)
    for ch in range(NCHUNK):
        for k0 in range(0, DC, 4):
            P.dma("sp", lambda e, k0=k0, ch=ch: e.dma_start(out=hT[:, k0:k0 + 4, :], in_=hTd[ch, :, k0:k0 + 4, :]), writes=["hT"], sem="hT")
        cx.load(rwb[:], rwd[ch], "rwb")

        def get_gu(ft):
            s = gi[0] % 2
            gi[0] += 1
            dep = [("Wg16", kq, ft // 4) for kq in range(4)] + [("Wu16", kq, ft // 4) for kq in range(4)]
            srcg = Wg16[:, ft * 128:(ft + 1) * 128].rearrange("(c p) n -> p c n", p=128)
            srcu = Wu16[:, ft * 128:(ft + 1) * 128].rearrange("(c p) n -> p c n", p=128)
            P.dma("sp", lambda e, s=s, srcg=srcg: e.dma_start(out=wgt[s][:], in_=srcg), reads=dep, writes=[("wgt", s)])
            P.dma("sp", lambda e, s=s, srcu=srcu: e.dma_start(out=wut[s][:], in_=srcu), reads=dep, writes=[("wut", s)])
            return wgt[s], wut[s], [("wgt", s), ("wut", s)]

        def get_d(fg):
            keys = []
            for j in range(FG):
                ft = fg * FG + j
                dep = [("Wd16", ft // 4, cb) for cb in range(D // 512)]
                P.dma("sp", lambda e, j=j, ft=ft: e.dma_start(out=wdt[:, j, :], in_=Wd16[ft * 128:(ft + 1) * 128, :]), reads=dep, writes=[("wdt", j)])
                keys.append(("wdt", j))
            return wdt, keys

        def out_evac(fg, dc, half, ps, pk):
            hs = slice(half * 512, (half + 1) * 512)
            if fg == 0:
                P.op("dve", lambda e: e.tensor_copy(out=acc[:, dc, hs], in_=ps[:]), reads=[pk], writes=[("acc", dc, half)])
            else:
                P.op("dve", lambda e: e.tensor_tensor(out=acc[:, dc, hs], in0=ps[:], in1=acc[:, dc, hs], op=ALU.add),
                     reads=[pk, ("acc", dc, half)], writes=[("acc", dc, half)])

        emit_ffn(cx, bufs, hT, ["hT"], nft, get_gu, get_d, out_evac, "moe", rwb=rwb, rwkey="rwb")
        for dc in range(DC):
            s = dc % 2
            P.op("act", lambda e, s=s, dc=dc: e.activation(out=o16c[s][:], in_=acc[:, dc, :], func=AF.Identity),
                 reads=[("acc", dc, 0), ("acc", dc, 1)], writes=[("oc", s)])
            cx.store(po[ch, :, dc, :], o16c[s][:], [("oc", s)], ("po", ch, dc), sem=("st_oc", s))
    return cx.finish()


def build_final():
    cx = Ctx()
    P = cx.P
    xTd = cx.din("xT", [128, DC, T], F32)
    pd = cx.din("partials", [NEXP, 128, DC, T], BF16)
    gfd = cx.din("gatef", [128, DC], F32)
    gnd = cx.din("gfin", [128, DC], F32)
    out = cx.dout("outT", [128, DC, T], F32)
    cx.alloc_psum(4)
    xT = cx.sb("xT_sb", [128, DC, T], F32)
    gf = cx.sb("gf_sb", [128, DC], F32)
    gfin = cx.sb("gfin_sb", [128, DC], F32)
    zero = cx.sb("zero_sb", [128, DC], F32)
    ones16 = cx.sb("ones16", [128, 128], BF16)
    P.op("pool", lambda e: e.memset(ones16[:], 1.0), writes=["ones16"])
    P.op("pool", lambda e: e.memset(zero[:], 0.0), writes=["modv"])
    for k0 in range(0, DC, 4):
        P.dma("sp", lambda e, k0=k0: e.dma_start(out=xT[:, k0:k0 + 4, :], in_=xTd[:, k0:k0 + 4, :]), writes=["xT_in"], sem="xT")
    cx.load(gf[:], gfd, "gf")
    cx.load(gfin[:], gnd, "gfin")
    pb = [cx.sb("pb%d" % i, [128, NEXP, T], BF16) for i in range(2)]
    acc = [cx.sb("facc%d" % i, [128, T], F32) for i in range(2)]
    for kc in range(DC):
        s = kc % 2
        for e_ in range(NEXP):
            P.dma("sp", lambda e, s=s, e_=e_, kc=kc: e.dma_start(out=pb[s][:, e_, :], in_=pd[e_, :, kc, :]), writes=[("pb", s)], sem=("pb", s))
        P.op("dve", lambda e, s=s: e.tensor_tensor(out=acc[s][:], in0=pb[s][:, 0, :], in1=pb[s][:, 1, :], op=ALU.add), reads=[("pb", s)], writes=[("facc", s)])
        for e_ in range(2, NEXP):
            P.op("dve", lambda e, s=s, e_=e_: e.tensor_tensor(out=acc[s][:], in0=acc[s][:], in1=pb[s][:, e_, :], op=ALU.add),
                 reads=[("pb", s), ("facc", s)], writes=[("facc", s)])
        P.op("dve", lambda e, s=s, kc=kc: e.scalar_tensor_tensor(out=xT[:, kc, :], in0=acc[s][:], scalar=gf[:, kc:kc + 1], in1=xT[:, kc, :],
                                                              op0=ALU.mult, op1=ALU.add), reads=[("facc", s), "gf", "xT_in"], writes=["xT"])
    oT = cx.sb("oT_sb", [128, DC // 2, T], F32)
    P.op("dve", lambda e: e.tensor_copy(out=gfin[:], in_=gfin[:]), reads=["gfin"], writes=["modv"])
    rstd = emit_rmsnorm_stats(cx, xT, "xT", ones16, "nf")
    tmpf = [cx.sb("nf_t%d" % i, [128, T], F32) for i in range(2)]
    for kc in range(DC):
        s = kc % 2
        P.op("dve", lambda e, s=s, kc=kc: e.tensor_tensor(out=tmpf[s][:], in0=xT[:, kc, :], in1=rstd[:], op=ALU.mult),
             reads=["xT", "nf_rstd"], writes=[("nft", s)])
        P.op("act", lambda e, s=s, kc=kc: e.activation(out=tmpf[s][:], in_=tmpf[s][:], func=AF.Identity, scale=gfin[:, kc:kc + 1]),
             reads=[("nft", s), "modv"], writes=[("nft", s)])
        cx.store(out[:, kc, :], tmpf[s][:], [("nft", s)], ("out", kc), sem=("st_nft", s))
    return cx.finish()


def emit_rmsnorm_stats(cx, xT, xkey, ones16, tag):
    P = cx.P
    sq = [cx.sb("%s_sq%d" % (tag, i), [128, 512], BF16) for i in range(2)]
    rstd = cx.sb("%s_rstd" % tag, [128, T], F32)
    for half in range(T // 512):
        hs = slice(half * 512, (half + 1) * 512)
        ps, pk = cx.psum()
        for kc in range(DC):
            s = kc % 2
            P.op("act", lambda e, s=s, kc=kc, hs=hs: e.activation(out=sq[s][:], in_=xT[:, kc, hs], func=AF.Square),
                 reads=[xkey], writes=[(tag, "sq", s)])
            P.op("pe", lambda e, s=s, kc=kc, ps=ps: e.matmul(ps[:], lhsT=ones16[:], rhs=sq[s][:], start=(kc == 0), stop=(kc == DC - 1)),
                 reads=[(tag, "sq", s), "ones16"], writes=[pk], signal=True)
        rk = tag + "_rstd"
        P.op("dve", lambda e, ps=ps, hs=hs: e.tensor_scalar(out=rstd[:, hs], in0=ps[:], scalar1=1.0 / D, scalar2=EPS, op0=ALU.mult, op1=ALU.add),
             reads=[pk, rk], writes=[rk])
        P.op("act", lambda e, hs=hs: e.activation(out=rstd[:, hs], in_=rstd[:, hs], func=AF.Sqrt), reads=[rk], writes=[rk])
        P.op("dve", lambda e, hs=hs: e.reciprocal(out=rstd[:, hs], in_=rstd[:, hs]), reads=[rk], writes=[rk])
    return rstd


def run_ffn(xTs, h2s, mod_l, inp):
    nc = get_nc("ffn", build_ffn)
    wg = np.ascontiguousarray(inp["ffn_w_gate"][0]); wu = np.ascontiguousarray(inp["ffn_w_up"][0]); wd = np.ascontiguousarray(inp["ffn_w_down"][0])
    ins = []
    for i in range(NCORES):
        b = i // 4
        ins.append({"xT": xTs[i], "h2T": h2s[i], "gatef": np.ascontiguousarray(modv_layout(mod_l[b])[:, 5, :]),
                    "w_gate": wg, "w_up": wu, "w_down": wd})
    res = run(nc, ins)
    return [res[i]["xT_out"] for i in range(NCORES)]


def run_moe(h2s, rws, inp):
    nc = get_nc("moe", build_moe)
    h2all = np.ascontiguousarray(np.stack(h2s))
    rwall = np.stack(rws)
    ins = []
    for e in range(NCORES):
        rwb = np.ascontiguousarray(np.broadcast_to(rwall[:, None, :, e], (NCHUNK, 128, T)))
        ins.append({"h2T": h2all, "rwb": rwb, "w_gate": np.ascontiguousarray(inp["moe_w_gate"][0][e]),
                    "w_up": np.ascontiguousarray(inp["moe_w_up"][0][e]), "w_down": np.ascontiguousarray(inp["moe_w_down"][0][e])})
    res = run(nc, ins)
    return [res[e]["partial"] for e in range(NCORES)]


def run_final(xTs, partials, mod_l, gfin):
    nc = get_nc("final", build_final)
    ins = []
    for i in range(NCORES):
        b = i // 4
        ins.append({"xT": xTs[i], "partials": np.ascontiguousarray(np.stack([partials[e][i] for e in range(NEXP)])),
                    "gatef": np.ascontiguousarray(modv_layout(mod_l[b])[:, 5, :]), "gfin": vec_fm(gfin)})
    res = run(nc, ins)
    out = np.zeros((2, SEQ, D), np.float32)
    for i in range(NCORES):
        b, q = i // 4, i % 4
        out[b, q * T:(q + 1) * T, :] = res[i]["outT"].transpose(1, 0, 2).reshape(D, T).T
    return out


def kernel(**inputs):
    inp = {k: np.asarray(v) for k, v in inputs.items()}
    mod = run_mod(inp["c"], inp["w_mod"], inp["b_mod"])
    xTs = x_to_cores(inp["x"])
    out = None
    for l in range(2):
        projT = run_inproj(xTs, mod[l], inp["norm_mix_g"][l], np.ascontiguousarray(inp["w_in"][l]))
        ypreT = run_ssm(projT, l, inp)
        yattT = run_attn(projT, inp["rel_bias"])
        moe = (l % 2 == 1)
        xTs, h2s, rws = run_post(xTs, ypreT, yattT, projT, mod[l], l, inp, moe)
        if not moe:
            xTs = run_ffn(xTs, h2s, mod[l], inp)
        else:
            hcs, posms, overflow = run_route(h2s, rws)
            if not overflow:
                ys = run_moe3(hcs, inp)
                out = run_final3(xTs, ys, posms, rws, mod[l], inp["final_norm_g"])
            else:
                partials = run_moe(h2s, rws, inp)
                out = run_final(xTs, partials, mod[l], inp["final_norm_g"])
    return out


CAP = 4096
NTOK = 8192
NTT = NTOK // 128


def build_moe2():
    cx = Ctx()
    P = cx.P
    nc = cx.nc
    h2d = cx.din("h2tm", [NTOK, D], BF16)
    rwd = cx.din("rwc", [128, NTT], F32)
    Ld = cx.din("Ltri", [128, 128], F32)
    idd = cx.din("ident", [128, 128], F32)
    Wg = cx.din("w_gate", [D, DFFE], F32)
    Wu = cx.din("w_up", [D, DFFE], F32)
    Wd = cx.din("w_down", [DFFE, D], F32)
    po = cx.dout("partial", [NTOK, D], BF16)
    Wg16 = nc.dram_tensor("Wg16", [D, DFFE], BF16, kind="Internal").ap()
    Wu16 = nc.dram_tensor("Wu16", [D, DFFE], BF16, kind="Internal").ap()
    Wd16 = nc.dram_tensor("Wd16", [DFFE, D], BF16, kind="Internal").ap()
    Xc = nc.dram_tensor("Xc", [CAP, D], BF16, kind="Internal").ap()
    Yc = nc.dram_tensor("Yc", [CAP, D], F32, kind="Internal").ap()
    for i in range(6):
        cx.ps.append(cx.st.enter_context(nc.psum_tensor("ps%d" % i, [128, 512], F32)))
    psb = [cx.st.enter_context(nc.psum_tensor("psb%d" % i, [128, 1024], BF16)) for i in range(2)]
    nft = DFFE // 128
    rwc = cx.sb("rwc_sb", [128, NTT], F32)
    posi = cx.sb("posi", [128, NTT], I32)
    identb = cx.sb("identb", [128, 128], BF16)
    cx.load(rwc[:], rwd, "rwc")

    cx.phase_begin()
    stg = [cx.sb("cv_st%d" % i, [128, 4, 512], F32) for i in range(3)]
    o16 = [cx.sb("cv_o%d" % i, [128, 4, 512], BF16) for i in range(3)]
    Ltri = cx.sb("Ltri_sb", [128, 128], F32)
    identf = cx.sb("identf", [128, 128], F32)
    ones32 = cx.sb("ones32", [128, 128], F32)
    onesr = cx.sb("onesr", [128, NTT], F32)
    m = cx.sb("m_sb", [128, NTT], F32)
    S = cx.sb("S_sb", [128, NTT], F32)
    incl = cx.sb("incl", [128, NTT], F32)
    posf = cx.sb("posf", [128, NTT], F32)
    z16 = cx.sb("z16", [128, D], BF16)
    hrow = [cx.sb("hrow%d" % i, [128, D], BF16) for i in range(4)]
    cx.load(Ltri[:], Ld, "Ltri")
    cx.load(identf[:], idd, "identf")
    P.op("dve", lambda e: e.tensor_copy(out=identb[:], in_=identf[:]), reads=["identf"], writes=["identb"])
    P.op("pool", lambda e: e.memset(ones32[:], 1.0), writes=["ones32"])
    P.op("pool", lambda e: e.memset(onesr[:], 1.0), writes=["onesr"])
    P.op("pool", lambda e: e.memset(z16[:], 0.0), writes=["z16"])
    C = "cmp"
    P.op("dve", lambda e: e.tensor_scalar(out=m[:], in0=rwc[:], scalar1=0.0, scalar2=None, op0=ALU.is_gt), reads=["rwc"], writes=[C])
    ps, pk = cx.psum()
    P.op("pe", lambda e: e.matmul(ps[:, 0:NTT], lhsT=Ltri[:], rhs=m[:], start=True, stop=True), reads=[C, "Ltri"], writes=[pk])
    P.op("pe", lambda e: e.matmul(ps[:, NTT:2 * NTT], lhsT=ones32[:], rhs=m[:], start=True, stop=True), reads=[C, "ones32"], writes=[pk])
    P.op("dve", lambda e: e.tensor_copy(out=S[:], in_=ps[:, NTT:2 * NTT]), reads=[pk, C], writes=[C])
    P.op("dve", lambda e: e.tensor_tensor_scan(out=incl[:], data0=onesr[:], data1=S[:], initial=0.0, op0=ALU.mult, op1=ALU.add),
         reads=[C, "onesr"], writes=[C])
    P.op("dve", lambda e: e.tensor_tensor(out=incl[:], in0=incl[:], in1=S[:], op=ALU.subtract), reads=[C], writes=[C])
    P.op("dve", lambda e: e.tensor_tensor(out=posf[:], in0=ps[:, 0:NTT], in1=incl[:], op=ALU.add), reads=[pk, C], writes=[C])
    P.op("dve", lambda e: e.tensor_scalar(out=m[:], in0=m[:], scalar1=-1.0e6, scalar2=1.0e6, op0=ALU.mult, op1=ALU.add), reads=[C], writes=[C])
    P.op("dve", lambda e: e.tensor_tensor(out=posf[:], in0=posf[:], in1=m[:], op=ALU.add), reads=[C], writes=[C])
    P.op("dve", lambda e: e.tensor_copy(out=posi[:], in_=posf[:]), reads=[C], writes=["posi"])
    for r in range(CAP // 128):
        P.dma("sp", lambda e, r=r: e.dma_start(out=Xc[r * 128:(r + 1) * 128, :], in_=z16[:]), reads=["z16"], writes=[("Xcz", r)], sem="xcz")
    xcz = [("Xcz", r) for r in range(CAP // 128)]
    for j in range(NTT):
        s = j % 4
        cx.load(hrow[s][:], h2d[j * 128:(j + 1) * 128, :], ("hrow", s))
        P.dma("pool", lambda e, j=j, s=s: e.indirect_dma_start(
            out=Xc[:, :], out_offset=bass.IndirectOffsetOnAxis(ap=posi[:, j:j + 1], axis=0), in_=hrow[s][:, :], in_offset=None,
            bounds_check=CAP - 1, oob_is_err=False), reads=[("hrow", s), "posi"] + (xcz if j < 4 else []), writes=[("Xcs", j)], sem=("sc", s))
    xcs = [("Xcs", j) for j in range(NTT)]
    ci = 0
    for (Wsrc, Wdst, name, KC_, ncols) in ((Wg, Wg16, "Wg16", DC, DFFE), (Wu, Wu16, "Wu16", DC, DFFE), (Wd, Wd16, "Wd16", nft, D)):
        for kq in range(0, KC_, 4):
            for cb in range(ncols // 512):
                s = ci % 3
                ci += 1
                src = Wsrc[kq * 128:(kq + 4) * 128, cb * 512:(cb + 1) * 512].rearrange("(c p) n -> p c n", p=128)
                dst = Wdst[kq * 128:(kq + 4) * 128, cb * 512:(cb + 1) * 512].rearrange("(c p) n -> p c n", p=128)
                P.dma("sp", lambda e, s=s, src=src: e.dma_start(out=stg[s][:], in_=src), writes=[("cvst", s)])
                cx.copy(cx.conv_eng(), o16[s][:], stg[s][:], [("cvst", s)], [("cvo", s)])
                P.dma("act", lambda e, s=s, dst=dst: e.dma_start(out=dst, in_=o16[s][:]), reads=[("cvo", s)], writes=[(name, kq // 4, cb)],
                      sem=("cvout", s))
    cx.phase_end()

    cx.phase_begin()
    hT = cx.sb("hT_sb", [128, DC, T], BF16)
    accR = cx.sb("accR", [128, T // 128, D], F32)
    wgt = [cx.sb("wg%d" % i, [128, DC, 128], BF16) for i in range(2)]
    wut = [cx.sb("wu%d" % i, [128, DC, 128], BF16) for i in range(2)]
    wdt = cx.sb("wdt", [128, FG, D], BF16)
    xrow = [cx.sb("xrow%d" % i, [128, D], BF16) for i in range(2)]
    g16 = [cx.sb("g16_%d" % i, [128, FG, T], BF16) for i in range(2)]
    sgl = [cx.sb("sg%d" % i, [128, 512], F32) for i in range(2)]
    gi = 0
    it = 0
    ti = 0
    for ch in range(CAP // T):
        for rt in range(T // 128):
            s = (ch * 8 + rt) % 2
            row0 = ch * T + rt * 128
            cx.P.dma("sp", lambda e, s=s, row0=row0: e.dma_start(out=xrow[s][:], in_=Xc[row0:row0 + 128, :]),
                     reads=(xcs + xcz) if (ch == 0 and rt < 2) else [], writes=[("xrow", s)])
            for kq in range(DC // 4):
                pb = psb[ti % 2]
                pbk = ("psb", ti % 2)
                ti += 1
                for q in range(4):
                    kc = kq * 4 + q
                    P.op("pe", lambda e, pb=pb, q=q, kc=kc, s=s: e.transpose(pb[:, q * 128:(q + 1) * 128], xrow[s][:, kc * 128:(kc + 1) * 128], identb[:]),
                         reads=[("xrow", s), "identb"], writes=[pbk], signal=(q == 3))
                eng = "act" if (ti % 2 == 0) else "dve"
                dst = hT[:, kq * 4:(kq + 1) * 4, rt * 128:(rt + 1) * 128]
                srcv = pb[:, 0:512].rearrange("p (k n) -> p k n", k=4)
                cx.copy(eng, dst, srcv, [pbk], ["hT"])
        for fg in range(nft // FG):
            gb = g16[fg % 2]
            for j in range(FG):
                ft = fg * FG + j
                s = gi % 2
                gi += 1
                dep = [("Wg16", kq, ft // 4) for kq in range(4)] + [("Wu16", kq, ft // 4) for kq in range(4)]
                srcg = Wg16[:, ft * 128:(ft + 1) * 128].rearrange("(c p) n -> p c n", p=128)
                srcu = Wu16[:, ft * 128:(ft + 1) * 128].rearrange("(c p) n -> p c n", p=128)
                P.dma("sp", lambda e, s=s, srcg=srcg: e.dma_start(out=wgt[s][:], in_=srcg), reads=dep if ch == 0 else [], writes=[("wgt", s)])
                P.dma("sp", lambda e, s=s, srcu=srcu: e.dma_start(out=wut[s][:], in_=srcu), reads=dep if ch == 0 else [], writes=[("wut", s)])
                for half in range(T // 512):
                    hs = slice(half * 512, (half + 1) * 512)
                    psg, pkg = cx.psum()
                    mm_group(cx, psg[:], pkg, [(wgt[s][:, kc, :], hT[:, kc, hs]) for kc in range(DC)], [("wgt", s), "hT"])
                    psu, pku = cx.psum()
                    mm_group(cx, psu[:], pku, [(wut[s][:, kc, :], hT[:, kc, hs]) for kc in range(DC)], [("wut", s), "hT"])
                    s2 = it % 2
                    it += 1
                    P.op("act", lambda e, s2=s2, psg=psg: e.activation(out=sgl[s2][:], in_=psg[:], func=AF.Silu), reads=[pkg], writes=[("sg", s2)])
                    P.op("dve", lambda e, s2=s2, psu=psu, gb=gb, j=j, hs=hs: e.tensor_tensor(out=gb[:, j, hs], in0=sgl[s2][:], in1=psu[:], op=ALU.mult),
                         reads=[("sg", s2), pku], writes=[("g16", fg % 2, j)])
            for j in range(FG):
                ft = fg * FG + j
                dep = [("Wd16", ft // 4, cb) for cb in range(D // 512)]
                P.dma("sp", lambda e, j=j, ft=ft: e.dma_start(out=wdt[:, j, :], in_=Wd16[ft * 128:(ft + 1) * 128, :]),
                      reads=dep if ch == 0 else [], writes=[("wdt", j)])
            di = 0
            for rt in range(T // 128):
                for db in range(D // 512):
                    ps, pk = cx.psum()
                    mm_group(cx, ps[:], pk, [(gb[:, j, rt * 128:(rt + 1) * 128], wdt[:, j, db * 512:(db + 1) * 512]) for j in range(FG)],
                             [("wdt", j) for j in range(FG)] + [("g16", fg % 2, j) for j in range(FG)])
                    dsl = accR[:, rt, db * 512:(db + 1) * 512]
                    eng = "dve" if (di % 4 != 3) else "pool"
                    di += 1
                    if fg == 0:
                        P.op("dve", lambda e, dsl=dsl, ps=ps: e.tensor_copy(out=dsl, in_=ps[:]), reads=[pk], writes=[("accR", rt, db)])
                    else:
                        P.op("dve", lambda e, dsl=dsl, ps=ps: e.tensor_tensor(out=dsl, in0=ps[:], in1=dsl, op=ALU.add),
                             reads=[pk, ("accR", rt, db)], writes=[("accR", rt, db)])
        for rt in range(T // 128):
            row0 = ch * T + rt * 128
            P.dma("sp", lambda e, rt=rt, row0=row0: e.dma_start(out=Yc[row0:row0 + 128, :], in_=accR[:, rt, :]),
                  reads=[("accR", rt, db) for db in range(D // 512)], writes=[("Yc", ch, rt)], sem=("st_acc", rt % 2))
    ycs = [("Yc", ch, rt) for ch in range(CAP // T) for rt in range(T // 128)]
    cx.phase_end()

    cx.phase_begin()
    zt = [cx.sb("zt%d" % i, [128, D], F32) for i in range(3)]
    ot = [cx.sb("ot%d" % i, [128, D], BF16) for i in range(3)]
    for i in range(3):
        P.op("pool", lambda e, i=i: e.memset(zt[i][:], 0.0), writes=[("zt", i)])
    for j in range(NTT):
        s = j % 3
        P.dma("pool", lambda e, j=j, s=s: e.indirect_dma_start(
            out=zt[s][:, :], out_offset=None, in_=Yc[:, :], in_offset=bass.IndirectOffsetOnAxis(ap=posi[:, j:j + 1], axis=0),
            bounds_check=CAP - 1, oob_is_err=False), reads=["posi", ("zt", s)], writes=[("zt", s)], sem=("ga", s))
        if j % 2 == 0:
            P.op("dve", lambda e, j=j, s=s: e.tensor_scalar(out=ot[s][:], in0=zt[s][:], scalar1=rwc[:, j:j + 1], scalar2=None, op0=ALU.mult),
                 reads=[("zt", s), "rwc"], writes=[("ot", s)])
        else:
            P.op("act", lambda e, j=j, s=s: e.activation(out=ot[s][:], in_=zt[s][:], func=AF.Identity, scale=rwc[:, j:j + 1]),
                 reads=[("zt", s), "rwc"], writes=[("ot", s)])
        cx.store(po[j * 128:(j + 1) * 128, :], ot[s][:], [("ot", s)], ("po", j), sem=("st_ot", s))
    cx.phase_end()
    return cx.finish()


def build_final2():
    cx = Ctx()
    P = cx.P
    NT_ = T // 128
    xd = cx.din("x", [T, D], F32)
    pd = cx.din("partials", [NEXP, T, D], BF16)
    gfd = cx.din("gatef", [128, D], F32)
    gnd = cx.din("gfin", [128, D], F32)
    out = cx.dout("out", [T, D], F32)
    gf = cx.sb("gf_sb", [128, D], F32)
    gfin = cx.sb("gfin_sb", [128, D], F32)
    cx.load(gf[:], gfd, "gf")
    cx.load(gfin[:], gnd, "gfin")
    xt = [cx.sb("xt%d" % i, [128, D], F32) for i in range(2)]
    pb = [cx.sb("pb%d" % i, [128, NEXP, D], BF16) for i in range(2)]
    acc = [cx.sb("acc%d" % i, [128, D], F32) for i in range(2)]
    sqj = cx.sb("sqj", [128, D], F32)
    ss = [cx.sb("ss%d" % i, [128, 1], F32) for i in range(2)]
    for tt in range(NT_):
        s = tt % 2
        rows = slice(tt * 128, (tt + 1) * 128)
        cx.load(xt[s][:], xd[rows, :], ("xt", s))
        for e_ in range(NEXP):
            P.dma("sp", lambda e, s=s, e_=e_, rows=rows: e.dma_start(out=pb[s][:, e_, :], in_=pd[e_, rows, :]), writes=[("pb", s, e_)], sem=("pb", s))
        pbk = [("pb", s, e_) for e_ in range(NEXP)]
        P.op("dve", lambda e, s=s: e.tensor_tensor(out=acc[s][:], in0=pb[s][:, 0, :], in1=pb[s][:, 1, :], op=ALU.add), reads=pbk, writes=[("acc", s)])
        for e_ in range(2, NEXP):
            eng = "dve" if e_ % 2 == 0 else "pool"
            P.op(eng, lambda e, s=s, e_=e_: e.tensor_tensor(out=acc[s][:], in0=acc[s][:], in1=pb[s][:, e_, :], op=ALU.add),
                 reads=pbk + [("acc", s)], writes=[("acc", s)])
        P.op("dve", lambda e, s=s: e.tensor_tensor(out=acc[s][:], in0=acc[s][:], in1=gf[:], op=ALU.mult), reads=[("acc", s), "gf"], writes=[("acc", s)])
        P.op("dve", lambda e, s=s: e.tensor_tensor(out=xt[s][:], in0=xt[s][:], in1=acc[s][:], op=ALU.add), reads=[("acc", s), ("xt", s)], writes=[("xt", s)])
        P.op("act", lambda e, s=s: e.activation(out=sqj[:], in_=xt[s][:], func=AF.Square, accum_out=ss[s][:]), reads=[("xt", s)], writes=["sqj", ("ss", s)])
        P.op("dve", lambda e, s=s: e.tensor_scalar(out=ss[s][:], in0=ss[s][:], scalar1=1.0 / D, scalar2=EPS, op0=ALU.mult, op1=ALU.add),
             reads=[("ss", s)], writes=[("ss", s)])
        P.op("act", lambda e, s=s: e.activation(out=ss[s][:], in_=ss[s][:], func=AF.Sqrt), reads=[("ss", s)], writes=[("ss", s)])
        P.op("dve", lambda e, s=s: e.reciprocal(out=ss[s][:], in_=ss[s][:]), reads=[("ss", s)], writes=[("ss", s)])
        P.op("dve", lambda e, s=s: e.scalar_tensor_tensor(out=acc[s][:], in0=xt[s][:], scalar=ss[s][:, 0:1], in1=gfin[:], op0=ALU.mult, op1=ALU.mult),
             reads=[("xt", s), ("ss", s), "gfin", ("acc", s)], writes=[("acc", s)])
        cx.store(out[rows, :], acc[s][:], [("acc", s)], ("out", tt), sem=("st_acc", s))
    return cx.finish()


def run_moe2(h2s, rws, inp):
    nc = get_nc("moe2", build_moe2)
    h2tm = np.ascontiguousarray(np.concatenate([h.transpose(1, 0, 2).reshape(D, T).T for h in h2s], axis=0))
    rwall = np.concatenate(rws, axis=0)
    kk = np.arange(128)
    Ltri = (kk[:, None] < kk[None, :]).astype(np.float32)
    ins = []
    for e in range(NCORES):
        ins.append({"h2tm": h2tm, "rwc": np.ascontiguousarray(rwall[:, e].reshape(NTT, 128).T), "Ltri": Ltri,
                    "ident": np.eye(128, dtype=np.float32),
                    "w_gate": np.ascontiguousarray(inp["moe_w_gate"][0][e]), "w_up": np.ascontiguousarray(inp["moe_w_up"][0][e]),
                    "w_down": np.ascontiguousarray(inp["moe_w_down"][0][e])})
    res = run(nc, ins)
    return [res[e]["partial"] for e in range(NCORES)]


def run_final2(xTs, partials, mod_l, gfin):
    nc = get_nc("final2", build_final2)
    ins = []
    for i in range(NCORES):
        b = i // 4
        gatef = mod_l[b].reshape(6, D)[5]
        ins.append({"x": np.ascontiguousarray(xTs[i].transpose(1, 0, 2).reshape(D, T).T),
                    "partials": np.ascontiguousarray(np.stack([partials[e][i * T:(i + 1) * T] for e in range(NEXP)])),
                    "gatef": np.ascontiguousarray(np.broadcast_to(gatef[None, :], (128, D))),
                    "gfin": np.ascontiguousarray(np.broadcast_to(gfin[None, :], (128, D)))})
    res = run(nc, ins)
    out = np.zeros((2, SEQ, D), np.float32)
    for i in range(NCORES):
        b, q = i // 4, i % 4
        out[b, q * T:(q + 1) * T, :] = res[i]["out"]
    return out


SEG = 512
NR = SEG // 128


def build_route():
    cx = Ctx()
    P = cx.P
    nc = cx.nc
    NT_ = T // 128
    hTd = cx.din("h2T", [128, DC, T], BF16)
    rwd = cx.din("rw", [128, NT_, NEXP], F32)
    Ld = cx.din("Ltri", [128, 128], F32)
    idd = cx.din("ident", [128, 128], F32)
    iod = cx.din("iota", [128, 128], F32)
    hco = cx.dout("hc", [NEXP, 128, DC, SEG], BF16)
    pso = cx.dout("posm", [128, NT_, NEXP], F32)
    ovo = cx.dout("ovf", [128, 1], F32)
    for i in range(6):
        cx.ps.append(cx.st.enter_context(nc.psum_tensor("ps%d" % i, [128, 512], F32)))
    psb = [cx.st.enter_context(nc.psum_tensor("psb%d" % i, [128, 1024], BF16)) for i in range(2)]
    hT = cx.sb("hT_sb", [128, DC, T], BF16)
    htm = cx.sb("htm", [128, NT_, D], BF16)
    ovf = cx.sb("ovf_sb", [128, 1], F32)
    rw = cx.sb("rw_sb", [128, NT_, NEXP], F32)
    Ltri = cx.sb("Ltri_sb", [128, 128], F32)
    identf = cx.sb("identf", [128, 128], F32)
    identb = cx.sb("identb", [128, 128], BF16)
    iota = cx.sb("iota_sb", [128, 128], F32)
    ones32 = cx.sb("ones32", [128, 128], F32)
    for k0 in range(0, DC, 4):
        P.dma("sp", lambda e, k0=k0: e.dma_start(out=hT[:, k0:k0 + 4, :], in_=hTd[:, k0:k0 + 4, :]), writes=["hT"], sem="hT")
    cx.load(rw[:], rwd, "rw")
    cx.load(Ltri[:], Ld, "Ltri")
    cx.load(identf[:], idd, "identf")
    cx.load(iota[:], iod, "iota")
    P.op("dve", lambda e: e.tensor_copy(out=identb[:], in_=identf[:]), reads=["identf"], writes=["identb"])
    P.op("pool", lambda e: e.memset(ones32[:], 1.0), writes=["ones32"])
    ti = 0
    for tt in range(NT_):
        for kq in range(DC // 4):
            pb = psb[ti % 2]
            pbk = ("psb", ti % 2)
            ti += 1
            for q in range(4):
                kc = kq * 4 + q
                P.op("pe", lambda e, pb=pb, q=q, kc=kc, tt=tt: e.transpose(pb[:, q * 128:(q + 1) * 128], hT[:, kc, tt * 128:(tt + 1) * 128], identb[:]),
                     reads=["hT", "identb"], writes=[pbk], signal=(q == 3))
            cx.copy("act" if ti % 2 else "dve", htm[:, tt, kq * 512:(kq + 1) * 512], pb[:, 0:512], [pbk], [("htm", tt)])
    C = "cmp"
    NF = NT_ * NEXP
    m = cx.sb("m_sb", [128, NT_, NEXP], F32)
    S = cx.sb("S_sb", [128, NT_, NEXP], F32)
    offs = cx.sb("offs", [128, NT_, NEXP], F32)
    pos = cx.sb("pos", [128, NT_, NEXP], F32)
    posr = cx.sb("posr", [128, NR, NT_, NEXP], F32)
    mf = m[:].rearrange("p t e -> p (t e)")
    P.op("dve", lambda e: e.tensor_scalar(out=m[:], in0=rw[:], scalar1=0.0, scalar2=None, op0=ALU.is_gt), reads=["rw"], writes=[C])
    ps, pk = cx.psum()
    P.op("pe", lambda e: e.matmul(ps[:, 0:NF], lhsT=Ltri[:], rhs=mf, start=True, stop=True), reads=[C, "Ltri"], writes=[pk])
    P.op("pe", lambda e: e.matmul(ps[:, NF:2 * NF], lhsT=ones32[:], rhs=mf, start=True, stop=True), reads=[C, "ones32"], writes=[pk])
    P.op("dve", lambda e: e.tensor_copy(out=S[:].rearrange("p t e -> p (t e)"), in_=ps[:, NF:2 * NF]), reads=[pk, C], writes=[C])
    P.op("pool", lambda e: e.memset(offs[:, 0, :], 0.0), reads=[C], writes=[C])
    for tt in range(1, NT_):
        P.op("dve", lambda e, tt=tt: e.tensor_tensor(out=offs[:, tt, :], in0=offs[:, tt - 1, :], in1=S[:, tt - 1, :], op=ALU.add), reads=[C], writes=[C])
    P.op("dve", lambda e: e.tensor_tensor(out=pos[:].rearrange("p t e -> p (t e)"), in0=ps[:, 0:NF], in1=offs[:].rearrange("p t e -> p (t e)"), op=ALU.add),
         reads=[pk, C], writes=[C])
    P.op("dve", lambda e: e.tensor_scalar(out=pos[:], in0=pos[:], scalar1=1.0e6, scalar2=None, op0=ALU.add), reads=[C], writes=[C])
    P.op("dve", lambda e: e.tensor_tensor(out=pos[:], in0=pos[:], in1=m[:], op=ALU.mult), reads=[C], writes=[C])
    P.op("dve", lambda e: e.tensor_scalar(out=pos[:], in0=pos[:], scalar1=-1.0e6, scalar2=None, op0=ALU.add), reads=[C], writes=[C])
    cx.store(pso, pos[:], [C], "pso")
    P.op("dve", lambda e: e.tensor_reduce(out=ovf[:], in_=pos[:].rearrange("p t e -> p (t e)"), axis=mybir.AxisListType.X, op=ALU.max), reads=[C], writes=["ovf"])
    P.op("dve", lambda e: e.tensor_scalar(out=ovf[:], in0=ovf[:], scalar1=float(SEG), scalar2=None, op0=ALU.is_ge), reads=["ovf"], writes=["ovf"])
    cx.store(ovo, ovf[:], ["ovf"], "ovo")
    for r in range(NR):
        P.op("dve", lambda e, r=r: e.tensor_scalar(out=posr[:, r, :, :], in0=pos[:], scalar1=-128.0 * r, scalar2=None, op0=ALU.add), reads=[C], writes=[C])
    Pm = [cx.sb("Pm%d" % i, [128, NT_, NR, 128], BF16) for i in range(2)]
    hcb = [cx.sb("hcb%d" % i, [128, DC, SEG], BF16) for i in range(2)]
    htk = [("htm", tt) for tt in range(NT_)]
    for ex in range(NEXP):
        s = ex % 2
        for tt in range(NT_):
            for r in range(min(tt, NR - 1) + 1):
                eng = "dve" if (tt + r) % 2 == 0 else "pool"
                P.op(eng, lambda e, s=s, tt=tt, r=r, ex=ex: e.tensor_scalar(out=Pm[s][:, tt, r, :], in0=iota[:], scalar1=posr[:, r, tt, ex:ex + 1],
                                                                        scalar2=None, op0=ALU.is_equal), reads=[C, "iota"], writes=[("Pm", s)])
        for r in range(NR):
            for kq in range(DC // 4):
                ps, pk = cx.psum()
                for q in range(4):
                    for tt in range(r, NT_):
                        kc = kq * 4 + q
                        P.op("pe", lambda e, ps=ps, q=q, kc=kc, tt=tt, r=r, s=s: e.matmul(
                            ps[:, q * 128:(q + 1) * 128], lhsT=htm[:, tt, kc * 128:(kc + 1) * 128], rhs=Pm[s][:, tt, r, :],
                            start=(tt == r), stop=(tt == NT_ - 1)), reads=htk + [("Pm", s)], writes=[pk], signal=(tt == NT_ - 1 and q == 3))
                cx.copy("act" if (kq % 2) else "dve", hcb[s][:, kq * 4:(kq + 1) * 4, r * 128:(r + 1) * 128],
                        ps[:].rearrange("p (k n) -> p k n", k=4), [pk], [("hcb", s)])
        cx.store(hco[ex].rearrange("p k n -> p (k n)"), hcb[s][:].rearrange("p k n -> p (k n)"), [("hcb", s)], ("hco", ex), sem=("st_hcb", s))
    return cx.finish()


NCH3 = CAP // T


def build_moe3():
    cx = Ctx()
    cx.ARENA_BYTES = 196 * 1024
    P = cx.P
    nc = cx.nc
    hcd = cx.din("hc", [NCH3, 128, DC, T], BF16)
    Wg = cx.din("w_gate", [D, DFFE], F32)
    Wu = cx.din("w_up", [D, DFFE], F32)
    Wd = cx.din("w_down", [DFFE, D], F32)
    yo = cx.dout("y", [CAP, D], BF16)
    Wg16 = nc.dram_tensor("Wg16", [D, DFFE], BF16, kind="Internal").ap()
    Wu16 = nc.dram_tensor("Wu16", [D, DFFE], BF16, kind="Internal").ap()
    Wd16 = nc.dram_tensor("Wd16", [DFFE, D], BF16, kind="Internal").ap()
    cx.alloc_psum(8)
    nft = DFFE // 128
    cx.phase_begin()
    stg = [cx.sb("cv_st%d" % i, [128, 4, 512], F32) for i in range(3)]
    o16 = [cx.sb("cv_o%d" % i, [128, 4, 512], BF16) for i in range(3)]
    cvi = [0]

    def convert_group(fg_):
        if fg_ >= nft // FG:
            return
        pieces = [(Wg, Wg16, "Wg16", kq, fg_) for kq in range(0, DC, 4)] + [(Wu, Wu16, "Wu16", kq, fg_) for kq in range(0, DC, 4)] + \
                 [(Wd, Wd16, "Wd16", fg_ * 4, cb) for cb in range(D // 512)]
        for (Wsrc, Wdst, name, kq, cb) in pieces:
            s = cvi[0] % 3
            cvi[0] += 1
            src = Wsrc[kq * 128:(kq + 4) * 128, cb * 512:(cb + 1) * 512].rearrange("(c p) n -> p c n", p=128)
            dst = Wdst[kq * 128:(kq + 4) * 128, cb * 512:(cb + 1) * 512].rearrange("(c p) n -> p c n", p=128)
            P.dma("sp", lambda e, s=s, src=src: e.dma_start(out=stg[s][:], in_=src), writes=[("cvst", s)])
            cx.copy(cx.conv_eng(), o16[s][:], stg[s][:], [("cvst", s)], [("cvo", s)])
            P.dma("pool", lambda e, s=s, dst=dst: e.dma_start(out=dst, in_=o16[s][:]), reads=[("cvo", s)], writes=[(name, kq // 4, cb)],
                  sem=("cvout", s))

    convert_group(0)
    convert_group(1)
    hT = cx.sb("hT_sb", [128, DC, T], BF16)
    accR = cx.sb("accR", [128, T // 128, D], F32)
    wgt = [cx.sb("wg%d" % i, [128, DC, 128], BF16) for i in range(2)]
    wut = [cx.sb("wu%d" % i, [128, DC, 128], BF16) for i in range(2)]
    wdt = cx.sb("wdt", [128, FG, D], BF16)
    g16 = [cx.sb("g16_%d" % i, [128, FG, T], BF16) for i in range(2)]
    sgl = [cx.sb("sg%d" % i, [128, 512], F32) for i in range(2)]
    orow = [cx.sb("orow%d" % i, [128, D], BF16) for i in range(2)]
    gi = 0
    it = 0
    for ch in range(NCH3):
        for k0 in range(0, DC, 4):
            P.dma("sp", lambda e, k0=k0, ch=ch: e.dma_start(out=hT[:, k0:k0 + 4, :], in_=hcd[ch, :, k0:k0 + 4, :]), writes=["hT"], sem="hT")
        for fg in range(nft // FG):
            gb = g16[fg % 2]
            if ch == 0:
                convert_group(fg + 2)
            for j in range(FG):
                ft = fg * FG + j
                s = gi % 2
                gi += 1
                dep = [("Wg16", kq, ft // 4) for kq in range(4)] + [("Wu16", kq, ft // 4) for kq in range(4)]
                srcg = Wg16[:, ft * 128:(ft + 1) * 128].rearrange("(c p) n -> p c n", p=128)
                srcu = Wu16[:, ft * 128:(ft + 1) * 128].rearrange("(c p) n -> p c n", p=128)
                P.dma("sp", lambda e, s=s, srcg=srcg: e.dma_start(out=wgt[s][:], in_=srcg), reads=dep if ch == 0 else [], writes=[("wgt", s)])
                P.dma("sp", lambda e, s=s, srcu=srcu: e.dma_start(out=wut[s][:], in_=srcu), reads=dep if ch == 0 else [], writes=[("wut", s)])
                for half in range(T // 512):
                    hs = slice(half * 512, (half + 1) * 512)
                    psg, pkg = cx.psum()
                    mm_group(cx, psg[:], pkg, [(wgt[s][:, kc, :], hT[:, kc, hs]) for kc in range(DC)], [("wgt", s), "hT"])
                    psu, pku = cx.psum()
                    mm_group(cx, psu[:], pku, [(wut[s][:, kc, :], hT[:, kc, hs]) for kc in range(DC)], [("wut", s), "hT"])
                    s2 = it % 2
                    it += 1
                    P.op("act", lambda e, s2=s2, psg=psg: e.activation(out=sgl[s2][:], in_=psg[:], func=AF.Silu), reads=[pkg], writes=[("sg", s2)])
                    P.op("dve", lambda e, s2=s2, psu=psu, gb=gb, j=j, hs=hs: e.tensor_tensor(out=gb[:, j, hs], in0=sgl[s2][:], in1=psu[:], op=ALU.mult),
                         reads=[("sg", s2), pku], writes=[("g16", fg % 2, j)])
            for j in range(FG):
                ft = fg * FG + j
                dep = [("Wd16", ft // 4, cb) for cb in range(D // 512)]
                P.dma("sp", lambda e, j=j, ft=ft: e.dma_start(out=wdt[:, j, :], in_=Wd16[ft * 128:(ft + 1) * 128, :]),
                      reads=dep if ch == 0 else [], writes=[("wdt", j)])
            for rt in range(T // 128):
                for db in range(D // 512):
                    ps, pk = cx.psum()
                    mm_group(cx, ps[:], pk, [(gb[:, j, rt * 128:(rt + 1) * 128], wdt[:, j, db * 512:(db + 1) * 512]) for j in range(FG)],
                             [("wdt", j) for j in range(FG)] + [("g16", fg % 2, j) for j in range(FG)])
                    dsl = accR[:, rt, db * 512:(db + 1) * 512]
                    if fg == 0:
                        P.op("dve", lambda e, dsl=dsl, ps=ps: e.tensor_copy(out=dsl, in_=ps[:]), reads=[pk], writes=[("accR", rt, db)])
                    else:
                        P.op("dve", lambda e, dsl=dsl, ps=ps: e.tensor_tensor(out=dsl, in0=ps[:], in1=dsl, op=ALU.add),
                             reads=[pk, ("accR", rt, db)], writes=[("accR", rt, db)])
        for rt in range(T // 128):
            s = rt % 2
            row0 = ch * T + rt * 128
            P.op("act", lambda e, s=s, rt=rt: e.activation(out=orow[s][:], in_=accR[:, rt, :], func=AF.Identity),
                 reads=[("accR", rt, db) for db in range(D // 512)], writes=[("orow", s)])
            cx.store(yo[row0:row0 + 128, :], orow[s][:], [("orow", s)], ("yo", ch, rt), sem=("st_orow", s))
    cx.phase_end()
    return cx.finish()


def build_final3():
    cx = Ctx()
    P = cx.P
    NT_ = T // 128
    xd = cx.din("x", [T, D], F32)
    yd = cx.din("yseg", [NEXP, SEG, D], BF16)
    pbd = cx.din("posb", [128, NEXP, T], F32)
    rwd = cx.din("rwt", [128, NT_, NEXP], F32)
    sld = cx.din("slotidx", [128, NR], F32)
    gfd = cx.din("gatef", [128, D], F32)
    gnd = cx.din("gfin", [128, D], F32)
    out = cx.dout("out", [T, D], F32)
    cx.alloc_psum(8)
    acc = cx.sb("acc", [128, NT_, D], F32)
    posb = cx.sb("posb_sb", [128, NEXP, T], F32)
    rwt = cx.sb("rwt_sb", [128, NT_, NEXP], F32)
    slot = cx.sb("slot_sb", [128, NR], F32)
    gf = cx.sb("gf_sb", [128, D], F32)
    gfin = cx.sb("gfin_sb", [128, D], F32)
    cx.load(posb[:].rearrange("p e t -> p (e t)"), pbd.rearrange("p e t -> p (e t)"), "posb")
    cx.load(rwt[:], rwd, "rwt")
    cx.load(slot[:], sld, "slot")
    cx.load(gf[:], gfd, "gf")
    cx.load(gfin[:], gnd, "gfin")
    ye = [cx.sb("ye%d" % i, [128, NR, D], BF16) for i in range(2)]
    PwT = [cx.sb("PwT%d" % i, [128, NR, T], BF16) for i in range(2)]
    for ex in range(NEXP):
        s = ex % 2
        for r in range(NR):
            P.dma("sp", lambda e, s=s, r=r, ex=ex: e.dma_start(out=ye[s][:, r, :], in_=yd[ex, r * 128:(r + 1) * 128, :]), writes=[("ye", s, r)], sem=("ye", s, r))
            P.op("dve" if r % 2 == 0 else "pool", lambda e, s=s, r=r, ex=ex: e.tensor_scalar(
                out=PwT[s][:, r, :], in0=posb[:, ex, :], scalar1=slot[:, r:r + 1], scalar2=None, op0=ALU.is_equal),
                reads=["posb", "slot"], writes=[("PwT", s, r)])
        for tt in range(NT_):
            nr = min(tt, NR - 1) + 1
            for db in range(D // 512):
                ps, pk = cx.psum()
                mm_group(cx, ps[:], pk, [(PwT[s][:, r, tt * 128:(tt + 1) * 128], ye[s][:, r, db * 512:(db + 1) * 512]) for r in range(nr)],
                         [("PwT", s, r) for r in range(nr)] + [("ye", s, r) for r in range(nr)])
                dsl = acc[:, tt, db * 512:(db + 1) * 512]
                if ex == 0:
                    P.op("dve", lambda e, dsl=dsl, ps=ps, tt=tt, ex=ex: e.tensor_scalar(out=dsl, in0=ps[:], scalar1=rwt[:, tt, ex:ex + 1], scalar2=None, op0=ALU.mult),
                         reads=[pk, "rwt"], writes=[("acc", tt, db)])
                else:
                    P.op("dve", lambda e, dsl=dsl, ps=ps, tt=tt, ex=ex: e.scalar_tensor_tensor(out=dsl, in0=ps[:], scalar=rwt[:, tt, ex:ex + 1], in1=dsl,
                                                                                     op0=ALU.mult, op1=ALU.add),
                         reads=[pk, "rwt", ("acc", tt, db)], writes=[("acc", tt, db)])
    xt = [cx.sb("xt%d" % i, [128, D], F32) for i in range(2)]
    sqj = cx.sb("sqj", [128, D], F32)
    ss = [cx.sb("ss%d" % i, [128, 1], F32) for i in range(2)]
    for tt in range(NT_):
        s = tt % 2
        rows = slice(tt * 128, (tt + 1) * 128)
        ak = [("acc", tt, db) for db in range(D // 512)]
        cx.load(xt[s][:], xd[rows, :], ("xt", s))
        P.op("pool", lambda e, tt=tt: e.tensor_tensor(out=acc[:, tt, :], in0=acc[:, tt, :], in1=gf[:], op=ALU.mult), reads=ak + ["gf"], writes=ak)
        P.op("dve", lambda e, s=s, tt=tt: e.tensor_tensor(out=xt[s][:], in0=xt[s][:], in1=acc[:, tt, :], op=ALU.add), reads=ak + [("xt", s)], writes=[("xt", s)])
        P.op("act", lambda e, s=s: e.activation(out=sqj[:], in_=xt[s][:], func=AF.Square, accum_out=ss[s][:]), reads=[("xt", s)], writes=["sqj", ("ss", s)])
        P.op("dve", lambda e, s=s: e.tensor_scalar(out=ss[s][:], in0=ss[s][:], scalar1=1.0 / D, scalar2=EPS, op0=ALU.mult, op1=ALU.add),
             reads=[("ss", s)], writes=[("ss", s)])
        P.op("act", lambda e, s=s: e.activation(out=ss[s][:], in_=ss[s][:], func=AF.Sqrt), reads=[("ss", s)], writes=[("ss", s)])
        P.op("dve", lambda e, s=s: e.reciprocal(out=ss[s][:], in_=ss[s][:]), reads=[("ss", s)], writes=[("ss", s)])
        P.op("dve", lambda e, s=s, tt=tt: e.scalar_tensor_tensor(out=acc[:, tt, :], in0=xt[s][:], scalar=ss[s][:, 0:1], in1=gfin[:], op0=ALU.mult, op1=ALU.mult),
             reads=[("xt", s), ("ss", s), "gfin"] + ak, writes=ak)
        cx.store(out[rows, :], acc[:, tt, :], ak, ("out", tt), sem=("st_out", s))
    return cx.finish()


def route_consts():
    kk = np.arange(128)
    return {"Ltri": (kk[:, None] < kk[None, :]).astype(np.float32), "ident": np.eye(128, dtype=np.float32),
            "iota": np.ascontiguousarray(np.broadcast_to(kk[None, :].astype(np.float32), (128, 128)))}


def rw_layout(rw):
    return np.ascontiguousarray(rw.reshape(T // 128, 128, NEXP).transpose(1, 0, 2))


def run_route(h2s, rws):
    nc = get_nc("route", build_route)
    cst = route_consts()
    ins = [dict(h2T=h2s[i], rw=rw_layout(rws[i]), **cst) for i in range(NCORES)]
    res = run(nc, ins)
    overflow = any(bool(np.any(res[i]["ovf"] > 0.5)) for i in range(NCORES))
    return [res[i]["hc"] for i in range(NCORES)], [res[i]["posm"] for i in range(NCORES)], overflow


def run_moe3(hcs, inp):
    nc = get_nc("moe3", build_moe3)
    ins = []
    for e in range(NCORES):
        hcat = np.concatenate([hcs[i][e] for i in range(NCORES)], axis=2)
        hc = np.ascontiguousarray(hcat.reshape(128, DC, NCH3, T).transpose(2, 0, 1, 3))
        ins.append({"hc": hc, "w_gate": np.ascontiguousarray(inp["moe_w_gate"][0][e]), "w_up": np.ascontiguousarray(inp["moe_w_up"][0][e]),
                    "w_down": np.ascontiguousarray(inp["moe_w_down"][0][e])})
    res = run(nc, ins)
    return [res[e]["y"] for e in range(NCORES)]


def run_final3(xTs, ys, posms, rws, mod_l, gfin):
    nc = get_nc("final3", build_final3)
    ins = []
    slotidx = (np.arange(128)[:, None] + 128 * np.arange(NR)[None, :]).astype(np.float32)
    for i in range(NCORES):
        b = i // 4
        gatef = mod_l[b].reshape(6, D)[5]
        posm = posms[i]
        pos_et = posm.transpose(2, 1, 0).reshape(NEXP, T)
        ins.append({"x": np.ascontiguousarray(xTs[i].transpose(1, 0, 2).reshape(D, T).T),
                    "yseg": np.ascontiguousarray(np.stack([ys[e][i * SEG:(i + 1) * SEG] for e in range(NEXP)])),
                    "posb": np.ascontiguousarray(np.broadcast_to(pos_et[None], (128, NEXP, T))),
                    "rwt": rw_layout(rws[i]), "slotidx": slotidx,
                    "gatef": np.ascontiguousarray(np.broadcast_to(gatef[None, :], (128, D))),
                    "gfin": np.ascontiguousarray(np.broadcast_to(gfin[None, :], (128, D)))})
    res = run(nc, ins)
    out = np.zeros((2, SEQ, D), np.float32)
    for i in range(NCORES):
        b, q = i // 4, i % 4
        out[b, q * T:(q + 1) * T, :] = res[i]["out"]
    return out
```
